# Optimizing a Trainium2 kernel written in Bass

```python
import jax, jax.numpy as jnp
from jax import lax
import numpy as np

D_MODEL = 1024
BATCH = 8
SEQ = 4096
DEPTH = 2

GRID_W = 64
CTX_LEN = 256
EPS = 1e-6
ROPE_THETA = 10000.0
Q_BLOCK = 128
LRU_WIDTH = 512
LRU_BLOCKS = 8
LRU_BLOCK = LRU_WIDTH // LRU_BLOCKS
CONV_W = 4
LRU_C = 8.0
MLA_HEADS = 8
MLA_Q_RANK = 256
MLA_KV_RANK = 128
MLA_NOPE = 64
MLA_ROPE = 32
MLA_V = 64
MLA_SCALE = (MLA_NOPE + MLA_ROPE) ** -0.5
GQA_HEADS = 8
GQA_KV_HEADS = 2
GQA_GROUP = GQA_HEADS // GQA_KV_HEADS
GQA_DIM = 64
GQA_SCALE = GQA_DIM ** -0.5
N_BRANCH = 3
BRANCH_W = 512
N_GROUPS = 4
EXPERTS_PER_GROUP = 8
N_EXPERTS = N_GROUPS * EXPERTS_PER_GROUP
TOP_K = 2
D_EXPERT = 256
KV_SIZES = (LRU_WIDTH, MLA_KV_RANK, MLA_ROPE, GQA_KV_HEADS * GQA_DIM, GQA_KV_HEADS * GQA_DIM)
Q_SIZES = (LRU_WIDTH, MLA_Q_RANK, GQA_HEADS * GQA_DIM, N_BRANCH * D_MODEL)
KV_SIDE = LRU_WIDTH + MLA_KV_RANK + MLA_ROPE + 2 * GQA_KV_HEADS * GQA_DIM
D_IN = KV_SIDE + LRU_WIDTH + MLA_Q_RANK + GQA_HEADS * GQA_DIM + N_BRANCH * D_MODEL

kernel_name = "hybrid_flow_backbone"


def rms_norm(x, g):
    xf = x.astype(jnp.float32)
    y = xf * lax.rsqrt(jnp.mean(xf * xf, axis=-1, keepdims=True) + EPS)
    return (y * g.astype(jnp.float32)).astype(x.dtype)


def modulate(h, shift, scale):
    return h * (1 + scale) + shift


def split_cols(u, sizes):
    idx = [int(i) for i in np.cumsum(sizes)[:-1]]
    return jnp.split(u, idx, axis=-1)


def rope_tables(n, rot_dim):
    rows = n // GRID_W
    row = jnp.repeat(jnp.arange(rows), GRID_W).astype(jnp.float32)
    col = jnp.tile(jnp.arange(GRID_W), rows).astype(jnp.float32)
    quarter = rot_dim // 4
    freqs = ROPE_THETA ** (-jnp.arange(quarter, dtype=jnp.float32) / quarter)
    ang = jnp.concatenate([row[:, None] * freqs, col[:, None] * freqs], axis=-1)
    return jnp.cos(ang), jnp.sin(ang)


def apply_rope(x, cos, sin):
    xf = x.astype(jnp.float32)
    half = x.shape[-1] // 2
    x1, x2 = xf[..., :half], xf[..., half:]
    return jnp.concatenate([x1 * cos - x2 * sin, x1 * sin + x2 * cos], axis=-1).astype(x.dtype)


def attend(q, k, v, scale):
    s = jnp.einsum('bhgqd,bhkd->bhgqk', q, k, preferred_element_type=jnp.float32) * scale
    p = jax.nn.softmax(s, axis=-1).astype(v.dtype)
    return jnp.einsum('bhgqk,bhkd->bhgqd', p, v)


def blocked_attend(q, k, v, scale):
    b, h, g, n, d = q.shape
    qb = jnp.moveaxis(q.reshape(b, h, g, n // Q_BLOCK, Q_BLOCK, d), 3, 0)
    ob = lax.map(lambda qi: attend(qi, k, v, scale), qb)
    return jnp.moveaxis(ob, 0, 3).reshape(b, h, g, n, v.shape[-1])


def merge_heads(o):
    b, h, g, n, d = o.shape
    return o.transpose(0, 3, 1, 2, 4).reshape(b, n, h * g * d)


def dwconv(x, w, bias):
    pad = (CONV_W // 2, CONV_W - 1 - CONV_W // 2)
    y = lax.conv_general_dilated(x, w[:, None, :], window_strides=(1,), padding=[pad],
                                 dimension_numbers=('NWC', 'WIO', 'NWC'),
                                 feature_group_count=x.shape[-1])
    return y + bias


def lru_coeffs(xc, wa, ba, wi, bi, lam):
    b, s, _ = xc.shape
    xb = xc.reshape(b, s, LRU_BLOCKS, LRU_BLOCK)
    r = jax.nn.sigmoid(jnp.einsum('bsnc,ncd->bsnd', xb, wa.astype(jnp.float32)).reshape(b, s, -1) + ba)
    i = jax.nn.sigmoid(jnp.einsum('bsnc,ncd->bsnd', xb, wi.astype(jnp.float32)).reshape(b, s, -1) + bi)
    log_a = -LRU_C * r * jax.nn.softplus(-lam.astype(jnp.float32))
    a = jnp.exp(log_a)
    mult = jnp.sqrt(-jnp.expm1(2.0 * log_a))
    return a, mult * i * xc


def linear_scan(a, b, h0, reverse):
    if h0 is not None:
        idx = -1 if reverse else 0
        b = b.at[:, idx].add(a[:, idx] * h0)

    def combine(l, r):
        al, bl = l
        ar, br = r
        return al * ar, ar * bl + br

    _, h = lax.associative_scan(combine, (a, b), reverse=reverse, axis=1)
    return h


def rglru(xr_lat, xr_ctx, conv_w, conv_b, wa, ba, wi, bi, lam, need_ctx):
    xl = dwconv(xr_lat, conv_w, conv_b).astype(jnp.float32)
    xc = dwconv(xr_ctx, conv_w, conv_b).astype(jnp.float32)
    y_lat = jnp.zeros_like(xl)
    y_ctx = jnp.zeros_like(xc) if need_ctx else None
    for d, rev in enumerate((False, True)):
        a_c, b_c = lru_coeffs(xc, wa[d], ba[d], wi[d], bi[d], lam[d])
        h_c = linear_scan(a_c, b_c, None, rev)
        h_final = h_c[:, 0] if rev else h_c[:, -1]
        a_l, b_l = lru_coeffs(xl, wa[d], ba[d], wi[d], bi[d], lam[d])
        y_lat = y_lat + linear_scan(a_l, b_l, h_final, rev)
        if need_ctx:
            y_ctx = y_ctx + h_c
    y_lat = y_lat.astype(xr_lat.dtype)
    return y_lat, (y_ctx.astype(xr_ctx.dtype) if need_ctx else None)


def mla_kv(ckv, kr, g_kv, w_ukv, cos, sin):
    b, n, _ = ckv.shape
    kv = (rms_norm(ckv, g_kv) @ w_ukv).reshape(b, n, MLA_HEADS, MLA_NOPE + MLA_V).transpose(0, 2, 1, 3)
    k_nope, v = kv[..., :MLA_NOPE], kv[..., MLA_NOPE:]
    kr = kr[:, None]
    if cos is not None:
        kr = apply_rope(kr, cos, sin)
    k = jnp.concatenate([k_nope, jnp.broadcast_to(kr, (b, MLA_HEADS, n, MLA_ROPE))], axis=-1)
    return k, v


def mla_q(cq, g_q, w_uq, cos, sin):
    b, n, _ = cq.shape
    q = (rms_norm(cq, g_q) @ w_uq).reshape(b, n, MLA_HEADS, MLA_NOPE + MLA_ROPE).transpose(0, 2, 1, 3)
    qn, qr = q[..., :MLA_NOPE], q[..., MLA_NOPE:]
    if cos is not None:
        qr = apply_rope(qr, cos, sin)
    return jnp.concatenate([qn, qr], axis=-1)[:, :, None]


def gqa_heads(t, n_heads, g, cos, sin):
    b, n, _ = t.shape
    t = t.reshape(b, n, n_heads, GQA_DIM).transpose(0, 2, 1, 3)
    if g is not None:
        t = rms_norm(t, g)
    if cos is not None:
        t = apply_rope(t, cos, sin)
    return t


def merge_branches(y_rnn, y_mla, y_gqa, mg, w_branch, w_out):
    br = jnp.stack([y_rnn, y_mla, y_gqa], axis=-2)
    proj = jnp.einsum('bnkc,kcd->bnkd', br, w_branch)
    gates = jax.nn.sigmoid(mg.reshape(mg.shape[:-1] + (N_BRANCH, D_MODEL)))
    return jnp.sum(gates * proj, axis=-2) @ w_out


def mixer(x, ctx, sh, sc, csh, csc, g_mix, w_in, conv_w, conv_b, lru_wa, lru_ba, lru_wi, lru_bi,
          lru_lambda, mla_gq, mla_wuq, mla_gkv, mla_wukv, gqa_gq, gqa_gk, w_branch, w_out,
          ropes, update_ctx):
    cos_m, sin_m, cos_g, sin_g = ropes
    h = modulate(rms_norm(x, g_mix), sh[:, None], sc[:, None])
    hc = modulate(rms_norm(ctx, g_mix), csh, csc)
    xr, ckv, kr, gk, gv, rg, cq, gq, mg = split_cols(h @ w_in, KV_SIZES + Q_SIZES)
    if update_ctx:
        xr_c, ckv_c, kr_c, gk_c, gv_c, rg_c, cq_c, gq_c, mg_c = split_cols(hc @ w_in, KV_SIZES + Q_SIZES)
    else:
        xr_c, ckv_c, kr_c, gk_c, gv_c = split_cols(hc @ w_in[:, :KV_SIDE], KV_SIZES)

    lru_l, lru_c = rglru(xr, xr_c, conv_w, conv_b, lru_wa, lru_ba, lru_wi, lru_bi, lru_lambda, update_ctx)
    y_rnn = lru_l * jax.nn.gelu(rg)

    k_c, v_c = mla_kv(ckv_c, kr_c, mla_gkv, mla_wukv, None, None)
    k_l, v_l = mla_kv(ckv, kr, mla_gkv, mla_wukv, cos_m, sin_m)
    q_l = mla_q(cq, mla_gq, mla_wuq, cos_m, sin_m)
    y_mla = merge_heads(blocked_attend(q_l, jnp.concatenate([k_c, k_l], axis=2),
                                       jnp.concatenate([v_c, v_l], axis=2), MLA_SCALE))

    b, n, _ = x.shape
    gk_ch = gqa_heads(gk_c, GQA_KV_HEADS, gqa_gk, None, None)
    gv_ch = gqa_heads(gv_c, GQA_KV_HEADS, None, None, None)
    gk_lh = gqa_heads(gk, GQA_KV_HEADS, gqa_gk, cos_g, sin_g)
    gv_lh = gqa_heads(gv, GQA_KV_HEADS, None, None, None)
    gq_lh = gqa_heads(gq, GQA_HEADS, gqa_gq, cos_g, sin_g).reshape(b, GQA_KV_HEADS, GQA_GROUP, n, GQA_DIM)
    y_gqa = merge_heads(blocked_attend(gq_lh, jnp.concatenate([gk_ch, gk_lh], axis=2),
                                       jnp.concatenate([gv_ch, gv_lh], axis=2), GQA_SCALE))

    out = merge_branches(y_rnn, y_mla, y_gqa, mg, w_branch, w_out)
    if not update_ctx:
        return out, None

    nc = ctx.shape[1]
    yc_rnn = lru_c * jax.nn.gelu(rg_c)
    qc = mla_q(cq_c, mla_gq, mla_wuq, None, None)
    yc_mla = merge_heads(attend(qc, k_c, v_c, MLA_SCALE))
    gq_ch = gqa_heads(gq_c, GQA_HEADS, gqa_gq, None, None).reshape(b, GQA_KV_HEADS, GQA_GROUP, nc, GQA_DIM)
    yc_gqa = merge_heads(attend(gq_ch, gk_ch, gv_ch, GQA_SCALE))
    out_c = merge_branches(yc_rnn, yc_mla, yc_gqa, mg_c, w_branch, w_out)
    return out, out_c


def hier_moe(h, wg, bg, we, be, w1, w3, w2):
    b, n, d = h.shape
    t = h.reshape(-1, d)
    glog = (t @ wg + bg).astype(jnp.float32)
    gprob = jax.nn.softmax(glog, axis=-1)
    _, gsel = lax.top_k(glog, 1)
    pg = jnp.take_along_axis(gprob, gsel, axis=-1)
    elog = (t @ we + be).astype(jnp.float32).reshape(-1, N_GROUPS, EXPERTS_PER_GROUP)
    idx = jnp.broadcast_to(gsel[:, :, None], (t.shape[0], 1, EXPERTS_PER_GROUP))
    elog = jnp.take_along_axis(elog, idx, axis=1)[:, 0]
    top_v, top_i = lax.top_k(elog, TOP_K)
    pe = jax.nn.softmax(top_v, axis=-1) * pg
    eidx = gsel * EXPERTS_PER_GROUP + top_i
    comb = jnp.sum(jax.nn.one_hot(eidx, N_EXPERTS, dtype=jnp.float32) * pe[..., None], axis=1).astype(t.dtype)
    out = jnp.zeros_like(t)
    for e in range(N_EXPERTS):
        he = jax.nn.silu(t @ w1[e]) * (t @ w3[e])
        out = out + comb[:, e:e + 1] * (he @ w2[e])
    return out.reshape(b, n, d)


def setup_inputs(seed: int = 0) -> dict:
    key = jax.random.key(seed)
    ks = iter(jax.random.split(key, 40))
    L, D = DEPTH, D_MODEL

    def nrm(shape, scale):
        return jax.random.normal(next(ks), shape, jnp.float32) * scale

    def gain(shape):
        return 1.0 + nrm(shape, 0.02)

    u = jax.random.uniform(next(ks), (L, 2, LRU_WIDTH), jnp.float32, minval=0.9, maxval=0.999)
    s = u ** (1.0 / LRU_C)
    lam = jnp.log(s) - jnp.log1p(-s)
    return {
        "x": nrm((BATCH, SEQ, D), 1.0),
        "c": nrm((BATCH, D), 1.0),
        "ctx": nrm((BATCH, CTX_LEN, D), 1.0),
        "c_ctx": nrm((D,), 1.0),
        "w_mod": nrm((L, D, 6 * D), 0.5 * D ** -0.5),
        "b_mod": nrm((L, 6 * D), 0.01),
        "g_mix": gain((L, D)),
        "g_ffn": gain((L, D)),
        "w_in": nrm((L, D, D_IN), D ** -0.5),
        "conv_w": nrm((L, CONV_W, LRU_WIDTH), CONV_W ** -0.5),
        "conv_b": nrm((L, LRU_WIDTH), 0.01),
        "lru_wa": nrm((L, 2, LRU_BLOCKS, LRU_BLOCK, LRU_BLOCK), LRU_BLOCK ** -0.5),
        "lru_ba": nrm((L, 2, LRU_WIDTH), 0.01),
        "lru_wi": nrm((L, 2, LRU_BLOCKS, LRU_BLOCK, LRU_BLOCK), LRU_BLOCK ** -0.5),
        "lru_bi": nrm((L, 2, LRU_WIDTH), 0.01),
        "lru_lambda": lam,
        "mla_gq": gain((L, MLA_Q_RANK)),
        "mla_wuq": nrm((L, MLA_Q_RANK, MLA_HEADS * (MLA_NOPE + MLA_ROPE)), MLA_Q_RANK ** -0.5),
        "mla_gkv": gain((L, MLA_KV_RANK)),
        "mla_wukv": nrm((L, MLA_KV_RANK, MLA_HEADS * (MLA_NOPE + MLA_V)), MLA_KV_RANK ** -0.5),
        "gqa_gq": gain((L, GQA_DIM)),
        "gqa_gk": gain((L, GQA_DIM)),
        "w_branch": nrm((L, N_BRANCH, BRANCH_W, D), BRANCH_W ** -0.5),
        "w_out": nrm((L, D, D), D ** -0.5),
        "moe_wg": nrm((L, D, N_GROUPS), D ** -0.5),
        "moe_bg": nrm((L, N_GROUPS), 0.01),
        "moe_we": nrm((L, D, N_EXPERTS), D ** -0.5),
        "moe_be": nrm((L, N_EXPERTS), 0.01),
        "moe_w1": nrm((L, N_EXPERTS, D, D_EXPERT), D ** -0.5),
        "moe_w3": nrm((L, N_EXPERTS, D, D_EXPERT), D ** -0.5),
        "moe_w2": nrm((L, N_EXPERTS, D_EXPERT, D), D_EXPERT ** -0.5),
        "g_final": gain((D,)),
    }


def reference(x, c, ctx, c_ctx, w_mod, b_mod, g_mix, g_ffn, w_in, conv_w, conv_b, lru_wa, lru_ba,
              lru_wi, lru_bi, lru_lambda, mla_gq, mla_wuq, mla_gkv, mla_wukv, gqa_gq, gqa_gk,
              w_branch, w_out, moe_wg, moe_bg, moe_we, moe_be, moe_w1, moe_w3, moe_w2, g_final):
    n = x.shape[1]
    nc = ctx.shape[1]
    ropes = rope_tables(n, MLA_ROPE) + rope_tables(n, GQA_DIM)
    for l in range(DEPTH):
        update_ctx = l < DEPTH - 1
        mod = jax.nn.silu(c) @ w_mod[l] + b_mod[l]
        mod_c = jax.nn.silu(c_ctx) @ w_mod[l] + b_mod[l]
        sh_a, sc_a, g_a, sh_f, sc_f, g_f = jnp.split(mod, 6, axis=-1)
        csh_a, csc_a, cg_a, csh_f, csc_f, cg_f = jnp.split(mod_c, 6, axis=-1)
        out, out_c = mixer(x, ctx, sh_a, sc_a, csh_a, csc_a, g_mix[l], w_in[l], conv_w[l], conv_b[l],
                           lru_wa[l], lru_ba[l], lru_wi[l], lru_bi[l], lru_lambda[l], mla_gq[l],
                           mla_wuq[l], mla_gkv[l], mla_wukv[l], gqa_gq[l], gqa_gk[l], w_branch[l],
                           w_out[l], ropes, update_ctx)
        x = x + g_a[:, None] * out
        h = modulate(rms_norm(x, g_ffn[l]), sh_f[:, None], sc_f[:, None])
        if update_ctx:
            ctx = ctx + cg_a * out_c
            hc = modulate(rms_norm(ctx, g_ffn[l]), csh_f, csc_f)
            y = hier_moe(jnp.concatenate([hc, h], axis=1), moe_wg[l], moe_bg[l], moe_we[l], moe_be[l],
                         moe_w1[l], moe_w3[l], moe_w2[l])
            ctx = ctx + cg_f * y[:, :nc]
            x = x + g_f[:, None] * y[:, nc:]
        else:
            x = x + g_f[:, None] * hier_moe(h, moe_wg[l], moe_bg[l], moe_we[l], moe_be[l],
                                            moe_w1[l], moe_w3[l], moe_w2[l])
    return rms_norm(x, g_final)
```

```python
import numpy as np
import ml_dtypes
import concourse.bass as bass
import concourse.mybir as mybir
from concourse.bass_utils import run_bass_kernel_spmd

F32 = mybir.dt.float32
BF16 = mybir.dt.bfloat16
AF = mybir.ActivationFunctionType
ALU = mybir.AluOpType
AX = mybir.AxisListType

D = 1024
NCTX = 256
SEQ = 4096
T = NCTX + SEQ
NT = T // 128
DIN = 5280
EPS = 1e-6
C_XR, C_CKV, C_KR, C_GK, C_GV, C_RG, C_CQ, C_GQ, C_MG = 0, 512, 640, 672, 800, 928, 1440, 1696, 2208
MLA_SCALE = 96 ** -0.5
GQA_SCALE = 64 ** -0.5
NE = 32
DE = 256
import os as _os
DBG_SKIP_ROUTER = bool(_os.environ.get('DBG_SKIP_ROUTER'))
DBG_STOP = int(_os.environ.get('DBG_STOP', '99'))


class Buf:
    __slots__ = ("name", "lastw", "readers")

    def __init__(self, name=""):
        self.name = name
        self.lastw = None
        self.readers = []


class Op:
    __slots__ = ("eng", "fn", "deps", "dma", "tok", "needed", "idx")


class Prog:
    COMPUTE = ("pe", "act", "dve", "pool")
    NPOOL = 12
    UID = 0

    def __init__(self, nc):
        self.nc = nc
        self.ops = []
        self.q = {k: [] for k in ("pe", "act", "dve", "pool", "sp")}

    def add(self, eng, fn, reads=(), writes=(), dma=False):
        op = Op()
        op.eng, op.fn, op.dma, op.tok, op.needed = eng, fn, dma, None, False
        op.idx = len(self.ops)
        deps = set()
        for b in reads:
            if b.lastw is not None:
                deps.add(b.lastw)
        for b in writes:
            if b.lastw is not None:
                deps.add(b.lastw)
            deps.update(b.readers)
        op.deps = deps
        for b in reads:
            if b in writes:
                continue
            if not dma:
                b.readers = [r for r in b.readers if self.ops[r].dma or self.ops[r].eng != eng]
            b.readers.append(op.idx)
        for b in writes:
            b.lastw = op.idx
            b.readers = []
        self.ops.append(op)
        self.q[eng].append(op)
        return op

    def emit(self):
        nc, ops = self.nc, self.ops
        for op in ops:
            for d in op.deps:
                dop = ops[d]
                if dop.eng == "pe" and op.eng == "pe" and not dop.dma and not op.dma:
                    continue
                dop.needed = True
        Prog.UID += 1
        u = Prog.UID
        sems = {k: nc.alloc_semaphore("s%d_%s" % (u, k)) for k in self.COMPUTE}
        dsem = {k: [nc.alloc_semaphore("d%d_%s_%d" % (u, k, i)) for i in range(self.NPOOL)] for k in self.q}
        cnt = {k: 0 for k in self.COMPUTE}
        dcnt = {k: 0 for k in self.q}
        prewait = {}
        for op in ops:
            if op.dma:
                k = dcnt[op.eng]
                dcnt[op.eng] += 1
                s = dsem[op.eng][k % self.NPOOL]
                op.tok = (s, 16 * (k // self.NPOOL + 1))
                if k >= self.NPOOL:
                    prewait[op.idx] = (s, 16 * (k // self.NPOOL))
            elif op.needed:
                cnt[op.eng] += 1
                op.tok = (sems[op.eng], cnt[op.eng])
        engines = {"pe": "tensor", "act": "scalar", "dve": "vector", "pool": "gpsimd", "sp": "sync"}
        with nc.Block() as block:
            def make(k):
                def body(e):
                    known = {}
                    for op in self.q[k]:
                        waits = []
                        if op.idx in prewait:
                            waits.append(prewait[op.idx])
                        for d in sorted(op.deps):
                            dop = ops[d]
                            if dop.tok is None:
                                continue
                            if dop.eng == "pe" and k == "pe" and not dop.dma and not op.dma:
                                continue
                            waits.append(dop.tok)
                        for (s, v) in waits:
                            if known.get(id(s), 0) >= v:
                                continue
                            known[id(s)] = v
                            e.wait_ge(s, v)
                        ins = op.fn(e)
                        if op.tok is not None:
                            ins.then_inc(op.tok[0], 16 if op.dma else 1)
                    if k == "sp":
                        for kk in self.q:
                            n = dcnt[kk]
                            for j in range(min(n, self.NPOOL)):
                                uses = (n - j + self.NPOOL - 1) // self.NPOOL
                                e.wait_ge(dsem[kk][j], 16 * uses)
                return body
            for k, attr in engines.items():
                getattr(block, attr)(make(k))


class Tl:
    def __init__(self, t, name=""):
        self.t = t
        self.b = Buf(name)

    def __getitem__(self, k):
        return self.t[k]


def _bufs(lst):
    return [x.b if isinstance(x, Tl) else x for x in lst]


class Ph:
    def __init__(self, nc, ps):
        self.nc = nc
        self.P = Prog(nc)
        self.ps = ps
        self.pb = [Tl(ps[:, i * 512:(i + 1) * 512], "pb%d" % i) for i in range(8)]
        self.n = 0
        Ph.UID += 1
        self.uid = Ph.UID

    UID = 0

    def sb(self, shape, dt=F32, name=None):
        self.n += 1
        t = self.nc.alloc_sbuf_tensor("%s_%d_%d" % (name or "t", self.uid, self.n), list(shape), dt)
        return Tl(t, name or "t")

    def add(self, eng, fn, r, w, dma=False):
        return self.P.add(eng, fn, _bufs(r), _bufs(w), dma=dma)

    def dma(self, out, in_, r=(), w=(), eng="sp", **kw):
        return self.add(eng, lambda e: e.dma_start(out=out, in_=in_, **kw), r, w, dma=True)

    def mm(self, out, lhsT, rhs, start, stop, r, w):
        return self.add("pe", lambda e: e.matmul(out, lhsT=lhsT, rhs=rhs, start=start, stop=stop), r, w)

    def tr(self, out, in_, ident, r, w):
        return self.add("pe", lambda e: e.transpose(out=out, in_=in_, identity=ident), r, w)

    def act(self, out, in_, func, r, w, bias=None, scale=None, accum=None):
        kw = {}
        if bias is not None:
            kw["bias"] = bias
        if scale is not None:
            kw["scale"] = scale
        if accum is not None:
            kw["accum_out"] = accum
        return self.add("act", lambda e: e.activation(out=out, in_=in_, func=func, **kw), r, w)

    def tt(self, eng, out, in0, in1, op, r, w):
        return self.add(eng, lambda e: e.tensor_tensor(out=out, in0=in0, in1=in1, op=op), r, w)

    def ts(self, eng, out, in0, s1, s2, op0, op1, r, w):
        if op1 is None:
            return self.add(eng, lambda e: e.tensor_scalar(out=out, in0=in0, scalar1=s1, scalar2=None, op0=op0), r, w)
        return self.add(eng, lambda e: e.tensor_scalar(out=out, in0=in0, scalar1=s1, scalar2=s2, op0=op0, op1=op1), r, w)

    def stt(self, eng, out, in0, sc, in1, op0, op1, r, w):
        return self.add(eng, lambda e: e.scalar_tensor_tensor(out=out, in0=in0, scalar=sc, in1=in1, op0=op0, op1=op1), r, w)

    def cp(self, eng, out, in_, r, w):
        if eng == "act":
            return self.add("act", lambda e: e.activation(out=out, in_=in_, func=AF.Copy), r, w)
        return self.add(eng, lambda e: e.tensor_copy(out=out, in_=in_), r, w)

    def memset(self, eng, ap, val, w):
        return self.add(eng, lambda e: e.memset(ap, val), [], w)

    def recip(self, out, in_, r, w):
        return self.add("dve", lambda e: e.reciprocal(out=out, in_=in_), r, w)

    def finish(self):
        self.P.emit()


def run_phase(nc, ps, fn, *args):
    with nc.cleanup_on_exit():
        ph = Ph(nc, ps)
        fn(ph, *args)
        ph.finish()
        nc.all_engine_barrier()


def token_blocks(t0, t1, bs=512):
    out = []
    t = t0
    while t < t1:
        n = min(bs, t1 - t)
        out.append((t, n))
        t += n
    return out


def make_identity(ph, dt, name):
    idf = ph.sb([128, 128], F32, name + "f")
    ph.memset("pool", idf[:], 0.0, [idf])
    ph.add("pool", lambda e: e.affine_select(out=idf[:], in_=idf[:], pattern=[[-1, 128]], compare_op=ALU.not_equal,
                                            fill=1.0, base=0, channel_multiplier=1), [idf], [idf])
    if dt == F32:
        return idf
    idb = ph.sb([128, 128], dt, name)
    ph.cp("pool", idb[:], idf[:], [idf], [idb])
    return idb


def phase_mod(ph, dr, l):
    cc = ph.sb([128, 2, 8], F32, "cc")
    sc = ph.sb([128, 2, 8], F32, "sc")
    ph.dma(cc[:], dr["cc"].rearrange("r (p k) -> p r k", k=8), [], [cc])
    ph.act(sc[:], cc[:], AF.Silu, [cc], [sc])
    mods = ph.sb([2, 6 * D], F32, "mods")
    bm = ph.sb([2, 6 * D], F32, "bm")
    ph.dma(bm[:], dr["b_mod"][l:l + 1, :].partition_broadcast(2), [], [bm])
    gm = ph.sb([2, D], F32, "gm")
    gf = ph.sb([2, D], F32, "gf")
    ph.dma(gm[:], dr["g_mix"][l:l + 1, :].partition_broadcast(2), [], [gm])
    ph.dma(gf[:], dr["g_ffn"][l:l + 1, :].partition_broadcast(2), [], [gf])
    wv = dr["w_mod"][l].rearrange("(p k) n -> p k n", k=8)
    wb = [ph.sb([128, 8, 512], F32, "wb") for _ in range(2)]
    for nb in range(12):
        w = wb[nb % 2]
        ph.dma(w[:], wv[:, :, nb * 512:(nb + 1) * 512], [], [w])
        pb = ph.pb[nb % 2]
        for k in range(8):
            ph.mm(pb[0:2, :], sc[:, :, k], w[:, k, :], k == 0, k == 7, [sc, w], [pb])
        ph.tt("dve", mods[:, nb * 512:(nb + 1) * 512], pb[0:2, :], bm[:, nb * 512:(nb + 1) * 512], ALU.add, [pb, bm], [mods])
    ph.stt("dve", mods[:, D:2 * D], mods[:, D:2 * D], 1.0, gm[:], ALU.add, ALU.mult, [mods, gm], [mods])
    ph.stt("dve", mods[:, 4 * D:5 * D], mods[:, 4 * D:5 * D], 1.0, gf[:], ALU.add, ALU.mult, [mods, gf], [mods])
    ph.dma(dr["modv"][:, :], mods[:], [mods], [])


def load_bc(ph, dr, row, idx, name):
    t = ph.sb([128, D], F32, name)
    ph.dma(t[:], dr["modv"][row:row + 1, idx * D:(idx + 1) * D].partition_broadcast(128), [], [t])
    return t


def rms_modulate(ph, xt, A, B, hout, junk, ss, rstd, h32=None):
    ph.act(junk[:], xt[:], AF.Square, [xt], [junk, ss], accum=ss[:, 0:1])
    ph.act(rstd[:, 0:1], ss[:, 0:1], AF.Sqrt, [ss], [rstd], bias=ph.epsc[:, 0:1], scale=1.0 / D)
    ph.recip(rstd[:, 0:1], rstd[:, 0:1], [rstd], [rstd])
    tmp = h32 if h32 is not None else junk
    ph.stt("dve", tmp[:], xt[:], rstd[:, 0:1], A[:], ALU.mult, ALU.mult, [xt, rstd, A], [tmp])
    ph.tt("dve", hout[:], tmp[:], B[:], ALU.add, [tmp, B], [hout])


def eps_const(ph):
    ph.epsc = ph.sb([128, 1], F32, "eps")
    ph.memset("pool", ph.epsc[:], EPS, [ph.epsc])


def phase_inproj(ph, dr, l, xsrc):
    nc = ph.nc
    eps_const(ph)
    win = ph.sb([128, 8, DIN], BF16, "win")
    stg = [ph.sb([128, 1320], F32, "stg") for _ in range(2)]
    wv = dr["w_in"][l].rearrange("(k p) n -> p k n", p=128)
    i = 0
    for k in range(8):
        for c4 in range(4):
            s = stg[i % 2]
            ph.dma(s[:], wv[:, k, c4 * 1320:(c4 + 1) * 1320], [], [s])
            ph.cp("dve" if i % 2 == 0 else "pool", win[:, k, c4 * 1320:(c4 + 1) * 1320], s[:], [s], [win])
            i += 1
    wkr = ph.sb([128, 8, 96], BF16, "wkr")
    wkrs = ph.sb([128, 8, 96], BF16, "wkrs")
    ph.memset("pool", wkr[:], 0.0, [wkr])
    ph.memset("pool", wkrs[:], 0.0, [wkrs])
    ph.cp("pool", wkr[:, :, 64:96], win[:, :, C_KR:C_KR + 32], [win], [wkr])
    ph.cp("pool", wkrs[:, :, 64:80], win[:, :, C_KR + 16:C_KR + 32], [win], [wkrs])
    ph.cp("pool", wkrs[:, :, 80:96], win[:, :, C_KR:C_KR + 16], [win], [wkrs])
    wgks = ph.sb([128, 8, 128], BF16, "wgks")
    wgqs = ph.sb([128, 8, 512], BF16, "wgqs")
    for (dst, c0, n) in ((wgks, C_GK, 128), (wgqs, C_GQ, 512)):
        sv = win[:, :, c0:c0 + n].rearrange("p k (h two d) -> p k h two d", two=2, d=32)
        dv = dst[:, :, :].rearrange("p k (h two d) -> p k h two d", two=2, d=32)
        ph.cp("pool", dv[:, :, :, 0, :], sv[:, :, :, 1, :], [win], [dst])
        ph.cp("pool", dv[:, :, :, 1, :], sv[:, :, :, 0, :], [win], [dst])
    gq = ph.sb([128, 2], F32, "gq")
    ph.dma(gq[:], dr["mla_gq"][l].rearrange("(c p) -> p c", p=128), [], [gq], allow_slow_non_contiguous=True)
    gkv = ph.sb([128, 1], F32, "gkv")
    ph.dma(gkv[:], dr["mla_gkv"][l].rearrange("(p o) -> p o", o=1), [], [gkv])
    wuqf = ph.sb([128, 2, 768], F32, "wuqf")
    ph.dma(wuqf[:], dr["mla_wuq"][l].rearrange("(c p) n -> p c n", p=128), [], [wuqf])
    wuq = ph.sb([128, 2, 768], BF16, "wuq")
    wuqs = ph.sb([128, 2, 768], BF16, "wuqs")
    for c in range(2):
        ph.ts("dve", wuq[:, c, :], wuqf[:, c, :], gq[:, c:c + 1], None, ALU.mult, None, [wuqf, gq], [wuq])
    ph.cp("pool", wuqs[:], wuq[:], [wuq], [wuqs])
    v1 = wuq[:, :, :].rearrange("p c (h d) -> p c h d", d=96)
    v2 = wuqs[:, :, :].rearrange("p c (h d) -> p c h d", d=96)
    ph.cp("pool", v2[:, :, :, 64:80], v1[:, :, :, 80:96], [wuq], [wuqs])
    ph.cp("pool", v2[:, :, :, 80:96], v1[:, :, :, 64:80], [wuq], [wuqs])
    wkvf = ph.sb([128, 1024], F32, "wkvf")
    ph.dma(wkvf[:], dr["mla_wukv"][l], [], [wkvf])
    wkv = ph.sb([128, 2, 512], BF16, "wkv")
    sv = wkvf[:, :].rearrange("p (h two d) -> p two h d", two=2, d=64)
    for two in range(2):
        ph.ts("dve", wkv[:, two, :].rearrange("p (h d) -> p h d", d=64), sv[:, two, :, :], gkv[:, 0:1], None, ALU.mult, None,
              [wkvf, gkv], [wkv])
    ones = ph.sb([128, 128], BF16, "ones")
    ph.memset("pool", ones[:], 1.0, [ones])
    bones = ph.sb([128, 128], BF16, "bones")
    ph.memset("pool", bones[:], 0.0, [bones])
    ph.memset("pool", bones[0:64, 0:64], 1.0, [bones])
    ph.memset("pool", bones[64:128, 64:128], 1.0, [bones])
    ident = make_identity(ph, BF16, "ident")
    gcol = ph.sb([128, 4], F32, "gcol")
    for j, nm in ((0, "gqa_gq"), (2, "gqa_gk")):
        src = dr[nm][l].rearrange("(d o) -> d o", o=1)
        for hh in range(2):
            ph.dma(gcol[hh * 64:hh * 64 + 64, j:j + 1], src[0:64, :], [], [gcol])
            ph.dma(gcol[hh * 64:hh * 64 + 32, j + 1:j + 2], src[32:64, :], [], [gcol])
            ph.dma(gcol[hh * 64 + 32:hh * 64 + 64, j + 1:j + 2], src[0:32, :], [], [gcol])
    Al = load_bc(ph, dr, 0, 1, "Al")
    Bl = load_bc(ph, dr, 0, 0, "Bl")
    Ac = load_bc(ph, dr, 1, 1, "Ac")
    Bc = load_bc(ph, dr, 1, 0, "Bc")

    xt = [ph.sb([128, D], F32, "xt") for _ in range(2)]
    junk = ph.sb([128, D], F32, "junk")
    hb = [ph.sb([128, D], BF16, "hb") for _ in range(2)]
    ss = ph.sb([128, 1], F32, "ss")
    rstd = ph.sb([128, 1], F32, "rstd")
    hT = [ph.sb([128, 8, 512], BF16, "hT") for _ in range(2)]
    tabm = [ph.sb([96, 2, 512], BF16, "tabm") for _ in range(2)]
    tabg = [ph.sb([128, 2, 512], BF16, "tabg") for _ in range(2)]
    NOB = 6
    ob = [ph.sb([128, 512], BF16, "ob") for _ in range(NOB)]
    NF = 6
    fb = [ph.sb([128, 512], F32, "fb") for _ in range(NF)]
    nck = ph.sb([128, 512], BF16, "nck")
    ncq = [ph.sb([128, 512], BF16, "ncq") for _ in range(2)]
    ctr = {"ob": 0, "fb": 0, "pb": 0, "ev": 0}

    def nob():
        ctr["ob"] += 1
        return ob[ctr["ob"] % NOB]

    def nfb():
        ctr["fb"] += 1
        return fb[ctr["fb"] % NF]

    def npb():
        ctr["pb"] += 1
        return ph.pb[ctr["pb"] % 8]

    def evac_eng():
        ctr["ev"] += 1
        return "act" if ctr["ev"] % 2 == 0 else "dve"

    def proj(lhs_tile, c0, m, hTb, n, extra_r=()):
        pb = npb()
        for k in range(8):
            ph.mm(pb[0:m, 0:n], lhs_tile[:, k, c0:c0 + m], hTb[:, k, 0:n], k == 0, k == 7, [lhs_tile, hTb], [pb])
        return pb

    def store(dst_ap, src_tile, src_ap, eng="pool"):
        ph.dma(dst_ap, src_ap, [src_tile], [], eng=eng)

    bi = 0
    for (t0, n) in token_blocks(0, T):
        hTb = hT[bi % 2]
        tm = tabm[bi % 2]
        tg = tabg[bi % 2]
        ph.dma(tm[64:96, :, 0:n], dr["ropem"][:, :, t0:t0 + n].rearrange("a r t -> r a t"), [], [tm])
        for hh in range(2):
            ph.dma(tg[hh * 64:hh * 64 + 64, :, 0:n], dr["ropeg"][:, :, t0:t0 + n].rearrange("a r t -> r a t"), [], [tg])
        for j in range(n // 128):
            tok = t0 + j * 128
            x_ = xt[j % 2]
            h_ = hb[j % 2]
            ph.dma(x_[:], xsrc[tok:tok + 128, :], [], [x_])
            isctx = tok < NCTX
            rms_modulate(ph, x_, Ac if isctx else Al, Bc if isctx else Bl, h_, junk, ss, rstd)
            pb = npb()
            pbv = pb.t.bitcast(BF16)
            for k in range(8):
                ph.tr(pbv[:, k * 128:(k + 1) * 128], h_[:, k * 128:(k + 1) * 128], ident[:], [h_, ident], [pb])
            ph.cp(evac_eng(), hTb[:, :, j * 128:(j + 1) * 128], pbv[:, :].rearrange("p (k t) -> p k t", t=128), [pb], [hTb])
        sl = slice(t0, t0 + n)
        for c in range(4):
            pb = proj(win, C_XR + c * 128, 128, hTb, n)
            o = nob()
            ph.cp(evac_eng(), o[:, 0:n], pb[:, 0:n], [pb], [o])
            store(dr["xrT"][c * 128:(c + 1) * 128, sl], o, o[:, 0:n])
        for c in range(4):
            pb = proj(win, C_RG + c * 128, 128, hTb, n)
            o = nob()
            ph.act(o[:, 0:n], pb[:, 0:n], AF.Gelu_apprx_tanh, [pb], [o])
            store(dr["rgT"][c * 128:(c + 1) * 128, sl], o, o[:, 0:n])
        for c in range(24):
            pb = proj(win, C_MG + c * 128, 128, hTb, n)
            o = nob()
            ph.act(o[:, 0:n], pb[:, 0:n], AF.Sigmoid, [pb], [o])
            store(dr["gatesT"][c * 128:(c + 1) * 128, sl], o, o[:, 0:n])
        for j in range(n // 128):
            pb = npb()
            for k in range(8):
                ph.mm(pb[:, 0:128], hTb[:, k, j * 128:(j + 1) * 128], win[:, k, C_GV:C_GV + 128], k == 0, k == 7, [hTb, win], [pb])
            o = nob()
            ph.cp(evac_eng(), o[:, 0:128], pb[:, 0:128], [pb], [o])
            store(dr["vg"][t0 + j * 128:t0 + (j + 1) * 128, :], o, o[:, 0:128])

        def rstd_bc(sq_list, ones_t, count):
            pb = npb()
            for i_, sq in enumerate(sq_list):
                ph.mm(pb[:, 0:n], ones_t[:], sq[:, 0:n], i_ == 0, i_ == len(sq_list) - 1, [ones_t, sq], [pb])
            r_ = nfb()
            ph.act(r_[:, 0:n], pb[:, 0:n], AF.Sqrt, [pb], [r_], bias=ph.epsc[:, 0:1], scale=1.0 / count)
            ph.recip(r_[:, 0:n], r_[:, 0:n], [r_], [r_])
            return r_

        pa = proj(win, C_CKV, 128, hTb, n)
        sq = nob()
        ph.act(sq[:, 0:n], pa[:, 0:n], AF.Square, [pa], [sq])
        r_ = rstd_bc([sq], ones, 128)
        ph.tt("dve", nck[:, 0:n], pa[:, 0:n], r_[:, 0:n], ALU.mult, [pa, r_], [nck])
        for hp in range(4):
            pb = npb()
            ph.mm(pb[:, 0:n], wkv[:, 0, hp * 128:(hp + 1) * 128], nck[:, 0:n], True, True, [wkv, nck], [pb])
            o = nob()
            ph.cp(evac_eng(), o[:, 0:n], pb[:, 0:n], [pb], [o])
            for hh in range(2):
                store(dr["kmT"][2 * hp + hh, 0:64, sl], o, o[hh * 64:hh * 64 + 64, 0:n])
        for j in range(n // 128):
            pb = npb()
            ph.mm(pb[:, :], nck[:, j * 128:(j + 1) * 128], wkv[:, 1, :], True, True, [nck, wkv], [pb])
            o = nob()
            ph.cp(evac_eng(), o[:, :], pb[:, :], [pb], [o])
            store(dr["vm"][t0 + j * 128:t0 + (j + 1) * 128, :], o, o[:, :])

        def rope96(pa, pb_, dst_tile):
            t1 = nfb()
            t2 = nfb()
            ph.tt("dve", t1[64:96, 0:n], pa[64:96, 0:n], tm[64:96, 0, 0:n], ALU.mult, [pa, tm], [t1])
            ph.tt("dve", t2[64:96, 0:n], pb_[64:96, 0:n], tm[64:96, 1, 0:n], ALU.mult, [pb_, tm], [t2])
            ph.tt("pool", dst_tile[64:96, 0:n], t1[64:96, 0:n], t2[64:96, 0:n], ALU.add, [t1, t2], [dst_tile])

        pa = proj(wkr, 0, 96, hTb, n)
        pb_ = proj(wkrs, 0, 96, hTb, n)
        o = nob()
        rope96(pa, pb_, o)
        for h in range(8):
            store(dr["kmT"][h, 64:96, sl], o, o[64:96, 0:n], eng="sp" if h % 2 else "pool")
        pc = [proj(win, C_CQ + c * 128, 128, hTb, n) for c in range(2)]
        sqs = []
        for c in range(2):
            s_ = nob()
            ph.act(s_[:, 0:n], pc[c][:, 0:n], AF.Square, [pc[c]], [s_])
            sqs.append(s_)
        r_ = rstd_bc(sqs, ones, 256)
        for c in range(2):
            ph.tt("dve", ncq[c][:, 0:n], pc[c][:, 0:n], r_[:, 0:n], ALU.mult, [pc[c], r_], [ncq[c]])
        for h in range(8):
            pa = npb()
            pb_ = npb()
            for c in range(2):
                ph.mm(pa[0:96, 0:n], wuq[:, c, h * 96:(h + 1) * 96], ncq[c][:, 0:n], c == 0, c == 1, [wuq, ncq[c]], [pa])
            for c in range(2):
                ph.mm(pb_[0:96, 0:n], wuqs[:, c, h * 96:(h + 1) * 96], ncq[c][:, 0:n], c == 0, c == 1, [wuqs, ncq[c]], [pb_])
            o = nob()
            ph.cp("act", o[0:64, 0:n], pa[0:64, 0:n], [pa], [o])
            rope96(pa, pb_, o)
            store(dr["qmT"][h, :, sl], o, o[0:96, 0:n], eng="sp" if h % 2 else "pool")

        def gqa_chunk(c0, wsw, csw, gj, dst_fn):
            pa = proj(win, c0, 128, hTb, n)
            pb_ = proj(wsw, csw, 128, hTb, n)
            sq = nob()
            ph.act(sq[:, 0:n], pa[:, 0:n], AF.Square, [pa], [sq])
            r_ = rstd_bc([sq], bones, 64)
            t1 = nfb()
            t2 = nfb()
            ph.stt("dve", t1[:, 0:n], pa[:, 0:n], gcol[:, gj:gj + 1], tg[:, 0, 0:n], ALU.mult, ALU.mult, [pa, gcol, tg], [t1])
            ph.stt("dve", t2[:, 0:n], pb_[:, 0:n], gcol[:, gj + 1:gj + 2], tg[:, 1, 0:n], ALU.mult, ALU.mult, [pb_, gcol, tg], [t2])
            ph.tt("pool", t1[:, 0:n], t1[:, 0:n], t2[:, 0:n], ALU.add, [t1, t2], [t1])
            o = nob()
            ph.tt("pool", o[:, 0:n], t1[:, 0:n], r_[:, 0:n], ALU.mult, [t1, r_], [o])
            dst_fn(o)

        def st_gk(o):
            for hh in range(2):
                store(dr["kgT"][hh, :, sl], o, o[hh * 64:hh * 64 + 64, 0:n])
        gqa_chunk(C_GK, wgks, 0, 2, st_gk)
        for c in range(4):
            def st_gq(o, c=c):
                for hh in range(2):
                    store(dr["qgT"][2 * c + hh, :, sl], o, o[hh * 64:hh * 64 + 64, 0:n])
            gqa_chunk(C_GQ + c * 128, wgqs, c * 128, 0, st_gq)
        bi += 1


def phase_lru(ph, dr, l):
    blocks = token_blocks(0, T)
    xp = ph.sb([128, T + 6], BF16, "xp")
    xc = ph.sb([128, T], F32, "xc")
    xcb = ph.sb([128, T], BF16, "xcb")
    rg = ph.sb([128, T], BF16, "rg")
    ysum = ph.sb([128, T], F32, "ysum")
    abuf = ph.sb([128, T], F32, "abuf")
    ibuf = ph.sb([128, T], F32, "ibuf")
    mbuf = ph.sb([128, T], F32, "mbuf")
    hbuf = ph.sb([128, T], F32, "hbuf")
    yo = ph.sb([128, T], BF16, "yo")
    for c in range(4):
        cs = slice(c * 128, (c + 1) * 128)
        ph.memset("pool", xp[:, 0:2], 0.0, [xp])
        ph.memset("pool", xp[:, 258:261], 0.0, [xp])
        ph.memset("pool", xp[:, T + 5:T + 6], 0.0, [xp])
        ph.dma(xp[:, 2:258], dr["xrT"][cs, 0:NCTX], [], [xp])
        ph.dma(xp[:, 261:261 + SEQ], dr["xrT"][cs, NCTX:T], [], [xp])
        cw = ph.sb([128, 4], F32, "cw")
        ph.dma(cw[:], dr["conv_w"][l].rearrange("j c -> c j")[cs, :], [], [cw], allow_slow_non_contiguous=True)
        cb = ph.sb([128, 1], F32, "cb")
        ph.dma(cb[:], dr["conv_b"][l].rearrange("(c o) -> c o", o=1)[cs, :], [], [cb])
        for (o0, i0, n) in ((0, 0, NCTX), (NCTX, 259, SEQ)):
            ph.ts("dve", xc[:, o0:o0 + n], xp[:, i0:i0 + n], cw[:, 0:1], cb[:, 0:1], ALU.mult, ALU.add, [xp, cw, cb], [xc])
            for j in range(1, 4):
                ph.stt("dve", xc[:, o0:o0 + n], xp[:, i0 + j:i0 + j + n], cw[:, j:j + 1], xc[:, o0:o0 + n],
                       ALU.mult, ALU.add, [xp, cw, xc], [xc])
        ph.cp("act", xcb[:], xc[:], [xc], [xcb])
        ph.dma(rg[:], dr["rgT"][cs, :], [], [rg])
        for d in range(2):
            wst = ph.sb([128, 2, 128], F32, "wst")
            ph.memset("pool", wst[:], 0.0, [wst])
            for g_, nm in enumerate(("lru_wa", "lru_wi")):
                for hb_ in range(2):
                    ph.dma(wst[hb_ * 64:hb_ * 64 + 64, g_, hb_ * 64:hb_ * 64 + 64], dr[nm][l, d, 2 * c + hb_], [wst], [wst])
            wbd = ph.sb([128, 2, 128], BF16, "wbd")
            ph.cp("pool", wbd[:], wst[:], [wst], [wbd])
            col = ph.sb([128, 8], F32, "col")
            for j, nm in enumerate(("lru_ba", "lru_bi", "lru_lambda")):
                ph.dma(col[:, j:j + 1], dr[nm][l, d].rearrange("(c o) -> c o", o=1)[cs, :], [], [col])
            ph.act(col[:, 4:5], col[:, 2:3], AF.Exp, [col], [col], scale=-1.0)
            ph.act(col[:, 5:6], col[:, 4:5], AF.Ln, [col], [col], bias=1.0)
            ph.ts("dve", col[:, 2:3], col[:, 5:6], -8.0, None, ALU.mult, None, [col], [col])
            ph.ts("dve", col[:, 3:4], col[:, 5:6], -16.0, None, ALU.mult, None, [col], [col])
            for bi, (t0, n) in enumerate(blocks):
                pr = ph.pb[(2 * bi) % 8]
                pi = ph.pb[(2 * bi + 1) % 8]
                ph.mm(pr[:, 0:n], wbd[:, 0, :], xcb[:, t0:t0 + n], True, True, [wbd, xcb], [pr])
                ph.mm(pi[:, 0:n], wbd[:, 1, :], xcb[:, t0:t0 + n], True, True, [wbd, xcb], [pi])
                ph.act(abuf[:, t0:t0 + n], pr[:, 0:n], AF.Sigmoid, [pr, col], [abuf], bias=col[:, 0:1])
                ph.act(ibuf[:, t0:t0 + n], pi[:, 0:n], AF.Sigmoid, [pi, col], [ibuf], bias=col[:, 1:2])
            ph.act(mbuf[:], abuf[:], AF.Exp, [abuf, col], [mbuf], scale=col[:, 3:4])
            ph.act(abuf[:], abuf[:], AF.Exp, [abuf, col], [abuf], scale=col[:, 2:3])
            ph.act(mbuf[:], mbuf[:], AF.Sqrt, [mbuf], [mbuf], bias=1.0, scale=-1.0)
            ph.tt("pool", ibuf[:], ibuf[:], mbuf[:], ALU.mult, [ibuf, mbuf], [ibuf])
            ph.tt("pool", ibuf[:], ibuf[:], xc[:], ALU.mult, [ibuf, xc], [ibuf])
            dst = ysum if d == 0 else hbuf
            if d == 0:
                ph.add("dve", lambda e, dst=dst: e.tensor_tensor_scan(out=dst[:, :], data0=abuf[:, :], data1=ibuf[:, :], initial=0.0,
                                                                      op0=ALU.mult, op1=ALU.add), [abuf, ibuf], [dst])
            else:
                ph.add("dve", lambda e, dst=dst: e.tensor_tensor_scan(out=dst[:, 0:NCTX][:, ::-1], data0=abuf[:, 0:NCTX][:, ::-1],
                                                                      data1=ibuf[:, 0:NCTX][:, ::-1], initial=0.0,
                                                                      op0=ALU.mult, op1=ALU.add), [abuf, ibuf], [dst])
                ph.add("dve", lambda e, dst=dst: e.tensor_tensor_scan(out=dst[:, NCTX:T][:, ::-1], data0=abuf[:, NCTX:T][:, ::-1],
                                                                      data1=ibuf[:, NCTX:T][:, ::-1], initial=dst[:, 0:1],
                                                                      op0=ALU.mult, op1=ALU.add), [abuf, ibuf, dst], [dst])
                ph.tt("pool", ysum[:], ysum[:], hbuf[:], ALU.add, [ysum, hbuf], [ysum])
        ph.tt("pool", yo[:], ysum[:], rg[:], ALU.mult, [ysum, rg], [yo])
        ph.dma(dr["yT"][0, cs, :], yo[:], [yo], [])


def phase_attn(ph, dr, l, with_ctx):
    onesf = ph.sb([128, 64], F32, "onesf")
    ph.memset("pool", onesf[:], 1.0, [onesf])
    NB = 2
    kT = [ph.sb([96, T], BF16, "kT") for _ in range(NB)]
    qT = [ph.sb([96, T], BF16, "qT") for _ in range(NB)]
    va = [ph.sb([128, NT, 65], BF16, "va") for _ in range(NB)]
    for v_ in va:
        ph.memset("pool", v_[:, :, 64:65], 1.0, [v_])
    NPT = 4
    pT = [ph.sb([128, 1024], BF16, "pT") for _ in range(NPT)]
    osb = [ph.sb([65, 512], F32, "osb") for _ in range(2)]
    yo = [ph.sb([64, 512], BF16, "yo") for _ in range(2)]
    sp_ = [Tl(ph.ps[:, 0:1024], "S0"), Tl(ph.ps[:, 1024:2048], "S1"), Tl(ph.ps[:, 2048:3072], "S2")]
    acc = [ph.pb[6], ph.pb[7]]
    cnt = {"s": 0, "p": 0, "a": 0}
    hi = 0
    for br in (1, 2):
        d = 96 if br == 1 else 64
        scale = MLA_SCALE if br == 1 else GQA_SCALE
        for h in range(8):
            k_, q_, v_ = kT[hi % NB], qT[hi % NB], va[hi % NB]
            hi += 1
            if br == 1:
                ph.dma(k_[0:96, :], dr["kmT"][h], [], [k_])
                ph.dma(q_[0:96, :], dr["qmT"][h], [], [q_])
                vsrc = dr["vm"][:, h * 64:(h + 1) * 64].rearrange("(c p) d -> p c d", p=128)
                ph.dma(v_[:, 0:17, 0:64], vsrc[:, 0:17, :], [], [v_])
                ph.dma(v_[:, 17:NT, 0:64], vsrc[:, 17:NT, :], [], [v_])
            else:
                ph.dma(k_[0:64, :], dr["kgT"][h // 4], [], [k_])
                ph.dma(q_[0:64, :], dr["qgT"][h], [], [q_])
                vsrc = dr["vg"][:, (h // 4) * 64:(h // 4 + 1) * 64].rearrange("(c p) d -> p c d", p=128)
                ph.dma(v_[:, 0:17, 0:64], vsrc[:, 0:17, :], [], [v_])
                ph.dma(v_[:, 17:NT, 0:64], vsrc[:, 17:NT, :], [], [v_])
            qblocks = [(t0, n, NT) for (t0, n) in token_blocks(NCTX, T)]
            if with_ctx:
                qblocks = [(0, NCTX, NCTX // 128)] + qblocks
            for (t0, n, nkc) in qblocks:
                a_ = acc[cnt["a"] % 2]
                cnt["a"] += 1
                for g0 in range(0, nkc, 2):
                    s_ = sp_[cnt["s"] % 3]
                    cnt["s"] += 1
                    p_ = pT[cnt["p"] % NPT]
                    cnt["p"] += 1
                    for u in range(2):
                        kc = g0 + u
                        ph.mm(s_[:, u * 512:u * 512 + n], k_[0:d, kc * 128:(kc + 1) * 128], q_[0:d, t0:t0 + n], True, True, [k_, q_], [s_])
                    if n == 512:
                        ph.act(p_[:, :], s_[:, :], AF.Exp, [s_], [p_], scale=scale)
                    else:
                        sv = s_[:, :].rearrange("p (u t) -> p u t", u=2)[:, :, 0:n]
                        pv = p_[:, :].rearrange("p (u t) -> p u t", u=2)[:, :, 0:n]
                        ph.act(pv, sv, AF.Exp, [s_], [p_], scale=scale)
                    for u in range(2):
                        kc = g0 + u
                        ph.mm(a_[0:65, 0:n], v_[:, kc, :], p_[:, u * 512:u * 512 + n], kc == 0, kc == nkc - 1, [v_, p_], [a_])
                o_ = osb[cnt["a"] % 2]
                y_ = yo[cnt["a"] % 2]
                ph.cp("act", o_[:, 0:n], a_[0:65, 0:n], [a_], [o_])
                ph.recip(o_[64:65, 0:n], o_[64:65, 0:n], [o_], [o_])
                bc = a_
                ph.mm(bc[0:64, 0:n], onesf[64:65, 0:64], o_[64:65, 0:n], True, True, [onesf, o_], [bc])
                ph.tt("dve", y_[:, 0:n], o_[0:64, 0:n], bc[0:64, 0:n], ALU.mult, [o_, bc], [y_])
                ph.dma(dr["yT"][br, h * 64:(h + 1) * 64, t0:t0 + n], y_[:, 0:n], [y_], [], eng="pool")


def phase_merge(ph, dr, l, xsrc, tstart):
    eps_const(ph)
    wbr = ph.sb([128, 3, 4, D], BF16, "wbr")
    wo = ph.sb([128, 8, D], BF16, "wo")
    stg = [ph.sb([128, 2, D], F32, "stg") for _ in range(2)]
    si = 0
    for k in range(3):
        for hf in range(2):
            s = stg[si % 2]
            ph.dma(s[:], dr["w_branch"][l, k].rearrange("(c p) n -> p c n", p=128)[:, hf * 2:(hf + 1) * 2, :], [], [s])
            ph.cp("dve" if si % 2 else "pool", wbr[:, k, hf * 2:(hf + 1) * 2, :], s[:], [s], [wbr])
            si += 1
    for hf in range(4):
        s = stg[si % 2]
        ph.dma(s[:], dr["w_out"][l].rearrange("(c p) n -> p c n", p=128)[:, hf * 2:(hf + 1) * 2, :], [], [s])
        ph.cp("dve" if si % 2 else "pool", wo[:, hf * 2:(hf + 1) * 2, :], s[:], [s], [wo])
        si += 1
    wr = ph.sb([128, 8, 36], F32, "wr")
    ph.dma(wr[:, :, 0:4], dr["moe_wg"][l].rearrange("(c p) n -> p c n", p=128), [], [wr])
    ph.dma(wr[:, :, 4:36], dr["moe_we"][l].rearrange("(c p) n -> p c n", p=128), [], [wr])
    br_ = ph.sb([128, 36], F32, "br")
    ph.dma(br_[:, 0:4], dr["moe_bg"][l:l + 1, :].partition_broadcast(128), [], [br_])
    ph.dma(br_[:, 4:36], dr["moe_be"][l:l + 1, :].partition_broadcast(128), [], [br_])
    wrb = ph.sb([128, 8, 36], BF16, "wrb")
    ph.cp("pool", wrb[:], wr[:], [wr], [wrb])
    identb = make_identity(ph, BF16, "identb")
    hb16 = [ph.sb([128, D], BF16, "hb16") for _ in range(2)]
    G = {}
    for row in ((0, 1) if tstart == 0 else (0,)):
        G[row] = (load_bc(ph, dr, row, 2, "Ga"), load_bc(ph, dr, row, 4, "Af"), load_bc(ph, dr, row, 3, "Bf"))
    yb = [ph.sb([128, 3, 4, 512], BF16, "yb") for _ in range(2)]
    gb = [ph.sb([128, 24, 512], BF16, "gb") for _ in range(1)]
    mg = [ph.sb([128, 8, 512], BF16, "mgd") for _ in range(2)]
    tmpf = [ph.sb([128, 512], F32, "tmpf") for _ in range(3)]
    accf = ph.sb([128, 512], F32, "accf")
    xt = [ph.sb([128, D], F32, "xt") for _ in range(2)]
    x2 = [ph.sb([128, D], F32, "x2") for _ in range(2)]
    junk = ph.sb([128, D], F32, "junk")
    hTb = [ph.sb([128, 8, 128], BF16, "hTb") for _ in range(2)]
    ss = ph.sb([128, 1], F32, "ss")
    rstd = ph.sb([128, 1], F32, "rstd")
    sm = ph.sb([128, 64], F32, "sm")
    lg = ph.sb([128, 36], F32, "lg")
    mk = ph.sb([128, 4], F32, "mk")
    es = ph.sb([128, 4, 8], F32, "es")
    e8 = ph.sb([128, 8], F32, "e8")
    m8 = ph.sb([128, 8], F32, "m8")
    cb_ = [ph.sb([128, 32], F32, "comb") for _ in range(2)]
    c1 = ph.sb([128, 32], F32, "c1")
    c2 = ph.sb([128, 32], F32, "c2")
    ctr = {"pb": 0, "t": 0}

    def npb():
        ctr["pb"] += 1
        return ph.pb[ctr["pb"] % 8]

    bi = 0
    for (t0, n) in token_blocks(tstart, T):
        if DBG_STOP < 1:
            break
        sl = slice(t0, t0 + n)
        y_, g_, m_ = yb[bi % 2], gb[0], mg[bi % 2]
        bi += 1
        for k in range(3):
            ph.dma(y_[:, k, :, 0:n], dr["yT"][k, :, sl].rearrange("(c p) t -> p c t", p=128), [], [y_])
        for k in range(3):
            ph.dma(g_[:, k * 8:(k + 1) * 8, 0:n], dr["gatesT"][k * D:(k + 1) * D, sl].rearrange("(c p) t -> p c t", p=128), [], [g_])
        for oc in range(8):
            for k in range(3):
                pb = npb()
                for c in range(4):
                    ph.mm(pb[:, 0:n], wbr[:, k, c, oc * 128:(oc + 1) * 128], y_[:, k, c, 0:n], c == 0, c == 3, [wbr, y_], [pb])
                if k == 0:
                    ph.tt("dve", accf[:, 0:n], pb[:, 0:n], g_[:, oc, 0:n], ALU.mult, [pb, g_], [accf])
                else:
                    tf = tmpf[ctr["t"] % 3]
                    ctr["t"] += 1
                    ph.tt("dve", tf[:, 0:n], pb[:, 0:n], g_[:, k * 8 + oc, 0:n], ALU.mult, [pb, g_], [tf])
                    if k == 1:
                        ph.tt("pool", accf[:, 0:n], accf[:, 0:n], tf[:, 0:n], ALU.add, [accf, tf], [accf])
                    else:
                        ph.tt("pool", m_[:, oc, 0:n], accf[:, 0:n], tf[:, 0:n], ALU.add, [accf, tf], [m_])
        for j in range(n // 128):
            if DBG_STOP < 2:
                break
            tok = t0 + j * 128
            row = 1 if tok < NCTX else 0
            Ga, Af, Bf = G[row]
            x_ = xt[j % 2]
            xo = x2[j % 2]
            ph.dma(x_[:], xsrc[tok:tok + 128, :], [], [x_])
            for hf in range(2):
                pb = npb()
                for c in range(8):
                    ph.mm(pb[:, :], m_[:, c, j * 128:(j + 1) * 128], wo[:, c, hf * 512:(hf + 1) * 512], c == 0, c == 7, [m_, wo], [pb])
                ph.tt("dve", junk[:, hf * 512:(hf + 1) * 512], pb[:, :], Ga[:, hf * 512:(hf + 1) * 512], ALU.mult, [pb, Ga], [junk])
            ph.tt("pool", xo[:], junk[:], x_[:], ALU.add, [junk, x_], [xo])
            ph.dma(dr["x2"][tok:tok + 128, :], xo[:], [xo], [], eng="pool")
            if DBG_STOP < 3:
                continue
            hbt = hb16[j % 2]
            rms_modulate(ph, xo, Af, Bf, hbt, junk, ss, rstd)
            hb_ = hTb[j % 2]
            pb = npb()
            pbv = pb.t.bitcast(BF16)
            for c in range(8):
                ph.tr(pbv[:, c * 128:(c + 1) * 128], hbt[:, c * 128:(c + 1) * 128], identb[:], [hbt, identb], [pb])
            ph.cp("act", hb_[:, :, :], pbv[:, :].rearrange("p (c t) -> p c t", t=128), [pb], [hb_])
            ph.dma(dr["h2T"][:, tok:tok + 128].rearrange("(c p) t -> p c t", p=128), hb_[:], [hb_], [], eng="pool")
            if DBG_SKIP_ROUTER:
                continue
            pb = npb()
            for c in range(8):
                ph.mm(pb[:, 0:36], hb_[:, c, :], wrb[:, c, :], c == 0, c == 7, [hb_, wrb], [pb])
            ph.tt("dve", lg[:], pb[:, 0:36], br_[:], ALU.add, [pb, br_], [lg])
            ph.add("dve", lambda e: e.tensor_reduce(out=sm[:, 0:1], in_=lg[:, 0:4], axis=AX.X, op=ALU.max), _bufs([lg]), _bufs([sm]))
            ph.ts("dve", mk[:], lg[:, 0:4], sm[:, 0:1], None, ALU.is_equal, None, [lg, sm], [mk])
            ph.ts("dve", sm[:, 1:2], sm[:, 0:1], -1.0, None, ALU.mult, None, [sm], [sm])
            ph.act(sm[:, 8:12], lg[:, 0:4], AF.Exp, [lg, sm], [sm], bias=sm[:, 1:2], accum=sm[:, 2:3])
            ph.recip(sm[:, 3:4], sm[:, 2:3], [sm], [sm])
            ev = lg[:, 4:36].rearrange("p (g e) -> p g e", e=8)
            ph.tt("dve", es[:], ev, mk[:, :].unsqueeze(2).to_broadcast([128, 4, 8]), ALU.mult, [lg, mk], [es])
            ph.add("dve", lambda e: e.tensor_reduce(out=e8[:], in_=es[:].rearrange("p g e -> p e g"), axis=AX.X, op=ALU.add),
                   _bufs([es]), _bufs([e8]))
            ph.add("dve", lambda e: e.max(out=m8[:], in_=e8[:]), _bufs([e8]), _bufs([m8]))
            ph.tt("dve", sm[:, 4:5], m8[:, 0:1], m8[:, 1:2], ALU.subtract, [m8], [sm])
            ph.act(sm[:, 5:6], sm[:, 4:5], AF.Sigmoid, [sm], [sm])
            ph.tt("dve", sm[:, 5:6], sm[:, 5:6], sm[:, 3:4], ALU.mult, [sm], [sm])
            ph.tt("dve", sm[:, 6:7], sm[:, 3:4], sm[:, 5:6], ALU.subtract, [sm], [sm])
            cbt = cb_[j % 2]
            ph.ts("dve", c1[:], lg[:, 4:36], m8[:, 0:1], sm[:, 5:6], ALU.is_equal, ALU.mult, [lg, m8, sm], [c1])
            ph.ts("dve", c2[:], lg[:, 4:36], m8[:, 1:2], sm[:, 6:7], ALU.is_equal, ALU.mult, [lg, m8, sm], [c2])
            ph.tt("dve", c1[:], c1[:], c2[:], ALU.add, [c1, c2], [c1])
            ph.tt("dve", cbt[:].rearrange("p (g e) -> p g e", e=8), c1[:].rearrange("p (g e) -> p g e", e=8),
                  mk[:, :].unsqueeze(2).to_broadcast([128, 4, 8]), ALU.mult, [c1, mk], [cbt])
            ph.dma(dr["comb"][tok:tok + 128, :], cbt[:], [cbt], [], eng="pool")


def phase_moe(ph, dr, l, tstart, final):
    eps_const(ph)
    ntok = T - tstart
    half = ntok // 2
    assert half % 128 == 0
    nth = half // 128
    Gf = {}
    for row in ((0, 1) if tstart == 0 else (0,)):
        Gf[row] = load_bc(ph, dr, row, 5, "Gf")
    if final:
        gfin = ph.sb([128, D], F32, "gfin")
        ph.dma(gfin[:], dr["g_final"].rearrange("(o n) -> o n", o=1).partition_broadcast(128), [], [gfin])
    hT = ph.sb([128, 8, half], BF16, "hT")
    acc = ph.sb([128, nth, D], F32, "acc")
    comb = ph.sb([128, nth, NE], F32, "comb")
    s13 = [ph.sb([128, 8, 256], F32, "s13") for _ in range(2)]
    s2 = [ph.sb([128, 2, D], F32, "s2") for _ in range(1)]
    w13 = [ph.sb([128, 8, 512], BF16, "w13") for _ in range(2)]
    w2 = [ph.sb([128, 2, D], BF16, "w2") for _ in range(2)]
    sg = [ph.sb([128, 2, 512], F32, "sg") for _ in range(2)]
    he = [ph.sb([128, 2, 512], BF16, "he") for _ in range(2)]
    xt = [ph.sb([128, D], F32, "xt") for _ in range(2)]
    junk = ph.sb([128, D], F32, "junk")
    ss = ph.sb([128, 1], F32, "ss")
    rstd = ph.sb([128, 1], F32, "rstd")
    ctr = {"he": 0, "pd": 0}
    for hf in range(2):
        h0 = tstart + hf * half
        ph.dma(hT[:, :, :], dr["h2T"][:, h0:h0 + half].rearrange("(c p) t -> p c t", p=128), [], [hT])
        ph.dma(comb[:, :, :], dr["comb"][h0:h0 + half, :].rearrange("(j p) e -> p j e", p=128), [], [comb])
        for e_ in range(NE):
            a2, b13, b2 = s2[0], w13[e_ % 2], w2[e_ % 2]
            ph.dma(s13[0][:], dr["moe_w1"][l, e_].rearrange("(c p) n -> p c n", p=128), [], [s13[0]])
            ph.dma(s13[1][:], dr["moe_w3"][l, e_].rearrange("(c p) n -> p c n", p=128), [], [s13[1]])
            ph.dma(a2[:], dr["moe_w2"][l, e_].rearrange("(c p) n -> p c n", p=128), [], [a2])
            ph.cp("pool", b13[:, :, 0:256], s13[0][:], [s13[0]], [b13])
            ph.cp("pool", b13[:, :, 256:512], s13[1][:], [s13[1]], [b13])
            ph.cp("pool", b2[:], a2[:], [a2], [b2])
            for (b0, n) in token_blocks(0, half):
                pbs = [ph.pb[m] for m in range(4)]
                for m in range(4):
                    for c in range(8):
                        ph.mm(pbs[m][:, 0:n], b13[:, c, m * 128:(m + 1) * 128], hT[:, c, b0:b0 + n], c == 0, c == 7, [b13, hT], [pbs[m]])
                s_ = sg[ctr["he"] % 2]
                h_ = he[ctr["he"] % 2]
                ctr["he"] += 1
                for m in range(2):
                    ph.act(s_[:, m, 0:n], pbs[m][:, 0:n], AF.Silu, [pbs[m]], [s_])
                    ph.tt("dve", h_[:, m, 0:n], pbs[2 + m][:, 0:n], s_[:, m, 0:n], ALU.mult, [pbs[2 + m], s_], [h_])
                for j in range(n // 128):
                    tj = (b0 // 128) + j
                    for nh in range(2):
                        pd = ph.pb[4 + ctr["pd"] % 4]
                        ctr["pd"] += 1
                        for c in range(2):
                            ph.mm(pd[:, :], h_[:, c, j * 128:(j + 1) * 128], b2[:, c, nh * 512:(nh + 1) * 512], c == 0, c == 1, [h_, b2], [pd])
                        asl = acc[:, tj, nh * 512:(nh + 1) * 512]
                        if e_ == 0:
                            ph.ts("dve", asl, pd[:, :], comb[:, tj, e_:e_ + 1], None, ALU.mult, None, [pd, comb], [acc])
                        else:
                            ph.stt("dve", asl, pd[:, :], comb[:, tj, e_:e_ + 1], asl, ALU.mult, ALU.add, [pd, comb, acc], [acc])
        for tj in range(nth):
            tok = h0 + tj * 128
            row = 1 if tok < NCTX else 0
            x_ = xt[tj % 2]
            ph.dma(x_[:], dr["x2"][tok:tok + 128, :], [], [x_])
            ph.tt("pool", acc[:, tj, :], acc[:, tj, :], Gf[row][:], ALU.mult, [acc, Gf[row]], [acc])
            ph.tt("pool", x_[:], x_[:], acc[:, tj, :], ALU.add, [x_, acc], [x_])
            if not final:
                ph.dma(dr["xres"][tok:tok + 128, :], x_[:], [x_], [], eng="pool")
            else:
                ph.act(junk[:], x_[:], AF.Square, [x_], [junk, ss], accum=ss[:, 0:1])
                ph.act(rstd[:, 0:1], ss[:, 0:1], AF.Sqrt, [ss], [rstd], bias=ph.epsc[:, 0:1], scale=1.0 / D)
                ph.recip(rstd[:, 0:1], rstd[:, 0:1], [rstd], [rstd])
                ph.stt("dve", x_[:], x_[:], rstd[:, 0:1], gfin[:], ALU.mult, ALU.mult, [x_, rstd, gfin], [x_])
                ph.dma(dr["out"][tok - NCTX:tok - NCTX + 128, :], x_[:], [x_], [], eng="pool")


WEIGHTS = [("w_mod", [2, D, 6 * D]), ("b_mod", [2, 6 * D]), ("g_mix", [2, D]), ("g_ffn", [2, D]), ("w_in", [2, D, DIN]),
           ("conv_w", [2, 4, 512]), ("conv_b", [2, 512]), ("lru_wa", [2, 2, 8, 64, 64]), ("lru_ba", [2, 2, 512]),
           ("lru_wi", [2, 2, 8, 64, 64]), ("lru_bi", [2, 2, 512]), ("lru_lambda", [2, 2, 512]), ("mla_gq", [2, 256]),
           ("mla_wuq", [2, 256, 768]), ("mla_gkv", [2, 128]), ("mla_wukv", [2, 128, 1024]), ("gqa_gq", [2, 64]),
           ("gqa_gk", [2, 64]), ("w_branch", [2, 3, 512, D]), ("w_out", [2, D, D]), ("moe_wg", [2, D, 4]), ("moe_bg", [2, 4]),
           ("moe_we", [2, D, 32]), ("moe_be", [2, 32]), ("moe_w1", [2, NE, D, DE]), ("moe_w3", [2, NE, D, DE]),
           ("moe_w2", [2, NE, DE, D]), ("g_final", [D])]

SCRATCH = [("modv", [2, 6 * D], F32), ("xres", [T, D], F32), ("x2", [T, D], F32), ("xrT", [512, T], BF16),
           ("rgT", [512, T], BF16), ("gatesT", [3 * D, T], BF16), ("kmT", [8, 96, T], BF16), ("qmT", [8, 96, T], BF16),
           ("vm", [T, 512], BF16), ("kgT", [2, 64, T], BF16), ("qgT", [8, 64, T], BF16), ("vg", [T, 128], BF16),
           ("yT", [3, 512, T], BF16), ("h2T", [D, T], BF16), ("comb", [T, NE], F32)]


def build_nc(phases=None, debug=()):
    nc = bass.Bass("TRN2", target_bir_lowering=False)
    dr = {}
    dr["xin"] = nc.dram_tensor("xin", [T, D], F32, kind="ExternalInput").ap()
    dr["cc"] = nc.dram_tensor("cc", [2, D], F32, kind="ExternalInput").ap()
    dr["ropem"] = nc.dram_tensor("ropem", [2, 32, T], BF16, kind="ExternalInput").ap()
    dr["ropeg"] = nc.dram_tensor("ropeg", [2, 64, T], BF16, kind="ExternalInput").ap()
    for nm, shp in WEIGHTS:
        dr[nm] = nc.dram_tensor(nm, shp, F32, kind="ExternalInput").ap()
    dr["out"] = nc.dram_tensor("out", [SEQ, D], F32, kind="ExternalOutput").ap()
    for nm, shp, dt in SCRATCH:
        if nm in debug:
            dr[nm] = nc.dram_tensor(nm, shp, dt, kind="ExternalOutput").ap()
        else:
            dr[nm] = nc.dram_tensor(nm, shp, dt).ap()
    ps = nc.alloc_psum_tensor("ps", [128, 4096], F32)
    for l in range(2):
        xsrc = dr["xin"] if l == 0 else dr["xres"]
        last = l == 1
        tstart = NCTX if last else 0
        plan = [("mod", phase_mod, (dr, l)), ("inproj", phase_inproj, (dr, l, xsrc)), ("lru", phase_lru, (dr, l)),
                ("attn", phase_attn, (dr, l, not last)), ("merge", phase_merge, (dr, l, xsrc, tstart)),
                ("moe", phase_moe, (dr, l, tstart, last))]
        for nm, fn, args in plan:
            if phases is not None and (l, nm) not in phases:
                continue
            run_phase(nc, ps, fn, *args)
    return nc


def rope_consts():
    def tab(rot):
        q = rot // 4
        pos = np.arange(SEQ)
        row = (pos // 64).astype(np.float32)
        col = (pos % 64).astype(np.float32)
        freqs = (np.float32(10000.0) ** (-np.arange(q, dtype=np.float32) / np.float32(q))).astype(np.float32)
        ang = np.concatenate([row[:, None] * freqs, col[:, None] * freqs], axis=-1).astype(np.float32)
        cos, sin = np.cos(ang).T, np.sin(ang).T
        C = np.ones((rot, T), np.float32)
        S = np.zeros((rot, T), np.float32)
        C[:, NCTX:] = np.concatenate([cos, cos], axis=0)
        S[:, NCTX:] = np.concatenate([-sin, sin], axis=0)
        return np.stack([C, S]).astype(ml_dtypes.bfloat16)
    return tab(32), tab(64)


_CACHE = {}


def kernel(**inputs):
    x = np.asarray(inputs["x"], np.float32)
    ctx = np.asarray(inputs["ctx"], np.float32)
    c = np.asarray(inputs["c"], np.float32)
    c_ctx = np.asarray(inputs["c_ctx"], np.float32)
    B = x.shape[0]
    if "nc" not in _CACHE:
        _CACHE["nc"] = build_nc()
    nc = _CACHE["nc"]
    ropem, ropeg = rope_consts()
    shared = {nm: np.ascontiguousarray(np.asarray(inputs[nm], np.float32)) for nm, _ in WEIGHTS}
    shared["ropem"] = ropem
    shared["ropeg"] = ropeg
    in_maps = []
    for b in range(B):
        m = dict(shared)
        m["xin"] = np.ascontiguousarray(np.concatenate([ctx[b], x[b]], axis=0))
        m["cc"] = np.ascontiguousarray(np.stack([c[b], c_ctx], axis=0))
        in_maps.append(m)
    res = run_bass_kernel_spmd(nc, in_maps, core_ids=list(range(B)))
    return np.stack([np.asarray(r["out"], np.float32) for r in res.results], axis=0)
```

```python
import numpy as np
import ml_dtypes
import concourse.bass as bass
import concourse.mybir as mybir
from concourse.bass_utils import run_bass_kernel_spmd

F32 = mybir.dt.float32
BF16 = mybir.dt.bfloat16
AF = mybir.ActivationFunctionType
ALU = mybir.AluOpType
AX = mybir.AxisListType

D = 1024
NCTX = 256
SEQ = 4096
T = NCTX + SEQ
NT = T // 128
DIN = 5280
EPS = 1e-6
C_XR, C_CKV, C_KR, C_GK, C_GV, C_RG, C_CQ, C_GQ, C_MG = 0, 512, 640, 672, 800, 928, 1440, 1696, 2208
MLA_SCALE = 96 ** -0.5
GQA_SCALE = 64 ** -0.5
NE = 32
DE = 256
import os as _os
DBG_SKIP_ROUTER = bool(_os.environ.get('DBG_SKIP_ROUTER'))
DBG_STOP = int(_os.environ.get('DBG_STOP', '99'))


class Buf:
    __slots__ = ("name", "lastw", "readers")

    def __init__(self, name=""):
        self.name = name
        self.lastw = None
        self.readers = []


class Op:
    __slots__ = ("eng", "fn", "deps", "dma", "tok", "needed", "idx")


class Prog:
    COMPUTE = ("pe", "act", "dve", "pool")
    NPOOL = 12
    UID = 0

    def __init__(self, nc):
        self.nc = nc
        self.ops = []
        self.q = {k: [] for k in ("pe", "act", "dve", "pool", "sp")}

    def add(self, eng, fn, reads=(), writes=(), dma=False):
        op = Op()
        op.eng, op.fn, op.dma, op.tok, op.needed = eng, fn, dma, None, False
        op.idx = len(self.ops)
        deps = set()
        for b in reads:
            if b.lastw is not None:
                deps.add(b.lastw)
        for b in writes:
            if b.lastw is not None:
                deps.add(b.lastw)
            deps.update(b.readers)
        op.deps = deps
        for b in reads:
            if b in writes:
                continue
            if not dma:
                b.readers = [r for r in b.readers if self.ops[r].dma or self.ops[r].eng != eng]
            b.readers.append(op.idx)
        for b in writes:
            b.lastw = op.idx
            b.readers = []
        self.ops.append(op)
        self.q[eng].append(op)
        return op

    def emit(self):
        nc, ops = self.nc, self.ops
        for op in ops:
            for d in op.deps:
                dop = ops[d]
                if dop.eng == "pe" and op.eng == "pe" and not dop.dma and not op.dma:
                    continue
                dop.needed = True
        Prog.UID += 1
        u = Prog.UID
        sems = {k: nc.alloc_semaphore("s%d_%s" % (u, k)) for k in self.COMPUTE}
        dsem = {k: [nc.alloc_semaphore("d%d_%s_%d" % (u, k, i)) for i in range(self.NPOOL)] for k in self.q}
        cnt = {k: 0 for k in self.COMPUTE}
        dcnt = {k: 0 for k in self.q}
        prewait = {}
        for op in ops:
            if op.dma:
                k = dcnt[op.eng]
                dcnt[op.eng] += 1
                s = dsem[op.eng][k % self.NPOOL]
                op.tok = (s, 16 * (k // self.NPOOL + 1))
                if k >= self.NPOOL:
                    prewait[op.idx] = (s, 16 * (k // self.NPOOL))
            elif op.needed:
                cnt[op.eng] += 1
                op.tok = (sems[op.eng], cnt[op.eng])
        engines = {"pe": "tensor", "act": "scalar", "dve": "vector", "pool": "gpsimd", "sp": "sync"}
        with nc.Block() as block:
            def make(k):
                def body(e):
                    known = {}
                    for op in self.q[k]:
                        waits = []
                        if op.idx in prewait:
                            waits.append(prewait[op.idx])
                        for d in sorted(op.deps):
                            dop = ops[d]
                            if dop.tok is None:
                                continue
                            if dop.eng == "pe" and k == "pe" and not dop.dma and not op.dma:
                                continue
                            waits.append(dop.tok)
                        for (s, v) in waits:
                            if known.get(id(s), 0) >= v:
                                continue
                            known[id(s)] = v
                            e.wait_ge(s, v)
                        ins = op.fn(e)
                        if op.tok is not None:
                            ins.then_inc(op.tok[0], 16 if op.dma else 1)
                    if k == "sp":
                        for kk in self.q:
                            n = dcnt[kk]
                            for j in range(min(n, self.NPOOL)):
                                uses = (n - j + self.NPOOL - 1) // self.NPOOL
                                e.wait_ge(dsem[kk][j], 16 * uses)
                return body
            for k, attr in engines.items():
                getattr(block, attr)(make(k))


class Tl:
    def __init__(self, t, name=""):
        self.t = t
        self.b = Buf(name)

    def __getitem__(self, k):
        return self.t[k]


def _bufs(lst):
    return [x.b if isinstance(x, Tl) else x for x in lst]


class Ph:
    def __init__(self, nc, ps):
        self.nc = nc
        self.P = Prog(nc)
        self.ps = ps
        self.pb = [Tl(ps[:, i * 512:(i + 1) * 512], "pb%d" % i) for i in range(8)]
        self.n = 0
        Ph.UID += 1
        self.uid = Ph.UID

    UID = 0

    def sb(self, shape, dt=F32, name=None):
        self.n += 1
        t = self.nc.alloc_sbuf_tensor("%s_%d_%d" % (name or "t", self.uid, self.n), list(shape), dt)
        return Tl(t, name or "t")

    def add(self, eng, fn, r, w, dma=False):
        return self.P.add(eng, fn, _bufs(r), _bufs(w), dma=dma)

    def dma(self, out, in_, r=(), w=(), eng="sp", **kw):
        return self.add(eng, lambda e: e.dma_start(out=out, in_=in_, **kw), r, w, dma=True)

    def mm(self, out, lhsT, rhs, start, stop, r, w):
        return self.add("pe", lambda e: e.matmul(out, lhsT=lhsT, rhs=rhs, start=start, stop=stop), r, w)

    def tr(self, out, in_, ident, r, w):
        return self.add("pe", lambda e: e.transpose(out=out, in_=in_, identity=ident), r, w)

    def act(self, out, in_, func, r, w, bias=None, scale=None, accum=None):
        kw = {}
        if bias is not None:
            kw["bias"] = bias
        if scale is not None:
            kw["scale"] = scale
        if accum is not None:
            kw["accum_out"] = accum
        return self.add("act", lambda e: e.activation(out=out, in_=in_, func=func, **kw), r, w)

    def tt(self, eng, out, in0, in1, op, r, w):
        return self.add(eng, lambda e: e.tensor_tensor(out=out, in0=in0, in1=in1, op=op), r, w)

    def ts(self, eng, out, in0, s1, s2, op0, op1, r, w):
        if op1 is None:
            return self.add(eng, lambda e: e.tensor_scalar(out=out, in0=in0, scalar1=s1, scalar2=None, op0=op0), r, w)
        return self.add(eng, lambda e: e.tensor_scalar(out=out, in0=in0, scalar1=s1, scalar2=s2, op0=op0, op1=op1), r, w)

    def stt(self, eng, out, in0, sc, in1, op0, op1, r, w):
        return self.add(eng, lambda e: e.scalar_tensor_tensor(out=out, in0=in0, scalar=sc, in1=in1, op0=op0, op1=op1), r, w)

    def cp(self, eng, out, in_, r, w):
        if eng == "act":
            return self.add("act", lambda e: e.activation(out=out, in_=in_, func=AF.Copy), r, w)
        return self.add(eng, lambda e: e.tensor_copy(out=out, in_=in_), r, w)

    def memset(self, eng, ap, val, w):
        return self.add(eng, lambda e: e.memset(ap, val), [], w)

    def recip(self, out, in_, r, w):
        return self.add("dve", lambda e: e.reciprocal(out=out, in_=in_), r, w)

    def finish(self):
        self.P.emit()


def run_phase(nc, ps, fn, *args):
    with nc.cleanup_on_exit():
        ph = Ph(nc, ps)
        fn(ph, *args)
        ph.finish()
        nc.all_engine_barrier()


def token_blocks(t0, t1, bs=512):
    out = []
    t = t0
    while t < t1:
        n = min(bs, t1 - t)
        out.append((t, n))
        t += n
    return out


def make_identity(ph, dt, name):
    idf = ph.sb([128, 128], F32, name + "f")
    ph.memset("pool", idf[:], 0.0, [idf])
    ph.add("pool", lambda e: e.affine_select(out=idf[:], in_=idf[:], pattern=[[-1, 128]], compare_op=ALU.not_equal,
                                            fill=1.0, base=0, channel_multiplier=1), [idf], [idf])
    if dt == F32:
        return idf
    idb = ph.sb([128, 128], dt, name)
    ph.cp("pool", idb[:], idf[:], [idf], [idb])
    return idb


def phase_mod(ph, dr, l):
    cc = ph.sb([128, 2, 8], F32, "cc")
    sc = ph.sb([128, 2, 8], F32, "sc")
    ph.dma(cc[:], dr["cc"].rearrange("r (p k) -> p r k", k=8), [], [cc])
    ph.act(sc[:], cc[:], AF.Silu, [cc], [sc])
    mods = ph.sb([2, 6 * D], F32, "mods")
    bm = ph.sb([2, 6 * D], F32, "bm")
    ph.dma(bm[:], dr["b_mod"][l:l + 1, :].partition_broadcast(2), [], [bm])
    gm = ph.sb([2, D], F32, "gm")
    gf = ph.sb([2, D], F32, "gf")
    ph.dma(gm[:], dr["g_mix"][l:l + 1, :].partition_broadcast(2), [], [gm])
    ph.dma(gf[:], dr["g_ffn"][l:l + 1, :].partition_broadcast(2), [], [gf])
    wv = dr["w_mod"][l].rearrange("(p k) n -> p k n", k=8)
    wb = [ph.sb([128, 8, 512], F32, "wb") for _ in range(2)]
    for nb in range(12):
        w = wb[nb % 2]
        ph.dma(w[:], wv[:, :, nb * 512:(nb + 1) * 512], [], [w])
        pb = ph.pb[nb % 2]
        for k in range(8):
            ph.mm(pb[0:2, :], sc[:, :, k], w[:, k, :], k == 0, k == 7, [sc, w], [pb])
        ph.tt("dve", mods[:, nb * 512:(nb + 1) * 512], pb[0:2, :], bm[:, nb * 512:(nb + 1) * 512], ALU.add, [pb, bm], [mods])
    ph.stt("dve", mods[:, D:2 * D], mods[:, D:2 * D], 1.0, gm[:], ALU.add, ALU.mult, [mods, gm], [mods])
    ph.stt("dve", mods[:, 4 * D:5 * D], mods[:, 4 * D:5 * D], 1.0, gf[:], ALU.add, ALU.mult, [mods, gf], [mods])
    ph.dma(dr["modv"][:, :], mods[:], [mods], [])


def load_bc(ph, dr, row, idx, name):
    t = ph.sb([128, D], F32, name)
    ph.dma(t[:], dr["modv"][row:row + 1, idx * D:(idx + 1) * D].partition_broadcast(128), [], [t])
    return t


def rms_modulate(ph, xt, A, B, hout, junk, ss, rstd, h32=None):
    ph.act(junk[:], xt[:], AF.Square, [xt], [junk, ss], accum=ss[:, 0:1])
    ph.act(rstd[:, 0:1], ss[:, 0:1], AF.Sqrt, [ss], [rstd], bias=ph.epsc[:, 0:1], scale=1.0 / D)
    ph.recip(rstd[:, 0:1], rstd[:, 0:1], [rstd], [rstd])
    tmp = h32 if h32 is not None else junk
    ph.stt("dve", tmp[:], xt[:], rstd[:, 0:1], A[:], ALU.mult, ALU.mult, [xt, rstd, A], [tmp])
    ph.tt("dve", hout[:], tmp[:], B[:], ALU.add, [tmp, B], [hout])


def eps_const(ph):
    ph.epsc = ph.sb([128, 1], F32, "eps")
    ph.memset("pool", ph.epsc[:], EPS, [ph.epsc])


def phase_inproj(ph, dr, l, xsrc):
    nc = ph.nc
    eps_const(ph)
    win = ph.sb([128, 8, DIN], BF16, "win")
    stg = [ph.sb([128, 1320], F32, "stg") for _ in range(2)]
    wv = dr["w_in"][l].rearrange("(k p) n -> p k n", p=128)
    i = 0
    for k in range(8):
        for c4 in range(4):
            s = stg[i % 2]
            ph.dma(s[:], wv[:, k, c4 * 1320:(c4 + 1) * 1320], [], [s])
            ph.cp("dve" if i % 2 == 0 else "pool", win[:, k, c4 * 1320:(c4 + 1) * 1320], s[:], [s], [win])
            i += 1
    wkr = ph.sb([128, 8, 96], BF16, "wkr")
    wkrs = ph.sb([128, 8, 96], BF16, "wkrs")
    ph.memset("pool", wkr[:], 0.0, [wkr])
    ph.memset("pool", wkrs[:], 0.0, [wkrs])
    ph.cp("pool", wkr[:, :, 64:96], win[:, :, C_KR:C_KR + 32], [win], [wkr])
    ph.cp("pool", wkrs[:, :, 64:80], win[:, :, C_KR + 16:C_KR + 32], [win], [wkrs])
    ph.cp("pool", wkrs[:, :, 80:96], win[:, :, C_KR:C_KR + 16], [win], [wkrs])
    wgks = ph.sb([128, 8, 128], BF16, "wgks")
    wgqs = ph.sb([128, 8, 512], BF16, "wgqs")
    for (dst, c0, n) in ((wgks, C_GK, 128), (wgqs, C_GQ, 512)):
        sv = win[:, :, c0:c0 + n].rearrange("p k (h two d) -> p k h two d", two=2, d=32)
        dv = dst[:, :, :].rearrange("p k (h two d) -> p k h two d", two=2, d=32)
        ph.cp("pool", dv[:, :, :, 0, :], sv[:, :, :, 1, :], [win], [dst])
        ph.cp("pool", dv[:, :, :, 1, :], sv[:, :, :, 0, :], [win], [dst])
    gq = ph.sb([128, 2], F32, "gq")
    ph.dma(gq[:], dr["mla_gq"][l].rearrange("(c p) -> p c", p=128), [], [gq], allow_slow_non_contiguous=True)
    gkv = ph.sb([128, 1], F32, "gkv")
    ph.dma(gkv[:], dr["mla_gkv"][l].rearrange("(p o) -> p o", o=1), [], [gkv])
    wuqf = ph.sb([128, 2, 768], F32, "wuqf")
    ph.dma(wuqf[:], dr["mla_wuq"][l].rearrange("(c p) n -> p c n", p=128), [], [wuqf])
    wuq = ph.sb([128, 2, 768], BF16, "wuq")
    wuqs = ph.sb([128, 2, 768], BF16, "wuqs")
    for c in range(2):
        ph.ts("dve", wuq[:, c, :], wuqf[:, c, :], gq[:, c:c + 1], None, ALU.mult, None, [wuqf, gq], [wuq])
    ph.cp("pool", wuqs[:], wuq[:], [wuq], [wuqs])
    v1 = wuq[:, :, :].rearrange("p c (h d) -> p c h d", d=96)
    v2 = wuqs[:, :, :].rearrange("p c (h d) -> p c h d", d=96)
    ph.cp("pool", v2[:, :, :, 64:80], v1[:, :, :, 80:96], [wuq], [wuqs])
    ph.cp("pool", v2[:, :, :, 80:96], v1[:, :, :, 64:80], [wuq], [wuqs])
    wkvf = ph.sb([128, 1024], F32, "wkvf")
    ph.dma(wkvf[:], dr["mla_wukv"][l], [], [wkvf])
    wkv = ph.sb([128, 2, 512], BF16, "wkv")
    sv = wkvf[:, :].rearrange("p (h two d) -> p two h d", two=2, d=64)
    for two in range(2):
        ph.ts("dve", wkv[:, two, :].rearrange("p (h d) -> p h d", d=64), sv[:, two, :, :], gkv[:, 0:1], None, ALU.mult, None,
              [wkvf, gkv], [wkv])
    ones = ph.sb([128, 128], BF16, "ones")
    ph.memset("pool", ones[:], 1.0, [ones])
    bones = ph.sb([128, 128], BF16, "bones")
    ph.memset("pool", bones[:], 0.0, [bones])
    ph.memset("pool", bones[0:64, 0:64], 1.0, [bones])
    ph.memset("pool", bones[64:128, 64:128], 1.0, [bones])
    ident = make_identity(ph, BF16, "ident")
    gcol = ph.sb([128, 4], F32, "gcol")
    for j, nm in ((0, "gqa_gq"), (2, "gqa_gk")):
        src = dr[nm][l].rearrange("(d o) -> d o", o=1)
        for hh in range(2):
            ph.dma(gcol[hh * 64:hh * 64 + 64, j:j + 1], src[0:64, :], [], [gcol])
            ph.dma(gcol[hh * 64:hh * 64 + 32, j + 1:j + 2], src[32:64, :], [], [gcol])
            ph.dma(gcol[hh * 64 + 32:hh * 64 + 64, j + 1:j + 2], src[0:32, :], [], [gcol])
    Al = load_bc(ph, dr, 0, 1, "Al")
    Bl = load_bc(ph, dr, 0, 0, "Bl")
    Ac = load_bc(ph, dr, 1, 1, "Ac")
    Bc = load_bc(ph, dr, 1, 0, "Bc")

    xt = [ph.sb([128, D], F32, "xt") for _ in range(2)]
    junk = ph.sb([128, D], F32, "junk")
    hb = [ph.sb([128, D], BF16, "hb") for _ in range(2)]
    ss = ph.sb([128, 1], F32, "ss")
    rstd = ph.sb([128, 1], F32, "rstd")
    hT = [ph.sb([128, 8, 512], BF16, "hT") for _ in range(2)]
    tabm = [ph.sb([96, 2, 512], BF16, "tabm") for _ in range(2)]
    tabg = [ph.sb([128, 2, 512], BF16, "tabg") for _ in range(2)]
    NOB = 6
    ob = [ph.sb([128, 512], BF16, "ob") for _ in range(NOB)]
    NF = 6
    fb = [ph.sb([128, 512], F32, "fb") for _ in range(NF)]
    nck = ph.sb([128, 512], BF16, "nck")
    ncq = [ph.sb([128, 512], BF16, "ncq") for _ in range(2)]
    ctr = {"ob": 0, "fb": 0, "pb": 0, "ev": 0}

    def nob():
        ctr["ob"] += 1
        return ob[ctr["ob"] % NOB]

    def nfb():
        ctr["fb"] += 1
        return fb[ctr["fb"] % NF]

    def npb():
        ctr["pb"] += 1
        return ph.pb[ctr["pb"] % 8]

    def evac_eng():
        ctr["ev"] += 1
        return "act" if ctr["ev"] % 2 == 0 else "dve"

    def proj(lhs_tile, c0, m, hTb, n, extra_r=()):
        pb = npb()
        for k in range(8):
            ph.mm(pb[0:m, 0:n], lhs_tile[:, k, c0:c0 + m], hTb[:, k, 0:n], k == 0, k == 7, [lhs_tile, hTb], [pb])
        return pb

    def store(dst_ap, src_tile, src_ap, eng="pool"):
        ph.dma(dst_ap, src_ap, [src_tile], [], eng=eng)

    bi = 0
    for (t0, n) in token_blocks(0, T):
        hTb = hT[bi % 2]
        tm = tabm[bi % 2]
        tg = tabg[bi % 2]
        ph.dma(tm[64:96, :, 0:n], dr["ropem"][:, :, t0:t0 + n].rearrange("a r t -> r a t"), [], [tm])
        for hh in range(2):
            ph.dma(tg[hh * 64:hh * 64 + 64, :, 0:n], dr["ropeg"][:, :, t0:t0 + n].rearrange("a r t -> r a t"), [], [tg])
        for j in range(n // 128):
            tok = t0 + j * 128
            x_ = xt[j % 2]
            h_ = hb[j % 2]
            ph.dma(x_[:], xsrc[tok:tok + 128, :], [], [x_])
            isctx = tok < NCTX
            rms_modulate(ph, x_, Ac if isctx else Al, Bc if isctx else Bl, h_, junk, ss, rstd)
            pb = npb()
            pbv = pb.t.bitcast(BF16)
            for k in range(8):
                ph.tr(pbv[:, k * 128:(k + 1) * 128], h_[:, k * 128:(k + 1) * 128], ident[:], [h_, ident], [pb])
            ph.cp(evac_eng(), hTb[:, :, j * 128:(j + 1) * 128], pbv[:, :].rearrange("p (k t) -> p k t", t=128), [pb], [hTb])
        sl = slice(t0, t0 + n)
        for c in range(4):
            pb = proj(win, C_XR + c * 128, 128, hTb, n)
            o = nob()
            ph.cp(evac_eng(), o[:, 0:n], pb[:, 0:n], [pb], [o])
            store(dr["xrT"][c * 128:(c + 1) * 128, sl], o, o[:, 0:n])
        for c in range(4):
            pb = proj(win, C_RG + c * 128, 128, hTb, n)
            o = nob()
            ph.act(o[:, 0:n], pb[:, 0:n], AF.Gelu_apprx_tanh, [pb], [o])
            store(dr["rgT"][c * 128:(c + 1) * 128, sl], o, o[:, 0:n])
        for c in range(24):
            pb = proj(win, C_MG + c * 128, 128, hTb, n)
            o = nob()
            ph.act(o[:, 0:n], pb[:, 0:n], AF.Sigmoid, [pb], [o])
            store(dr["gatesT"][c * 128:(c + 1) * 128, sl], o, o[:, 0:n])
        for j in range(n // 128):
            pb = npb()
            for k in range(8):
                ph.mm(pb[:, 0:128], hTb[:, k, j * 128:(j + 1) * 128], win[:, k, C_GV:C_GV + 128], k == 0, k == 7, [hTb, win], [pb])
            o = nob()
            ph.cp(evac_eng(), o[:, 0:128], pb[:, 0:128], [pb], [o])
            store(dr["vg"][t0 + j * 128:t0 + (j + 1) * 128, :], o, o[:, 0:128])

        def rstd_bc(sq_list, ones_t, count):
            pb = npb()
            for i_, sq in enumerate(sq_list):
                ph.mm(pb[:, 0:n], ones_t[:], sq[:, 0:n], i_ == 0, i_ == len(sq_list) - 1, [ones_t, sq], [pb])
            r_ = nfb()
            ph.act(r_[:, 0:n], pb[:, 0:n], AF.Sqrt, [pb], [r_], bias=ph.epsc[:, 0:1], scale=1.0 / count)
            ph.recip(r_[:, 0:n], r_[:, 0:n], [r_], [r_])
            return r_

        pa = proj(win, C_CKV, 128, hTb, n)
        sq = nob()
        ph.act(sq[:, 0:n], pa[:, 0:n], AF.Square, [pa], [sq])
        r_ = rstd_bc([sq], ones, 128)
        ph.tt("dve", nck[:, 0:n], pa[:, 0:n], r_[:, 0:n], ALU.mult, [pa, r_], [nck])
        for hp in range(4):
            pb = npb()
            ph.mm(pb[:, 0:n], wkv[:, 0, hp * 128:(hp + 1) * 128], nck[:, 0:n], True, True, [wkv, nck], [pb])
            o = nob()
            ph.cp(evac_eng(), o[:, 0:n], pb[:, 0:n], [pb], [o])
            for hh in range(2):
                store(dr["kmT"][2 * hp + hh, 0:64, sl], o, o[hh * 64:hh * 64 + 64, 0:n])
        for j in range(n // 128):
            pb = npb()
            ph.mm(pb[:, :], nck[:, j * 128:(j + 1) * 128], wkv[:, 1, :], True, True, [nck, wkv], [pb])
            o = nob()
            ph.cp(evac_eng(), o[:, :], pb[:, :], [pb], [o])
            store(dr["vm"][t0 + j * 128:t0 + (j + 1) * 128, :], o, o[:, :])

        def rope96(pa, pb_, dst_tile):
            t1 = nfb()
            t2 = nfb()
            ph.tt("dve", t1[64:96, 0:n], pa[64:96, 0:n], tm[64:96, 0, 0:n], ALU.mult, [pa, tm], [t1])
            ph.tt("dve", t2[64:96, 0:n], pb_[64:96, 0:n], tm[64:96, 1, 0:n], ALU.mult, [pb_, tm], [t2])
            ph.tt("pool", dst_tile[64:96, 0:n], t1[64:96, 0:n], t2[64:96, 0:n], ALU.add, [t1, t2], [dst_tile])

        pa = proj(wkr, 0, 96, hTb, n)
        pb_ = proj(wkrs, 0, 96, hTb, n)
        o = nob()
        rope96(pa, pb_, o)
        for h in range(8):
            store(dr["kmT"][h, 64:96, sl], o, o[64:96, 0:n], eng="sp" if h % 2 else "pool")
        pc = [proj(win, C_CQ + c * 128, 128, hTb, n) for c in range(2)]
        sqs = []
        for c in range(2):
            s_ = nob()
            ph.act(s_[:, 0:n], pc[c][:, 0:n], AF.Square, [pc[c]], [s_])
            sqs.append(s_)
        r_ = rstd_bc(sqs, ones, 256)
        for c in range(2):
            ph.tt("dve", ncq[c][:, 0:n], pc[c][:, 0:n], r_[:, 0:n], ALU.mult, [pc[c], r_], [ncq[c]])
        for h in range(8):
            pa = npb()
            pb_ = npb()
            for c in range(2):
                ph.mm(pa[0:96, 0:n], wuq[:, c, h * 96:(h + 1) * 96], ncq[c][:, 0:n], c == 0, c == 1, [wuq, ncq[c]], [pa])
            for c in range(2):
                ph.mm(pb_[0:96, 0:n], wuqs[:, c, h * 96:(h + 1) * 96], ncq[c][:, 0:n], c == 0, c == 1, [wuqs, ncq[c]], [pb_])
            o = nob()
            ph.cp("act", o[0:64, 0:n], pa[0:64, 0:n], [pa], [o])
            rope96(pa, pb_, o)
            store(dr["qmT"][h, :, sl], o, o[0:96, 0:n], eng="sp" if h % 2 else "pool")

        def gqa_chunk(c0, wsw, csw, gj, dst_fn):
            pa = proj(win, c0, 128, hTb, n)
            pb_ = proj(wsw, csw, 128, hTb, n)
            sq = nob()
            ph.act(sq[:, 0:n], pa[:, 0:n], AF.Square, [pa], [sq])
            r_ = rstd_bc([sq], bones, 64)
            t1 = nfb()
            t2 = nfb()
            ph.stt("dve", t1[:, 0:n], pa[:, 0:n], gcol[:, gj:gj + 1], tg[:, 0, 0:n], ALU.mult, ALU.mult, [pa, gcol, tg], [t1])
            ph.stt("dve", t2[:, 0:n], pb_[:, 0:n], gcol[:, gj + 1:gj + 2], tg[:, 1, 0:n], ALU.mult, ALU.mult, [pb_, gcol, tg], [t2])
            ph.tt("pool", t1[:, 0:n], t1[:, 0:n], t2[:, 0:n], ALU.add, [t1, t2], [t1])
            o = nob()
            ph.tt("pool", o[:, 0:n], t1[:, 0:n], r_[:, 0:n], ALU.mult, [t1, r_], [o])
            dst_fn(o)

        def st_gk(o):
            for hh in range(2):
                store(dr["kgT"][hh, :, sl], o, o[hh * 64:hh * 64 + 64, 0:n])
        gqa_chunk(C_GK, wgks, 0, 2, st_gk)
        for c in range(4):
            def st_gq(o, c=c):
                for hh in range(2):
                    store(dr["qgT"][2 * c + hh, :, sl], o, o[hh * 64:hh * 64 + 64, 0:n])
            gqa_chunk(C_GQ + c * 128, wgqs, c * 128, 0, st_gq)
        bi += 1


def phase_lru(ph, dr, l):
    blocks = token_blocks(0, T)
    xp = ph.sb([128, T + 6], BF16, "xp")
    xc = ph.sb([128, T], F32, "xc")
    xcb = ph.sb([128, T], BF16, "xcb")
    rg = ph.sb([128, T], BF16, "rg")
    ysum = ph.sb([128, T], F32, "ysum")
    abuf = ph.sb([128, T], F32, "abuf")
    ibuf = ph.sb([128, T], F32, "ibuf")
    mbuf = ph.sb([128, T], F32, "mbuf")
    hbuf = ph.sb([128, T], F32, "hbuf")
    yo = ph.sb([128, T], BF16, "yo")
    for c in range(4):
        cs = slice(c * 128, (c + 1) * 128)
        ph.memset("pool", xp[:, 0:2], 0.0, [xp])
        ph.memset("pool", xp[:, 258:261], 0.0, [xp])
        ph.memset("pool", xp[:, T + 5:T + 6], 0.0, [xp])
        ph.dma(xp[:, 2:258], dr["xrT"][cs, 0:NCTX], [], [xp])
        ph.dma(xp[:, 261:261 + SEQ], dr["xrT"][cs, NCTX:T], [], [xp])
        cw = ph.sb([128, 4], F32, "cw")
        ph.dma(cw[:], dr["conv_w"][l].rearrange("j c -> c j")[cs, :], [], [cw], allow_slow_non_contiguous=True)
        cb = ph.sb([128, 1], F32, "cb")
        ph.dma(cb[:], dr["conv_b"][l].rearrange("(c o) -> c o", o=1)[cs, :], [], [cb])
        for (o0, i0, n) in ((0, 0, NCTX), (NCTX, 259, SEQ)):
            ph.ts("dve", xc[:, o0:o0 + n], xp[:, i0:i0 + n], cw[:, 0:1], cb[:, 0:1], ALU.mult, ALU.add, [xp, cw, cb], [xc])
            for j in range(1, 4):
                ph.stt("dve", xc[:, o0:o0 + n], xp[:, i0 + j:i0 + j + n], cw[:, j:j + 1], xc[:, o0:o0 + n],
                       ALU.mult, ALU.add, [xp, cw, xc], [xc])
        ph.cp("act", xcb[:], xc[:], [xc], [xcb])
        ph.dma(rg[:], dr["rgT"][cs, :], [], [rg])
        for d in range(2):
            wst = ph.sb([128, 2, 128], F32, "wst")
            ph.memset("pool", wst[:], 0.0, [wst])
            for g_, nm in enumerate(("lru_wa", "lru_wi")):
                for hb_ in range(2):
                    ph.dma(wst[hb_ * 64:hb_ * 64 + 64, g_, hb_ * 64:hb_ * 64 + 64], dr[nm][l, d, 2 * c + hb_], [wst], [wst])
            wbd = ph.sb([128, 2, 128], BF16, "wbd")
            ph.cp("pool", wbd[:], wst[:], [wst], [wbd])
            col = ph.sb([128, 8], F32, "col")
            for j, nm in enumerate(("lru_ba", "lru_bi", "lru_lambda")):
                ph.dma(col[:, j:j + 1], dr[nm][l, d].rearrange("(c o) -> c o", o=1)[cs, :], [], [col])
            ph.act(col[:, 4:5], col[:, 2:3], AF.Exp, [col], [col], scale=-1.0)
            ph.act(col[:, 5:6], col[:, 4:5], AF.Ln, [col], [col], bias=1.0)
            ph.ts("dve", col[:, 2:3], col[:, 5:6], -8.0, None, ALU.mult, None, [col], [col])
            ph.ts("dve", col[:, 3:4], col[:, 5:6], -16.0, None, ALU.mult, None, [col], [col])
            for bi, (t0, n) in enumerate(blocks):
                pr = ph.pb[(2 * bi) % 8]
                pi = ph.pb[(2 * bi + 1) % 8]
                ph.mm(pr[:, 0:n], wbd[:, 0, :], xcb[:, t0:t0 + n], True, True, [wbd, xcb], [pr])
                ph.mm(pi[:, 0:n], wbd[:, 1, :], xcb[:, t0:t0 + n], True, True, [wbd, xcb], [pi])
                ph.act(abuf[:, t0:t0 + n], pr[:, 0:n], AF.Sigmoid, [pr, col], [abuf], bias=col[:, 0:1])
                ph.act(ibuf[:, t0:t0 + n], pi[:, 0:n], AF.Sigmoid, [pi, col], [ibuf], bias=col[:, 1:2])
            ph.act(mbuf[:], abuf[:], AF.Exp, [abuf, col], [mbuf], scale=col[:, 3:4])
            ph.act(abuf[:], abuf[:], AF.Exp, [abuf, col], [abuf], scale=col[:, 2:3])
            ph.act(mbuf[:], mbuf[:], AF.Sqrt, [mbuf], [mbuf], bias=1.0, scale=-1.0)
            ph.tt("pool", ibuf[:], ibuf[:], mbuf[:], ALU.mult, [ibuf, mbuf], [ibuf])
            ph.tt("pool", ibuf[:], ibuf[:], xc[:], ALU.mult, [ibuf, xc], [ibuf])
            dst = ysum if d == 0 else hbuf
            if d == 0:
                ph.add("dve", lambda e, dst=dst: e.tensor_tensor_scan(out=dst[:, :], data0=abuf[:, :], data1=ibuf[:, :], initial=0.0,
                                                                      op0=ALU.mult, op1=ALU.add), [abuf, ibuf], [dst])
            else:
                ph.add("dve", lambda e, dst=dst: e.tensor_tensor_scan(out=dst[:, 0:NCTX][:, ::-1], data0=abuf[:, 0:NCTX][:, ::-1],
                                                                      data1=ibuf[:, 0:NCTX][:, ::-1], initial=0.0,
                                                                      op0=ALU.mult, op1=ALU.add), [abuf, ibuf], [dst])
                ph.add("dve", lambda e, dst=dst: e.tensor_tensor_scan(out=dst[:, NCTX:T][:, ::-1], data0=abuf[:, NCTX:T][:, ::-1],
                                                                      data1=ibuf[:, NCTX:T][:, ::-1], initial=dst[:, 0:1],
                                                                      op0=ALU.mult, op1=ALU.add), [abuf, ibuf, dst], [dst])
                ph.tt("pool", ysum[:], ysum[:], hbuf[:], ALU.add, [ysum, hbuf], [ysum])
        ph.tt("pool", yo[:], ysum[:], rg[:], ALU.mult, [ysum, rg], [yo])
        ph.dma(dr["yT"][0, cs, :], yo[:], [yo], [])


def phase_attn(ph, dr, l, with_ctx):
    NB = 2
    kT = [ph.sb([96, T], BF16, "kT") for _ in range(NB)]
    qT = [ph.sb([96, T], BF16, "qT") for _ in range(NB)]
    kTg = [ph.sb([128, T], BF16, "kTg") for _ in range(NB)]
    qTg = [ph.sb([128, T], BF16, "qTg") for _ in range(NB)]
    for t_ in kTg + qTg:
        ph.memset("pool", t_[64:128, :], 0.0, [t_])
    va = [ph.sb([128, NT, 128], BF16, "va") for _ in range(NB)]
    for v_ in va:
        ph.memset("pool", v_[:, :, 64:128], 1.0, [v_])
    NPT = 4
    pT = [ph.sb([128, 1024], BF16, "pT") for _ in range(NPT)]
    osb = [ph.sb([64, 512], F32, "osb") for _ in range(2)]
    yo = [ph.sb([64, 512], BF16, "yo") for _ in range(2)]
    sp_ = [Tl(ph.ps[:, 0:1024], "S0"), Tl(ph.ps[:, 1024:2048], "S1"), Tl(ph.ps[:, 2048:3072], "S2")]
    acc = [ph.pb[6], ph.pb[7]]
    heads = [(br, h) for br in (1, 2) for h in range(8)]

    def load_head(hi):
        br, h = heads[hi]
        k_, q_, v_ = (kT if br == 1 else kTg)[hi % NB], (qT if br == 1 else qTg)[hi % NB], va[hi % NB]
        if br == 1:
            ph.dma(k_[0:96, :], dr["kmT"][h], [], [k_])
            ph.dma(q_[0:96, :], dr["qmT"][h], [], [q_])
            vsrc = dr["vm"][:, h * 64:(h + 1) * 64].rearrange("(c p) d -> p c d", p=128)
        else:
            ph.dma(k_[0:64, :], dr["kgT"][h // 4], [], [k_])
            ph.dma(q_[0:64, :], dr["qgT"][h], [], [q_])
            vsrc = dr["vg"][:, (h // 4) * 64:(h // 4 + 1) * 64].rearrange("(c p) d -> p c d", p=128)
        ph.dma(v_[:, 0:17, 0:64], vsrc[:, 0:17, :], [], [v_])
        ph.dma(v_[:, 17:NT, 0:64], vsrc[:, 17:NT, :], [], [v_])

    items = []
    blk = 0
    for hi, (br, h) in enumerate(heads):
        d = 96 if br == 1 else 64
        scale = MLA_SCALE if br == 1 else GQA_SCALE
        qblocks = [(t0, n, NT) for (t0, n) in token_blocks(NCTX, T)]
        if with_ctx:
            qblocks = [(0, NCTX, NCTX // 128)] + qblocks
        first = True
        for (t0, n, nkc) in qblocks:
            for g0 in range(0, nkc, 2):
                items.append(dict(hi=hi, br=br, h=h, d=d, scale=scale, t0=t0, n=n, nkc=nkc, g0=g0, blk=blk,
                                  pre=first, last=(g0 + 2 >= nkc), idx=len(items)))
                first = False
            blk += 1

    def S(it):
        i = it["idx"]
        k_, q_ = (kT if it["br"] == 1 else kTg)[it["hi"] % NB], (qT if it["br"] == 1 else qTg)[it["hi"] % NB]
        s_, p_ = sp_[i % 3], pT[i % NPT]
        n, t0 = it["n"], it["t0"]
        d = 96 if it["br"] == 1 else 128
        for u in range(2):
            kc = it["g0"] + u
            ph.mm(s_[:, u * 512:u * 512 + n], k_[0:d, kc * 128:(kc + 1) * 128], q_[0:d, t0:t0 + n], True, True, [k_, q_], [s_])
        if n == 512:
            ph.act(p_[:, :], s_[:, :], AF.Exp, [s_], [p_], scale=it["scale"])
        else:
            sv = s_[:, :].rearrange("p (u t) -> p u t", u=2)[:, :, 0:n]
            pv = p_[:, :].rearrange("p (u t) -> p u t", u=2)[:, :, 0:n]
            ph.act(pv, sv, AF.Exp, [s_], [p_], scale=it["scale"])

    def PV(it):
        i = it["idx"]
        v_, p_ = va[it["hi"] % NB], pT[i % NPT]
        a_ = acc[it["blk"] % 2]
        n = it["n"]
        for u in range(2):
            kc = it["g0"] + u
            ph.mm(a_[:, 0:n], v_[:, kc, :], p_[:, u * 512:u * 512 + n], kc == 0, kc == it["nkc"] - 1, [v_, p_], [a_])
        if it["last"]:
            o_, y_ = osb[it["blk"] % 2], yo[it["blk"] % 2]
            t0, h, br = it["t0"], it["h"], it["br"]
            ph.recip(o_[0:64, 0:n], a_[64:128, 0:n], [a_], [o_])
            ph.tt("dve", y_[:, 0:n], a_[0:64, 0:n], o_[0:64, 0:n], ALU.mult, [o_, a_], [y_])
            ph.dma(dr["yT"][br, h * 64:(h + 1) * 64, t0:t0 + n], y_[:, 0:n], [y_], [], eng="pool")

    load_head(0)
    N = len(items)
    for i in range(N + 1):
        if i < N:
            S(items[i])
        if 0 <= i - 1 < N:
            PV(items[i - 1])
        if i < N and items[i]["pre"] and items[i]["hi"] + 1 < len(heads):
            load_head(items[i]["hi"] + 1)


def phase_merge(ph, dr, l, xsrc, tstart):
    eps_const(ph)
    wbr = ph.sb([128, 3, 4, D], BF16, "wbr")
    wo = ph.sb([128, 8, D], BF16, "wo")
    stg = [ph.sb([128, 2, D], F32, "stg") for _ in range(2)]
    si = 0
    for k in range(3):
        for hf in range(2):
            s = stg[si % 2]
            ph.dma(s[:], dr["w_branch"][l, k].rearrange("(c p) n -> p c n", p=128)[:, hf * 2:(hf + 1) * 2, :], [], [s])
            ph.cp("dve" if si % 2 else "pool", wbr[:, k, hf * 2:(hf + 1) * 2, :], s[:], [s], [wbr])
            si += 1
    for hf in range(4):
        s = stg[si % 2]
        ph.dma(s[:], dr["w_out"][l].rearrange("(c p) n -> p c n", p=128)[:, hf * 2:(hf + 1) * 2, :], [], [s])
        ph.cp("dve" if si % 2 else "pool", wo[:, hf * 2:(hf + 1) * 2, :], s[:], [s], [wo])
        si += 1
    wr = ph.sb([128, 8, 36], F32, "wr")
    ph.dma(wr[:, :, 0:4], dr["moe_wg"][l].rearrange("(c p) n -> p c n", p=128), [], [wr])
    ph.dma(wr[:, :, 4:36], dr["moe_we"][l].rearrange("(c p) n -> p c n", p=128), [], [wr])
    br_ = ph.sb([128, 36], F32, "br")
    ph.dma(br_[:, 0:4], dr["moe_bg"][l:l + 1, :].partition_broadcast(128), [], [br_])
    ph.dma(br_[:, 4:36], dr["moe_be"][l:l + 1, :].partition_broadcast(128), [], [br_])
    wrb = ph.sb([128, 8, 36], BF16, "wrb")
    ph.cp("pool", wrb[:], wr[:], [wr], [wrb])
    identb = make_identity(ph, BF16, "identb")
    hb16 = [ph.sb([128, D], BF16, "hb16") for _ in range(2)]
    G = {}
    for row in ((0, 1) if tstart == 0 else (0,)):
        G[row] = (load_bc(ph, dr, row, 2, "Ga"), load_bc(ph, dr, row, 4, "Af"), load_bc(ph, dr, row, 3, "Bf"))
    yb = [ph.sb([128, 3, 4, 512], BF16, "yb") for _ in range(2)]
    gb = [ph.sb([128, 24, 512], BF16, "gb") for _ in range(1)]
    mg = [ph.sb([128, 8, 512], BF16, "mgd") for _ in range(2)]
    tmpf = [ph.sb([128, 512], F32, "tmpf") for _ in range(3)]
    accf = ph.sb([128, 512], F32, "accf")
    xt = [ph.sb([128, D], F32, "xt") for _ in range(2)]
    x2 = [ph.sb([128, D], F32, "x2") for _ in range(2)]
    junk = ph.sb([128, D], F32, "junk")
    hTb = [ph.sb([128, 8, 128], BF16, "hTb") for _ in range(2)]
    ss = ph.sb([128, 1], F32, "ss")
    rstd = ph.sb([128, 1], F32, "rstd")
    sm = ph.sb([128, 64], F32, "sm")
    lg = ph.sb([128, 36], F32, "lg")
    mk = ph.sb([128, 4], F32, "mk")
    es = ph.sb([128, 4, 8], F32, "es")
    e8 = ph.sb([128, 8], F32, "e8")
    m8 = ph.sb([128, 8], F32, "m8")
    cb_ = [ph.sb([128, 32], F32, "comb") for _ in range(2)]
    c1 = ph.sb([128, 32], F32, "c1")
    c2 = ph.sb([128, 32], F32, "c2")
    ctr = {"pb": 0, "t": 0}

    def npb():
        ctr["pb"] += 1
        return ph.pb[ctr["pb"] % 8]

    bi = 0
    for (t0, n) in token_blocks(tstart, T):
        if DBG_STOP < 1:
            break
        sl = slice(t0, t0 + n)
        y_, g_, m_ = yb[bi % 2], gb[0], mg[bi % 2]
        bi += 1
        for k in range(3):
            ph.dma(y_[:, k, :, 0:n], dr["yT"][k, :, sl].rearrange("(c p) t -> p c t", p=128), [], [y_])
        for k in range(3):
            ph.dma(g_[:, k * 8:(k + 1) * 8, 0:n], dr["gatesT"][k * D:(k + 1) * D, sl].rearrange("(c p) t -> p c t", p=128), [], [g_])
        for oc in range(8):
            for k in range(3):
                pb = npb()
                for c in range(4):
                    ph.mm(pb[:, 0:n], wbr[:, k, c, oc * 128:(oc + 1) * 128], y_[:, k, c, 0:n], c == 0, c == 3, [wbr, y_], [pb])
                if k == 0:
                    ph.tt("dve", accf[:, 0:n], pb[:, 0:n], g_[:, oc, 0:n], ALU.mult, [pb, g_], [accf])
                else:
                    tf = tmpf[ctr["t"] % 3]
                    ctr["t"] += 1
                    ph.tt("dve", tf[:, 0:n], pb[:, 0:n], g_[:, k * 8 + oc, 0:n], ALU.mult, [pb, g_], [tf])
                    if k == 1:
                        ph.tt("pool", accf[:, 0:n], accf[:, 0:n], tf[:, 0:n], ALU.add, [accf, tf], [accf])
                    else:
                        ph.tt("pool", m_[:, oc, 0:n], accf[:, 0:n], tf[:, 0:n], ALU.add, [accf, tf], [m_])
        for j in range(n // 128):
            if DBG_STOP < 2:
                break
            tok = t0 + j * 128
            row = 1 if tok < NCTX else 0
            Ga, Af, Bf = G[row]
            x_ = xt[j % 2]
            xo = x2[j % 2]
            ph.dma(x_[:], xsrc[tok:tok + 128, :], [], [x_])
            for hf in range(2):
                pb = npb()
                for c in range(8):
                    ph.mm(pb[:, :], m_[:, c, j * 128:(j + 1) * 128], wo[:, c, hf * 512:(hf + 1) * 512], c == 0, c == 7, [m_, wo], [pb])
                ph.tt("dve", junk[:, hf * 512:(hf + 1) * 512], pb[:, :], Ga[:, hf * 512:(hf + 1) * 512], ALU.mult, [pb, Ga], [junk])
            ph.tt("pool", xo[:], junk[:], x_[:], ALU.add, [junk, x_], [xo])
            ph.dma(dr["x2"][tok:tok + 128, :], xo[:], [xo], [], eng="pool")
            if DBG_STOP < 3:
                continue
            hbt = hb16[j % 2]
            rms_modulate(ph, xo, Af, Bf, hbt, junk, ss, rstd)
            hb_ = hTb[j % 2]
            pb = npb()
            pbv = pb.t.bitcast(BF16)
            for c in range(8):
                ph.tr(pbv[:, c * 128:(c + 1) * 128], hbt[:, c * 128:(c + 1) * 128], identb[:], [hbt, identb], [pb])
            ph.cp("act", hb_[:, :, :], pbv[:, :].rearrange("p (c t) -> p c t", t=128), [pb], [hb_])
            ph.dma(dr["h2T"][:, tok:tok + 128].rearrange("(c p) t -> p c t", p=128), hb_[:], [hb_], [], eng="pool")
            if DBG_SKIP_ROUTER:
                continue
            pb = npb()
            for c in range(8):
                ph.mm(pb[:, 0:36], hb_[:, c, :], wrb[:, c, :], c == 0, c == 7, [hb_, wrb], [pb])
            ph.tt("dve", lg[:], pb[:, 0:36], br_[:], ALU.add, [pb, br_], [lg])
            ph.add("dve", lambda e: e.tensor_reduce(out=sm[:, 0:1], in_=lg[:, 0:4], axis=AX.X, op=ALU.max), _bufs([lg]), _bufs([sm]))
            ph.ts("dve", mk[:], lg[:, 0:4], sm[:, 0:1], None, ALU.is_equal, None, [lg, sm], [mk])
            ph.ts("dve", sm[:, 1:2], sm[:, 0:1], -1.0, None, ALU.mult, None, [sm], [sm])
            ph.act(sm[:, 8:12], lg[:, 0:4], AF.Exp, [lg, sm], [sm], bias=sm[:, 1:2], accum=sm[:, 2:3])
            ph.recip(sm[:, 3:4], sm[:, 2:3], [sm], [sm])
            ev = lg[:, 4:36].rearrange("p (g e) -> p g e", e=8)
            ph.tt("dve", es[:], ev, mk[:, :].unsqueeze(2).to_broadcast([128, 4, 8]), ALU.mult, [lg, mk], [es])
            ph.add("dve", lambda e: e.tensor_reduce(out=e8[:], in_=es[:].rearrange("p g e -> p e g"), axis=AX.X, op=ALU.add),
                   _bufs([es]), _bufs([e8]))
            ph.add("dve", lambda e: e.max(out=m8[:], in_=e8[:]), _bufs([e8]), _bufs([m8]))
            ph.tt("dve", sm[:, 4:5], m8[:, 0:1], m8[:, 1:2], ALU.subtract, [m8], [sm])
            ph.act(sm[:, 5:6], sm[:, 4:5], AF.Sigmoid, [sm], [sm])
            ph.tt("dve", sm[:, 5:6], sm[:, 5:6], sm[:, 3:4], ALU.mult, [sm], [sm])
            ph.tt("dve", sm[:, 6:7], sm[:, 3:4], sm[:, 5:6], ALU.subtract, [sm], [sm])
            cbt = cb_[j % 2]
            ph.ts("dve", c1[:], lg[:, 4:36], m8[:, 0:1], sm[:, 5:6], ALU.is_equal, ALU.mult, [lg, m8, sm], [c1])
            ph.ts("dve", c2[:], lg[:, 4:36], m8[:, 1:2], sm[:, 6:7], ALU.is_equal, ALU.mult, [lg, m8, sm], [c2])
            ph.tt("dve", c1[:], c1[:], c2[:], ALU.add, [c1, c2], [c1])
            ph.tt("dve", cbt[:].rearrange("p (g e) -> p g e", e=8), c1[:].rearrange("p (g e) -> p g e", e=8),
                  mk[:, :].unsqueeze(2).to_broadcast([128, 4, 8]), ALU.mult, [c1, mk], [cbt])
            ph.dma(dr["comb"][tok:tok + 128, :], cbt[:], [cbt], [], eng="pool")


def phase_moe(ph, dr, l, tstart, final):
    eps_const(ph)
    ntok = T - tstart
    half = ntok // 2
    assert half % 128 == 0
    nth = half // 128
    Gf = {}
    for row in ((0, 1) if tstart == 0 else (0,)):
        Gf[row] = load_bc(ph, dr, row, 5, "Gf")
    if final:
        gfin = ph.sb([128, D], F32, "gfin")
        ph.dma(gfin[:], dr["g_final"].rearrange("(o n) -> o n", o=1).partition_broadcast(128), [], [gfin])
    hT = ph.sb([128, 8, half], BF16, "hT")
    acc = ph.sb([128, nth, D], F32, "acc")
    comb = ph.sb([128, nth, NE], F32, "comb")
    s13 = [ph.sb([128, 8, 256], F32, "s13") for _ in range(2)]
    s2 = [ph.sb([128, 2, D], F32, "s2") for _ in range(1)]
    w13 = [ph.sb([128, 8, 512], BF16, "w13") for _ in range(2)]
    w2 = [ph.sb([128, 2, D], BF16, "w2") for _ in range(2)]
    sg = [ph.sb([128, 2, 512], F32, "sg") for _ in range(2)]
    he = [ph.sb([128, 2, 512], BF16, "he") for _ in range(2)]
    xt = [ph.sb([128, D], F32, "xt") for _ in range(2)]
    junk = ph.sb([128, D], F32, "junk")
    ss = ph.sb([128, 1], F32, "ss")
    rstd = ph.sb([128, 1], F32, "rstd")
    ctr = {"pd": 0}
    for hf in range(2):
        h0 = tstart + hf * half
        ph.dma(hT[:, :, :], dr["h2T"][:, h0:h0 + half].rearrange("(c p) t -> p c t", p=128), [], [hT])
        ph.dma(comb[:, :, :], dr["comb"][h0:h0 + half, :].rearrange("(j p) e -> p j e", p=128), [], [comb])

        def load_w(e_):
            a2, b13, b2 = s2[0], w13[e_ % 2], w2[e_ % 2]
            ph.dma(s13[0][:], dr["moe_w1"][l, e_].rearrange("(c p) n -> p c n", p=128), [], [s13[0]])
            ph.dma(s13[1][:], dr["moe_w3"][l, e_].rearrange("(c p) n -> p c n", p=128), [], [s13[1]])
            ph.dma(a2[:], dr["moe_w2"][l, e_].rearrange("(c p) n -> p c n", p=128), [], [a2])
            ph.cp("pool", b13[:, :, 0:256], s13[0][:], [s13[0]], [b13])
            ph.cp("pool", b13[:, :, 256:512], s13[1][:], [s13[1]], [b13])
            ph.cp("pool", b2[:], a2[:], [a2], [b2])

        items = []
        for e_ in range(NE):
            for bi_, (b0, n) in enumerate(token_blocks(0, half)):
                items.append(dict(e=e_, b0=b0, n=n, first=(bi_ == 0), idx=len(items)))

        def UP(it):
            i, e_, b0, n = it["idx"], it["e"], it["b0"], it["n"]
            b13 = w13[e_ % 2]
            s_, h_ = sg[i % 2], he[i % 2]
            for m in range(2):
                pg, pu = ph.pb[2 * m], ph.pb[2 * m + 1]
                for c in range(8):
                    ph.mm(pg[:, 0:n], b13[:, c, m * 128:(m + 1) * 128], hT[:, c, b0:b0 + n], c == 0, c == 7, [b13, hT], [pg])
                for c in range(8):
                    ph.mm(pu[:, 0:n], b13[:, c, 256 + m * 128:256 + (m + 1) * 128], hT[:, c, b0:b0 + n], c == 0, c == 7, [b13, hT], [pu])
                ph.act(s_[:, m, 0:n], pg[:, 0:n], AF.Silu, [pg], [s_])
                ph.tt("dve", h_[:, m, 0:n], pu[:, 0:n], s_[:, m, 0:n], ALU.mult, [pu, s_], [h_])

        def DN(it):
            i, e_, b0, n = it["idx"], it["e"], it["b0"], it["n"]
            b2 = w2[e_ % 2]
            h_ = he[i % 2]
            for j in range(n // 128):
                tj = (b0 // 128) + j
                for nh in range(2):
                    pd = ph.pb[4 + ctr["pd"] % 4]
                    ctr["pd"] += 1
                    for c in range(2):
                        ph.mm(pd[:, :], h_[:, c, j * 128:(j + 1) * 128], b2[:, c, nh * 512:(nh + 1) * 512], c == 0, c == 1, [h_, b2], [pd])
                    asl = acc[:, tj, nh * 512:(nh + 1) * 512]
                    if e_ == 0:
                        ph.ts("dve", asl, pd[:, :], comb[:, tj, e_:e_ + 1], None, ALU.mult, None, [pd, comb], [acc])
                    else:
                        ph.stt("dve", asl, pd[:, :], comb[:, tj, e_:e_ + 1], asl, ALU.mult, ALU.add, [pd, comb, acc], [acc])

        load_w(0)
        N = len(items)
        for i in range(N + 1):
            if i < N:
                UP(items[i])
            if i >= 1:
                DN(items[i - 1])
            if i < N and items[i]["first"] and items[i]["e"] + 1 < NE:
                load_w(items[i]["e"] + 1)
        for tj in range(nth):
            tok = h0 + tj * 128
            row = 1 if tok < NCTX else 0
            x_ = xt[tj % 2]
            ph.dma(x_[:], dr["x2"][tok:tok + 128, :], [], [x_])
            ph.tt("pool", acc[:, tj, :], acc[:, tj, :], Gf[row][:], ALU.mult, [acc, Gf[row]], [acc])
            ph.tt("pool", x_[:], x_[:], acc[:, tj, :], ALU.add, [x_, acc], [x_])
            if not final:
                ph.dma(dr["xres"][tok:tok + 128, :], x_[:], [x_], [], eng="pool")
            else:
                ph.act(junk[:], x_[:], AF.Square, [x_], [junk, ss], accum=ss[:, 0:1])
                ph.act(rstd[:, 0:1], ss[:, 0:1], AF.Sqrt, [ss], [rstd], bias=ph.epsc[:, 0:1], scale=1.0 / D)
                ph.recip(rstd[:, 0:1], rstd[:, 0:1], [rstd], [rstd])
                ph.stt("dve", x_[:], x_[:], rstd[:, 0:1], gfin[:], ALU.mult, ALU.mult, [x_, rstd, gfin], [x_])
                ph.dma(dr["out"][tok - NCTX:tok - NCTX + 128, :], x_[:], [x_], [], eng="pool")


WEIGHTS = [("w_mod", [2, D, 6 * D]), ("b_mod", [2, 6 * D]), ("g_mix", [2, D]), ("g_ffn", [2, D]), ("w_in", [2, D, DIN]),
           ("conv_w", [2, 4, 512]), ("conv_b", [2, 512]), ("lru_wa", [2, 2, 8, 64, 64]), ("lru_ba", [2, 2, 512]),
           ("lru_wi", [2, 2, 8, 64, 64]), ("lru_bi", [2, 2, 512]), ("lru_lambda", [2, 2, 512]), ("mla_gq", [2, 256]),
           ("mla_wuq", [2, 256, 768]), ("mla_gkv", [2, 128]), ("mla_wukv", [2, 128, 1024]), ("gqa_gq", [2, 64]),
           ("gqa_gk", [2, 64]), ("w_branch", [2, 3, 512, D]), ("w_out", [2, D, D]), ("moe_wg", [2, D, 4]), ("moe_bg", [2, 4]),
           ("moe_we", [2, D, 32]), ("moe_be", [2, 32]), ("moe_w1", [2, NE, D, DE]), ("moe_w3", [2, NE, D, DE]),
           ("moe_w2", [2, NE, DE, D]), ("g_final", [D])]

SCRATCH = [("modv", [2, 6 * D], F32), ("xres", [T, D], F32), ("x2", [T, D], F32), ("xrT", [512, T], BF16),
           ("rgT", [512, T], BF16), ("gatesT", [3 * D, T], BF16), ("kmT", [8, 96, T], BF16), ("qmT", [8, 96, T], BF16),
           ("vm", [T, 512], BF16), ("kgT", [2, 64, T], BF16), ("qgT", [8, 64, T], BF16), ("vg", [T, 128], BF16),
           ("yT", [3, 512, T], BF16), ("h2T", [D, T], BF16), ("comb", [T, NE], F32)]


def build_nc(phases=None, debug=()):
    nc = bass.Bass("TRN2", target_bir_lowering=False)
    dr = {}
    dr["xin"] = nc.dram_tensor("xin", [T, D], F32, kind="ExternalInput").ap()
    dr["cc"] = nc.dram_tensor("cc", [2, D], F32, kind="ExternalInput").ap()
    dr["ropem"] = nc.dram_tensor("ropem", [2, 32, T], BF16, kind="ExternalInput").ap()
    dr["ropeg"] = nc.dram_tensor("ropeg", [2, 64, T], BF16, kind="ExternalInput").ap()
    for nm, shp in WEIGHTS:
        dr[nm] = nc.dram_tensor(nm, shp, F32, kind="ExternalInput").ap()
    dr["out"] = nc.dram_tensor("out", [SEQ, D], F32, kind="ExternalOutput").ap()
    for nm, shp, dt in SCRATCH:
        if nm in debug:
            dr[nm] = nc.dram_tensor(nm, shp, dt, kind="ExternalOutput").ap()
        else:
            dr[nm] = nc.dram_tensor(nm, shp, dt).ap()
    ps = nc.alloc_psum_tensor("ps", [128, 4096], F32)
    for l in range(2):
        xsrc = dr["xin"] if l == 0 else dr["xres"]
        last = l == 1
        tstart = NCTX if last else 0
        plan = [("mod", phase_mod, (dr, l)), ("inproj", phase_inproj, (dr, l, xsrc)), ("lru", phase_lru, (dr, l)),
                ("attn", phase_attn, (dr, l, not last)), ("merge", phase_merge, (dr, l, xsrc, tstart)),
                ("moe", phase_moe, (dr, l, tstart, last))]
        for nm, fn, args in plan:
            if phases is not None and (l, nm) not in phases:
                continue
            run_phase(nc, ps, fn, *args)
    return nc


def rope_consts():
    def tab(rot):
        q = rot // 4
        pos = np.arange(SEQ)
        row = (pos // 64).astype(np.float32)
        col = (pos % 64).astype(np.float32)
        freqs = (np.float32(10000.0) ** (-np.arange(q, dtype=np.float32) / np.float32(q))).astype(np.float32)
        ang = np.concatenate([row[:, None] * freqs, col[:, None] * freqs], axis=-1).astype(np.float32)
        cos, sin = np.cos(ang).T, np.sin(ang).T
        C = np.ones((rot, T), np.float32)
        S = np.zeros((rot, T), np.float32)
        C[:, NCTX:] = np.concatenate([cos, cos], axis=0)
        S[:, NCTX:] = np.concatenate([-sin, sin], axis=0)
        return np.stack([C, S]).astype(ml_dtypes.bfloat16)
    return tab(32), tab(64)


_CACHE = {}


def kernel(**inputs):
    x = np.asarray(inputs["x"], np.float32)
    ctx = np.asarray(inputs["ctx"], np.float32)
    c = np.asarray(inputs["c"], np.float32)
    c_ctx = np.asarray(inputs["c_ctx"], np.float32)
    B = x.shape[0]
    if "nc" not in _CACHE:
        _CACHE["nc"] = build_nc()
    nc = _CACHE["nc"]
    ropem, ropeg = rope_consts()
    shared = {nm: np.ascontiguousarray(np.asarray(inputs[nm], np.float32)) for nm, _ in WEIGHTS}
    shared["ropem"] = ropem
    shared["ropeg"] = ropeg
    in_maps = []
    for b in range(B):
        m = dict(shared)
        m["xin"] = np.ascontiguousarray(np.concatenate([ctx[b], x[b]], axis=0))
        m["cc"] = np.ascontiguousarray(np.stack([c[b], c_ctx], axis=0))
        in_maps.append(m)
    res = run_bass_kernel_spmd(nc, in_maps, core_ids=list(range(B)))
    return np.stack([np.asarray(r["out"], np.float32) for r in res.results], axis=0)
```

```python
import numpy as np
import ml_dtypes
import concourse.bass as bass
import concourse.mybir as mybir
from concourse.bass_utils import run_bass_kernel_spmd

F32 = mybir.dt.float32
BF16 = mybir.dt.bfloat16
AF = mybir.ActivationFunctionType
ALU = mybir.AluOpType
AX = mybir.AxisListType

D = 1024
NCTX = 256
SEQ = 4096
T = NCTX + SEQ
NT = T // 128
DIN = 5280
EPS = 1e-6
C_XR, C_CKV, C_KR, C_GK, C_GV, C_RG, C_CQ, C_GQ, C_MG = 0, 512, 640, 672, 800, 928, 1440, 1696, 2208
MLA_SCALE = 96 ** -0.5
GQA_SCALE = 64 ** -0.5
NE = 32
DE = 256
import os as _os
DBG_SKIP_ROUTER = bool(_os.environ.get('DBG_SKIP_ROUTER'))
DBG_STOP = int(_os.environ.get('DBG_STOP', '99'))


class Buf:
    __slots__ = ("name", "lastw", "readers")

    def __init__(self, name=""):
        self.name = name
        self.lastw = None
        self.readers = []


class Op:
    __slots__ = ("eng", "fn", "deps", "dma", "tok", "needed", "idx")


class Prog:
    COMPUTE = ("pe", "act", "dve", "pool")
    NPOOL = 12
    UID = 0

    def __init__(self, nc):
        self.nc = nc
        self.ops = []
        self.q = {k: [] for k in ("pe", "act", "dve", "pool", "sp")}

    def add(self, eng, fn, reads=(), writes=(), dma=False):
        op = Op()
        op.eng, op.fn, op.dma, op.tok, op.needed = eng, fn, dma, None, False
        op.idx = len(self.ops)
        deps = set()
        for b in reads:
            if b.lastw is not None:
                deps.add(b.lastw)
        for b in writes:
            if b.lastw is not None:
                deps.add(b.lastw)
            deps.update(b.readers)
        op.deps = deps
        for b in reads:
            if b in writes:
                continue
            if not dma:
                b.readers = [r for r in b.readers if self.ops[r].dma or self.ops[r].eng != eng]
            b.readers.append(op.idx)
        for b in writes:
            b.lastw = op.idx
            b.readers = []
        self.ops.append(op)
        self.q[eng].append(op)
        return op

    def emit(self):
        nc, ops = self.nc, self.ops
        for op in ops:
            for d in op.deps:
                dop = ops[d]
                if dop.eng == "pe" and op.eng == "pe" and not dop.dma and not op.dma:
                    continue
                dop.needed = True
        Prog.UID += 1
        u = Prog.UID
        sems = {k: nc.alloc_semaphore("s%d_%s" % (u, k)) for k in self.COMPUTE}
        dsem = {k: [nc.alloc_semaphore("d%d_%s_%d" % (u, k, i)) for i in range(self.NPOOL)] for k in self.q}
        cnt = {k: 0 for k in self.COMPUTE}
        dcnt = {k: 0 for k in self.q}
        prewait = {}
        for op in ops:
            if op.dma:
                k = dcnt[op.eng]
                dcnt[op.eng] += 1
                s = dsem[op.eng][k % self.NPOOL]
                op.tok = (s, 16 * (k // self.NPOOL + 1))
                if k >= self.NPOOL:
                    prewait[op.idx] = (s, 16 * (k // self.NPOOL))
            elif op.needed:
                cnt[op.eng] += 1
                op.tok = (sems[op.eng], cnt[op.eng])
        engines = {"pe": "tensor", "act": "scalar", "dve": "vector", "pool": "gpsimd", "sp": "sync"}
        with nc.Block() as block:
            def make(k):
                def body(e):
                    known = {}
                    for op in self.q[k]:
                        waits = []
                        if op.idx in prewait:
                            waits.append(prewait[op.idx])
                        for d in sorted(op.deps):
                            dop = ops[d]
                            if dop.tok is None:
                                continue
                            if dop.eng == "pe" and k == "pe" and not dop.dma and not op.dma:
                                continue
                            waits.append(dop.tok)
                        for (s, v) in waits:
                            if known.get(id(s), 0) >= v:
                                continue
                            known[id(s)] = v
                            e.wait_ge(s, v)
                        ins = op.fn(e)
                        if op.tok is not None:
                            ins.then_inc(op.tok[0], 16 if op.dma else 1)
                    if k == "sp":
                        for kk in self.q:
                            n = dcnt[kk]
                            for j in range(min(n, self.NPOOL)):
                                uses = (n - j + self.NPOOL - 1) // self.NPOOL
                                e.wait_ge(dsem[kk][j], 16 * uses)
                return body
            for k, attr in engines.items():
                getattr(block, attr)(make(k))


class Tl:
    def __init__(self, t, name=""):
        self.t = t
        self.b = Buf(name)

    def __getitem__(self, k):
        return self.t[k]


def _bufs(lst):
    return [x.b if isinstance(x, Tl) else x for x in lst]


class Ph:
    def __init__(self, nc, ps):
        self.nc = nc
        self.P = Prog(nc)
        self.ps = ps
        self.pb = [Tl(ps[:, i * 512:(i + 1) * 512], "pb%d" % i) for i in range(8)]
        self.n = 0
        Ph.UID += 1
        self.uid = Ph.UID

    UID = 0

    def sb(self, shape, dt=F32, name=None):
        self.n += 1
        t = self.nc.alloc_sbuf_tensor("%s_%d_%d" % (name or "t", self.uid, self.n), list(shape), dt)
        return Tl(t, name or "t")

    def add(self, eng, fn, r, w, dma=False):
        return self.P.add(eng, fn, _bufs(r), _bufs(w), dma=dma)

    def dma(self, out, in_, r=(), w=(), eng="sp", **kw):
        return self.add(eng, lambda e: e.dma_start(out=out, in_=in_, **kw), r, w, dma=True)

    def mm(self, out, lhsT, rhs, start, stop, r, w):
        return self.add("pe", lambda e: e.matmul(out, lhsT=lhsT, rhs=rhs, start=start, stop=stop), r, w)

    def tr(self, out, in_, ident, r, w):
        return self.add("pe", lambda e: e.transpose(out=out, in_=in_, identity=ident), r, w)

    def act(self, out, in_, func, r, w, bias=None, scale=None, accum=None):
        kw = {}
        if bias is not None:
            kw["bias"] = bias
        if scale is not None:
            kw["scale"] = scale
        if accum is not None:
            kw["accum_out"] = accum
        return self.add("act", lambda e: e.activation(out=out, in_=in_, func=func, **kw), r, w)

    def tt(self, eng, out, in0, in1, op, r, w):
        return self.add(eng, lambda e: e.tensor_tensor(out=out, in0=in0, in1=in1, op=op), r, w)

    def ts(self, eng, out, in0, s1, s2, op0, op1, r, w):
        if op1 is None:
            return self.add(eng, lambda e: e.tensor_scalar(out=out, in0=in0, scalar1=s1, scalar2=None, op0=op0), r, w)
        return self.add(eng, lambda e: e.tensor_scalar(out=out, in0=in0, scalar1=s1, scalar2=s2, op0=op0, op1=op1), r, w)

    def stt(self, eng, out, in0, sc, in1, op0, op1, r, w):
        return self.add(eng, lambda e: e.scalar_tensor_tensor(out=out, in0=in0, scalar=sc, in1=in1, op0=op0, op1=op1), r, w)

    def cp(self, eng, out, in_, r, w):
        if eng == "act":
            return self.add("act", lambda e: e.activation(out=out, in_=in_, func=AF.Copy), r, w)
        return self.add(eng, lambda e: e.tensor_copy(out=out, in_=in_), r, w)

    def memset(self, eng, ap, val, w):
        return self.add(eng, lambda e: e.memset(ap, val), [], w)

    def recip(self, out, in_, r, w):
        return self.add("dve", lambda e: e.reciprocal(out=out, in_=in_), r, w)

    def breg(self, e, val):
        if not hasattr(self, "_regs"):
            self._regs = {}
        if val not in self._regs:
            r = e.alloc_register("bnd_%d_%d" % (self.uid, val))
            e.reg_mov(r, val)
            self._regs[val] = r
        return self._regs[val]

    def finish(self):
        self.P.emit()


def run_phase(nc, ps, fn, *args):
    with nc.cleanup_on_exit():
        ph = Ph(nc, ps)
        fn(ph, *args)
        ph.finish()
        nc.all_engine_barrier()


def token_blocks(t0, t1, bs=512):
    out = []
    t = t0
    while t < t1:
        n = min(bs, t1 - t)
        out.append((t, n))
        t += n
    return out


def make_identity(ph, dt, name):
    idf = ph.sb([128, 128], F32, name + "f")
    ph.memset("pool", idf[:], 0.0, [idf])
    ph.add("pool", lambda e: e.affine_select(out=idf[:], in_=idf[:], pattern=[[-1, 128]], compare_op=ALU.not_equal,
                                            fill=1.0, base=0, channel_multiplier=1), [idf], [idf])
    if dt == F32:
        return idf
    idb = ph.sb([128, 128], dt, name)
    ph.cp("pool", idb[:], idf[:], [idf], [idb])
    return idb


def phase_mod(ph, dr, l):
    cc = ph.sb([128, 2, 8], F32, "cc")
    sc = ph.sb([128, 2, 8], F32, "sc")
    ph.dma(cc[:], dr["cc"].rearrange("r (p k) -> p r k", k=8), [], [cc])
    ph.act(sc[:], cc[:], AF.Silu, [cc], [sc])
    mods = ph.sb([2, 6 * D], F32, "mods")
    bm = ph.sb([2, 6 * D], F32, "bm")
    ph.dma(bm[:], dr["b_mod"][l:l + 1, :].partition_broadcast(2), [], [bm])
    gm = ph.sb([2, D], F32, "gm")
    gf = ph.sb([2, D], F32, "gf")
    ph.dma(gm[:], dr["g_mix"][l:l + 1, :].partition_broadcast(2), [], [gm])
    ph.dma(gf[:], dr["g_ffn"][l:l + 1, :].partition_broadcast(2), [], [gf])
    wv = dr["w_mod"][l].rearrange("(p k) n -> p k n", k=8)
    wb = [ph.sb([128, 8, 512], F32, "wb") for _ in range(2)]
    for nb in range(12):
        w = wb[nb % 2]
        ph.dma(w[:], wv[:, :, nb * 512:(nb + 1) * 512], [], [w])
        pb = ph.pb[nb % 2]
        for k in range(8):
            ph.mm(pb[0:2, :], sc[:, :, k], w[:, k, :], k == 0, k == 7, [sc, w], [pb])
        ph.tt("dve", mods[:, nb * 512:(nb + 1) * 512], pb[0:2, :], bm[:, nb * 512:(nb + 1) * 512], ALU.add, [pb, bm], [mods])
    ph.stt("dve", mods[:, D:2 * D], mods[:, D:2 * D], 1.0, gm[:], ALU.add, ALU.mult, [mods, gm], [mods])
    ph.stt("dve", mods[:, 4 * D:5 * D], mods[:, 4 * D:5 * D], 1.0, gf[:], ALU.add, ALU.mult, [mods, gf], [mods])
    ph.dma(dr["modv"][:, :], mods[:], [mods], [])


def load_bc(ph, dr, row, idx, name):
    t = ph.sb([128, D], F32, name)
    ph.dma(t[:], dr["modv"][row:row + 1, idx * D:(idx + 1) * D].partition_broadcast(128), [], [t])
    return t


def rms_modulate(ph, xt, A, B, hout, junk, ss, rstd, h32=None):
    ph.act(junk[:], xt[:], AF.Square, [xt], [junk, ss], accum=ss[:, 0:1])
    ph.act(rstd[:, 0:1], ss[:, 0:1], AF.Sqrt, [ss], [rstd], bias=ph.epsc[:, 0:1], scale=1.0 / D)
    ph.recip(rstd[:, 0:1], rstd[:, 0:1], [rstd], [rstd])
    tmp = h32 if h32 is not None else junk
    ph.stt("dve", tmp[:], xt[:], rstd[:, 0:1], A[:], ALU.mult, ALU.mult, [xt, rstd, A], [tmp])
    ph.tt("dve", hout[:], tmp[:], B[:], ALU.add, [tmp, B], [hout])


def eps_const(ph):
    ph.epsc = ph.sb([128, 1], F32, "eps")
    ph.memset("pool", ph.epsc[:], EPS, [ph.epsc])


def phase_inproj(ph, dr, l, xsrc):
    nc = ph.nc
    eps_const(ph)
    win = ph.sb([128, 8, DIN], BF16, "win")
    stg = [ph.sb([128, 1320], F32, "stg") for _ in range(2)]
    wv = dr["w_in"][l].rearrange("(k p) n -> p k n", p=128)
    i = 0
    for k in range(8):
        for c4 in range(4):
            s = stg[i % 2]
            ph.dma(s[:], wv[:, k, c4 * 1320:(c4 + 1) * 1320], [], [s])
            ph.cp("dve" if i % 2 == 0 else "pool", win[:, k, c4 * 1320:(c4 + 1) * 1320], s[:], [s], [win])
            i += 1
    wkr = ph.sb([128, 8, 96], BF16, "wkr")
    wkrs = ph.sb([128, 8, 96], BF16, "wkrs")
    ph.memset("pool", wkr[:], 0.0, [wkr])
    ph.memset("pool", wkrs[:], 0.0, [wkrs])
    ph.cp("pool", wkr[:, :, 64:96], win[:, :, C_KR:C_KR + 32], [win], [wkr])
    ph.cp("pool", wkrs[:, :, 64:80], win[:, :, C_KR + 16:C_KR + 32], [win], [wkrs])
    ph.cp("pool", wkrs[:, :, 80:96], win[:, :, C_KR:C_KR + 16], [win], [wkrs])
    wgks = ph.sb([128, 8, 128], BF16, "wgks")
    wgqs = ph.sb([128, 8, 512], BF16, "wgqs")
    for (dst, c0, n) in ((wgks, C_GK, 128), (wgqs, C_GQ, 512)):
        sv = win[:, :, c0:c0 + n].rearrange("p k (h two d) -> p k h two d", two=2, d=32)
        dv = dst[:, :, :].rearrange("p k (h two d) -> p k h two d", two=2, d=32)
        ph.cp("pool", dv[:, :, :, 0, :], sv[:, :, :, 1, :], [win], [dst])
        ph.cp("pool", dv[:, :, :, 1, :], sv[:, :, :, 0, :], [win], [dst])
    gq = ph.sb([128, 2], F32, "gq")
    ph.dma(gq[:], dr["mla_gq"][l].rearrange("(c p) -> p c", p=128), [], [gq], allow_slow_non_contiguous=True)
    gkv = ph.sb([128, 1], F32, "gkv")
    ph.dma(gkv[:], dr["mla_gkv"][l].rearrange("(p o) -> p o", o=1), [], [gkv])
    wuqf = ph.sb([128, 2, 768], F32, "wuqf")
    ph.dma(wuqf[:], dr["mla_wuq"][l].rearrange("(c p) n -> p c n", p=128), [], [wuqf])
    wuq = ph.sb([128, 2, 768], BF16, "wuq")
    wuqs = ph.sb([128, 2, 768], BF16, "wuqs")
    for c in range(2):
        ph.ts("dve", wuq[:, c, :], wuqf[:, c, :], gq[:, c:c + 1], None, ALU.mult, None, [wuqf, gq], [wuq])
    ph.cp("pool", wuqs[:], wuq[:], [wuq], [wuqs])
    v1 = wuq[:, :, :].rearrange("p c (h d) -> p c h d", d=96)
    v2 = wuqs[:, :, :].rearrange("p c (h d) -> p c h d", d=96)
    ph.cp("pool", v2[:, :, :, 64:80], v1[:, :, :, 80:96], [wuq], [wuqs])
    ph.cp("pool", v2[:, :, :, 80:96], v1[:, :, :, 64:80], [wuq], [wuqs])
    wkvf = ph.sb([128, 1024], F32, "wkvf")
    ph.dma(wkvf[:], dr["mla_wukv"][l], [], [wkvf])
    wkv = ph.sb([128, 2, 512], BF16, "wkv")
    sv = wkvf[:, :].rearrange("p (h two d) -> p two h d", two=2, d=64)
    for two in range(2):
        ph.ts("dve", wkv[:, two, :].rearrange("p (h d) -> p h d", d=64), sv[:, two, :, :], gkv[:, 0:1], None, ALU.mult, None,
              [wkvf, gkv], [wkv])
    ones = ph.sb([128, 128], BF16, "ones")
    ph.memset("pool", ones[:], 1.0, [ones])
    bones = ph.sb([128, 128], BF16, "bones")
    ph.memset("pool", bones[:], 0.0, [bones])
    ph.memset("pool", bones[0:64, 0:64], 1.0, [bones])
    ph.memset("pool", bones[64:128, 64:128], 1.0, [bones])
    ident = make_identity(ph, BF16, "ident")
    gcol = ph.sb([128, 4], F32, "gcol")
    for j, nm in ((0, "gqa_gq"), (2, "gqa_gk")):
        src = dr[nm][l].rearrange("(d o) -> d o", o=1)
        for hh in range(2):
            ph.dma(gcol[hh * 64:hh * 64 + 64, j:j + 1], src[0:64, :], [], [gcol])
            ph.dma(gcol[hh * 64:hh * 64 + 32, j + 1:j + 2], src[32:64, :], [], [gcol])
            ph.dma(gcol[hh * 64 + 32:hh * 64 + 64, j + 1:j + 2], src[0:32, :], [], [gcol])
    Al = load_bc(ph, dr, 0, 1, "Al")
    Bl = load_bc(ph, dr, 0, 0, "Bl")
    Ac = load_bc(ph, dr, 1, 1, "Ac")
    Bc = load_bc(ph, dr, 1, 0, "Bc")

    xt = [ph.sb([128, D], F32, "xt") for _ in range(2)]
    junk = ph.sb([128, D], F32, "junk")
    hb = [ph.sb([128, D], BF16, "hb") for _ in range(2)]
    ss = ph.sb([128, 1], F32, "ss")
    rstd = ph.sb([128, 1], F32, "rstd")
    hT = [ph.sb([128, 8, 512], BF16, "hT") for _ in range(2)]
    tabm = [ph.sb([96, 2, 512], BF16, "tabm") for _ in range(2)]
    tabg = [ph.sb([128, 2, 512], BF16, "tabg") for _ in range(2)]
    NOB = 6
    ob = [ph.sb([128, 512], BF16, "ob") for _ in range(NOB)]
    NF = 6
    fb = [ph.sb([128, 512], F32, "fb") for _ in range(NF)]
    nck = ph.sb([128, 512], BF16, "nck")
    ncq = [ph.sb([128, 512], BF16, "ncq") for _ in range(2)]
    ctr = {"ob": 0, "fb": 0, "pb": 0, "ev": 0}

    def nob():
        ctr["ob"] += 1
        return ob[ctr["ob"] % NOB]

    def nfb():
        ctr["fb"] += 1
        return fb[ctr["fb"] % NF]

    def npb():
        ctr["pb"] += 1
        return ph.pb[ctr["pb"] % 8]

    def evac_eng():
        ctr["ev"] += 1
        return "act" if ctr["ev"] % 2 == 0 else "dve"

    def proj(lhs_tile, c0, m, hTb, n, extra_r=()):
        pb = npb()
        for k in range(8):
            ph.mm(pb[0:m, 0:n], lhs_tile[:, k, c0:c0 + m], hTb[:, k, 0:n], k == 0, k == 7, [lhs_tile, hTb], [pb])
        return pb

    def store(dst_ap, src_tile, src_ap, eng="pool"):
        ph.dma(dst_ap, src_ap, [src_tile], [], eng=eng)

    bi = 0
    for (t0, n) in token_blocks(0, T):
        hTb = hT[bi % 2]
        tm = tabm[bi % 2]
        tg = tabg[bi % 2]
        ph.dma(tm[64:96, :, 0:n], dr["ropem"][:, :, t0:t0 + n].rearrange("a r t -> r a t"), [], [tm])
        for hh in range(2):
            ph.dma(tg[hh * 64:hh * 64 + 64, :, 0:n], dr["ropeg"][:, :, t0:t0 + n].rearrange("a r t -> r a t"), [], [tg])
        for j in range(n // 128):
            tok = t0 + j * 128
            x_ = xt[j % 2]
            h_ = hb[j % 2]
            ph.dma(x_[:], xsrc[tok:tok + 128, :], [], [x_])
            isctx = tok < NCTX
            rms_modulate(ph, x_, Ac if isctx else Al, Bc if isctx else Bl, h_, junk, ss, rstd)
            pb = npb()
            pbv = pb.t.bitcast(BF16)
            for k in range(8):
                ph.tr(pbv[:, k * 128:(k + 1) * 128], h_[:, k * 128:(k + 1) * 128], ident[:], [h_, ident], [pb])
            ph.cp(evac_eng(), hTb[:, :, j * 128:(j + 1) * 128], pbv[:, :].rearrange("p (k t) -> p k t", t=128), [pb], [hTb])
        sl = slice(t0, t0 + n)
        for c in range(4):
            pb = proj(win, C_XR + c * 128, 128, hTb, n)
            o = nob()
            ph.cp(evac_eng(), o[:, 0:n], pb[:, 0:n], [pb], [o])
            store(dr["xrT"][c * 128:(c + 1) * 128, sl], o, o[:, 0:n])
        for c in range(4):
            pb = proj(win, C_RG + c * 128, 128, hTb, n)
            o = nob()
            ph.act(o[:, 0:n], pb[:, 0:n], AF.Gelu_apprx_tanh, [pb], [o])
            store(dr["rgT"][c * 128:(c + 1) * 128, sl], o, o[:, 0:n])
        for c in range(24):
            pb = proj(win, C_MG + c * 128, 128, hTb, n)
            o = nob()
            ph.act(o[:, 0:n], pb[:, 0:n], AF.Sigmoid, [pb], [o])
            store(dr["gatesT"][c * 128:(c + 1) * 128, sl], o, o[:, 0:n])
        for j in range(n // 128):
            pb = npb()
            for k in range(8):
                ph.mm(pb[:, 0:128], hTb[:, k, j * 128:(j + 1) * 128], win[:, k, C_GV:C_GV + 128], k == 0, k == 7, [hTb, win], [pb])
            o = nob()
            ph.cp(evac_eng(), o[:, 0:128], pb[:, 0:128], [pb], [o])
            store(dr["vg"][t0 + j * 128:t0 + (j + 1) * 128, :], o, o[:, 0:128])

        def rstd_bc(sq_list, ones_t, count):
            pb = npb()
            for i_, sq in enumerate(sq_list):
                ph.mm(pb[:, 0:n], ones_t[:], sq[:, 0:n], i_ == 0, i_ == len(sq_list) - 1, [ones_t, sq], [pb])
            r_ = nfb()
            ph.act(r_[:, 0:n], pb[:, 0:n], AF.Sqrt, [pb], [r_], bias=ph.epsc[:, 0:1], scale=1.0 / count)
            ph.recip(r_[:, 0:n], r_[:, 0:n], [r_], [r_])
            return r_

        pa = proj(win, C_CKV, 128, hTb, n)
        sq = nob()
        ph.act(sq[:, 0:n], pa[:, 0:n], AF.Square, [pa], [sq])
        r_ = rstd_bc([sq], ones, 128)
        ph.tt("dve", nck[:, 0:n], pa[:, 0:n], r_[:, 0:n], ALU.mult, [pa, r_], [nck])
        for hp in range(4):
            pb = npb()
            ph.mm(pb[:, 0:n], wkv[:, 0, hp * 128:(hp + 1) * 128], nck[:, 0:n], True, True, [wkv, nck], [pb])
            o = nob()
            ph.cp(evac_eng(), o[:, 0:n], pb[:, 0:n], [pb], [o])
            for hh in range(2):
                store(dr["kmT"][2 * hp + hh, 0:64, sl], o, o[hh * 64:hh * 64 + 64, 0:n])
        for j in range(n // 128):
            pb = npb()
            ph.mm(pb[:, :], nck[:, j * 128:(j + 1) * 128], wkv[:, 1, :], True, True, [nck, wkv], [pb])
            o = nob()
            ph.cp(evac_eng(), o[:, :], pb[:, :], [pb], [o])
            store(dr["vm"][t0 + j * 128:t0 + (j + 1) * 128, :], o, o[:, :])

        def rope96(pa, pb_, dst_tile):
            t1 = nfb()
            t2 = nfb()
            ph.tt("dve", t1[64:96, 0:n], pa[64:96, 0:n], tm[64:96, 0, 0:n], ALU.mult, [pa, tm], [t1])
            ph.tt("dve", t2[64:96, 0:n], pb_[64:96, 0:n], tm[64:96, 1, 0:n], ALU.mult, [pb_, tm], [t2])
            ph.tt("pool", dst_tile[64:96, 0:n], t1[64:96, 0:n], t2[64:96, 0:n], ALU.add, [t1, t2], [dst_tile])

        pa = proj(wkr, 0, 96, hTb, n)
        pb_ = proj(wkrs, 0, 96, hTb, n)
        o = nob()
        rope96(pa, pb_, o)
        for h in range(8):
            store(dr["kmT"][h, 64:96, sl], o, o[64:96, 0:n], eng="sp" if h % 2 else "pool")
        pc = [proj(win, C_CQ + c * 128, 128, hTb, n) for c in range(2)]
        sqs = []
        for c in range(2):
            s_ = nob()
            ph.act(s_[:, 0:n], pc[c][:, 0:n], AF.Square, [pc[c]], [s_])
            sqs.append(s_)
        r_ = rstd_bc(sqs, ones, 256)
        for c in range(2):
            ph.tt("dve", ncq[c][:, 0:n], pc[c][:, 0:n], r_[:, 0:n], ALU.mult, [pc[c], r_], [ncq[c]])
        for h in range(8):
            pa = npb()
            pb_ = npb()
            for c in range(2):
                ph.mm(pa[0:96, 0:n], wuq[:, c, h * 96:(h + 1) * 96], ncq[c][:, 0:n], c == 0, c == 1, [wuq, ncq[c]], [pa])
            for c in range(2):
                ph.mm(pb_[0:96, 0:n], wuqs[:, c, h * 96:(h + 1) * 96], ncq[c][:, 0:n], c == 0, c == 1, [wuqs, ncq[c]], [pb_])
            o = nob()
            ph.cp("act", o[0:64, 0:n], pa[0:64, 0:n], [pa], [o])
            rope96(pa, pb_, o)
            store(dr["qmT"][h, :, sl], o, o[0:96, 0:n], eng="sp" if h % 2 else "pool")

        def gqa_chunk(c0, wsw, csw, gj, dst_fn):
            pa = proj(win, c0, 128, hTb, n)
            pb_ = proj(wsw, csw, 128, hTb, n)
            sq = nob()
            ph.act(sq[:, 0:n], pa[:, 0:n], AF.Square, [pa], [sq])
            r_ = rstd_bc([sq], bones, 64)
            t1 = nfb()
            t2 = nfb()
            ph.stt("dve", t1[:, 0:n], pa[:, 0:n], gcol[:, gj:gj + 1], tg[:, 0, 0:n], ALU.mult, ALU.mult, [pa, gcol, tg], [t1])
            ph.stt("dve", t2[:, 0:n], pb_[:, 0:n], gcol[:, gj + 1:gj + 2], tg[:, 1, 0:n], ALU.mult, ALU.mult, [pb_, gcol, tg], [t2])
            ph.tt("pool", t1[:, 0:n], t1[:, 0:n], t2[:, 0:n], ALU.add, [t1, t2], [t1])
            o = nob()
            ph.tt("pool", o[:, 0:n], t1[:, 0:n], r_[:, 0:n], ALU.mult, [t1, r_], [o])
            dst_fn(o)

        def st_gk(o):
            for hh in range(2):
                store(dr["kgT"][hh, :, sl], o, o[hh * 64:hh * 64 + 64, 0:n])
        gqa_chunk(C_GK, wgks, 0, 2, st_gk)
        for c in range(4):
            def st_gq(o, c=c):
                for hh in range(2):
                    store(dr["qgT"][2 * c + hh, :, sl], o, o[hh * 64:hh * 64 + 64, 0:n])
            gqa_chunk(C_GQ + c * 128, wgqs, c * 128, 0, st_gq)
        bi += 1


def phase_lru(ph, dr, l):
    blocks = token_blocks(0, T)
    xp = ph.sb([128, T + 6], BF16, "xp")
    xc = ph.sb([128, T], F32, "xc")
    xcb = ph.sb([128, T], BF16, "xcb")
    rg = ph.sb([128, T], BF16, "rg")
    ysum = ph.sb([128, T], F32, "ysum")
    abuf = ph.sb([128, T], F32, "abuf")
    ibuf = ph.sb([128, T], F32, "ibuf")
    mbuf = ph.sb([128, T], F32, "mbuf")
    hbuf = ph.sb([128, T], F32, "hbuf")
    yo = ph.sb([128, T], BF16, "yo")
    for c in range(4):
        cs = slice(c * 128, (c + 1) * 128)
        ph.memset("pool", xp[:, 0:2], 0.0, [xp])
        ph.memset("pool", xp[:, 258:261], 0.0, [xp])
        ph.memset("pool", xp[:, T + 5:T + 6], 0.0, [xp])
        ph.dma(xp[:, 2:258], dr["xrT"][cs, 0:NCTX], [], [xp])
        ph.dma(xp[:, 261:261 + SEQ], dr["xrT"][cs, NCTX:T], [], [xp])
        cw = ph.sb([128, 4], F32, "cw")
        ph.dma(cw[:], dr["conv_w"][l].rearrange("j c -> c j")[cs, :], [], [cw], allow_slow_non_contiguous=True)
        cb = ph.sb([128, 1], F32, "cb")
        ph.dma(cb[:], dr["conv_b"][l].rearrange("(c o) -> c o", o=1)[cs, :], [], [cb])
        for (o0, i0, n) in ((0, 0, NCTX), (NCTX, 259, SEQ)):
            ph.ts("dve", xc[:, o0:o0 + n], xp[:, i0:i0 + n], cw[:, 0:1], cb[:, 0:1], ALU.mult, ALU.add, [xp, cw, cb], [xc])
            for j in range(1, 4):
                ph.stt("dve", xc[:, o0:o0 + n], xp[:, i0 + j:i0 + j + n], cw[:, j:j + 1], xc[:, o0:o0 + n],
                       ALU.mult, ALU.add, [xp, cw, xc], [xc])
        ph.cp("act", xcb[:], xc[:], [xc], [xcb])
        ph.dma(rg[:], dr["rgT"][cs, :], [], [rg])
        for d in range(2):
            wst = ph.sb([128, 2, 128], F32, "wst")
            ph.memset("pool", wst[:], 0.0, [wst])
            for g_, nm in enumerate(("lru_wa", "lru_wi")):
                for hb_ in range(2):
                    ph.dma(wst[hb_ * 64:hb_ * 64 + 64, g_, hb_ * 64:hb_ * 64 + 64], dr[nm][l, d, 2 * c + hb_], [wst], [wst])
            wbd = ph.sb([128, 2, 128], BF16, "wbd")
            ph.cp("pool", wbd[:], wst[:], [wst], [wbd])
            col = ph.sb([128, 8], F32, "col")
            for j, nm in enumerate(("lru_ba", "lru_bi", "lru_lambda")):
                ph.dma(col[:, j:j + 1], dr[nm][l, d].rearrange("(c o) -> c o", o=1)[cs, :], [], [col])
            ph.act(col[:, 4:5], col[:, 2:3], AF.Exp, [col], [col], scale=-1.0)
            ph.act(col[:, 5:6], col[:, 4:5], AF.Ln, [col], [col], bias=1.0)
            ph.ts("dve", col[:, 2:3], col[:, 5:6], -8.0, None, ALU.mult, None, [col], [col])
            ph.ts("dve", col[:, 3:4], col[:, 5:6], -16.0, None, ALU.mult, None, [col], [col])
            for bi, (t0, n) in enumerate(blocks):
                pr = ph.pb[(2 * bi) % 8]
                pi = ph.pb[(2 * bi + 1) % 8]
                ph.mm(pr[:, 0:n], wbd[:, 0, :], xcb[:, t0:t0 + n], True, True, [wbd, xcb], [pr])
                ph.mm(pi[:, 0:n], wbd[:, 1, :], xcb[:, t0:t0 + n], True, True, [wbd, xcb], [pi])
                ph.act(abuf[:, t0:t0 + n], pr[:, 0:n], AF.Sigmoid, [pr, col], [abuf], bias=col[:, 0:1])
                ph.act(ibuf[:, t0:t0 + n], pi[:, 0:n], AF.Sigmoid, [pi, col], [ibuf], bias=col[:, 1:2])
            ph.act(mbuf[:], abuf[:], AF.Exp, [abuf, col], [mbuf], scale=col[:, 3:4])
            ph.act(abuf[:], abuf[:], AF.Exp, [abuf, col], [abuf], scale=col[:, 2:3])
            ph.act(mbuf[:], mbuf[:], AF.Sqrt, [mbuf], [mbuf], bias=1.0, scale=-1.0)
            ph.tt("pool", ibuf[:], ibuf[:], mbuf[:], ALU.mult, [ibuf, mbuf], [ibuf])
            ph.tt("pool", ibuf[:], ibuf[:], xc[:], ALU.mult, [ibuf, xc], [ibuf])
            dst = ysum if d == 0 else hbuf
            if d == 0:
                ph.add("dve", lambda e, dst=dst: e.tensor_tensor_scan(out=dst[:, :], data0=abuf[:, :], data1=ibuf[:, :], initial=0.0,
                                                                      op0=ALU.mult, op1=ALU.add), [abuf, ibuf], [dst])
            else:
                ph.add("dve", lambda e, dst=dst: e.tensor_tensor_scan(out=dst[:, 0:NCTX][:, ::-1], data0=abuf[:, 0:NCTX][:, ::-1],
                                                                      data1=ibuf[:, 0:NCTX][:, ::-1], initial=0.0,
                                                                      op0=ALU.mult, op1=ALU.add), [abuf, ibuf], [dst])
                ph.add("dve", lambda e, dst=dst: e.tensor_tensor_scan(out=dst[:, NCTX:T][:, ::-1], data0=abuf[:, NCTX:T][:, ::-1],
                                                                      data1=ibuf[:, NCTX:T][:, ::-1], initial=dst[:, 0:1],
                                                                      op0=ALU.mult, op1=ALU.add), [abuf, ibuf, dst], [dst])
                ph.tt("pool", ysum[:], ysum[:], hbuf[:], ALU.add, [ysum, hbuf], [ysum])
        ph.tt("pool", yo[:], ysum[:], rg[:], ALU.mult, [ysum, rg], [yo])
        ph.dma(dr["yT"][0, cs, :], yo[:], [yo], [])


def phase_attn(ph, dr, l, with_ctx):
    NB = 2
    kT = [ph.sb([96, T], BF16, "kT") for _ in range(NB)]
    qT = [ph.sb([96, T], BF16, "qT") for _ in range(NB)]
    kTg = [ph.sb([128, T], BF16, "kTg") for _ in range(NB)]
    qTg = [ph.sb([128, T], BF16, "qTg") for _ in range(NB)]
    for t_ in kTg + qTg:
        ph.memset("pool", t_[64:128, :], 0.0, [t_])
    va = [ph.sb([128, NT, 128], BF16, "va") for _ in range(NB)]
    for v_ in va:
        ph.memset("pool", v_[:, :, 64:128], 1.0, [v_])
    NPT = 4
    pT = [ph.sb([128, 1024], BF16, "pT") for _ in range(NPT)]
    osb = [ph.sb([64, 512], F32, "osb") for _ in range(2)]
    yo = [ph.sb([64, 512], BF16, "yo") for _ in range(2)]
    sp_ = [Tl(ph.ps[:, 0:1024], "S0"), Tl(ph.ps[:, 1024:2048], "S1"), Tl(ph.ps[:, 2048:3072], "S2")]
    acc = [ph.pb[6], ph.pb[7]]
    heads = [(br, h) for br in (1, 2) for h in range(8)]

    def load_head(hi):
        br, h = heads[hi]
        k_, q_, v_ = (kT if br == 1 else kTg)[hi % NB], (qT if br == 1 else qTg)[hi % NB], va[hi % NB]
        if br == 1:
            ph.dma(k_[0:96, :], dr["kmT"][h], [], [k_])
            ph.dma(q_[0:96, :], dr["qmT"][h], [], [q_])
            vsrc = dr["vm"][:, h * 64:(h + 1) * 64].rearrange("(c p) d -> p c d", p=128)
        else:
            ph.dma(k_[0:64, :], dr["kgT"][h // 4], [], [k_])
            ph.dma(q_[0:64, :], dr["qgT"][h], [], [q_])
            vsrc = dr["vg"][:, (h // 4) * 64:(h // 4 + 1) * 64].rearrange("(c p) d -> p c d", p=128)
        ph.dma(v_[:, 0:17, 0:64], vsrc[:, 0:17, :], [], [v_])
        ph.dma(v_[:, 17:NT, 0:64], vsrc[:, 17:NT, :], [], [v_])

    items = []
    blk = 0
    for hi, (br, h) in enumerate(heads):
        d = 96 if br == 1 else 64
        scale = MLA_SCALE if br == 1 else GQA_SCALE
        qblocks = [(t0, n, NT) for (t0, n) in token_blocks(NCTX, T)]
        if with_ctx:
            qblocks = [(0, NCTX, NCTX // 128)] + qblocks
        first = True
        for (t0, n, nkc) in qblocks:
            for g0 in range(0, nkc, 2):
                items.append(dict(hi=hi, br=br, h=h, d=d, scale=scale, t0=t0, n=n, nkc=nkc, g0=g0, blk=blk,
                                  pre=first, last=(g0 + 2 >= nkc), idx=len(items)))
                first = False
            blk += 1

    def S(it):
        i = it["idx"]
        k_, q_ = (kT if it["br"] == 1 else kTg)[it["hi"] % NB], (qT if it["br"] == 1 else qTg)[it["hi"] % NB]
        s_, p_ = sp_[i % 3], pT[i % NPT]
        n, t0 = it["n"], it["t0"]
        d = 96 if it["br"] == 1 else 128
        for u in range(2):
            kc = it["g0"] + u
            ph.mm(s_[:, u * 512:u * 512 + n], k_[0:d, kc * 128:(kc + 1) * 128], q_[0:d, t0:t0 + n], True, True, [k_, q_], [s_])
        if n == 512:
            ph.act(p_[:, :], s_[:, :], AF.Exp, [s_], [p_], scale=it["scale"])
        else:
            sv = s_[:, :].rearrange("p (u t) -> p u t", u=2)[:, :, 0:n]
            pv = p_[:, :].rearrange("p (u t) -> p u t", u=2)[:, :, 0:n]
            ph.act(pv, sv, AF.Exp, [s_], [p_], scale=it["scale"])

    def PV(it):
        i = it["idx"]
        v_, p_ = va[it["hi"] % NB], pT[i % NPT]
        a_ = acc[it["blk"] % 2]
        n = it["n"]
        for u in range(2):
            kc = it["g0"] + u
            ph.mm(a_[:, 0:n], v_[:, kc, :], p_[:, u * 512:u * 512 + n], kc == 0, kc == it["nkc"] - 1, [v_, p_], [a_])
        if it["last"]:
            o_, y_ = osb[it["blk"] % 2], yo[it["blk"] % 2]
            t0, h, br = it["t0"], it["h"], it["br"]
            ph.recip(o_[0:64, 0:n], a_[64:128, 0:n], [a_], [o_])
            ph.tt("dve", y_[:, 0:n], a_[0:64, 0:n], o_[0:64, 0:n], ALU.mult, [o_, a_], [y_])
            ph.dma(dr["yT"][br, h * 64:(h + 1) * 64, t0:t0 + n], y_[:, 0:n], [y_], [], eng="pool")

    load_head(0)
    N = len(items)
    for i in range(N + 1):
        if i < N:
            S(items[i])
        if 0 <= i - 1 < N:
            PV(items[i - 1])
        if i < N and items[i]["pre"] and items[i]["hi"] + 1 < len(heads):
            load_head(items[i]["hi"] + 1)


def phase_merge(ph, dr, l, xsrc, tstart):
    eps_const(ph)
    wbr = ph.sb([128, 3, 4, D], BF16, "wbr")
    wo = ph.sb([128, 8, D], BF16, "wo")
    stg = [ph.sb([128, 2, D], F32, "stg") for _ in range(2)]
    si = 0
    for k in range(3):
        for hf in range(2):
            s = stg[si % 2]
            ph.dma(s[:], dr["w_branch"][l, k].rearrange("(c p) n -> p c n", p=128)[:, hf * 2:(hf + 1) * 2, :], [], [s])
            ph.cp("dve" if si % 2 else "pool", wbr[:, k, hf * 2:(hf + 1) * 2, :], s[:], [s], [wbr])
            si += 1
    for hf in range(4):
        s = stg[si % 2]
        ph.dma(s[:], dr["w_out"][l].rearrange("(c p) n -> p c n", p=128)[:, hf * 2:(hf + 1) * 2, :], [], [s])
        ph.cp("dve" if si % 2 else "pool", wo[:, hf * 2:(hf + 1) * 2, :], s[:], [s], [wo])
        si += 1
    wr = ph.sb([128, 8, 36], F32, "wr")
    ph.dma(wr[:, :, 0:4], dr["moe_wg"][l].rearrange("(c p) n -> p c n", p=128), [], [wr])
    ph.dma(wr[:, :, 4:36], dr["moe_we"][l].rearrange("(c p) n -> p c n", p=128), [], [wr])
    br_ = ph.sb([128, 36], F32, "br")
    ph.dma(br_[:, 0:4], dr["moe_bg"][l:l + 1, :].partition_broadcast(128), [], [br_])
    ph.dma(br_[:, 4:36], dr["moe_be"][l:l + 1, :].partition_broadcast(128), [], [br_])
    wrb = ph.sb([128, 8, 36], BF16, "wrb")
    ph.cp("pool", wrb[:], wr[:], [wr], [wrb])
    identb = make_identity(ph, BF16, "identb")
    hb16 = [ph.sb([128, D], BF16, "hb16") for _ in range(2)]
    G = {}
    for row in ((0, 1) if tstart == 0 else (0,)):
        G[row] = (load_bc(ph, dr, row, 2, "Ga"), load_bc(ph, dr, row, 4, "Af"), load_bc(ph, dr, row, 3, "Bf"))
    yb = [ph.sb([128, 3, 4, 512], BF16, "yb") for _ in range(2)]
    gb = [ph.sb([128, 24, 512], BF16, "gb") for _ in range(1)]
    mg = [ph.sb([128, 8, 512], BF16, "mgd") for _ in range(2)]
    tmpf = [ph.sb([128, 512], F32, "tmpf") for _ in range(3)]
    accf = ph.sb([128, 512], F32, "accf")
    xt = [ph.sb([128, D], F32, "xt") for _ in range(2)]
    x2 = [ph.sb([128, D], F32, "x2") for _ in range(2)]
    junk = ph.sb([128, D], F32, "junk")
    hTb = [ph.sb([128, 8, 128], BF16, "hTb") for _ in range(2)]
    ss = ph.sb([128, 1], F32, "ss")
    rstd = ph.sb([128, 1], F32, "rstd")
    sm = ph.sb([128, 64], F32, "sm")
    lg = ph.sb([128, 36], F32, "lg")
    mk = ph.sb([128, 4], F32, "mk")
    es = ph.sb([128, 4, 8], F32, "es")
    e8 = ph.sb([128, 8], F32, "e8")
    m8 = ph.sb([128, 8], F32, "m8")
    cb_ = [ph.sb([128, 32], F32, "comb") for _ in range(2)]
    c1 = ph.sb([128, 32], F32, "c1")
    c2 = ph.sb([128, 32], F32, "c2")
    ctr = {"pb": 0, "t": 0}

    def npb():
        ctr["pb"] += 1
        return ph.pb[ctr["pb"] % 8]

    bi = 0
    for (t0, n) in token_blocks(tstart, T):
        if DBG_STOP < 1:
            break
        sl = slice(t0, t0 + n)
        y_, g_, m_ = yb[bi % 2], gb[0], mg[bi % 2]
        bi += 1
        for k in range(3):
            ph.dma(y_[:, k, :, 0:n], dr["yT"][k, :, sl].rearrange("(c p) t -> p c t", p=128), [], [y_])
        for k in range(3):
            ph.dma(g_[:, k * 8:(k + 1) * 8, 0:n], dr["gatesT"][k * D:(k + 1) * D, sl].rearrange("(c p) t -> p c t", p=128), [], [g_])
        for oc in range(8):
            for k in range(3):
                pb = npb()
                for c in range(4):
                    ph.mm(pb[:, 0:n], wbr[:, k, c, oc * 128:(oc + 1) * 128], y_[:, k, c, 0:n], c == 0, c == 3, [wbr, y_], [pb])
                if k == 0:
                    ph.tt("dve", accf[:, 0:n], pb[:, 0:n], g_[:, oc, 0:n], ALU.mult, [pb, g_], [accf])
                else:
                    tf = tmpf[ctr["t"] % 3]
                    ctr["t"] += 1
                    ph.tt("dve", tf[:, 0:n], pb[:, 0:n], g_[:, k * 8 + oc, 0:n], ALU.mult, [pb, g_], [tf])
                    if k == 1:
                        ph.tt("pool", accf[:, 0:n], accf[:, 0:n], tf[:, 0:n], ALU.add, [accf, tf], [accf])
                    else:
                        ph.tt("pool", m_[:, oc, 0:n], accf[:, 0:n], tf[:, 0:n], ALU.add, [accf, tf], [m_])
        for j in range(n // 128):
            if DBG_STOP < 2:
                break
            tok = t0 + j * 128
            row = 1 if tok < NCTX else 0
            Ga, Af, Bf = G[row]
            x_ = xt[j % 2]
            xo = x2[j % 2]
            ph.dma(x_[:], xsrc[tok:tok + 128, :], [], [x_])
            for hf in range(2):
                pb = npb()
                for c in range(8):
                    ph.mm(pb[:, :], m_[:, c, j * 128:(j + 1) * 128], wo[:, c, hf * 512:(hf + 1) * 512], c == 0, c == 7, [m_, wo], [pb])
                ph.tt("dve", junk[:, hf * 512:(hf + 1) * 512], pb[:, :], Ga[:, hf * 512:(hf + 1) * 512], ALU.mult, [pb, Ga], [junk])
            ph.tt("pool", xo[:], junk[:], x_[:], ALU.add, [junk, x_], [xo])
            ph.dma(dr["x2"][tok:tok + 128, :], xo[:], [xo], [], eng="pool")
            if DBG_STOP < 3:
                continue
            hbt = hb16[j % 2]
            rms_modulate(ph, xo, Af, Bf, hbt, junk, ss, rstd)
            hb_ = hTb[j % 2]
            pb = npb()
            pbv = pb.t.bitcast(BF16)
            for c in range(8):
                ph.tr(pbv[:, c * 128:(c + 1) * 128], hbt[:, c * 128:(c + 1) * 128], identb[:], [hbt, identb], [pb])
            ph.cp("act", hb_[:, :, :], pbv[:, :].rearrange("p (c t) -> p c t", t=128), [pb], [hb_])
            ph.dma(dr["h2tok"][tok:tok + 128, :], hbt[:], [hbt], [], eng="pool")
            if DBG_SKIP_ROUTER:
                continue
            pb = npb()
            for c in range(8):
                ph.mm(pb[:, 0:36], hb_[:, c, :], wrb[:, c, :], c == 0, c == 7, [hb_, wrb], [pb])
            ph.tt("dve", lg[:], pb[:, 0:36], br_[:], ALU.add, [pb, br_], [lg])
            ph.add("dve", lambda e: e.tensor_reduce(out=sm[:, 0:1], in_=lg[:, 0:4], axis=AX.X, op=ALU.max), _bufs([lg]), _bufs([sm]))
            ph.ts("dve", mk[:], lg[:, 0:4], sm[:, 0:1], None, ALU.is_equal, None, [lg, sm], [mk])
            ph.ts("dve", sm[:, 1:2], sm[:, 0:1], -1.0, None, ALU.mult, None, [sm], [sm])
            ph.act(sm[:, 8:12], lg[:, 0:4], AF.Exp, [lg, sm], [sm], bias=sm[:, 1:2], accum=sm[:, 2:3])
            ph.recip(sm[:, 3:4], sm[:, 2:3], [sm], [sm])
            ev = lg[:, 4:36].rearrange("p (g e) -> p g e", e=8)
            ph.tt("dve", es[:], ev, mk[:, :].unsqueeze(2).to_broadcast([128, 4, 8]), ALU.mult, [lg, mk], [es])
            ph.add("dve", lambda e: e.tensor_reduce(out=e8[:], in_=es[:].rearrange("p g e -> p e g"), axis=AX.X, op=ALU.add),
                   _bufs([es]), _bufs([e8]))
            ph.add("dve", lambda e: e.max(out=m8[:], in_=e8[:]), _bufs([e8]), _bufs([m8]))
            ph.tt("dve", sm[:, 4:5], m8[:, 0:1], m8[:, 1:2], ALU.subtract, [m8], [sm])
            ph.act(sm[:, 5:6], sm[:, 4:5], AF.Sigmoid, [sm], [sm])
            ph.tt("dve", sm[:, 5:6], sm[:, 5:6], sm[:, 3:4], ALU.mult, [sm], [sm])
            ph.tt("dve", sm[:, 6:7], sm[:, 3:4], sm[:, 5:6], ALU.subtract, [sm], [sm])
            cbt = cb_[j % 2]
            ph.ts("dve", c1[:], lg[:, 4:36], m8[:, 0:1], sm[:, 5:6], ALU.is_equal, ALU.mult, [lg, m8, sm], [c1])
            ph.ts("dve", c2[:], lg[:, 4:36], m8[:, 1:2], sm[:, 6:7], ALU.is_equal, ALU.mult, [lg, m8, sm], [c2])
            ph.tt("dve", c1[:], c1[:], c2[:], ALU.add, [c1, c2], [c1])
            ph.tt("dve", cbt[:].rearrange("p (g e) -> p g e", e=8), c1[:].rearrange("p (g e) -> p g e", e=8),
                  mk[:, :].unsqueeze(2).to_broadcast([128, 4, 8]), ALU.mult, [c1, mk], [cbt])
            ph.dma(dr["comb"][tok:tok + 128, :], cbt[:], [cbt], [], eng="pool")


def phase_moe(ph, dr, l, tstart, final):
    eps_const(ph)
    ntok = T - tstart
    half = ntok // 2
    assert half % 128 == 0
    nth = half // 128
    Gf = {}
    for row in ((0, 1) if tstart == 0 else (0,)):
        Gf[row] = load_bc(ph, dr, row, 5, "Gf")
    if final:
        gfin = ph.sb([128, D], F32, "gfin")
        ph.dma(gfin[:], dr["g_final"].rearrange("(o n) -> o n", o=1).partition_broadcast(128), [], [gfin])
    hT = ph.sb([128, 8, half], BF16, "hT")
    acc = ph.sb([128, nth, D], F32, "acc")
    comb = ph.sb([128, nth, NE], F32, "comb")
    s13 = [ph.sb([128, 8, 256], F32, "s13") for _ in range(2)]
    s2 = [ph.sb([128, 2, D], F32, "s2") for _ in range(1)]
    w13 = [ph.sb([128, 8, 512], BF16, "w13") for _ in range(2)]
    w2 = [ph.sb([128, 2, D], BF16, "w2") for _ in range(2)]
    sg = [ph.sb([128, 2, 512], F32, "sg") for _ in range(2)]
    he = [ph.sb([128, 2, 512], BF16, "he") for _ in range(2)]
    xt = [ph.sb([128, D], F32, "xt") for _ in range(2)]
    junk = ph.sb([128, D], F32, "junk")
    ss = ph.sb([128, 1], F32, "ss")
    rstd = ph.sb([128, 1], F32, "rstd")
    ctr = {"pd": 0}
    for hf in range(2):
        h0 = tstart + hf * half
        ph.dma(hT[:, :, :], dr["h2T"][:, h0:h0 + half].rearrange("(c p) t -> p c t", p=128), [], [hT])
        ph.dma(comb[:, :, :], dr["comb"][h0:h0 + half, :].rearrange("(j p) e -> p j e", p=128), [], [comb])

        def load_w(e_):
            a2, b13, b2 = s2[0], w13[e_ % 2], w2[e_ % 2]
            ph.dma(s13[0][:], dr["moe_w1"][l, e_].rearrange("(c p) n -> p c n", p=128), [], [s13[0]])
            ph.dma(s13[1][:], dr["moe_w3"][l, e_].rearrange("(c p) n -> p c n", p=128), [], [s13[1]])
            ph.dma(a2[:], dr["moe_w2"][l, e_].rearrange("(c p) n -> p c n", p=128), [], [a2])
            ph.cp("pool", b13[:, :, 0:256], s13[0][:], [s13[0]], [b13])
            ph.cp("pool", b13[:, :, 256:512], s13[1][:], [s13[1]], [b13])
            ph.cp("pool", b2[:], a2[:], [a2], [b2])

        items = []
        for e_ in range(NE):
            for bi_, (b0, n) in enumerate(token_blocks(0, half)):
                items.append(dict(e=e_, b0=b0, n=n, first=(bi_ == 0), idx=len(items)))

        def UP(it):
            i, e_, b0, n = it["idx"], it["e"], it["b0"], it["n"]
            b13 = w13[e_ % 2]
            s_, h_ = sg[i % 2], he[i % 2]
            for m in range(2):
                pg, pu = ph.pb[2 * m], ph.pb[2 * m + 1]
                for c in range(8):
                    ph.mm(pg[:, 0:n], b13[:, c, m * 128:(m + 1) * 128], hT[:, c, b0:b0 + n], c == 0, c == 7, [b13, hT], [pg])
                for c in range(8):
                    ph.mm(pu[:, 0:n], b13[:, c, 256 + m * 128:256 + (m + 1) * 128], hT[:, c, b0:b0 + n], c == 0, c == 7, [b13, hT], [pu])
                ph.act(s_[:, m, 0:n], pg[:, 0:n], AF.Silu, [pg], [s_])
                ph.tt("dve", h_[:, m, 0:n], pu[:, 0:n], s_[:, m, 0:n], ALU.mult, [pu, s_], [h_])

        def DN(it):
            i, e_, b0, n = it["idx"], it["e"], it["b0"], it["n"]
            b2 = w2[e_ % 2]
            h_ = he[i % 2]
            for j in range(n // 128):
                tj = (b0 // 128) + j
                for nh in range(2):
                    pd = ph.pb[4 + ctr["pd"] % 4]
                    ctr["pd"] += 1
                    for c in range(2):
                        ph.mm(pd[:, :], h_[:, c, j * 128:(j + 1) * 128], b2[:, c, nh * 512:(nh + 1) * 512], c == 0, c == 1, [h_, b2], [pd])
                    asl = acc[:, tj, nh * 512:(nh + 1) * 512]
                    if e_ == 0:
                        ph.ts("dve", asl, pd[:, :], comb[:, tj, e_:e_ + 1], None, ALU.mult, None, [pd, comb], [acc])
                    else:
                        ph.stt("dve", asl, pd[:, :], comb[:, tj, e_:e_ + 1], asl, ALU.mult, ALU.add, [pd, comb, acc], [acc])

        load_w(0)
        N = len(items)
        for i in range(N + 1):
            if i < N:
                UP(items[i])
            if i >= 1:
                DN(items[i - 1])
            if i < N and items[i]["first"] and items[i]["e"] + 1 < NE:
                load_w(items[i]["e"] + 1)
        for tj in range(nth):
            tok = h0 + tj * 128
            row = 1 if tok < NCTX else 0
            x_ = xt[tj % 2]
            ph.dma(x_[:], dr["x2"][tok:tok + 128, :], [], [x_])
            ph.tt("pool", acc[:, tj, :], acc[:, tj, :], Gf[row][:], ALU.mult, [acc, Gf[row]], [acc])
            ph.tt("pool", x_[:], x_[:], acc[:, tj, :], ALU.add, [x_, acc], [x_])
            if not final:
                ph.dma(dr["xres"][tok:tok + 128, :], x_[:], [x_], [], eng="pool")
            else:
                ph.act(junk[:], x_[:], AF.Square, [x_], [junk, ss], accum=ss[:, 0:1])
                ph.act(rstd[:, 0:1], ss[:, 0:1], AF.Sqrt, [ss], [rstd], bias=ph.epsc[:, 0:1], scale=1.0 / D)
                ph.recip(rstd[:, 0:1], rstd[:, 0:1], [rstd], [rstd])
                ph.stt("dve", x_[:], x_[:], rstd[:, 0:1], gfin[:], ALU.mult, ALU.mult, [x_, rstd, gfin], [x_])
                ph.dma(dr["out"][tok - NCTX:tok - NCTX + 128, :], x_[:], [x_], [], eng="pool")


SL = 512
I32 = mybir.dt.int32
BIG = 1.0e9


def n_chunks(tstart):
    return (2 * (T - tstart)) // SL + NE


def phase_route(ph, dr, l, tstart):
    ntt = (T - tstart) // 128
    nch_tot = n_chunks(tstart)
    W = ntt * NE
    comb = ph.sb([128, ntt, NE], F32, "comb")
    ph.dma(comb[:], dr["comb"][tstart:T, :].rearrange("(j p) e -> p j e", p=128), [], [comb])
    ltri = ph.sb([128, 128], BF16, "ltri")
    ph.dma(ltri[:], dr["cst_ltri"][:, :], [], [ltri])
    iota = ph.sb([128, 128], F32, "iota")
    ph.dma(iota[:], dr["cst_iota"][:, :], [], [iota])
    pidx = ph.sb([128, 1], F32, "pidx")
    ph.dma(pidx[:], dr["cst_pidx"][:, :], [], [pidx])
    ones = ph.sb([128, 128], BF16, "ones")
    ph.memset("pool", ones[:], 1.0, [ones])
    M = ph.sb([128, ntt, NE], BF16, "M")
    Mf = ph.sb([128, ntt, NE], F32, "Mf")
    ph.ts("dve", Mf[:], comb[:], 0.0, None, ALU.is_gt, None, [comb], [Mf])
    ph.cp("dve", M[:], Mf[:], [Mf], [M])
    rank = ph.sb([128, ntt, NE], F32, "rank")
    tot = ph.sb([128, ntt, NE], F32, "tot")
    Mfl = M[:, :, :].rearrange("p j e -> p (j e)")
    for (dst, lhs, pbi) in ((rank, ltri, 0), (tot, ones, 4)):
        dfl = dst[:, :, :].rearrange("p j e -> p (j e)")
        for i, c0 in enumerate(range(0, W, 512)):
            n = min(512, W - c0)
            pb = ph.pb[pbi + i]
            ph.mm(pb[:, 0:n], lhs[:], Mfl[:, c0:c0 + n], True, True, [lhs, M], [pb])
            ph.cp("act", dfl[:, c0:c0 + n], pb[:, 0:n], [pb], [dst])
    carry = ph.sb([128, ntt + 1, NE], F32, "carry")
    ph.memset("dve", carry[:, 0, :], 0.0, [carry])
    for j in range(ntt):
        ph.tt("dve", carry[:, j + 1, :], carry[:, j, :], tot[:, j, :], ALU.add, [carry, tot], [carry])
    cnt = carry[:, ntt, :]
    NK = (T - tstart) // SL + 1
    thr = ph.sb([128, NK], F32, "thr")
    ph.ts("dve", thr[:], iota[:, 0:NK], float(SL), None, ALU.mult, None, [iota], [thr])
    cmpk = ph.sb([128, NE, NK], F32, "cmpk")
    ph.tt("dve", cmpk[:], cnt.unsqueeze(2).to_broadcast([128, NE, NK]), thr[:, :].unsqueeze(1).to_broadcast([128, NE, NK]), ALU.is_gt,
          [carry, thr], [cmpk])
    sc = ph.sb([128, 8, NE], F32, "sc")
    ph.add("dve", lambda e: e.tensor_reduce(out=sc[:, 0, :], in_=cmpk[:], axis=AX.X, op=ALU.add), _bufs([cmpk]), _bufs([sc]))
    ph.memset("dve", sc[:, 4, :], 1.0, [sc])
    ph.add("dve", lambda e: e.tensor_tensor_scan(out=sc[:, 1, :], data0=sc[:, 4, :], data1=sc[:, 0, :], initial=0.0,
                                                 op0=ALU.mult, op1=ALU.add), _bufs([sc]), _bufs([sc]))
    ph.tt("dve", sc[:, 2, :], sc[:, 1, :], sc[:, 0, :], ALU.subtract, [sc], [sc])
    ph.ts("dve", sc[:, 3, :], sc[:, 2, :], float(SL), None, ALU.mult, None, [sc], [sc])
    posv = ph.sb([128, ntt, NE], F32, "posv")
    ph.tt("dve", posv[:], rank[:], carry[:, 0:ntt, :], ALU.add, [rank, carry], [posv])
    ph.stt("dve", posv[:], posv[:], 1.0, sc[:, 3, :].unsqueeze(1).to_broadcast([128, ntt, NE]), ALU.add, ALU.add, [posv, sc], [posv])
    ph.tt("dve", posv[:], posv[:], Mf[:], ALU.mult, [posv, Mf], [posv])
    pw = ph.sb([128, ntt, 4], F32, "pw")
    eq1 = ph.sb([128, ntt, NE], F32, "eq1")
    tmp = ph.sb([128, ntt, NE], F32, "tmp")
    ph.add("dve", lambda e: e.tensor_reduce(out=pw[:, :, 0], in_=posv[:], axis=AX.X, op=ALU.max), _bufs([posv]), _bufs([pw]))
    ph.tt("dve", eq1[:], posv[:], pw[:, :, 0:1].to_broadcast([128, ntt, NE]), ALU.is_equal, [posv, pw], [eq1])
    ph.tt("dve", tmp[:], eq1[:], posv[:], ALU.mult, [eq1, posv], [tmp])
    ph.tt("dve", tmp[:], posv[:], tmp[:], ALU.subtract, [posv, tmp], [tmp])
    ph.add("dve", lambda e: e.tensor_reduce(out=pw[:, :, 1], in_=tmp[:], axis=AX.X, op=ALU.max), _bufs([tmp]), _bufs([pw]))
    ph.tt("dve", tmp[:], eq1[:], comb[:], ALU.mult, [eq1, comb], [tmp])
    ph.add("dve", lambda e: e.tensor_reduce(out=pw[:, :, 2], in_=tmp[:], axis=AX.X, op=ALU.add), _bufs([tmp]), _bufs([pw]))
    ph.add("dve", lambda e: e.tensor_reduce(out=pw[:, :, 3], in_=comb[:], axis=AX.X, op=ALU.add), _bufs([comb]), _bufs([pw]))
    ph.tt("dve", pw[:, :, 3], pw[:, :, 3], pw[:, :, 2], ALU.subtract, [pw], [pw])
    posf = ph.sb([128, ntt, 2], F32, "posf")
    ph.ts("dve", posf[:], pw[:, :, 0:2], -1.0, None, ALU.add, None, [pw], [posf])
    posi = ph.sb([128, ntt, 2], I32, "posi")
    ph.cp("dve", posi[:], posf[:], [posf], [posi])
    ph.dma(dr["posi"][tstart:T, :].rearrange("(j p) k -> p j k", p=128), posi[:], [posi], [])
    ph.dma(dr["posw"][tstart:T, :].rearrange("(j p) k -> p j k", p=128), pw[:, :, 2:4], [pw], [])
    cmpc = ph.sb([128, nch_tot, NE], F32, "cmpc")
    ph.tt("dve", cmpc[:], sc[:, 2, :].unsqueeze(1).to_broadcast([128, nch_tot, NE]),
          iota[:, 0:nch_tot].unsqueeze(2).to_broadcast([128, nch_tot, NE]), ALU.is_le, [sc, iota], [cmpc])
    wf = ph.sb([128, 4, nch_tot], F32, "wf")
    ph.add("dve", lambda e: e.tensor_reduce(out=wf[:, 0, :], in_=cmpc[:], axis=AX.X, op=ALU.add), _bufs([cmpc]), _bufs([wf]))
    ph.ts("dve", wf[:, 1, :], wf[:, 0, :], -1.0, 128.0, ALU.add, ALU.mult, [wf], [wf])
    ph.ts("dve", wf[:, 1, :], wf[:, 1, :], pidx[:, 0:1], None, ALU.add, None, [wf, pidx], [wf])
    ph.ts("dve", wf[:, 2, :], iota[:, 0:nch_tot], sc[:, 1, NE - 1:NE], BIG, ALU.is_ge, ALU.mult, [iota, sc], [wf])
    ph.tt("dve", wf[:, 1, :], wf[:, 1, :], wf[:, 2, :], ALU.add, [wf], [wf])
    widx = ph.sb([128, nch_tot], I32, "widx")
    ph.cp("dve", widx[:], wf[:, 1, :], [wf], [widx])
    ph.dma(dr["widx"][:, 0:nch_tot], widx[:], [widx], [])
    xs_t = Tl(dr["xsort"], "xsort")
    xt = [ph.sb([128, D], BF16, "xt") for _ in range(3)]
    bound = nch_tot * SL - 1
    for j in range(ntt):
        x_ = xt[j % 3]
        tok = tstart + j * 128
        ph.dma(x_[:], dr["h2tok"][tok:tok + 128, :], [], [x_])
        for k in range(2):
            ph.add("pool", lambda e, x_=x_, j=j, k=k: e.indirect_dma_start(
                out=dr["xsort"][:, :], out_offset=bass.IndirectOffsetOnAxis(ap=posi[:, j, k:k + 1], axis=0),
                in_=x_[:, :], in_offset=None, bounds_check=ph.breg(e, bound), oob_is_err=False), _bufs([x_, posi]), _bufs([]), dma=True)


def phase_moe_sparse(ph, dr, l, tstart, final):
    eps_const(ph)
    nch_tot = n_chunks(tstart)
    ntt = (T - tstart) // 128
    widx = ph.sb([128, nch_tot], I32, "widx")
    ph.dma(widx[:], dr["widx"][:, 0:nch_tot], [], [widx])
    identb = make_identity(ph, BF16, "identb")
    st = [[ph.sb([128, 2048], F32, "st") for _ in range(3)] for _ in range(2)]
    w13 = [ph.sb([128, 8, 512], BF16, "w13") for _ in range(2)]
    w2 = [ph.sb([128, 2, D], BF16, "w2") for _ in range(2)]
    for t_ in st[0] + st[1]:
        ph.memset("pool", t_[:], 0.0, [t_])
    xs = [ph.sb([128, D], BF16, "xs") for _ in range(3)]
    xT = [ph.sb([128, 8, SL], BF16, "xT") for _ in range(2)]
    sg = [ph.sb([128, 2, SL], F32, "sg") for _ in range(2)]
    he = [ph.sb([128, 2, SL], BF16, "he") for _ in range(2)]
    yb = [ph.sb([128, D], BF16, "yb") for _ in range(3)]
    srcs = (dr["w1r%d" % l], dr["w3r%d" % l], dr["w2r%d" % l])
    ctr = {"x": 0, "y": 0, "pd": 0, "pt": 0}

    def LOADW(c):
        for i in range(3):
            s_ = st[c % 2][i]
            ph.add("pool", lambda e, s_=s_, i=i: e.indirect_dma_start(
                out=s_[:, :], out_offset=None, in_=srcs[i][:, :],
                in_offset=bass.IndirectOffsetOnAxis(ap=widx[:, c:c + 1], axis=0), bounds_check=ph.breg(e, NE * 128 - 1), oob_is_err=False),
                _bufs([widx]), _bufs([s_]), dma=True)
        b13, b2 = w13[c % 2], w2[c % 2]
        ph.cp("dve", b13[:, :, 0:256], st[c % 2][0][:, :].rearrange("p (k n) -> p k n", n=256), [st[c % 2][0]], [b13])
        ph.cp("act", b13[:, :, 256:512], st[c % 2][1][:, :].rearrange("p (k n) -> p k n", n=256), [st[c % 2][1]], [b13])
        ph.cp("dve", b2[:, :, :], st[c % 2][2][:, :].rearrange("p (k n) -> p k n", n=D), [st[c % 2][2]], [b2])

    def LOADX(c):
        xT_ = xT[c % 2]
        for s4 in range(SL // 128):
            x_ = xs[ctr["x"] % 3]
            ctr["x"] += 1
            r0 = c * SL + s4 * 128
            ph.dma(x_[:], dr["xsort"][r0:r0 + 128, :], [], [x_])
            pb = ph.pb[6 + ctr["pt"] % 2]
            ctr["pt"] += 1
            pbv = pb.t.bitcast(BF16)
            for k in range(8):
                ph.tr(pbv[:, k * 128:(k + 1) * 128], x_[:, k * 128:(k + 1) * 128], identb[:], [x_, identb], [pb])
            ph.cp("act" if s4 % 2 else "dve", xT_[:, :, s4 * 128:(s4 + 1) * 128], pbv[:, :].rearrange("p (k t) -> p k t", t=128), [pb], [xT_])

    def UP(c):
        b13, xT_ = w13[c % 2], xT[c % 2]
        s_, h_ = sg[c % 2], he[c % 2]
        for m in range(2):
            pg, pu = ph.pb[2 * m], ph.pb[2 * m + 1]
            for k in range(8):
                ph.mm(pg[:, :], b13[:, k, m * 128:(m + 1) * 128], xT_[:, k, :], k == 0, k == 7, [b13, xT_], [pg])
            for k in range(8):
                ph.mm(pu[:, :], b13[:, k, 256 + m * 128:256 + (m + 1) * 128], xT_[:, k, :], k == 0, k == 7, [b13, xT_], [pu])
            ph.act(s_[:, m, :], pg[:, :], AF.Silu, [pg], [s_])
            ph.tt("dve", h_[:, m, :], pu[:, :], s_[:, m, :], ALU.mult, [pu, s_], [h_])

    def DN(c):
        b2, h_ = w2[c % 2], he[c % 2]
        for s4 in range(SL // 128):
            y_ = yb[ctr["y"] % 3]
            ctr["y"] += 1
            for nh in range(2):
                pd = ph.pb[4 + ctr["pd"] % 2]
                ctr["pd"] += 1
                for k in range(2):
                    ph.mm(pd[:, :], h_[:, k, s4 * 128:(s4 + 1) * 128], b2[:, k, nh * 512:(nh + 1) * 512], k == 0, k == 1, [h_, b2], [pd])
                ph.cp("act" if nh else "dve", y_[:, nh * 512:(nh + 1) * 512], pd[:, :], [pd], [y_])
            r0 = c * SL + s4 * 128
            ph.dma(dr["ysort"][r0:r0 + 128, :], y_[:], [y_], [])

    LOADW(0)
    LOADX(0)
    for c in range(nch_tot + 1):
        if c < nch_tot:
            UP(c)
        if c >= 1:
            DN(c - 1)
        if c + 1 < nch_tot:
            LOADW(c + 1)
            LOADX(c + 1)

    Gf = {}
    for row in ((0, 1) if tstart == 0 else (0,)):
        Gf[row] = load_bc(ph, dr, row, 5, "Gf")
    if final:
        gfin = ph.sb([128, D], F32, "gfin")
        ph.dma(gfin[:], dr["g_final"].rearrange("(o n) -> o n", o=1).partition_broadcast(128), [], [gfin])
    posi = ph.sb([128, ntt, 2], I32, "posi")
    posw = ph.sb([128, ntt, 2], F32, "posw")
    ph.dma(posi[:], dr["posi"][tstart:T, :].rearrange("(j p) k -> p j k", p=128), [], [posi])
    ph.dma(posw[:], dr["posw"][tstart:T, :].rearrange("(j p) k -> p j k", p=128), [], [posw])
    ya = [[ph.sb([128, D], BF16, "ya") for _ in range(2)] for _ in range(2)]
    for a_ in ya[0] + ya[1]:
        ph.memset("pool", a_[:], 0.0, [a_])
    acc = [ph.sb([128, D], F32, "acc") for _ in range(2)]
    xt = [ph.sb([128, D], F32, "xt") for _ in range(2)]
    junk = ph.sb([128, D], F32, "junk")
    ss = ph.sb([128, 1], F32, "ss")
    rstd = ph.sb([128, 1], F32, "rstd")
    ys_t = Tl(dr["ysort"], "ysort")
    bound = nch_tot * SL - 1
    for j in range(ntt):
        tok = tstart + j * 128
        row = 1 if tok < NCTX else 0
        for k in range(2):
            a_ = ya[j % 2][k]
            ph.add("pool", lambda e, a_=a_, j=j, k=k: e.indirect_dma_start(
                out=a_[:, :], out_offset=None, in_=dr["ysort"][:, :],
                in_offset=bass.IndirectOffsetOnAxis(ap=posi[:, j, k:k + 1], axis=0), bounds_check=ph.breg(e, bound), oob_is_err=False),
                _bufs([posi]), _bufs([a_]), dma=True)
        x_, ac = xt[j % 2], acc[j % 2]
        ph.dma(x_[:], dr["x2"][tok:tok + 128, :], [], [x_])
        ph.ts("dve", ac[:], ya[j % 2][0][:], posw[:, j, 0:1], None, ALU.mult, None, [ya[j % 2][0], posw], [ac])
        ph.stt("dve", ac[:], ya[j % 2][1][:], posw[:, j, 1:2], ac[:], ALU.mult, ALU.add, [ya[j % 2][1], posw, ac], [ac])
        ph.tt("pool", ac[:], ac[:], Gf[row][:], ALU.mult, [ac, Gf[row]], [ac])
        ph.tt("pool", x_[:], x_[:], ac[:], ALU.add, [x_, ac], [x_])
        if not final:
            ph.dma(dr["xres"][tok:tok + 128, :], x_[:], [x_], [])
        else:
            ph.act(junk[:], x_[:], AF.Square, [x_], [junk, ss], accum=ss[:, 0:1])
            ph.act(rstd[:, 0:1], ss[:, 0:1], AF.Sqrt, [ss], [rstd], bias=ph.epsc[:, 0:1], scale=1.0 / D)
            ph.recip(rstd[:, 0:1], rstd[:, 0:1], [rstd], [rstd])
            ph.stt("dve", x_[:], x_[:], rstd[:, 0:1], gfin[:], ALU.mult, ALU.mult, [x_, rstd, gfin], [x_])
            ph.dma(dr["out"][tok - NCTX:tok - NCTX + 128, :], x_[:], [x_], [])


WEIGHTS = [("w_mod", [2, D, 6 * D]), ("b_mod", [2, 6 * D]), ("g_mix", [2, D]), ("g_ffn", [2, D]), ("w_in", [2, D, DIN]),
           ("conv_w", [2, 4, 512]), ("conv_b", [2, 512]), ("lru_wa", [2, 2, 8, 64, 64]), ("lru_ba", [2, 2, 512]),
           ("lru_wi", [2, 2, 8, 64, 64]), ("lru_bi", [2, 2, 512]), ("lru_lambda", [2, 2, 512]), ("mla_gq", [2, 256]),
           ("mla_wuq", [2, 256, 768]), ("mla_gkv", [2, 128]), ("mla_wukv", [2, 128, 1024]), ("gqa_gq", [2, 64]),
           ("gqa_gk", [2, 64]), ("w_branch", [2, 3, 512, D]), ("w_out", [2, D, D]), ("moe_wg", [2, D, 4]), ("moe_bg", [2, 4]),
           ("moe_we", [2, D, 32]), ("moe_be", [2, 32]), ("g_final", [D])]
RELAID = [("w1r0", [NE * 128, 2048]), ("w3r0", [NE * 128, 2048]), ("w2r0", [NE * 128, 2048]),
          ("w1r1", [NE * 128, 2048]), ("w3r1", [NE * 128, 2048]), ("w2r1", [NE * 128, 2048])]

SCRATCH = [("modv", [2, 6 * D], F32), ("xres", [T, D], F32), ("x2", [T, D], F32), ("xrT", [512, T], BF16),
           ("rgT", [512, T], BF16), ("gatesT", [3 * D, T], BF16), ("kmT", [8, 96, T], BF16), ("qmT", [8, 96, T], BF16),
           ("vm", [T, 512], BF16), ("kgT", [2, 64, T], BF16), ("qgT", [8, 64, T], BF16), ("vg", [T, 128], BF16),
           ("yT", [3, 512, T], BF16), ("h2tok", [T, D], BF16), ("comb", [T, NE], F32),
           ("xsort", [(2 * T // SL + NE) * SL, D], BF16), ("ysort", [(2 * T // SL + NE) * SL, D], BF16),
           ("posi", [T, 2], I32), ("posw", [T, 2], F32), ("widx", [128, 2 * T // SL + NE], I32)]


def build_nc(phases=None, debug=()):
    nc = bass.Bass("TRN2", target_bir_lowering=False)
    dr = {}
    dr["xin"] = nc.dram_tensor("xin", [T, D], F32, kind="ExternalInput").ap()
    dr["cc"] = nc.dram_tensor("cc", [2, D], F32, kind="ExternalInput").ap()
    dr["ropem"] = nc.dram_tensor("ropem", [2, 32, T], BF16, kind="ExternalInput").ap()
    dr["ropeg"] = nc.dram_tensor("ropeg", [2, 64, T], BF16, kind="ExternalInput").ap()
    for nm, shp in WEIGHTS + RELAID:
        dr[nm] = nc.dram_tensor(nm, shp, F32, kind="ExternalInput").ap()
    dr["cst_ltri"] = nc.dram_tensor("cst_ltri", [128, 128], BF16, kind="ExternalInput").ap()
    dr["cst_iota"] = nc.dram_tensor("cst_iota", [128, 128], F32, kind="ExternalInput").ap()
    dr["cst_pidx"] = nc.dram_tensor("cst_pidx", [128, 1], F32, kind="ExternalInput").ap()
    dr["out"] = nc.dram_tensor("out", [SEQ, D], F32, kind="ExternalOutput").ap()
    for nm, shp, dt in SCRATCH:
        if nm in debug:
            dr[nm] = nc.dram_tensor(nm, shp, dt, kind="ExternalOutput").ap()
        else:
            dr[nm] = nc.dram_tensor(nm, shp, dt).ap()
    ps = nc.alloc_psum_tensor("ps", [128, 4096], F32)
    for l in range(2):
        xsrc = dr["xin"] if l == 0 else dr["xres"]
        last = l == 1
        tstart = NCTX if last else 0
        plan = [("mod", phase_mod, (dr, l)), ("inproj", phase_inproj, (dr, l, xsrc)), ("lru", phase_lru, (dr, l)),
                ("attn", phase_attn, (dr, l, not last)), ("merge", phase_merge, (dr, l, xsrc, tstart)),
                ("route", phase_route, (dr, l, tstart)), ("moe", phase_moe_sparse, (dr, l, tstart, last))]
        for nm, fn, args in plan:
            if phases is not None and (l, nm) not in phases:
                continue
            run_phase(nc, ps, fn, *args)
    return nc


def rope_consts():
    def tab(rot):
        q = rot // 4
        pos = np.arange(SEQ)
        row = (pos // 64).astype(np.float32)
        col = (pos % 64).astype(np.float32)
        freqs = (np.float32(10000.0) ** (-np.arange(q, dtype=np.float32) / np.float32(q))).astype(np.float32)
        ang = np.concatenate([row[:, None] * freqs, col[:, None] * freqs], axis=-1).astype(np.float32)
        cos, sin = np.cos(ang).T, np.sin(ang).T
        C = np.ones((rot, T), np.float32)
        S = np.zeros((rot, T), np.float32)
        C[:, NCTX:] = np.concatenate([cos, cos], axis=0)
        S[:, NCTX:] = np.concatenate([-sin, sin], axis=0)
        return np.stack([C, S]).astype(ml_dtypes.bfloat16)
    return tab(32), tab(64)


def host_shared(inputs):
    shared = {nm: np.ascontiguousarray(np.asarray(inputs[nm], np.float32)) for nm, _ in WEIGHTS}
    ropem, ropeg = rope_consts()
    shared["ropem"] = ropem
    shared["ropeg"] = ropeg
    for l in range(2):
        w1 = np.asarray(inputs["moe_w1"][l], np.float32).reshape(NE, 8, 128, DE).transpose(0, 2, 1, 3)
        w3 = np.asarray(inputs["moe_w3"][l], np.float32).reshape(NE, 8, 128, DE).transpose(0, 2, 1, 3)
        w2 = np.asarray(inputs["moe_w2"][l], np.float32).reshape(NE, 2, 128, D).transpose(0, 2, 1, 3)
        shared["w1r%d" % l] = np.ascontiguousarray(w1).reshape(NE * 128, 2048)
        shared["w3r%d" % l] = np.ascontiguousarray(w3).reshape(NE * 128, 2048)
        shared["w2r%d" % l] = np.ascontiguousarray(w2).reshape(NE * 128, 2048)
    shared["cst_ltri"] = np.triu(np.ones((128, 128), np.float32), 1).astype(ml_dtypes.bfloat16)
    shared["cst_iota"] = np.ascontiguousarray(np.broadcast_to(np.arange(128, dtype=np.float32)[None, :], (128, 128)))
    shared["cst_pidx"] = np.arange(128, dtype=np.float32).reshape(128, 1)
    return shared


_CACHE = {}


def kernel(**inputs):
    x = np.asarray(inputs["x"], np.float32)
    ctx = np.asarray(inputs["ctx"], np.float32)
    c = np.asarray(inputs["c"], np.float32)
    c_ctx = np.asarray(inputs["c_ctx"], np.float32)
    B = x.shape[0]
    if "nc" not in _CACHE:
        _CACHE["nc"] = build_nc()
    nc = _CACHE["nc"]
    shared = host_shared(inputs)
    in_maps = []
    for b in range(B):
        m = dict(shared)
        m["xin"] = np.ascontiguousarray(np.concatenate([ctx[b], x[b]], axis=0))
        m["cc"] = np.ascontiguousarray(np.stack([c[b], c_ctx], axis=0))
        in_maps.append(m)
    res = run_bass_kernel_spmd(nc, in_maps, core_ids=list(range(B)))
    return np.stack([np.asarray(r["out"], np.float32) for r in res.results], axis=0)
```

```python
import numpy as np
import ml_dtypes
import concourse.bass as bass
import concourse.mybir as mybir
from concourse.bass_utils import run_bass_kernel_spmd

F32 = mybir.dt.float32
BF16 = mybir.dt.bfloat16
AF = mybir.ActivationFunctionType
ALU = mybir.AluOpType
AX = mybir.AxisListType

D = 1024
NCTX = 256
SEQ = 4096
T = NCTX + SEQ
NT = T // 128
DIN = 5280
EPS = 1e-6
C_XR, C_CKV, C_KR, C_GK, C_GV, C_RG, C_CQ, C_GQ, C_MG = 0, 512, 640, 672, 800, 928, 1440, 1696, 2208
MLA_SCALE = 96 ** -0.5
GQA_SCALE = 64 ** -0.5
NE = 32
DE = 256
import os as _os
DBG_SKIP_ROUTER = bool(_os.environ.get('DBG_SKIP_ROUTER'))
DBG_STOP = int(_os.environ.get('DBG_STOP', '99'))


class Buf:
    __slots__ = ("name", "lastw", "readers")

    def __init__(self, name=""):
        self.name = name
        self.lastw = None
        self.readers = []


class Op:
    __slots__ = ("eng", "fn", "deps", "dma", "tok", "needed", "idx")


class Prog:
    COMPUTE = ("pe", "act", "dve", "pool")
    NPOOL = 12
    UID = 0

    def __init__(self, nc):
        self.nc = nc
        self.ops = []
        self.q = {k: [] for k in ("pe", "act", "dve", "pool", "sp")}

    def add(self, eng, fn, reads=(), writes=(), dma=False):
        op = Op()
        op.eng, op.fn, op.dma, op.tok, op.needed = eng, fn, dma, None, False
        op.idx = len(self.ops)
        deps = set()
        for b in reads:
            if b.lastw is not None:
                deps.add(b.lastw)
        for b in writes:
            if b.lastw is not None:
                deps.add(b.lastw)
            deps.update(b.readers)
        op.deps = deps
        for b in reads:
            if b in writes:
                continue
            if not dma:
                b.readers = [r for r in b.readers if self.ops[r].dma or self.ops[r].eng != eng]
            b.readers.append(op.idx)
        for b in writes:
            b.lastw = op.idx
            b.readers = []
        self.ops.append(op)
        self.q[eng].append(op)
        return op

    def emit(self):
        nc, ops = self.nc, self.ops
        for op in ops:
            for d in op.deps:
                dop = ops[d]
                if dop.eng == "pe" and op.eng == "pe" and not dop.dma and not op.dma:
                    continue
                dop.needed = True
        Prog.UID += 1
        u = Prog.UID
        sems = {k: nc.alloc_semaphore("s%d_%s" % (u, k)) for k in self.COMPUTE}
        dsem = {k: [nc.alloc_semaphore("d%d_%s_%d" % (u, k, i)) for i in range(self.NPOOL)] for k in self.q}
        cnt = {k: 0 for k in self.COMPUTE}
        dcnt = {k: 0 for k in self.q}
        prewait = {}
        for op in ops:
            if op.dma:
                k = dcnt[op.eng]
                dcnt[op.eng] += 1
                s = dsem[op.eng][k % self.NPOOL]
                op.tok = (s, 16 * (k // self.NPOOL + 1))
                if k >= self.NPOOL:
                    prewait[op.idx] = (s, 16 * (k // self.NPOOL))
            elif op.needed:
                cnt[op.eng] += 1
                op.tok = (sems[op.eng], cnt[op.eng])
        engines = {"pe": "tensor", "act": "scalar", "dve": "vector", "pool": "gpsimd", "sp": "sync"}
        with nc.Block() as block:
            def make(k):
                def body(e):
                    known = {}
                    for op in self.q[k]:
                        waits = []
                        if op.idx in prewait:
                            waits.append(prewait[op.idx])
                        for d in sorted(op.deps):
                            dop = ops[d]
                            if dop.tok is None:
                                continue
                            if dop.eng == "pe" and k == "pe" and not dop.dma and not op.dma:
                                continue
                            waits.append(dop.tok)
                        for (s, v) in waits:
                            if known.get(id(s), 0) >= v:
                                continue
                            known[id(s)] = v
                            e.wait_ge(s, v)
                        ins = op.fn(e)
                        if op.tok is not None:
                            ins.then_inc(op.tok[0], 16 if op.dma else 1)
                    if k == "sp":
                        for kk in self.q:
                            n = dcnt[kk]
                            for j in range(min(n, self.NPOOL)):
                                uses = (n - j + self.NPOOL - 1) // self.NPOOL
                                e.wait_ge(dsem[kk][j], 16 * uses)
                return body
            for k, attr in engines.items():
                getattr(block, attr)(make(k))


class Tl:
    def __init__(self, t, name=""):
        self.t = t
        self.b = Buf(name)

    def __getitem__(self, k):
        return self.t[k]


def _bufs(lst):
    return [x.b if isinstance(x, Tl) else x for x in lst]


class Ph:
    def __init__(self, nc, ps):
        self.nc = nc
        self.P = Prog(nc)
        self.ps = ps
        self.pb = [Tl(ps[:, i * 512:(i + 1) * 512], "pb%d" % i) for i in range(8)]
        self.n = 0
        Ph.UID += 1
        self.uid = Ph.UID

    UID = 0

    def sb(self, shape, dt=F32, name=None):
        self.n += 1
        t = self.nc.alloc_sbuf_tensor("%s_%d_%d" % (name or "t", self.uid, self.n), list(shape), dt)
        return Tl(t, name or "t")

    def add(self, eng, fn, r, w, dma=False):
        return self.P.add(eng, fn, _bufs(r), _bufs(w), dma=dma)

    def dma(self, out, in_, r=(), w=(), eng="sp", **kw):
        return self.add(eng, lambda e: e.dma_start(out=out, in_=in_, **kw), r, w, dma=True)

    def mm(self, out, lhsT, rhs, start, stop, r, w):
        return self.add("pe", lambda e: e.matmul(out, lhsT=lhsT, rhs=rhs, start=start, stop=stop), r, w)

    def tr(self, out, in_, ident, r, w):
        return self.add("pe", lambda e: e.transpose(out=out, in_=in_, identity=ident), r, w)

    def act(self, out, in_, func, r, w, bias=None, scale=None, accum=None):
        kw = {}
        if bias is not None:
            kw["bias"] = bias
        if scale is not None:
            kw["scale"] = scale
        if accum is not None:
            kw["accum_out"] = accum
        return self.add("act", lambda e: e.activation(out=out, in_=in_, func=func, **kw), r, w)

    def tt(self, eng, out, in0, in1, op, r, w):
        return self.add(eng, lambda e: e.tensor_tensor(out=out, in0=in0, in1=in1, op=op), r, w)

    def ts(self, eng, out, in0, s1, s2, op0, op1, r, w):
        if op1 is None:
            return self.add(eng, lambda e: e.tensor_scalar(out=out, in0=in0, scalar1=s1, scalar2=None, op0=op0), r, w)
        return self.add(eng, lambda e: e.tensor_scalar(out=out, in0=in0, scalar1=s1, scalar2=s2, op0=op0, op1=op1), r, w)

    def stt(self, eng, out, in0, sc, in1, op0, op1, r, w):
        return self.add(eng, lambda e: e.scalar_tensor_tensor(out=out, in0=in0, scalar=sc, in1=in1, op0=op0, op1=op1), r, w)

    def cp(self, eng, out, in_, r, w):
        if eng == "act":
            return self.add("act", lambda e: e.activation(out=out, in_=in_, func=AF.Copy), r, w)
        return self.add(eng, lambda e: e.tensor_copy(out=out, in_=in_), r, w)

    def memset(self, eng, ap, val, w):
        return self.add(eng, lambda e: e.memset(ap, val), [], w)

    def recip(self, out, in_, r, w):
        return self.add("dve", lambda e: e.reciprocal(out=out, in_=in_), r, w)

    def breg(self, e, val):
        if not hasattr(self, "_regs"):
            self._regs = {}
        if val not in self._regs:
            r = e.alloc_register("bnd_%d_%d" % (self.uid, val))
            e.reg_mov(r, val)
            self._regs[val] = r
        return self._regs[val]

    def finish(self):
        self.P.emit()


def run_phase(nc, ps, fn, *args):
    with nc.cleanup_on_exit():
        ph = Ph(nc, ps)
        fn(ph, *args)
        ph.finish()
        nc.all_engine_barrier()


def token_blocks(t0, t1, bs=512):
    out = []
    t = t0
    while t < t1:
        n = min(bs, t1 - t)
        out.append((t, n))
        t += n
    return out


def make_identity(ph, dt, name):
    idf = ph.sb([128, 128], F32, name + "f")
    ph.memset("pool", idf[:], 0.0, [idf])
    ph.add("pool", lambda e: e.affine_select(out=idf[:], in_=idf[:], pattern=[[-1, 128]], compare_op=ALU.not_equal,
                                            fill=1.0, base=0, channel_multiplier=1), [idf], [idf])
    if dt == F32:
        return idf
    idb = ph.sb([128, 128], dt, name)
    ph.cp("pool", idb[:], idf[:], [idf], [idb])
    return idb


def phase_mod(ph, dr, l):
    cc = ph.sb([128, 2, 8], F32, "cc")
    sc = ph.sb([128, 2, 8], F32, "sc")
    ph.dma(cc[:], dr["cc"].rearrange("r (p k) -> p r k", k=8), [], [cc])
    ph.act(sc[:], cc[:], AF.Silu, [cc], [sc])
    mods = ph.sb([2, 6 * D], F32, "mods")
    bm = ph.sb([2, 6 * D], F32, "bm")
    ph.dma(bm[:], dr["b_mod"][l:l + 1, :].partition_broadcast(2), [], [bm])
    gm = ph.sb([2, D], F32, "gm")
    gf = ph.sb([2, D], F32, "gf")
    ph.dma(gm[:], dr["g_mix"][l:l + 1, :].partition_broadcast(2), [], [gm])
    ph.dma(gf[:], dr["g_ffn"][l:l + 1, :].partition_broadcast(2), [], [gf])
    wv = dr["w_mod"][l].rearrange("(p k) n -> p k n", k=8)
    wb = [ph.sb([128, 8, 512], F32, "wb") for _ in range(2)]
    for nb in range(12):
        w = wb[nb % 2]
        ph.dma(w[:], wv[:, :, nb * 512:(nb + 1) * 512], [], [w])
        pb = ph.pb[nb % 2]
        for k in range(8):
            ph.mm(pb[0:2, :], sc[:, :, k], w[:, k, :], k == 0, k == 7, [sc, w], [pb])
        ph.tt("dve", mods[:, nb * 512:(nb + 1) * 512], pb[0:2, :], bm[:, nb * 512:(nb + 1) * 512], ALU.add, [pb, bm], [mods])
    ph.stt("dve", mods[:, D:2 * D], mods[:, D:2 * D], 1.0, gm[:], ALU.add, ALU.mult, [mods, gm], [mods])
    ph.stt("dve", mods[:, 4 * D:5 * D], mods[:, 4 * D:5 * D], 1.0, gf[:], ALU.add, ALU.mult, [mods, gf], [mods])
    ph.dma(dr["modv"][:, :], mods[:], [mods], [])


def load_bc(ph, dr, row, idx, name):
    t = ph.sb([128, D], F32, name)
    ph.dma(t[:], dr["modv"][row:row + 1, idx * D:(idx + 1) * D].partition_broadcast(128), [], [t])
    return t


def rms_modulate(ph, xt, A, B, hout, junk, ss, rstd, h32=None):
    ph.act(junk[:], xt[:], AF.Square, [xt], [junk, ss], accum=ss[:, 0:1])
    ph.act(rstd[:, 0:1], ss[:, 0:1], AF.Sqrt, [ss], [rstd], bias=ph.epsc[:, 0:1], scale=1.0 / D)
    ph.recip(rstd[:, 0:1], rstd[:, 0:1], [rstd], [rstd])
    tmp = h32 if h32 is not None else junk
    ph.stt("dve", tmp[:], xt[:], rstd[:, 0:1], A[:], ALU.mult, ALU.mult, [xt, rstd, A], [tmp])
    ph.tt("dve", hout[:], tmp[:], B[:], ALU.add, [tmp, B], [hout])


def eps_const(ph):
    ph.epsc = ph.sb([128, 1], F32, "eps")
    ph.memset("pool", ph.epsc[:], EPS, [ph.epsc])


def phase_inproj(ph, dr, l, xsrc):
    nc = ph.nc
    eps_const(ph)
    win = ph.sb([128, 8, DIN], BF16, "win")
    stg = [ph.sb([128, 1320], F32, "stg") for _ in range(2)]
    wv = dr["w_in"][l].rearrange("(k p) n -> p k n", p=128)
    i = 0
    for k in range(8):
        for c4 in range(4):
            s = stg[i % 2]
            ph.dma(s[:], wv[:, k, c4 * 1320:(c4 + 1) * 1320], [], [s])
            ph.cp("dve" if i % 2 == 0 else "pool", win[:, k, c4 * 1320:(c4 + 1) * 1320], s[:], [s], [win])
            i += 1
    wkr = ph.sb([128, 8, 96], BF16, "wkr")
    wkrs = ph.sb([128, 8, 96], BF16, "wkrs")
    ph.memset("pool", wkr[:], 0.0, [wkr])
    ph.memset("pool", wkrs[:], 0.0, [wkrs])
    ph.cp("pool", wkr[:, :, 64:96], win[:, :, C_KR:C_KR + 32], [win], [wkr])
    ph.cp("pool", wkrs[:, :, 64:80], win[:, :, C_KR + 16:C_KR + 32], [win], [wkrs])
    ph.cp("pool", wkrs[:, :, 80:96], win[:, :, C_KR:C_KR + 16], [win], [wkrs])
    wgks = ph.sb([128, 8, 128], BF16, "wgks")
    wgqs = ph.sb([128, 8, 512], BF16, "wgqs")
    for (dst, c0, n) in ((wgks, C_GK, 128), (wgqs, C_GQ, 512)):
        sv = win[:, :, c0:c0 + n].rearrange("p k (h two d) -> p k h two d", two=2, d=32)
        dv = dst[:, :, :].rearrange("p k (h two d) -> p k h two d", two=2, d=32)
        ph.cp("pool", dv[:, :, :, 0, :], sv[:, :, :, 1, :], [win], [dst])
        ph.cp("pool", dv[:, :, :, 1, :], sv[:, :, :, 0, :], [win], [dst])
    gq = ph.sb([128, 2], F32, "gq")
    ph.dma(gq[:], dr["mla_gq"][l].rearrange("(c p) -> p c", p=128), [], [gq], allow_slow_non_contiguous=True)
    gkv = ph.sb([128, 1], F32, "gkv")
    ph.dma(gkv[:], dr["mla_gkv"][l].rearrange("(p o) -> p o", o=1), [], [gkv])
    wuqf = ph.sb([128, 2, 768], F32, "wuqf")
    ph.dma(wuqf[:], dr["mla_wuq"][l].rearrange("(c p) n -> p c n", p=128), [], [wuqf])
    wuq = ph.sb([128, 2, 768], BF16, "wuq")
    wuqs = ph.sb([128, 2, 768], BF16, "wuqs")
    for c in range(2):
        ph.ts("dve", wuq[:, c, :], wuqf[:, c, :], gq[:, c:c + 1], None, ALU.mult, None, [wuqf, gq], [wuq])
    ph.cp("pool", wuqs[:], wuq[:], [wuq], [wuqs])
    v1 = wuq[:, :, :].rearrange("p c (h d) -> p c h d", d=96)
    v2 = wuqs[:, :, :].rearrange("p c (h d) -> p c h d", d=96)
    ph.cp("pool", v2[:, :, :, 64:80], v1[:, :, :, 80:96], [wuq], [wuqs])
    ph.cp("pool", v2[:, :, :, 80:96], v1[:, :, :, 64:80], [wuq], [wuqs])
    wkvf = ph.sb([128, 1024], F32, "wkvf")
    ph.dma(wkvf[:], dr["mla_wukv"][l], [], [wkvf])
    wkv = ph.sb([128, 2, 512], BF16, "wkv")
    sv = wkvf[:, :].rearrange("p (h two d) -> p two h d", two=2, d=64)
    for two in range(2):
        ph.ts("dve", wkv[:, two, :].rearrange("p (h d) -> p h d", d=64), sv[:, two, :, :], gkv[:, 0:1], None, ALU.mult, None,
              [wkvf, gkv], [wkv])
    ones = ph.sb([128, 128], BF16, "ones")
    ph.memset("pool", ones[:], 1.0, [ones])
    bones = ph.sb([128, 128], BF16, "bones")
    ph.memset("pool", bones[:], 0.0, [bones])
    ph.memset("pool", bones[0:64, 0:64], 1.0, [bones])
    ph.memset("pool", bones[64:128, 64:128], 1.0, [bones])
    ident = make_identity(ph, BF16, "ident")
    gcol = ph.sb([128, 4], F32, "gcol")
    for j, nm in ((0, "gqa_gq"), (2, "gqa_gk")):
        src = dr[nm][l].rearrange("(d o) -> d o", o=1)
        for hh in range(2):
            ph.dma(gcol[hh * 64:hh * 64 + 64, j:j + 1], src[0:64, :], [], [gcol])
            ph.dma(gcol[hh * 64:hh * 64 + 32, j + 1:j + 2], src[32:64, :], [], [gcol])
            ph.dma(gcol[hh * 64 + 32:hh * 64 + 64, j + 1:j + 2], src[0:32, :], [], [gcol])
    Al = load_bc(ph, dr, 0, 1, "Al")
    Bl = load_bc(ph, dr, 0, 0, "Bl")
    Ac = load_bc(ph, dr, 1, 1, "Ac")
    Bc = load_bc(ph, dr, 1, 0, "Bc")

    xt = [ph.sb([128, D], F32, "xt") for _ in range(2)]
    junk = ph.sb([128, D], F32, "junk")
    hb = [ph.sb([128, D], BF16, "hb") for _ in range(2)]
    ss = ph.sb([128, 1], F32, "ss")
    rstd = ph.sb([128, 1], F32, "rstd")
    hT = [ph.sb([128, 8, 512], BF16, "hT") for _ in range(2)]
    tabm = [ph.sb([96, 2, 512], BF16, "tabm") for _ in range(2)]
    tabg = [ph.sb([128, 2, 512], BF16, "tabg") for _ in range(2)]
    NOB = 6
    ob = [ph.sb([128, 512], BF16, "ob") for _ in range(NOB)]
    NF = 6
    fb = [ph.sb([128, 512], F32, "fb") for _ in range(NF)]
    nck = ph.sb([128, 512], BF16, "nck")
    ncq = [ph.sb([128, 512], BF16, "ncq") for _ in range(2)]
    ctr = {"ob": 0, "fb": 0, "pb": 0, "ev": 0}

    def nob():
        ctr["ob"] += 1
        return ob[ctr["ob"] % NOB]

    def nfb():
        ctr["fb"] += 1
        return fb[ctr["fb"] % NF]

    def npb():
        ctr["pb"] += 1
        return ph.pb[ctr["pb"] % 8]

    def evac_eng():
        ctr["ev"] += 1
        return "act" if ctr["ev"] % 2 == 0 else "dve"

    def proj(lhs_tile, c0, m, hTb, n, extra_r=()):
        pb = npb()
        for k in range(8):
            ph.mm(pb[0:m, 0:n], lhs_tile[:, k, c0:c0 + m], hTb[:, k, 0:n], k == 0, k == 7, [lhs_tile, hTb], [pb])
        return pb

    def store(dst_ap, src_tile, src_ap, eng="pool"):
        ph.dma(dst_ap, src_ap, [src_tile], [], eng=eng)

    blocks = token_blocks(0, T)

    def PREP(bi, t0, n):
        hTb = hT[bi % 2]
        tm = tabm[bi % 2]
        tg = tabg[bi % 2]
        ph.dma(tm[64:96, :, 0:n], dr["ropem"][:, :, t0:t0 + n].rearrange("a r t -> r a t"), [], [tm])
        for hh in range(2):
            ph.dma(tg[hh * 64:hh * 64 + 64, :, 0:n], dr["ropeg"][:, :, t0:t0 + n].rearrange("a r t -> r a t"), [], [tg])
        for j in range(n // 128):
            tok = t0 + j * 128
            x_ = xt[j % 2]
            h_ = hb[j % 2]
            ph.dma(x_[:], xsrc[tok:tok + 128, :], [], [x_])
            isctx = tok < NCTX
            rms_modulate(ph, x_, Ac if isctx else Al, Bc if isctx else Bl, h_, junk, ss, rstd)
            pb = npb()
            pbv = pb.t.bitcast(BF16)
            for k in range(8):
                ph.tr(pbv[:, k * 128:(k + 1) * 128], h_[:, k * 128:(k + 1) * 128], ident[:], [h_, ident], [pb])
            ph.cp(evac_eng(), hTb[:, :, j * 128:(j + 1) * 128], pbv[:, :].rearrange("p (k t) -> p k t", t=128), [pb], [hTb])

    def MAIN(bi, t0, n, part):
        hTb = hT[bi % 2]
        tm = tabm[bi % 2]
        tg = tabg[bi % 2]
        sl = slice(t0, t0 + n)
        if part == 1:
            for c in range(4):
                pb = proj(win, C_XR + c * 128, 128, hTb, n)
                o = nob()
                ph.cp(evac_eng(), o[:, 0:n], pb[:, 0:n], [pb], [o])
                store(dr["xrT"][c * 128:(c + 1) * 128, sl], o, o[:, 0:n])
            for c in range(4):
                pb = proj(win, C_RG + c * 128, 128, hTb, n)
                o = nob()
                ph.act(o[:, 0:n], pb[:, 0:n], AF.Gelu_apprx_tanh, [pb], [o])
                store(dr["rgT"][c * 128:(c + 1) * 128, sl], o, o[:, 0:n])
            for c in range(24):
                pb = proj(win, C_MG + c * 128, 128, hTb, n)
                o = nob()
                ph.act(o[:, 0:n], pb[:, 0:n], AF.Sigmoid, [pb], [o])
                store(dr["gatesT"][c * 128:(c + 1) * 128, sl], o, o[:, 0:n])
            return
        for j in range(n // 128):
            pb = npb()
            for k in range(8):
                ph.mm(pb[:, 0:128], hTb[:, k, j * 128:(j + 1) * 128], win[:, k, C_GV:C_GV + 128], k == 0, k == 7, [hTb, win], [pb])
            o = nob()
            ph.cp(evac_eng(), o[:, 0:128], pb[:, 0:128], [pb], [o])
            store(dr["vg"][t0 + j * 128:t0 + (j + 1) * 128, :], o, o[:, 0:128])

        def rstd_bc(sq_list, ones_t, count):
            pb = npb()
            for i_, sq in enumerate(sq_list):
                ph.mm(pb[:, 0:n], ones_t[:], sq[:, 0:n], i_ == 0, i_ == len(sq_list) - 1, [ones_t, sq], [pb])
            r_ = nfb()
            ph.act(r_[:, 0:n], pb[:, 0:n], AF.Sqrt, [pb], [r_], bias=ph.epsc[:, 0:1], scale=1.0 / count)
            ph.recip(r_[:, 0:n], r_[:, 0:n], [r_], [r_])
            return r_

        pa = proj(win, C_CKV, 128, hTb, n)
        sq = nob()
        ph.act(sq[:, 0:n], pa[:, 0:n], AF.Square, [pa], [sq])
        r_ = rstd_bc([sq], ones, 128)
        ph.tt("dve", nck[:, 0:n], pa[:, 0:n], r_[:, 0:n], ALU.mult, [pa, r_], [nck])
        for hp in range(4):
            pb = npb()
            ph.mm(pb[:, 0:n], wkv[:, 0, hp * 128:(hp + 1) * 128], nck[:, 0:n], True, True, [wkv, nck], [pb])
            o = nob()
            ph.cp(evac_eng(), o[:, 0:n], pb[:, 0:n], [pb], [o])
            for hh in range(2):
                store(dr["kmT"][2 * hp + hh, 0:64, sl], o, o[hh * 64:hh * 64 + 64, 0:n])
        for j in range(n // 128):
            pb = npb()
            ph.mm(pb[:, :], nck[:, j * 128:(j + 1) * 128], wkv[:, 1, :], True, True, [nck, wkv], [pb])
            o = nob()
            ph.cp(evac_eng(), o[:, :], pb[:, :], [pb], [o])
            store(dr["vm"][t0 + j * 128:t0 + (j + 1) * 128, :], o, o[:, :])

        def rope96(pa, pb_, dst_tile):
            t1 = nfb()
            t2 = nfb()
            ph.tt("dve", t1[64:96, 0:n], pa[64:96, 0:n], tm[64:96, 0, 0:n], ALU.mult, [pa, tm], [t1])
            ph.tt("dve", t2[64:96, 0:n], pb_[64:96, 0:n], tm[64:96, 1, 0:n], ALU.mult, [pb_, tm], [t2])
            ph.tt("pool", dst_tile[64:96, 0:n], t1[64:96, 0:n], t2[64:96, 0:n], ALU.add, [t1, t2], [dst_tile])

        pa = proj(wkr, 0, 96, hTb, n)
        pb_ = proj(wkrs, 0, 96, hTb, n)
        o = nob()
        rope96(pa, pb_, o)
        for h in range(8):
            store(dr["kmT"][h, 64:96, sl], o, o[64:96, 0:n], eng="sp" if h % 2 else "pool")
        pc = [proj(win, C_CQ + c * 128, 128, hTb, n) for c in range(2)]
        sqs = []
        for c in range(2):
            s_ = nob()
            ph.act(s_[:, 0:n], pc[c][:, 0:n], AF.Square, [pc[c]], [s_])
            sqs.append(s_)
        r_ = rstd_bc(sqs, ones, 256)
        for c in range(2):
            ph.tt("dve", ncq[c][:, 0:n], pc[c][:, 0:n], r_[:, 0:n], ALU.mult, [pc[c], r_], [ncq[c]])
        for h in range(8):
            pa = npb()
            pb_ = npb()
            for c in range(2):
                ph.mm(pa[0:96, 0:n], wuq[:, c, h * 96:(h + 1) * 96], ncq[c][:, 0:n], c == 0, c == 1, [wuq, ncq[c]], [pa])
            for c in range(2):
                ph.mm(pb_[0:96, 0:n], wuqs[:, c, h * 96:(h + 1) * 96], ncq[c][:, 0:n], c == 0, c == 1, [wuqs, ncq[c]], [pb_])
            o = nob()
            ph.cp("act", o[0:64, 0:n], pa[0:64, 0:n], [pa], [o])
            rope96(pa, pb_, o)
            store(dr["qmT"][h, :, sl], o, o[0:96, 0:n], eng="sp" if h % 2 else "pool")

        def gqa_chunk(c0, wsw, csw, gj, dst_fn):
            pa = proj(win, c0, 128, hTb, n)
            pb_ = proj(wsw, csw, 128, hTb, n)
            sq = nob()
            ph.act(sq[:, 0:n], pa[:, 0:n], AF.Square, [pa], [sq])
            r_ = rstd_bc([sq], bones, 64)
            t1 = nfb()
            t2 = nfb()
            ph.stt("dve", t1[:, 0:n], pa[:, 0:n], gcol[:, gj:gj + 1], tg[:, 0, 0:n], ALU.mult, ALU.mult, [pa, gcol, tg], [t1])
            ph.stt("dve", t2[:, 0:n], pb_[:, 0:n], gcol[:, gj + 1:gj + 2], tg[:, 1, 0:n], ALU.mult, ALU.mult, [pb_, gcol, tg], [t2])
            ph.tt("pool", t1[:, 0:n], t1[:, 0:n], t2[:, 0:n], ALU.add, [t1, t2], [t1])
            o = nob()
            ph.tt("pool", o[:, 0:n], t1[:, 0:n], r_[:, 0:n], ALU.mult, [t1, r_], [o])
            dst_fn(o)

        def st_gk(o):
            for hh in range(2):
                store(dr["kgT"][hh, :, sl], o, o[hh * 64:hh * 64 + 64, 0:n])
        gqa_chunk(C_GK, wgks, 0, 2, st_gk)
        for c in range(4):
            def st_gq(o, c=c):
                for hh in range(2):
                    store(dr["qgT"][2 * c + hh, :, sl], o, o[hh * 64:hh * 64 + 64, 0:n])
            gqa_chunk(C_GQ + c * 128, wgqs, c * 128, 0, st_gq)


    PREP(0, *blocks[0])
    for bi, (t0, n) in enumerate(blocks):
        MAIN(bi, t0, n, 1)
        if bi + 1 < len(blocks):
            PREP(bi + 1, *blocks[bi + 1])
        MAIN(bi, t0, n, 2)


def phase_lru(ph, dr, l):
    blocks = token_blocks(0, T)
    xp = ph.sb([128, T + 6], BF16, "xp")
    xc = ph.sb([128, T], F32, "xc")
    xcb = ph.sb([128, T], BF16, "xcb")
    rg = ph.sb([128, T], BF16, "rg")
    ysum = ph.sb([128, T], F32, "ysum")
    abuf = ph.sb([128, T], F32, "abuf")
    ibuf = ph.sb([128, T], F32, "ibuf")
    mbuf = ph.sb([128, T], F32, "mbuf")
    hbuf = ph.sb([128, T], F32, "hbuf")
    yo = ph.sb([128, T], BF16, "yo")
    for c in range(4):
        cs = slice(c * 128, (c + 1) * 128)
        ph.memset("pool", xp[:, 0:2], 0.0, [xp])
        ph.memset("pool", xp[:, 258:261], 0.0, [xp])
        ph.memset("pool", xp[:, T + 5:T + 6], 0.0, [xp])
        ph.dma(xp[:, 2:258], dr["xrT"][cs, 0:NCTX], [], [xp])
        ph.dma(xp[:, 261:261 + SEQ], dr["xrT"][cs, NCTX:T], [], [xp])
        cw = ph.sb([128, 4], F32, "cw")
        ph.dma(cw[:], dr["conv_w"][l].rearrange("j c -> c j")[cs, :], [], [cw], allow_slow_non_contiguous=True)
        cb = ph.sb([128, 1], F32, "cb")
        ph.dma(cb[:], dr["conv_b"][l].rearrange("(c o) -> c o", o=1)[cs, :], [], [cb])
        for (o0, i0, n) in ((0, 0, NCTX), (NCTX, 259, SEQ)):
            ph.ts("dve", xc[:, o0:o0 + n], xp[:, i0:i0 + n], cw[:, 0:1], cb[:, 0:1], ALU.mult, ALU.add, [xp, cw, cb], [xc])
            for j in range(1, 4):
                ph.stt("dve", xc[:, o0:o0 + n], xp[:, i0 + j:i0 + j + n], cw[:, j:j + 1], xc[:, o0:o0 + n],
                       ALU.mult, ALU.add, [xp, cw, xc], [xc])
        ph.cp("act", xcb[:], xc[:], [xc], [xcb])
        ph.dma(rg[:], dr["rgT"][cs, :], [], [rg])
        for d in range(2):
            wst = ph.sb([128, 2, 128], F32, "wst")
            ph.memset("pool", wst[:], 0.0, [wst])
            for g_, nm in enumerate(("lru_wa", "lru_wi")):
                for hb_ in range(2):
                    ph.dma(wst[hb_ * 64:hb_ * 64 + 64, g_, hb_ * 64:hb_ * 64 + 64], dr[nm][l, d, 2 * c + hb_], [wst], [wst])
            wbd = ph.sb([128, 2, 128], BF16, "wbd")
            ph.cp("pool", wbd[:], wst[:], [wst], [wbd])
            col = ph.sb([128, 8], F32, "col")
            for j, nm in enumerate(("lru_ba", "lru_bi", "lru_lambda")):
                ph.dma(col[:, j:j + 1], dr[nm][l, d].rearrange("(c o) -> c o", o=1)[cs, :], [], [col])
            ph.act(col[:, 4:5], col[:, 2:3], AF.Exp, [col], [col], scale=-1.0)
            ph.act(col[:, 5:6], col[:, 4:5], AF.Ln, [col], [col], bias=1.0)
            ph.ts("dve", col[:, 2:3], col[:, 5:6], -8.0, None, ALU.mult, None, [col], [col])
            ph.ts("dve", col[:, 3:4], col[:, 5:6], -16.0, None, ALU.mult, None, [col], [col])
            for bi, (t0, n) in enumerate(blocks):
                pr = ph.pb[(2 * bi) % 8]
                pi = ph.pb[(2 * bi + 1) % 8]
                ph.mm(pr[:, 0:n], wbd[:, 0, :], xcb[:, t0:t0 + n], True, True, [wbd, xcb], [pr])
                ph.mm(pi[:, 0:n], wbd[:, 1, :], xcb[:, t0:t0 + n], True, True, [wbd, xcb], [pi])
                ph.act(abuf[:, t0:t0 + n], pr[:, 0:n], AF.Sigmoid, [pr, col], [abuf], bias=col[:, 0:1])
                ph.act(ibuf[:, t0:t0 + n], pi[:, 0:n], AF.Sigmoid, [pi, col], [ibuf], bias=col[:, 1:2])
            ph.act(mbuf[:], abuf[:], AF.Exp, [abuf, col], [mbuf], scale=col[:, 3:4])
            ph.act(abuf[:], abuf[:], AF.Exp, [abuf, col], [abuf], scale=col[:, 2:3])
            ph.act(mbuf[:], mbuf[:], AF.Sqrt, [mbuf], [mbuf], bias=1.0, scale=-1.0)
            ph.tt("pool", ibuf[:], ibuf[:], mbuf[:], ALU.mult, [ibuf, mbuf], [ibuf])
            ph.tt("pool", ibuf[:], ibuf[:], xc[:], ALU.mult, [ibuf, xc], [ibuf])
            dst = ysum if d == 0 else hbuf
            if d == 0:
                ph.add("dve", lambda e, dst=dst: e.tensor_tensor_scan(out=dst[:, :], data0=abuf[:, :], data1=ibuf[:, :], initial=0.0,
                                                                      op0=ALU.mult, op1=ALU.add), [abuf, ibuf], [dst])
            else:
                ph.add("dve", lambda e, dst=dst: e.tensor_tensor_scan(out=dst[:, 0:NCTX][:, ::-1], data0=abuf[:, 0:NCTX][:, ::-1],
                                                                      data1=ibuf[:, 0:NCTX][:, ::-1], initial=0.0,
                                                                      op0=ALU.mult, op1=ALU.add), [abuf, ibuf], [dst])
                ph.add("dve", lambda e, dst=dst: e.tensor_tensor_scan(out=dst[:, NCTX:T][:, ::-1], data0=abuf[:, NCTX:T][:, ::-1],
                                                                      data1=ibuf[:, NCTX:T][:, ::-1], initial=dst[:, 0:1],
                                                                      op0=ALU.mult, op1=ALU.add), [abuf, ibuf, dst], [dst])
                ph.tt("pool", ysum[:], ysum[:], hbuf[:], ALU.add, [ysum, hbuf], [ysum])
        ph.tt("pool", yo[:], ysum[:], rg[:], ALU.mult, [ysum, rg], [yo])
        ph.dma(dr["yT"][0, cs, :], yo[:], [yo], [])


def phase_attn(ph, dr, l, with_ctx):
    NB = 2
    kT = [ph.sb([96, T], BF16, "kT") for _ in range(NB)]
    qT = [ph.sb([96, T], BF16, "qT") for _ in range(NB)]
    kTg = [ph.sb([128, T], BF16, "kTg") for _ in range(NB)]
    qTg = [ph.sb([128, T], BF16, "qTg") for _ in range(NB)]
    for t_ in kTg + qTg:
        ph.memset("pool", t_[64:128, :], 0.0, [t_])
    va = [ph.sb([128, NT, 128], BF16, "va") for _ in range(NB)]
    for v_ in va:
        ph.memset("pool", v_[:, :, 64:128], 1.0, [v_])
    NPT = 4
    pT = [ph.sb([128, 1024], BF16, "pT") for _ in range(NPT)]
    osb = [ph.sb([64, 512], F32, "osb") for _ in range(2)]
    yo = [ph.sb([64, 512], BF16, "yo") for _ in range(2)]
    sp_ = [Tl(ph.ps[:, 0:1024], "S0"), Tl(ph.ps[:, 1024:2048], "S1"), Tl(ph.ps[:, 2048:3072], "S2")]
    acc = [ph.pb[6], ph.pb[7]]
    heads = [(br, h) for br in (1, 2) for h in range(8)]

    def load_head(hi):
        br, h = heads[hi]
        k_, q_, v_ = (kT if br == 1 else kTg)[hi % NB], (qT if br == 1 else qTg)[hi % NB], va[hi % NB]
        if br == 1:
            ph.dma(k_[0:96, :], dr["kmT"][h], [], [k_])
            ph.dma(q_[0:96, :], dr["qmT"][h], [], [q_])
            vsrc = dr["vm"][:, h * 64:(h + 1) * 64].rearrange("(c p) d -> p c d", p=128)
        else:
            ph.dma(k_[0:64, :], dr["kgT"][h // 4], [], [k_])
            ph.dma(q_[0:64, :], dr["qgT"][h], [], [q_])
            vsrc = dr["vg"][:, (h // 4) * 64:(h // 4 + 1) * 64].rearrange("(c p) d -> p c d", p=128)
        ph.dma(v_[:, 0:17, 0:64], vsrc[:, 0:17, :], [], [v_])
        ph.dma(v_[:, 17:NT, 0:64], vsrc[:, 17:NT, :], [], [v_])

    items = []
    blk = 0
    for hi, (br, h) in enumerate(heads):
        d = 96 if br == 1 else 64
        scale = MLA_SCALE if br == 1 else GQA_SCALE
        qblocks = [(t0, n, NT) for (t0, n) in token_blocks(NCTX, T)]
        if with_ctx:
            qblocks = [(0, NCTX, NCTX // 128)] + qblocks
        first = True
        for (t0, n, nkc) in qblocks:
            for g0 in range(0, nkc, 2):
                items.append(dict(hi=hi, br=br, h=h, d=d, scale=scale, t0=t0, n=n, nkc=nkc, g0=g0, blk=blk,
                                  pre=first, last=(g0 + 2 >= nkc), idx=len(items)))
                first = False
            blk += 1

    def S(it):
        i = it["idx"]
        k_, q_ = (kT if it["br"] == 1 else kTg)[it["hi"] % NB], (qT if it["br"] == 1 else qTg)[it["hi"] % NB]
        s_, p_ = sp_[i % 3], pT[i % NPT]
        n, t0 = it["n"], it["t0"]
        d = 96 if it["br"] == 1 else 128
        for u in range(2):
            kc = it["g0"] + u
            ph.mm(s_[:, u * 512:u * 512 + n], k_[0:d, kc * 128:(kc + 1) * 128], q_[0:d, t0:t0 + n], True, True, [k_, q_], [s_])
        if n == 512:
            ph.act(p_[:, :], s_[:, :], AF.Exp, [s_], [p_], scale=it["scale"])
        else:
            sv = s_[:, :].rearrange("p (u t) -> p u t", u=2)[:, :, 0:n]
            pv = p_[:, :].rearrange("p (u t) -> p u t", u=2)[:, :, 0:n]
            ph.act(pv, sv, AF.Exp, [s_], [p_], scale=it["scale"])

    def PV(it):
        i = it["idx"]
        v_, p_ = va[it["hi"] % NB], pT[i % NPT]
        a_ = acc[it["blk"] % 2]
        n = it["n"]
        for u in range(2):
            kc = it["g0"] + u
            ph.mm(a_[:, 0:n], v_[:, kc, :], p_[:, u * 512:u * 512 + n], kc == 0, kc == it["nkc"] - 1, [v_, p_], [a_])
        if it["last"]:
            o_, y_ = osb[it["blk"] % 2], yo[it["blk"] % 2]
            t0, h, br = it["t0"], it["h"], it["br"]
            ph.recip(o_[0:64, 0:n], a_[64:128, 0:n], [a_], [o_])
            ph.tt("dve", y_[:, 0:n], a_[0:64, 0:n], o_[0:64, 0:n], ALU.mult, [o_, a_], [y_])
            ph.dma(dr["yT"][br, h * 64:(h + 1) * 64, t0:t0 + n], y_[:, 0:n], [y_], [], eng="pool")

    load_head(0)
    N = len(items)
    for i in range(N + 1):
        if i < N:
            S(items[i])
        if 0 <= i - 1 < N:
            PV(items[i - 1])
        if i < N and items[i]["pre"] and items[i]["hi"] + 1 < len(heads):
            load_head(items[i]["hi"] + 1)


def phase_merge(ph, dr, l, xsrc, tstart):
    eps_const(ph)
    wbr = ph.sb([128, 3, 4, D], BF16, "wbr")
    wo = ph.sb([128, 8, D], BF16, "wo")
    stg = [ph.sb([128, 2, D], F32, "stg") for _ in range(2)]
    si = 0
    for k in range(3):
        for hf in range(2):
            s = stg[si % 2]
            ph.dma(s[:], dr["w_branch"][l, k].rearrange("(c p) n -> p c n", p=128)[:, hf * 2:(hf + 1) * 2, :], [], [s])
            ph.cp("dve" if si % 2 else "pool", wbr[:, k, hf * 2:(hf + 1) * 2, :], s[:], [s], [wbr])
            si += 1
    for hf in range(4):
        s = stg[si % 2]
        ph.dma(s[:], dr["w_out"][l].rearrange("(c p) n -> p c n", p=128)[:, hf * 2:(hf + 1) * 2, :], [], [s])
        ph.cp("dve" if si % 2 else "pool", wo[:, hf * 2:(hf + 1) * 2, :], s[:], [s], [wo])
        si += 1
    wr = ph.sb([128, 8, 36], F32, "wr")
    ph.dma(wr[:, :, 0:4], dr["moe_wg"][l].rearrange("(c p) n -> p c n", p=128), [], [wr])
    ph.dma(wr[:, :, 4:36], dr["moe_we"][l].rearrange("(c p) n -> p c n", p=128), [], [wr])
    br_ = ph.sb([128, 36], F32, "br")
    ph.dma(br_[:, 0:4], dr["moe_bg"][l:l + 1, :].partition_broadcast(128), [], [br_])
    ph.dma(br_[:, 4:36], dr["moe_be"][l:l + 1, :].partition_broadcast(128), [], [br_])
    wrb = ph.sb([128, 8, 36], BF16, "wrb")
    ph.cp("pool", wrb[:], wr[:], [wr], [wrb])
    identb = make_identity(ph, BF16, "identb")
    hb16 = [ph.sb([128, D], BF16, "hb16") for _ in range(2)]
    G = {}
    for row in ((0, 1) if tstart == 0 else (0,)):
        G[row] = (load_bc(ph, dr, row, 2, "Ga"), load_bc(ph, dr, row, 4, "Af"), load_bc(ph, dr, row, 3, "Bf"))
    yb = [ph.sb([128, 3, 4, 512], BF16, "yb") for _ in range(2)]
    gb = [ph.sb([128, 24, 512], BF16, "gb") for _ in range(1)]
    mg = [ph.sb([128, 8, 512], BF16, "mgd") for _ in range(2)]
    tmpf = [ph.sb([128, 512], F32, "tmpf") for _ in range(3)]
    accf = ph.sb([128, 512], F32, "accf")
    xt = [ph.sb([128, D], F32, "xt") for _ in range(2)]
    x2 = [ph.sb([128, D], F32, "x2") for _ in range(2)]
    junk = ph.sb([128, D], F32, "junk")
    hTb = [ph.sb([128, 8, 128], BF16, "hTb") for _ in range(2)]
    ss = ph.sb([128, 1], F32, "ss")
    rstd = ph.sb([128, 1], F32, "rstd")
    sm = ph.sb([128, 64], F32, "sm")
    lg = ph.sb([128, 36], F32, "lg")
    mk = ph.sb([128, 4], F32, "mk")
    es = ph.sb([128, 4, 8], F32, "es")
    e8 = ph.sb([128, 8], F32, "e8")
    m8 = ph.sb([128, 8], F32, "m8")
    cb_ = [ph.sb([128, 32], F32, "comb") for _ in range(2)]
    c1 = ph.sb([128, 32], F32, "c1")
    c2 = ph.sb([128, 32], F32, "c2")
    ctr = {"pb": 0, "t": 0}

    def npb():
        ctr["pb"] += 1
        return ph.pb[ctr["pb"] % 8]

    blocks = token_blocks(tstart, T)
    bufs_of_block = {}

    def PRO(bi, t0, n):
        sl = slice(t0, t0 + n)
        y_, g_, m_ = yb[bi % 2], gb[0], mg[bi % 2]
        for k in range(3):
            ph.dma(y_[:, k, :, 0:n], dr["yT"][k, :, sl].rearrange("(c p) t -> p c t", p=128), [], [y_])
        for k in range(3):
            ph.dma(g_[:, k * 8:(k + 1) * 8, 0:n], dr["gatesT"][k * D:(k + 1) * D, sl].rearrange("(c p) t -> p c t", p=128), [], [g_])
        for oc in range(8):
            for k in range(3):
                pb = npb()
                for c in range(4):
                    ph.mm(pb[:, 0:n], wbr[:, k, c, oc * 128:(oc + 1) * 128], y_[:, k, c, 0:n], c == 0, c == 3, [wbr, y_], [pb])
                if k == 0:
                    ph.tt("dve", accf[:, 0:n], pb[:, 0:n], g_[:, oc, 0:n], ALU.mult, [pb, g_], [accf])
                else:
                    tf = tmpf[ctr["t"] % 3]
                    ctr["t"] += 1
                    ph.tt("dve", tf[:, 0:n], pb[:, 0:n], g_[:, k * 8 + oc, 0:n], ALU.mult, [pb, g_], [tf])
                    if k == 1:
                        ph.tt("pool", accf[:, 0:n], accf[:, 0:n], tf[:, 0:n], ALU.add, [accf, tf], [accf])
                    else:
                        ph.tt("pool", m_[:, oc, 0:n], accf[:, 0:n], tf[:, 0:n], ALU.add, [accf, tf], [m_])

    items = []
    for bi, (t0, n) in enumerate(blocks):
        for j in range(n // 128):
            items.append(dict(bi=bi, t0=t0, n=n, j=j, gi=len(items)))

    def A(it):
        bi, t0, n, j, gi = it["bi"], it["t0"], it["n"], it["j"], it["gi"]
        if j == 0:
            PRO(bi, t0, n)
        m_ = mg[bi % 2]
        tok = t0 + j * 128
        row = 1 if tok < NCTX else 0
        Ga, Af, Bf = G[row]
        x_, xo = xt[gi % 2], x2[gi % 2]
        ph.dma(x_[:], xsrc[tok:tok + 128, :], [], [x_])
        for hf in range(2):
            pb = npb()
            for c in range(8):
                ph.mm(pb[:, :], m_[:, c, j * 128:(j + 1) * 128], wo[:, c, hf * 512:(hf + 1) * 512], c == 0, c == 7, [m_, wo], [pb])
            ph.tt("dve", junk[:, hf * 512:(hf + 1) * 512], pb[:, :], Ga[:, hf * 512:(hf + 1) * 512], ALU.mult, [pb, Ga], [junk])
        ph.tt("pool", xo[:], junk[:], x_[:], ALU.add, [junk, x_], [xo])
        ph.dma(dr["x2"][tok:tok + 128, :], xo[:], [xo], [], eng="pool")
        hbt = hb16[gi % 2]
        rms_modulate(ph, xo, Af, Bf, hbt, junk, ss, rstd)
        ph.dma(dr["h2tok"][tok:tok + 128, :], hbt[:], [hbt], [], eng="pool")

    def B(it):
        gi = it["gi"]
        tok = it["t0"] + it["j"] * 128
        hbt = hb16[gi % 2]
        hb_ = hTb[gi % 2]
        pb = npb()
        pbv = pb.t.bitcast(BF16)
        for c in range(8):
            ph.tr(pbv[:, c * 128:(c + 1) * 128], hbt[:, c * 128:(c + 1) * 128], identb[:], [hbt, identb], [pb])
        ph.cp("act", hb_[:, :, :], pbv[:, :].rearrange("p (c t) -> p c t", t=128), [pb], [hb_])
        pb = npb()
        for c in range(8):
            ph.mm(pb[:, 0:36], hb_[:, c, :], wrb[:, c, :], c == 0, c == 7, [hb_, wrb], [pb])
        ph.tt("dve", lg[:], pb[:, 0:36], br_[:], ALU.add, [pb, br_], [lg])
        ph.add("dve", lambda e: e.tensor_reduce(out=sm[:, 0:1], in_=lg[:, 0:4], axis=AX.X, op=ALU.max), _bufs([lg]), _bufs([sm]))
        ph.ts("dve", mk[:], lg[:, 0:4], sm[:, 0:1], None, ALU.is_equal, None, [lg, sm], [mk])
        ph.ts("dve", sm[:, 1:2], sm[:, 0:1], -1.0, None, ALU.mult, None, [sm], [sm])
        ph.act(sm[:, 8:12], lg[:, 0:4], AF.Exp, [lg, sm], [sm], bias=sm[:, 1:2], accum=sm[:, 2:3])
        ph.recip(sm[:, 3:4], sm[:, 2:3], [sm], [sm])
        ev = lg[:, 4:36].rearrange("p (g e) -> p g e", e=8)
        ph.tt("dve", es[:], ev, mk[:, :].unsqueeze(2).to_broadcast([128, 4, 8]), ALU.mult, [lg, mk], [es])
        ph.add("dve", lambda e: e.tensor_reduce(out=e8[:], in_=es[:].rearrange("p g e -> p e g"), axis=AX.X, op=ALU.add),
               _bufs([es]), _bufs([e8]))
        ph.add("dve", lambda e: e.max(out=m8[:], in_=e8[:]), _bufs([e8]), _bufs([m8]))
        ph.tt("dve", sm[:, 4:5], m8[:, 0:1], m8[:, 1:2], ALU.subtract, [m8], [sm])
        ph.act(sm[:, 5:6], sm[:, 4:5], AF.Sigmoid, [sm], [sm])
        ph.tt("dve", sm[:, 5:6], sm[:, 5:6], sm[:, 3:4], ALU.mult, [sm], [sm])
        ph.tt("dve", sm[:, 6:7], sm[:, 3:4], sm[:, 5:6], ALU.subtract, [sm], [sm])
        cbt = cb_[gi % 2]
        ph.ts("dve", c1[:], lg[:, 4:36], m8[:, 0:1], sm[:, 5:6], ALU.is_equal, ALU.mult, [lg, m8, sm], [c1])
        ph.ts("dve", c2[:], lg[:, 4:36], m8[:, 1:2], sm[:, 6:7], ALU.is_equal, ALU.mult, [lg, m8, sm], [c2])
        ph.tt("dve", c1[:], c1[:], c2[:], ALU.add, [c1, c2], [c1])
        ph.tt("dve", cbt[:].rearrange("p (g e) -> p g e", e=8), c1[:].rearrange("p (g e) -> p g e", e=8),
              mk[:, :].unsqueeze(2).to_broadcast([128, 4, 8]), ALU.mult, [c1, mk], [cbt])
        ph.dma(dr["comb"][tok:tok + 128, :], cbt[:], [cbt], [], eng="pool")

    N = len(items)
    for i in range(N + 1):
        if i < N:
            A(items[i])
        if i >= 1:
            B(items[i - 1])


def phase_moe(ph, dr, l, tstart, final):
    eps_const(ph)
    ntok = T - tstart
    half = ntok // 2
    assert half % 128 == 0
    nth = half // 128
    Gf = {}
    for row in ((0, 1) if tstart == 0 else (0,)):
        Gf[row] = load_bc(ph, dr, row, 5, "Gf")
    if final:
        gfin = ph.sb([128, D], F32, "gfin")
        ph.dma(gfin[:], dr["g_final"].rearrange("(o n) -> o n", o=1).partition_broadcast(128), [], [gfin])
    hT = ph.sb([128, 8, half], BF16, "hT")
    acc = ph.sb([128, nth, D], F32, "acc")
    comb = ph.sb([128, nth, NE], F32, "comb")
    s13 = [ph.sb([128, 8, 256], F32, "s13") for _ in range(2)]
    s2 = [ph.sb([128, 2, D], F32, "s2") for _ in range(1)]
    w13 = [ph.sb([128, 8, 512], BF16, "w13") for _ in range(2)]
    w2 = [ph.sb([128, 2, D], BF16, "w2") for _ in range(2)]
    sg = [ph.sb([128, 2, 512], F32, "sg") for _ in range(2)]
    he = [ph.sb([128, 2, 512], BF16, "he") for _ in range(2)]
    xt = [ph.sb([128, D], F32, "xt") for _ in range(2)]
    junk = ph.sb([128, D], F32, "junk")
    ss = ph.sb([128, 1], F32, "ss")
    rstd = ph.sb([128, 1], F32, "rstd")
    ctr = {"pd": 0}
    for hf in range(2):
        h0 = tstart + hf * half
        ph.dma(hT[:, :, :], dr["h2T"][:, h0:h0 + half].rearrange("(c p) t -> p c t", p=128), [], [hT])
        ph.dma(comb[:, :, :], dr["comb"][h0:h0 + half, :].rearrange("(j p) e -> p j e", p=128), [], [comb])

        def load_w(e_):
            a2, b13, b2 = s2[0], w13[e_ % 2], w2[e_ % 2]
            ph.dma(s13[0][:], dr["moe_w1"][l, e_].rearrange("(c p) n -> p c n", p=128), [], [s13[0]])
            ph.dma(s13[1][:], dr["moe_w3"][l, e_].rearrange("(c p) n -> p c n", p=128), [], [s13[1]])
            ph.dma(a2[:], dr["moe_w2"][l, e_].rearrange("(c p) n -> p c n", p=128), [], [a2])
            ph.cp("pool", b13[:, :, 0:256], s13[0][:], [s13[0]], [b13])
            ph.cp("pool", b13[:, :, 256:512], s13[1][:], [s13[1]], [b13])
            ph.cp("pool", b2[:], a2[:], [a2], [b2])

        items = []
        for e_ in range(NE):
            for bi_, (b0, n) in enumerate(token_blocks(0, half)):
                items.append(dict(e=e_, b0=b0, n=n, first=(bi_ == 0), idx=len(items)))

        def UP(it):
            i, e_, b0, n = it["idx"], it["e"], it["b0"], it["n"]
            b13 = w13[e_ % 2]
            s_, h_ = sg[i % 2], he[i % 2]
            for m in range(2):
                pg, pu = ph.pb[2 * m], ph.pb[2 * m + 1]
                for c in range(8):
                    ph.mm(pg[:, 0:n], b13[:, c, m * 128:(m + 1) * 128], hT[:, c, b0:b0 + n], c == 0, c == 7, [b13, hT], [pg])
                for c in range(8):
                    ph.mm(pu[:, 0:n], b13[:, c, 256 + m * 128:256 + (m + 1) * 128], hT[:, c, b0:b0 + n], c == 0, c == 7, [b13, hT], [pu])
                ph.act(s_[:, m, 0:n], pg[:, 0:n], AF.Silu, [pg], [s_])
                ph.tt("dve", h_[:, m, 0:n], pu[:, 0:n], s_[:, m, 0:n], ALU.mult, [pu, s_], [h_])

        def DN(it):
            i, e_, b0, n = it["idx"], it["e"], it["b0"], it["n"]
            b2 = w2[e_ % 2]
            h_ = he[i % 2]
            for j in range(n // 128):
                tj = (b0 // 128) + j
                for nh in range(2):
                    pd = ph.pb[4 + ctr["pd"] % 4]
                    ctr["pd"] += 1
                    for c in range(2):
                        ph.mm(pd[:, :], h_[:, c, j * 128:(j + 1) * 128], b2[:, c, nh * 512:(nh + 1) * 512], c == 0, c == 1, [h_, b2], [pd])
                    asl = acc[:, tj, nh * 512:(nh + 1) * 512]
                    if e_ == 0:
                        ph.ts("dve", asl, pd[:, :], comb[:, tj, e_:e_ + 1], None, ALU.mult, None, [pd, comb], [acc])
                    else:
                        ph.stt("dve", asl, pd[:, :], comb[:, tj, e_:e_ + 1], asl, ALU.mult, ALU.add, [pd, comb, acc], [acc])

        load_w(0)
        N = len(items)
        for i in range(N + 1):
            if i < N:
                UP(items[i])
            if i >= 1:
                DN(items[i - 1])
            if i < N and items[i]["first"] and items[i]["e"] + 1 < NE:
                load_w(items[i]["e"] + 1)
        for tj in range(nth):
            tok = h0 + tj * 128
            row = 1 if tok < NCTX else 0
            x_ = xt[tj % 2]
            ph.dma(x_[:], dr["x2"][tok:tok + 128, :], [], [x_])
            ph.tt("pool", acc[:, tj, :], acc[:, tj, :], Gf[row][:], ALU.mult, [acc, Gf[row]], [acc])
            ph.tt("pool", x_[:], x_[:], acc[:, tj, :], ALU.add, [x_, acc], [x_])
            if not final:
                ph.dma(dr["xres"][tok:tok + 128, :], x_[:], [x_], [], eng="pool")
            else:
                ph.act(junk[:], x_[:], AF.Square, [x_], [junk, ss], accum=ss[:, 0:1])
                ph.act(rstd[:, 0:1], ss[:, 0:1], AF.Sqrt, [ss], [rstd], bias=ph.epsc[:, 0:1], scale=1.0 / D)
                ph.recip(rstd[:, 0:1], rstd[:, 0:1], [rstd], [rstd])
                ph.stt("dve", x_[:], x_[:], rstd[:, 0:1], gfin[:], ALU.mult, ALU.mult, [x_, rstd, gfin], [x_])
                ph.dma(dr["out"][tok - NCTX:tok - NCTX + 128, :], x_[:], [x_], [], eng="pool")


SL = 512
I32 = mybir.dt.int32
BIG = 1.0e9


def n_chunks(tstart):
    return (2 * (T - tstart)) // SL + NE


def phase_route(ph, dr, l, tstart):
    ntt = (T - tstart) // 128
    nch_tot = n_chunks(tstart)
    W = ntt * NE
    comb = ph.sb([128, ntt, NE], F32, "comb")
    ph.dma(comb[:], dr["comb"][tstart:T, :].rearrange("(j p) e -> p j e", p=128), [], [comb])
    ltri = ph.sb([128, 128], BF16, "ltri")
    ph.dma(ltri[:], dr["cst_ltri"][:, :], [], [ltri])
    iota = ph.sb([128, 128], F32, "iota")
    ph.dma(iota[:], dr["cst_iota"][:, :], [], [iota])
    pidx = ph.sb([128, 1], F32, "pidx")
    ph.dma(pidx[:], dr["cst_pidx"][:, :], [], [pidx])
    ones = ph.sb([128, 128], BF16, "ones")
    ph.memset("pool", ones[:], 1.0, [ones])
    M = ph.sb([128, ntt, NE], BF16, "M")
    Mf = ph.sb([128, ntt, NE], F32, "Mf")
    ph.ts("dve", Mf[:], comb[:], 0.0, None, ALU.is_gt, None, [comb], [Mf])
    ph.cp("dve", M[:], Mf[:], [Mf], [M])
    rank = ph.sb([128, ntt, NE], F32, "rank")
    tot = ph.sb([128, ntt, NE], F32, "tot")
    Mfl = M[:, :, :].rearrange("p j e -> p (j e)")
    for (dst, lhs, pbi) in ((rank, ltri, 0), (tot, ones, 4)):
        dfl = dst[:, :, :].rearrange("p j e -> p (j e)")
        for i, c0 in enumerate(range(0, W, 512)):
            n = min(512, W - c0)
            pb = ph.pb[pbi + i]
            ph.mm(pb[:, 0:n], lhs[:], Mfl[:, c0:c0 + n], True, True, [lhs, M], [pb])
            ph.cp("act", dfl[:, c0:c0 + n], pb[:, 0:n], [pb], [dst])
    carry = ph.sb([128, ntt + 1, NE], F32, "carry")
    ph.memset("dve", carry[:, 0, :], 0.0, [carry])
    for j in range(ntt):
        ph.tt("dve", carry[:, j + 1, :], carry[:, j, :], tot[:, j, :], ALU.add, [carry, tot], [carry])
    cnt = carry[:, ntt, :]
    NK = (T - tstart) // SL + 1
    thr = ph.sb([128, NK], F32, "thr")
    ph.ts("dve", thr[:], iota[:, 0:NK], float(SL), None, ALU.mult, None, [iota], [thr])
    cmpk = ph.sb([128, NE, NK], F32, "cmpk")
    ph.tt("dve", cmpk[:], cnt.unsqueeze(2).to_broadcast([128, NE, NK]), thr[:, :].unsqueeze(1).to_broadcast([128, NE, NK]), ALU.is_gt,
          [carry, thr], [cmpk])
    sc = ph.sb([128, 8, NE], F32, "sc")
    ph.add("dve", lambda e: e.tensor_reduce(out=sc[:, 0, :], in_=cmpk[:], axis=AX.X, op=ALU.add), _bufs([cmpk]), _bufs([sc]))
    ph.memset("dve", sc[:, 4, :], 1.0, [sc])
    ph.add("dve", lambda e: e.tensor_tensor_scan(out=sc[:, 1, :], data0=sc[:, 4, :], data1=sc[:, 0, :], initial=0.0,
                                                 op0=ALU.mult, op1=ALU.add), _bufs([sc]), _bufs([sc]))
    ph.tt("dve", sc[:, 2, :], sc[:, 1, :], sc[:, 0, :], ALU.subtract, [sc], [sc])
    ph.ts("dve", sc[:, 3, :], sc[:, 2, :], float(SL), None, ALU.mult, None, [sc], [sc])
    posv = ph.sb([128, ntt, NE], F32, "posv")
    ph.tt("dve", posv[:], rank[:], carry[:, 0:ntt, :], ALU.add, [rank, carry], [posv])
    ph.stt("dve", posv[:], posv[:], 1.0, sc[:, 3, :].unsqueeze(1).to_broadcast([128, ntt, NE]), ALU.add, ALU.add, [posv, sc], [posv])
    ph.tt("dve", posv[:], posv[:], Mf[:], ALU.mult, [posv, Mf], [posv])
    pw = ph.sb([128, ntt, 4], F32, "pw")
    eq1 = ph.sb([128, ntt, NE], F32, "eq1")
    tmp = ph.sb([128, ntt, NE], F32, "tmp")
    ph.add("dve", lambda e: e.tensor_reduce(out=pw[:, :, 0], in_=posv[:], axis=AX.X, op=ALU.max), _bufs([posv]), _bufs([pw]))
    ph.tt("dve", eq1[:], posv[:], pw[:, :, 0:1].to_broadcast([128, ntt, NE]), ALU.is_equal, [posv, pw], [eq1])
    ph.tt("dve", tmp[:], eq1[:], posv[:], ALU.mult, [eq1, posv], [tmp])
    ph.tt("dve", tmp[:], posv[:], tmp[:], ALU.subtract, [posv, tmp], [tmp])
    ph.add("dve", lambda e: e.tensor_reduce(out=pw[:, :, 1], in_=tmp[:], axis=AX.X, op=ALU.max), _bufs([tmp]), _bufs([pw]))
    ph.tt("dve", tmp[:], eq1[:], comb[:], ALU.mult, [eq1, comb], [tmp])
    ph.add("dve", lambda e: e.tensor_reduce(out=pw[:, :, 2], in_=tmp[:], axis=AX.X, op=ALU.add), _bufs([tmp]), _bufs([pw]))
    ph.add("dve", lambda e: e.tensor_reduce(out=pw[:, :, 3], in_=comb[:], axis=AX.X, op=ALU.add), _bufs([comb]), _bufs([pw]))
    ph.tt("dve", pw[:, :, 3], pw[:, :, 3], pw[:, :, 2], ALU.subtract, [pw], [pw])
    posf = ph.sb([128, ntt, 2], F32, "posf")
    ph.ts("dve", posf[:], pw[:, :, 0:2], -1.0, None, ALU.add, None, [pw], [posf])
    posi = ph.sb([128, ntt, 2], I32, "posi")
    ph.cp("dve", posi[:], posf[:], [posf], [posi])
    ph.dma(dr["posi"][tstart:T, :].rearrange("(j p) k -> p j k", p=128), posi[:], [posi], [])
    ph.dma(dr["posw"][tstart:T, :].rearrange("(j p) k -> p j k", p=128), pw[:, :, 2:4], [pw], [])
    cmpc = ph.sb([128, nch_tot, NE], F32, "cmpc")
    ph.tt("dve", cmpc[:], sc[:, 2, :].unsqueeze(1).to_broadcast([128, nch_tot, NE]),
          iota[:, 0:nch_tot].unsqueeze(2).to_broadcast([128, nch_tot, NE]), ALU.is_le, [sc, iota], [cmpc])
    wf = ph.sb([128, 4, nch_tot], F32, "wf")
    ph.add("dve", lambda e: e.tensor_reduce(out=wf[:, 0, :], in_=cmpc[:], axis=AX.X, op=ALU.add), _bufs([cmpc]), _bufs([wf]))
    ph.ts("dve", wf[:, 1, :], wf[:, 0, :], -1.0, 128.0, ALU.add, ALU.mult, [wf], [wf])
    ph.ts("dve", wf[:, 1, :], wf[:, 1, :], pidx[:, 0:1], None, ALU.add, None, [wf, pidx], [wf])
    ph.ts("dve", wf[:, 2, :], iota[:, 0:nch_tot], sc[:, 1, NE - 1:NE], BIG, ALU.is_ge, ALU.mult, [iota, sc], [wf])
    ph.tt("dve", wf[:, 1, :], wf[:, 1, :], wf[:, 2, :], ALU.add, [wf], [wf])
    widx = ph.sb([128, nch_tot], I32, "widx")
    ph.cp("dve", widx[:], wf[:, 1, :], [wf], [widx])
    ph.dma(dr["widx"][:, 0:nch_tot], widx[:], [widx], [])
    xs_t = Tl(dr["xsort"], "xsort")
    xt = [ph.sb([128, D], BF16, "xt") for _ in range(3)]
    bound = nch_tot * SL - 1
    for j in range(ntt):
        x_ = xt[j % 3]
        tok = tstart + j * 128
        ph.dma(x_[:], dr["h2tok"][tok:tok + 128, :], [], [x_])
        for k in range(2):
            ph.add("pool", lambda e, x_=x_, j=j, k=k: e.indirect_dma_start(
                out=dr["xsort"][:, :], out_offset=bass.IndirectOffsetOnAxis(ap=posi[:, j, k:k + 1], axis=0),
                in_=x_[:, :], in_offset=None, bounds_check=ph.breg(e, bound), oob_is_err=False), _bufs([x_, posi]), _bufs([]), dma=True)


def phase_moe_sparse(ph, dr, l, tstart, final):
    eps_const(ph)
    nch_tot = n_chunks(tstart)
    ntt = (T - tstart) // 128
    widx = ph.sb([128, nch_tot], I32, "widx")
    ph.dma(widx[:], dr["widx"][:, 0:nch_tot], [], [widx])
    identb = make_identity(ph, BF16, "identb")
    st = [[ph.sb([128, 2048], F32, "st") for _ in range(3)] for _ in range(2)]
    w13 = [ph.sb([128, 8, 512], BF16, "w13") for _ in range(2)]
    w2 = [ph.sb([128, 2, D], BF16, "w2") for _ in range(2)]
    for t_ in st[0] + st[1]:
        ph.memset("pool", t_[:], 0.0, [t_])
    xs = [ph.sb([128, D], BF16, "xs") for _ in range(3)]
    xT = [ph.sb([128, 8, SL], BF16, "xT") for _ in range(2)]
    sg = [ph.sb([128, 2, SL], F32, "sg") for _ in range(2)]
    he = [ph.sb([128, 2, SL], BF16, "he") for _ in range(2)]
    yb = [ph.sb([128, D], BF16, "yb") for _ in range(3)]
    srcs = (dr["w1r%d" % l], dr["w3r%d" % l], dr["w2r%d" % l])
    ctr = {"x": 0, "y": 0, "pd": 0, "pt": 0}

    def LOADW(c):
        for i in range(3):
            s_ = st[c % 2][i]
            ph.add("pool", lambda e, s_=s_, i=i: e.indirect_dma_start(
                out=s_[:, :], out_offset=None, in_=srcs[i][:, :],
                in_offset=bass.IndirectOffsetOnAxis(ap=widx[:, c:c + 1], axis=0), bounds_check=ph.breg(e, NE * 128 - 1), oob_is_err=False),
                _bufs([widx]), _bufs([s_]), dma=True)
        b13, b2 = w13[c % 2], w2[c % 2]
        ph.cp("dve", b13[:, :, 0:256], st[c % 2][0][:, :].rearrange("p (k n) -> p k n", n=256), [st[c % 2][0]], [b13])
        ph.cp("act", b13[:, :, 256:512], st[c % 2][1][:, :].rearrange("p (k n) -> p k n", n=256), [st[c % 2][1]], [b13])
        ph.cp("dve", b2[:, :, :], st[c % 2][2][:, :].rearrange("p (k n) -> p k n", n=D), [st[c % 2][2]], [b2])

    def LOADX(c):
        xT_ = xT[c % 2]
        for s4 in range(SL // 128):
            x_ = xs[ctr["x"] % 3]
            ctr["x"] += 1
            r0 = c * SL + s4 * 128
            ph.dma(x_[:], dr["xsort"][r0:r0 + 128, :], [], [x_])
            pb = ph.pb[6 + ctr["pt"] % 2]
            ctr["pt"] += 1
            pbv = pb.t.bitcast(BF16)
            for k in range(8):
                ph.tr(pbv[:, k * 128:(k + 1) * 128], x_[:, k * 128:(k + 1) * 128], identb[:], [x_, identb], [pb])
            ph.cp("act" if s4 % 2 else "dve", xT_[:, :, s4 * 128:(s4 + 1) * 128], pbv[:, :].rearrange("p (k t) -> p k t", t=128), [pb], [xT_])

    def UP(c):
        b13, xT_ = w13[c % 2], xT[c % 2]
        s_, h_ = sg[c % 2], he[c % 2]
        for m in range(2):
            pg, pu = ph.pb[2 * m], ph.pb[2 * m + 1]
            for k in range(8):
                ph.mm(pg[:, :], b13[:, k, m * 128:(m + 1) * 128], xT_[:, k, :], k == 0, k == 7, [b13, xT_], [pg])
            for k in range(8):
                ph.mm(pu[:, :], b13[:, k, 256 + m * 128:256 + (m + 1) * 128], xT_[:, k, :], k == 0, k == 7, [b13, xT_], [pu])
            ph.act(s_[:, m, :], pg[:, :], AF.Silu, [pg], [s_])
            ph.tt("dve", h_[:, m, :], pu[:, :], s_[:, m, :], ALU.mult, [pu, s_], [h_])

    def DN(c):
        b2, h_ = w2[c % 2], he[c % 2]
        for s4 in range(SL // 128):
            y_ = yb[ctr["y"] % 3]
            ctr["y"] += 1
            for nh in range(2):
                pd = ph.pb[4 + ctr["pd"] % 2]
                ctr["pd"] += 1
                for k in range(2):
                    ph.mm(pd[:, :], h_[:, k, s4 * 128:(s4 + 1) * 128], b2[:, k, nh * 512:(nh + 1) * 512], k == 0, k == 1, [h_, b2], [pd])
                ph.cp("act" if nh else "dve", y_[:, nh * 512:(nh + 1) * 512], pd[:, :], [pd], [y_])
            r0 = c * SL + s4 * 128
            ph.dma(dr["ysort"][r0:r0 + 128, :], y_[:], [y_], [])

    LOADW(0)
    LOADX(0)
    for c in range(nch_tot + 1):
        if c < nch_tot:
            UP(c)
        if c >= 1:
            DN(c - 1)
        if c + 1 < nch_tot:
            LOADW(c + 1)
            LOADX(c + 1)

    Gf = {}
    for row in ((0, 1) if tstart == 0 else (0,)):
        Gf[row] = load_bc(ph, dr, row, 5, "Gf")
    if final:
        gfin = ph.sb([128, D], F32, "gfin")
        ph.dma(gfin[:], dr["g_final"].rearrange("(o n) -> o n", o=1).partition_broadcast(128), [], [gfin])
    posi = ph.sb([128, ntt, 2], I32, "posi")
    posw = ph.sb([128, ntt, 2], F32, "posw")
    ph.dma(posi[:], dr["posi"][tstart:T, :].rearrange("(j p) k -> p j k", p=128), [], [posi])
    ph.dma(posw[:], dr["posw"][tstart:T, :].rearrange("(j p) k -> p j k", p=128), [], [posw])
    ya = [[ph.sb([128, D], BF16, "ya") for _ in range(2)] for _ in range(2)]
    for a_ in ya[0] + ya[1]:
        ph.memset("pool", a_[:], 0.0, [a_])
    acc = [ph.sb([128, D], F32, "acc") for _ in range(2)]
    xt = [ph.sb([128, D], F32, "xt") for _ in range(2)]
    junk = ph.sb([128, D], F32, "junk")
    ss = ph.sb([128, 1], F32, "ss")
    rstd = ph.sb([128, 1], F32, "rstd")
    ys_t = Tl(dr["ysort"], "ysort")
    bound = nch_tot * SL - 1
    for j in range(ntt):
        tok = tstart + j * 128
        row = 1 if tok < NCTX else 0
        for k in range(2):
            a_ = ya[j % 2][k]
            ph.add("pool", lambda e, a_=a_, j=j, k=k: e.indirect_dma_start(
                out=a_[:, :], out_offset=None, in_=dr["ysort"][:, :],
                in_offset=bass.IndirectOffsetOnAxis(ap=posi[:, j, k:k + 1], axis=0), bounds_check=ph.breg(e, bound), oob_is_err=False),
                _bufs([posi]), _bufs([a_]), dma=True)
        x_, ac = xt[j % 2], acc[j % 2]
        ph.dma(x_[:], dr["x2"][tok:tok + 128, :], [], [x_])
        ph.ts("dve", ac[:], ya[j % 2][0][:], posw[:, j, 0:1], None, ALU.mult, None, [ya[j % 2][0], posw], [ac])
        ph.stt("dve", ac[:], ya[j % 2][1][:], posw[:, j, 1:2], ac[:], ALU.mult, ALU.add, [ya[j % 2][1], posw, ac], [ac])
        ph.tt("dve", ac[:], ac[:], Gf[row][:], ALU.mult, [ac, Gf[row]], [ac])
        ph.tt("dve", x_[:], x_[:], ac[:], ALU.add, [x_, ac], [x_])
        if not final:
            ph.dma(dr["xres"][tok:tok + 128, :], x_[:], [x_], [])
        else:
            ph.act(junk[:], x_[:], AF.Square, [x_], [junk, ss], accum=ss[:, 0:1])
            ph.act(rstd[:, 0:1], ss[:, 0:1], AF.Sqrt, [ss], [rstd], bias=ph.epsc[:, 0:1], scale=1.0 / D)
            ph.recip(rstd[:, 0:1], rstd[:, 0:1], [rstd], [rstd])
            ph.stt("dve", x_[:], x_[:], rstd[:, 0:1], gfin[:], ALU.mult, ALU.mult, [x_, rstd, gfin], [x_])
            ph.dma(dr["out"][tok - NCTX:tok - NCTX + 128, :], x_[:], [x_], [])


WEIGHTS = [("w_mod", [2, D, 6 * D]), ("b_mod", [2, 6 * D]), ("g_mix", [2, D]), ("g_ffn", [2, D]), ("w_in", [2, D, DIN]),
           ("conv_w", [2, 4, 512]), ("conv_b", [2, 512]), ("lru_wa", [2, 2, 8, 64, 64]), ("lru_ba", [2, 2, 512]),
           ("lru_wi", [2, 2, 8, 64, 64]), ("lru_bi", [2, 2, 512]), ("lru_lambda", [2, 2, 512]), ("mla_gq", [2, 256]),
           ("mla_wuq", [2, 256, 768]), ("mla_gkv", [2, 128]), ("mla_wukv", [2, 128, 1024]), ("gqa_gq", [2, 64]),
           ("gqa_gk", [2, 64]), ("w_branch", [2, 3, 512, D]), ("w_out", [2, D, D]), ("moe_wg", [2, D, 4]), ("moe_bg", [2, 4]),
           ("moe_we", [2, D, 32]), ("moe_be", [2, 32]), ("g_final", [D])]
RELAID = [("w1r0", [NE * 128, 2048]), ("w3r0", [NE * 128, 2048]), ("w2r0", [NE * 128, 2048]),
          ("w1r1", [NE * 128, 2048]), ("w3r1", [NE * 128, 2048]), ("w2r1", [NE * 128, 2048])]

SCRATCH = [("modv", [2, 6 * D], F32), ("xres", [T, D], F32), ("x2", [T, D], F32), ("xrT", [512, T], BF16),
           ("rgT", [512, T], BF16), ("gatesT", [3 * D, T], BF16), ("kmT", [8, 96, T], BF16), ("qmT", [8, 96, T], BF16),
           ("vm", [T, 512], BF16), ("kgT", [2, 64, T], BF16), ("qgT", [8, 64, T], BF16), ("vg", [T, 128], BF16),
           ("yT", [3, 512, T], BF16), ("h2tok", [T, D], BF16), ("comb", [T, NE], F32),
           ("xsort", [(2 * T // SL + NE) * SL, D], BF16), ("ysort", [(2 * T // SL + NE) * SL, D], BF16),
           ("posi", [T, 2], I32), ("posw", [T, 2], F32), ("widx", [128, 2 * T // SL + NE], I32)]


def build_nc(phases=None, debug=()):
    nc = bass.Bass("TRN2", target_bir_lowering=False)
    dr = {}
    dr["xin"] = nc.dram_tensor("xin", [T, D], F32, kind="ExternalInput").ap()
    dr["cc"] = nc.dram_tensor("cc", [2, D], F32, kind="ExternalInput").ap()
    dr["ropem"] = nc.dram_tensor("ropem", [2, 32, T], BF16, kind="ExternalInput").ap()
    dr["ropeg"] = nc.dram_tensor("ropeg", [2, 64, T], BF16, kind="ExternalInput").ap()
    for nm, shp in WEIGHTS + RELAID:
        dr[nm] = nc.dram_tensor(nm, shp, F32, kind="ExternalInput").ap()
    dr["cst_ltri"] = nc.dram_tensor("cst_ltri", [128, 128], BF16, kind="ExternalInput").ap()
    dr["cst_iota"] = nc.dram_tensor("cst_iota", [128, 128], F32, kind="ExternalInput").ap()
    dr["cst_pidx"] = nc.dram_tensor("cst_pidx", [128, 1], F32, kind="ExternalInput").ap()
    dr["out"] = nc.dram_tensor("out", [SEQ, D], F32, kind="ExternalOutput").ap()
    for nm, shp, dt in SCRATCH:
        if nm in debug:
            dr[nm] = nc.dram_tensor(nm, shp, dt, kind="ExternalOutput").ap()
        else:
            dr[nm] = nc.dram_tensor(nm, shp, dt).ap()
    ps = nc.alloc_psum_tensor("ps", [128, 4096], F32)
    for l in range(2):
        xsrc = dr["xin"] if l == 0 else dr["xres"]
        last = l == 1
        tstart = NCTX if last else 0
        plan = [("mod", phase_mod, (dr, l)), ("inproj", phase_inproj, (dr, l, xsrc)), ("lru", phase_lru, (dr, l)),
                ("attn", phase_attn, (dr, l, not last)), ("merge", phase_merge, (dr, l, xsrc, tstart)),
                ("route", phase_route, (dr, l, tstart)), ("moe", phase_moe_sparse, (dr, l, tstart, last))]
        for nm, fn, args in plan:
            if phases is not None and (l, nm) not in phases:
                continue
            run_phase(nc, ps, fn, *args)
    return nc


def rope_consts():
    def tab(rot):
        q = rot // 4
        pos = np.arange(SEQ)
        row = (pos // 64).astype(np.float32)
        col = (pos % 64).astype(np.float32)
        freqs = (np.float32(10000.0) ** (-np.arange(q, dtype=np.float32) / np.float32(q))).astype(np.float32)
        ang = np.concatenate([row[:, None] * freqs, col[:, None] * freqs], axis=-1).astype(np.float32)
        cos, sin = np.cos(ang).T, np.sin(ang).T
        C = np.ones((rot, T), np.float32)
        S = np.zeros((rot, T), np.float32)
        C[:, NCTX:] = np.concatenate([cos, cos], axis=0)
        S[:, NCTX:] = np.concatenate([-sin, sin], axis=0)
        return np.stack([C, S]).astype(ml_dtypes.bfloat16)
    return tab(32), tab(64)


def host_shared(inputs):
    shared = {nm: np.ascontiguousarray(np.asarray(inputs[nm], np.float32)) for nm, _ in WEIGHTS}
    ropem, ropeg = rope_consts()
    shared["ropem"] = ropem
    shared["ropeg"] = ropeg
    for l in range(2):
        w1 = np.asarray(inputs["moe_w1"][l], np.float32).reshape(NE, 8, 128, DE).transpose(0, 2, 1, 3)
        w3 = np.asarray(inputs["moe_w3"][l], np.float32).reshape(NE, 8, 128, DE).transpose(0, 2, 1, 3)
        w2 = np.asarray(inputs["moe_w2"][l], np.float32).reshape(NE, 2, 128, D).transpose(0, 2, 1, 3)
        shared["w1r%d" % l] = np.ascontiguousarray(w1).reshape(NE * 128, 2048)
        shared["w3r%d" % l] = np.ascontiguousarray(w3).reshape(NE * 128, 2048)
        shared["w2r%d" % l] = np.ascontiguousarray(w2).reshape(NE * 128, 2048)
    shared["cst_ltri"] = np.triu(np.ones((128, 128), np.float32), 1).astype(ml_dtypes.bfloat16)
    shared["cst_iota"] = np.ascontiguousarray(np.broadcast_to(np.arange(128, dtype=np.float32)[None, :], (128, 128)))
    shared["cst_pidx"] = np.arange(128, dtype=np.float32).reshape(128, 1)
    return shared


_CACHE = {}


def kernel(**inputs):
    x = np.asarray(inputs["x"], np.float32)
    ctx = np.asarray(inputs["ctx"], np.float32)
    c = np.asarray(inputs["c"], np.float32)
    c_ctx = np.asarray(inputs["c_ctx"], np.float32)
    B = x.shape[0]
    if "nc" not in _CACHE:
        _CACHE["nc"] = build_nc()
    nc = _CACHE["nc"]
    shared = host_shared(inputs)
    in_maps = []
    for b in range(B):
        m = dict(shared)
        m["xin"] = np.ascontiguousarray(np.concatenate([ctx[b], x[b]], axis=0))
        m["cc"] = np.ascontiguousarray(np.stack([c[b], c_ctx], axis=0))
        in_maps.append(m)
    res = run_bass_kernel_spmd(nc, in_maps, core_ids=list(range(B)))
    return np.stack([np.asarray(r["out"], np.float32) for r in res.results], axis=0)
```

```python
import numpy as np
import ml_dtypes
import concourse.bass as bass
import concourse.mybir as mybir
from concourse.bass_utils import run_bass_kernel_spmd

F32 = mybir.dt.float32
BF16 = mybir.dt.bfloat16
AF = mybir.ActivationFunctionType
ALU = mybir.AluOpType
AX = mybir.AxisListType

D = 1024
NCTX = 256
SEQ = 4096
T = NCTX + SEQ
NT = T // 128
DIN = 5280
EPS = 1e-6
C_XR, C_CKV, C_KR, C_GK, C_GV, C_RG, C_CQ, C_GQ, C_MG = 0, 512, 640, 672, 800, 928, 1440, 1696, 2208
MLA_SCALE = 96 ** -0.5
GQA_SCALE = 64 ** -0.5
NE = 32
DE = 256
import os as _os
DBG_SKIP_ROUTER = bool(_os.environ.get('DBG_SKIP_ROUTER'))
DBG_STOP = int(_os.environ.get('DBG_STOP', '99'))


class Buf:
    __slots__ = ("name", "lastw", "readers")

    def __init__(self, name=""):
        self.name = name
        self.lastw = None
        self.readers = []


class Op:
    __slots__ = ("eng", "fn", "deps", "dma", "tok", "needed", "idx")


class Prog:
    COMPUTE = ("pe", "act", "dve", "pool")
    NPOOL = 12
    UID = 0

    def __init__(self, nc):
        self.nc = nc
        self.ops = []
        self.q = {k: [] for k in ("pe", "act", "dve", "pool", "sp")}

    def add(self, eng, fn, reads=(), writes=(), dma=False):
        op = Op()
        op.eng, op.fn, op.dma, op.tok, op.needed = eng, fn, dma, None, False
        op.idx = len(self.ops)
        deps = set()
        for b in reads:
            if b.lastw is not None:
                deps.add(b.lastw)
        for b in writes:
            if b.lastw is not None:
                deps.add(b.lastw)
            deps.update(b.readers)
        op.deps = deps
        for b in reads:
            if b in writes:
                continue
            if not dma:
                b.readers = [r for r in b.readers if self.ops[r].dma or self.ops[r].eng != eng]
            b.readers.append(op.idx)
        for b in writes:
            b.lastw = op.idx
            b.readers = []
        self.ops.append(op)
        self.q[eng].append(op)
        return op

    def emit(self):
        nc, ops = self.nc, self.ops
        for op in ops:
            for d in op.deps:
                dop = ops[d]
                if dop.eng == "pe" and op.eng == "pe" and not dop.dma and not op.dma:
                    continue
                dop.needed = True
        Prog.UID += 1
        u = Prog.UID
        sems = {k: nc.alloc_semaphore("s%d_%s" % (u, k)) for k in self.COMPUTE}
        dsem = {k: [nc.alloc_semaphore("d%d_%s_%d" % (u, k, i)) for i in range(self.NPOOL)] for k in self.q}
        cnt = {k: 0 for k in self.COMPUTE}
        dcnt = {k: 0 for k in self.q}
        prewait = {}
        for op in ops:
            if op.dma:
                k = dcnt[op.eng]
                dcnt[op.eng] += 1
                s = dsem[op.eng][k % self.NPOOL]
                op.tok = (s, 16 * (k // self.NPOOL + 1))
                if k >= self.NPOOL:
                    prewait[op.idx] = (s, 16 * (k // self.NPOOL))
            elif op.needed:
                cnt[op.eng] += 1
                op.tok = (sems[op.eng], cnt[op.eng])
        engines = {"pe": "tensor", "act": "scalar", "dve": "vector", "pool": "gpsimd", "sp": "sync"}
        with nc.Block() as block:
            def make(k):
                def body(e):
                    known = {}
                    for op in self.q[k]:
                        waits = []
                        if op.idx in prewait:
                            waits.append(prewait[op.idx])
                        for d in sorted(op.deps):
                            dop = ops[d]
                            if dop.tok is None:
                                continue
                            if dop.eng == "pe" and k == "pe" and not dop.dma and not op.dma:
                                continue
                            waits.append(dop.tok)
                        for (s, v) in waits:
                            if known.get(id(s), 0) >= v:
                                continue
                            known[id(s)] = v
                            e.wait_ge(s, v)
                        ins = op.fn(e)
                        if op.tok is not None:
                            ins.then_inc(op.tok[0], 16 if op.dma else 1)
                    if k == "sp":
                        for kk in self.q:
                            n = dcnt[kk]
                            for j in range(min(n, self.NPOOL)):
                                uses = (n - j + self.NPOOL - 1) // self.NPOOL
                                e.wait_ge(dsem[kk][j], 16 * uses)
                return body
            for k, attr in engines.items():
                getattr(block, attr)(make(k))


class Tl:
    def __init__(self, t, name=""):
        self.t = t
        self.b = Buf(name)

    def __getitem__(self, k):
        return self.t[k]


def _bufs(lst):
    return [x.b if isinstance(x, Tl) else x for x in lst]


class Ph:
    def __init__(self, nc, ps):
        self.nc = nc
        self.P = Prog(nc)
        self.ps = ps
        self.pb = [Tl(ps[:, i * 512:(i + 1) * 512], "pb%d" % i) for i in range(8)]
        self.n = 0
        Ph.UID += 1
        self.uid = Ph.UID

    UID = 0

    def sb(self, shape, dt=F32, name=None):
        self.n += 1
        t = self.nc.alloc_sbuf_tensor("%s_%d_%d" % (name or "t", self.uid, self.n), list(shape), dt)
        return Tl(t, name or "t")

    def add(self, eng, fn, r, w, dma=False):
        return self.P.add(eng, fn, _bufs(r), _bufs(w), dma=dma)

    def dma(self, out, in_, r=(), w=(), eng="sp", **kw):
        return self.add(eng, lambda e: e.dma_start(out=out, in_=in_, **kw), r, w, dma=True)

    def mm(self, out, lhsT, rhs, start, stop, r, w):
        return self.add("pe", lambda e: e.matmul(out, lhsT=lhsT, rhs=rhs, start=start, stop=stop), r, w)

    def tr(self, out, in_, ident, r, w):
        return self.add("pe", lambda e: e.transpose(out=out, in_=in_, identity=ident), r, w)

    def act(self, out, in_, func, r, w, bias=None, scale=None, accum=None):
        kw = {}
        if bias is not None:
            kw["bias"] = bias
        if scale is not None:
            kw["scale"] = scale
        if accum is not None:
            kw["accum_out"] = accum
        return self.add("act", lambda e: e.activation(out=out, in_=in_, func=func, **kw), r, w)

    def tt(self, eng, out, in0, in1, op, r, w):
        return self.add(eng, lambda e: e.tensor_tensor(out=out, in0=in0, in1=in1, op=op), r, w)

    def ts(self, eng, out, in0, s1, s2, op0, op1, r, w):
        if op1 is None:
            return self.add(eng, lambda e: e.tensor_scalar(out=out, in0=in0, scalar1=s1, scalar2=None, op0=op0), r, w)
        return self.add(eng, lambda e: e.tensor_scalar(out=out, in0=in0, scalar1=s1, scalar2=s2, op0=op0, op1=op1), r, w)

    def stt(self, eng, out, in0, sc, in1, op0, op1, r, w):
        return self.add(eng, lambda e: e.scalar_tensor_tensor(out=out, in0=in0, scalar=sc, in1=in1, op0=op0, op1=op1), r, w)

    def cp(self, eng, out, in_, r, w):
        if eng == "act":
            return self.add("act", lambda e: e.activation(out=out, in_=in_, func=AF.Copy), r, w)
        return self.add(eng, lambda e: e.tensor_copy(out=out, in_=in_), r, w)

    def memset(self, eng, ap, val, w):
        return self.add(eng, lambda e: e.memset(ap, val), [], w)

    def recip(self, out, in_, r, w):
        return self.add("dve", lambda e: e.reciprocal(out=out, in_=in_), r, w)

    def breg(self, e, val):
        if not hasattr(self, "_regs"):
            self._regs = {}
        if val not in self._regs:
            r = e.alloc_register("bnd_%d_%d" % (self.uid, val))
            e.reg_mov(r, val)
            self._regs[val] = r
        return self._regs[val]

    def finish(self):
        self.P.emit()


def run_phase(nc, ps, fn, *args):
    with nc.cleanup_on_exit():
        ph = Ph(nc, ps)
        fn(ph, *args)
        ph.finish()
        nc.all_engine_barrier()


def token_blocks(t0, t1, bs=512):
    out = []
    t = t0
    while t < t1:
        n = min(bs, t1 - t)
        out.append((t, n))
        t += n
    return out


def make_identity(ph, dt, name):
    idf = ph.sb([128, 128], F32, name + "f")
    ph.memset("pool", idf[:], 0.0, [idf])
    ph.add("pool", lambda e: e.affine_select(out=idf[:], in_=idf[:], pattern=[[-1, 128]], compare_op=ALU.not_equal,
                                            fill=1.0, base=0, channel_multiplier=1), [idf], [idf])
    if dt == F32:
        return idf
    idb = ph.sb([128, 128], dt, name)
    ph.cp("pool", idb[:], idf[:], [idf], [idb])
    return idb


def phase_mod(ph, dr, l):
    cc = ph.sb([128, 2, 8], F32, "cc")
    sc = ph.sb([128, 2, 8], F32, "sc")
    ph.dma(cc[:], dr["cc"].rearrange("r (p k) -> p r k", k=8), [], [cc])
    ph.act(sc[:], cc[:], AF.Silu, [cc], [sc])
    mods = ph.sb([2, 6 * D], F32, "mods")
    bm = ph.sb([2, 6 * D], F32, "bm")
    ph.dma(bm[:], dr["b_mod"][l:l + 1, :].partition_broadcast(2), [], [bm])
    gm = ph.sb([2, D], F32, "gm")
    gf = ph.sb([2, D], F32, "gf")
    ph.dma(gm[:], dr["g_mix"][l:l + 1, :].partition_broadcast(2), [], [gm])
    ph.dma(gf[:], dr["g_ffn"][l:l + 1, :].partition_broadcast(2), [], [gf])
    wv = dr["w_mod"][l].rearrange("(p k) n -> p k n", k=8)
    wb = [ph.sb([128, 8, 512], F32, "wb") for _ in range(2)]
    for nb in range(12):
        w = wb[nb % 2]
        ph.dma(w[:], wv[:, :, nb * 512:(nb + 1) * 512], [], [w])
        pb = ph.pb[nb % 2]
        for k in range(8):
            ph.mm(pb[0:2, :], sc[:, :, k], w[:, k, :], k == 0, k == 7, [sc, w], [pb])
        ph.tt("dve", mods[:, nb * 512:(nb + 1) * 512], pb[0:2, :], bm[:, nb * 512:(nb + 1) * 512], ALU.add, [pb, bm], [mods])
    ph.stt("dve", mods[:, D:2 * D], mods[:, D:2 * D], 1.0, gm[:], ALU.add, ALU.mult, [mods, gm], [mods])
    ph.stt("dve", mods[:, 4 * D:5 * D], mods[:, 4 * D:5 * D], 1.0, gf[:], ALU.add, ALU.mult, [mods, gf], [mods])
    ph.dma(dr["modv"][:, :], mods[:], [mods], [])


def load_bc(ph, dr, row, idx, name):
    t = ph.sb([128, D], F32, name)
    ph.dma(t[:], dr["modv"][row:row + 1, idx * D:(idx + 1) * D].partition_broadcast(128), [], [t])
    return t


def rms_modulate(ph, xt, A, B, hout, junk, ss, rstd, h32=None):
    ph.act(junk[:], xt[:], AF.Square, [xt], [junk, ss], accum=ss[:, 0:1])
    ph.act(rstd[:, 0:1], ss[:, 0:1], AF.Sqrt, [ss], [rstd], bias=ph.epsc[:, 0:1], scale=1.0 / D)
    ph.recip(rstd[:, 0:1], rstd[:, 0:1], [rstd], [rstd])
    tmp = h32 if h32 is not None else junk
    ph.stt("dve", tmp[:], xt[:], rstd[:, 0:1], A[:], ALU.mult, ALU.mult, [xt, rstd, A], [tmp])
    ph.tt("dve", hout[:], tmp[:], B[:], ALU.add, [tmp, B], [hout])


def eps_const(ph):
    ph.epsc = ph.sb([128, 1], F32, "eps")
    ph.memset("pool", ph.epsc[:], EPS, [ph.epsc])


def phase_inproj(ph, dr, l, xsrc):
    nc = ph.nc
    eps_const(ph)
    win = ph.sb([128, 8, DIN], BF16, "win")
    stg = [ph.sb([128, 1320], F32, "stg") for _ in range(2)]
    wv = dr["w_in"][l].rearrange("(k p) n -> p k n", p=128)
    i = 0
    for k in range(8):
        for c4 in range(4):
            s = stg[i % 2]
            ph.dma(s[:], wv[:, k, c4 * 1320:(c4 + 1) * 1320], [], [s])
            ph.cp("dve" if i % 2 == 0 else "pool", win[:, k, c4 * 1320:(c4 + 1) * 1320], s[:], [s], [win])
            i += 1
    wkr = ph.sb([128, 8, 96], BF16, "wkr")
    wkrs = ph.sb([128, 8, 96], BF16, "wkrs")
    ph.memset("pool", wkr[:], 0.0, [wkr])
    ph.memset("pool", wkrs[:], 0.0, [wkrs])
    ph.cp("pool", wkr[:, :, 64:96], win[:, :, C_KR:C_KR + 32], [win], [wkr])
    ph.cp("pool", wkrs[:, :, 64:80], win[:, :, C_KR + 16:C_KR + 32], [win], [wkrs])
    ph.cp("pool", wkrs[:, :, 80:96], win[:, :, C_KR:C_KR + 16], [win], [wkrs])
    wgks = ph.sb([128, 8, 128], BF16, "wgks")
    wgqs = ph.sb([128, 8, 512], BF16, "wgqs")
    for (dst, c0, n) in ((wgks, C_GK, 128), (wgqs, C_GQ, 512)):
        sv = win[:, :, c0:c0 + n].rearrange("p k (h two d) -> p k h two d", two=2, d=32)
        dv = dst[:, :, :].rearrange("p k (h two d) -> p k h two d", two=2, d=32)
        ph.cp("pool", dv[:, :, :, 0, :], sv[:, :, :, 1, :], [win], [dst])
        ph.cp("pool", dv[:, :, :, 1, :], sv[:, :, :, 0, :], [win], [dst])
    gq = ph.sb([128, 2], F32, "gq")
    ph.dma(gq[:], dr["mla_gq"][l].rearrange("(c p) -> p c", p=128), [], [gq], allow_slow_non_contiguous=True)
    gkv = ph.sb([128, 1], F32, "gkv")
    ph.dma(gkv[:], dr["mla_gkv"][l].rearrange("(p o) -> p o", o=1), [], [gkv])
    wuqf = ph.sb([128, 2, 768], F32, "wuqf")
    ph.dma(wuqf[:], dr["mla_wuq"][l].rearrange("(c p) n -> p c n", p=128), [], [wuqf])
    wuq = ph.sb([128, 2, 768], BF16, "wuq")
    wuqs = ph.sb([128, 2, 768], BF16, "wuqs")
    for c in range(2):
        ph.ts("dve", wuq[:, c, :], wuqf[:, c, :], gq[:, c:c + 1], None, ALU.mult, None, [wuqf, gq], [wuq])
    ph.cp("pool", wuqs[:], wuq[:], [wuq], [wuqs])
    v1 = wuq[:, :, :].rearrange("p c (h d) -> p c h d", d=96)
    v2 = wuqs[:, :, :].rearrange("p c (h d) -> p c h d", d=96)
    ph.cp("pool", v2[:, :, :, 64:80], v1[:, :, :, 80:96], [wuq], [wuqs])
    ph.cp("pool", v2[:, :, :, 80:96], v1[:, :, :, 64:80], [wuq], [wuqs])
    wkvf = ph.sb([128, 1024], F32, "wkvf")
    ph.dma(wkvf[:], dr["mla_wukv"][l], [], [wkvf])
    wkv = ph.sb([128, 2, 512], BF16, "wkv")
    sv = wkvf[:, :].rearrange("p (h two d) -> p two h d", two=2, d=64)
    for two in range(2):
        ph.ts("dve", wkv[:, two, :].rearrange("p (h d) -> p h d", d=64), sv[:, two, :, :], gkv[:, 0:1], None, ALU.mult, None,
              [wkvf, gkv], [wkv])
    ones = ph.sb([128, 128], BF16, "ones")
    ph.memset("pool", ones[:], 1.0, [ones])
    bones = ph.sb([128, 128], BF16, "bones")
    ph.memset("pool", bones[:], 0.0, [bones])
    ph.memset("pool", bones[0:64, 0:64], 1.0, [bones])
    ph.memset("pool", bones[64:128, 64:128], 1.0, [bones])
    ident = make_identity(ph, BF16, "ident")
    gcol = ph.sb([128, 4], F32, "gcol")
    for j, nm in ((0, "gqa_gq"), (2, "gqa_gk")):
        src = dr[nm][l].rearrange("(d o) -> d o", o=1)
        for hh in range(2):
            ph.dma(gcol[hh * 64:hh * 64 + 64, j:j + 1], src[0:64, :], [], [gcol])
            ph.dma(gcol[hh * 64:hh * 64 + 32, j + 1:j + 2], src[32:64, :], [], [gcol])
            ph.dma(gcol[hh * 64 + 32:hh * 64 + 64, j + 1:j + 2], src[0:32, :], [], [gcol])
    Al = load_bc(ph, dr, 0, 1, "Al")
    Bl = load_bc(ph, dr, 0, 0, "Bl")
    Ac = load_bc(ph, dr, 1, 1, "Ac")
    Bc = load_bc(ph, dr, 1, 0, "Bc")

    xt = [ph.sb([128, D], F32, "xt") for _ in range(2)]
    junk = ph.sb([128, D], F32, "junk")
    hb = [ph.sb([128, D], BF16, "hb") for _ in range(2)]
    ss = ph.sb([128, 1], F32, "ss")
    rstd = ph.sb([128, 1], F32, "rstd")
    hT = [ph.sb([128, 8, 512], BF16, "hT") for _ in range(2)]
    tabm = [ph.sb([96, 2, 512], BF16, "tabm") for _ in range(2)]
    tabg = [ph.sb([128, 2, 512], BF16, "tabg") for _ in range(2)]
    NOB = 6
    ob = [ph.sb([128, 512], BF16, "ob") for _ in range(NOB)]
    NF = 6
    fb = [ph.sb([128, 512], F32, "fb") for _ in range(NF)]
    nck = ph.sb([128, 512], BF16, "nck")
    ncq = [ph.sb([128, 512], BF16, "ncq") for _ in range(2)]
    ctr = {"ob": 0, "fb": 0, "pb": 0, "ev": 0}

    def nob():
        ctr["ob"] += 1
        return ob[ctr["ob"] % NOB]

    def nfb():
        ctr["fb"] += 1
        return fb[ctr["fb"] % NF]

    def npb():
        ctr["pb"] += 1
        return ph.pb[ctr["pb"] % 8]

    def evac_eng():
        ctr["ev"] += 1
        return "act" if ctr["ev"] % 2 == 0 else "dve"

    def proj(lhs_tile, c0, m, hTb, n, extra_r=()):
        pb = npb()
        for k in range(8):
            ph.mm(pb[0:m, 0:n], lhs_tile[:, k, c0:c0 + m], hTb[:, k, 0:n], k == 0, k == 7, [lhs_tile, hTb], [pb])
        return pb

    def store(dst_ap, src_tile, src_ap, eng="pool"):
        ph.dma(dst_ap, src_ap, [src_tile], [], eng=eng)

    blocks = token_blocks(0, T)

    def PREP(bi, t0, n):
        hTb = hT[bi % 2]
        tm = tabm[bi % 2]
        tg = tabg[bi % 2]
        ph.dma(tm[64:96, :, 0:n], dr["ropem"][:, :, t0:t0 + n].rearrange("a r t -> r a t"), [], [tm])
        for hh in range(2):
            ph.dma(tg[hh * 64:hh * 64 + 64, :, 0:n], dr["ropeg"][:, :, t0:t0 + n].rearrange("a r t -> r a t"), [], [tg])
        for j in range(n // 128):
            tok = t0 + j * 128
            x_ = xt[j % 2]
            h_ = hb[j % 2]
            ph.dma(x_[:], xsrc[tok:tok + 128, :], [], [x_])
            isctx = tok < NCTX
            rms_modulate(ph, x_, Ac if isctx else Al, Bc if isctx else Bl, h_, junk, ss, rstd)
            pb = npb()
            pbv = pb.t.bitcast(BF16)
            for k in range(8):
                ph.tr(pbv[:, k * 128:(k + 1) * 128], h_[:, k * 128:(k + 1) * 128], ident[:], [h_, ident], [pb])
            ph.cp(evac_eng(), hTb[:, :, j * 128:(j + 1) * 128], pbv[:, :].rearrange("p (k t) -> p k t", t=128), [pb], [hTb])

    def MAIN(bi, t0, n, part):
        hTb = hT[bi % 2]
        tm = tabm[bi % 2]
        tg = tabg[bi % 2]
        sl = slice(t0, t0 + n)
        if part == 1:
            for c in range(4):
                pb = proj(win, C_XR + c * 128, 128, hTb, n)
                o = nob()
                ph.cp(evac_eng(), o[:, 0:n], pb[:, 0:n], [pb], [o])
                store(dr["xrT"][c * 128:(c + 1) * 128, sl], o, o[:, 0:n])
            for c in range(4):
                pb = proj(win, C_RG + c * 128, 128, hTb, n)
                o = nob()
                ph.act(o[:, 0:n], pb[:, 0:n], AF.Gelu_apprx_tanh, [pb], [o])
                store(dr["rgT"][c * 128:(c + 1) * 128, sl], o, o[:, 0:n])
            for c in range(24):
                pb = proj(win, C_MG + c * 128, 128, hTb, n)
                o = nob()
                ph.act(o[:, 0:n], pb[:, 0:n], AF.Sigmoid, [pb], [o])
                store(dr["gatesT"][c * 128:(c + 1) * 128, sl], o, o[:, 0:n])
            return
        for j in range(n // 128):
            pb = npb()
            for k in range(8):
                ph.mm(pb[:, 0:128], hTb[:, k, j * 128:(j + 1) * 128], win[:, k, C_GV:C_GV + 128], k == 0, k == 7, [hTb, win], [pb])
            o = nob()
            ph.cp(evac_eng(), o[:, 0:128], pb[:, 0:128], [pb], [o])
            store(dr["vg"][t0 + j * 128:t0 + (j + 1) * 128, :], o, o[:, 0:128])

        def rstd_bc(sq_list, ones_t, count):
            pb = npb()
            for i_, sq in enumerate(sq_list):
                ph.mm(pb[:, 0:n], ones_t[:], sq[:, 0:n], i_ == 0, i_ == len(sq_list) - 1, [ones_t, sq], [pb])
            r_ = nfb()
            ph.act(r_[:, 0:n], pb[:, 0:n], AF.Sqrt, [pb], [r_], bias=ph.epsc[:, 0:1], scale=1.0 / count)
            ph.recip(r_[:, 0:n], r_[:, 0:n], [r_], [r_])
            return r_

        pa = proj(win, C_CKV, 128, hTb, n)
        sq = nob()
        ph.act(sq[:, 0:n], pa[:, 0:n], AF.Square, [pa], [sq])
        r_ = rstd_bc([sq], ones, 128)
        ph.tt("dve", nck[:, 0:n], pa[:, 0:n], r_[:, 0:n], ALU.mult, [pa, r_], [nck])
        for hp in range(4):
            pb = npb()
            ph.mm(pb[:, 0:n], wkv[:, 0, hp * 128:(hp + 1) * 128], nck[:, 0:n], True, True, [wkv, nck], [pb])
            o = nob()
            ph.cp(evac_eng(), o[:, 0:n], pb[:, 0:n], [pb], [o])
            for hh in range(2):
                store(dr["kmT"][2 * hp + hh, 0:64, sl], o, o[hh * 64:hh * 64 + 64, 0:n])
        for j in range(n // 128):
            pb = npb()
            ph.mm(pb[:, :], nck[:, j * 128:(j + 1) * 128], wkv[:, 1, :], True, True, [nck, wkv], [pb])
            o = nob()
            ph.cp(evac_eng(), o[:, :], pb[:, :], [pb], [o])
            store(dr["vm"][t0 + j * 128:t0 + (j + 1) * 128, :], o, o[:, :])

        def rope96(pa, pb_, dst_tile):
            t1 = nfb()
            t2 = nfb()
            ph.tt("dve", t1[64:96, 0:n], pa[64:96, 0:n], tm[64:96, 0, 0:n], ALU.mult, [pa, tm], [t1])
            ph.tt("dve", t2[64:96, 0:n], pb_[64:96, 0:n], tm[64:96, 1, 0:n], ALU.mult, [pb_, tm], [t2])
            ph.tt("pool", dst_tile[64:96, 0:n], t1[64:96, 0:n], t2[64:96, 0:n], ALU.add, [t1, t2], [dst_tile])

        pa = proj(wkr, 0, 96, hTb, n)
        pb_ = proj(wkrs, 0, 96, hTb, n)
        o = nob()
        rope96(pa, pb_, o)
        for h in range(8):
            store(dr["kmT"][h, 64:96, sl], o, o[64:96, 0:n], eng="sp" if h % 2 else "pool")
        pc = [proj(win, C_CQ + c * 128, 128, hTb, n) for c in range(2)]
        sqs = []
        for c in range(2):
            s_ = nob()
            ph.act(s_[:, 0:n], pc[c][:, 0:n], AF.Square, [pc[c]], [s_])
            sqs.append(s_)
        r_ = rstd_bc(sqs, ones, 256)
        for c in range(2):
            ph.tt("dve", ncq[c][:, 0:n], pc[c][:, 0:n], r_[:, 0:n], ALU.mult, [pc[c], r_], [ncq[c]])
        for h in range(8):
            pa = npb()
            pb_ = npb()
            for c in range(2):
                ph.mm(pa[0:96, 0:n], wuq[:, c, h * 96:(h + 1) * 96], ncq[c][:, 0:n], c == 0, c == 1, [wuq, ncq[c]], [pa])
            for c in range(2):
                ph.mm(pb_[0:96, 0:n], wuqs[:, c, h * 96:(h + 1) * 96], ncq[c][:, 0:n], c == 0, c == 1, [wuqs, ncq[c]], [pb_])
            o = nob()
            ph.cp("act", o[0:64, 0:n], pa[0:64, 0:n], [pa], [o])
            rope96(pa, pb_, o)
            store(dr["qmT"][h, :, sl], o, o[0:96, 0:n], eng="sp" if h % 2 else "pool")

        def gqa_chunk(c0, wsw, csw, gj, dst_fn):
            pa = proj(win, c0, 128, hTb, n)
            pb_ = proj(wsw, csw, 128, hTb, n)
            sq = nob()
            ph.act(sq[:, 0:n], pa[:, 0:n], AF.Square, [pa], [sq])
            r_ = rstd_bc([sq], bones, 64)
            t1 = nfb()
            t2 = nfb()
            ph.stt("dve", t1[:, 0:n], pa[:, 0:n], gcol[:, gj:gj + 1], tg[:, 0, 0:n], ALU.mult, ALU.mult, [pa, gcol, tg], [t1])
            ph.stt("dve", t2[:, 0:n], pb_[:, 0:n], gcol[:, gj + 1:gj + 2], tg[:, 1, 0:n], ALU.mult, ALU.mult, [pb_, gcol, tg], [t2])
            ph.tt("pool", t1[:, 0:n], t1[:, 0:n], t2[:, 0:n], ALU.add, [t1, t2], [t1])
            o = nob()
            ph.tt("pool", o[:, 0:n], t1[:, 0:n], r_[:, 0:n], ALU.mult, [t1, r_], [o])
            dst_fn(o)

        def st_gk(o):
            for hh in range(2):
                store(dr["kgT"][hh, :, sl], o, o[hh * 64:hh * 64 + 64, 0:n])
        gqa_chunk(C_GK, wgks, 0, 2, st_gk)
        for c in range(4):
            def st_gq(o, c=c):
                for hh in range(2):
                    store(dr["qgT"][2 * c + hh, :, sl], o, o[hh * 64:hh * 64 + 64, 0:n])
            gqa_chunk(C_GQ + c * 128, wgqs, c * 128, 0, st_gq)


    PREP(0, *blocks[0])
    for bi, (t0, n) in enumerate(blocks):
        MAIN(bi, t0, n, 1)
        if bi + 1 < len(blocks):
            PREP(bi + 1, *blocks[bi + 1])
        MAIN(bi, t0, n, 2)


def phase_lru(ph, dr, l):
    blocks = token_blocks(0, T)
    xp = ph.sb([128, T + 6], BF16, "xp")
    xc = ph.sb([128, T], F32, "xc")
    xcb = ph.sb([128, T], BF16, "xcb")
    rg = ph.sb([128, T], BF16, "rg")
    ysum = ph.sb([128, T], F32, "ysum")
    abufs = [ph.sb([128, T], F32, "abuf") for _ in range(2)]
    ibufs = [ph.sb([128, T], F32, "ibuf") for _ in range(2)]
    mbufs = [ph.sb([128, T], F32, "mbuf") for _ in range(2)]
    hbuf = ph.sb([128, T], F32, "hbuf")
    yo = ph.sb([128, T], BF16, "yo")
    for c in range(4):
        cs = slice(c * 128, (c + 1) * 128)
        ph.memset("pool", xp[:, 0:2], 0.0, [xp])
        ph.memset("pool", xp[:, 258:261], 0.0, [xp])
        ph.memset("pool", xp[:, T + 5:T + 6], 0.0, [xp])
        ph.dma(xp[:, 2:258], dr["xrT"][cs, 0:NCTX], [], [xp])
        ph.dma(xp[:, 261:261 + SEQ], dr["xrT"][cs, NCTX:T], [], [xp])
        cw = ph.sb([128, 4], F32, "cw")
        ph.dma(cw[:], dr["conv_w"][l].rearrange("j c -> c j")[cs, :], [], [cw], allow_slow_non_contiguous=True)
        cb = ph.sb([128, 1], F32, "cb")
        ph.dma(cb[:], dr["conv_b"][l].rearrange("(c o) -> c o", o=1)[cs, :], [], [cb])
        for (o0, i0, n) in ((0, 0, NCTX), (NCTX, 259, SEQ)):
            ph.ts("dve", xc[:, o0:o0 + n], xp[:, i0:i0 + n], cw[:, 0:1], cb[:, 0:1], ALU.mult, ALU.add, [xp, cw, cb], [xc])
            for j in range(1, 4):
                ph.stt("dve", xc[:, o0:o0 + n], xp[:, i0 + j:i0 + j + n], cw[:, j:j + 1], xc[:, o0:o0 + n],
                       ALU.mult, ALU.add, [xp, cw, xc], [xc])
        ph.cp("act", xcb[:], xc[:], [xc], [xcb])
        ph.dma(rg[:], dr["rgT"][cs, :], [], [rg])
        for d in range(2):
            abuf, ibuf, mbuf = abufs[d], ibufs[d], mbufs[d]
            wst = ph.sb([128, 2, 128], F32, "wst")
            ph.memset("pool", wst[:], 0.0, [wst])
            for g_, nm in enumerate(("lru_wa", "lru_wi")):
                for hb_ in range(2):
                    ph.dma(wst[hb_ * 64:hb_ * 64 + 64, g_, hb_ * 64:hb_ * 64 + 64], dr[nm][l, d, 2 * c + hb_], [wst], [wst])
            wbd = ph.sb([128, 2, 128], BF16, "wbd")
            ph.cp("pool", wbd[:], wst[:], [wst], [wbd])
            col = ph.sb([128, 8], F32, "col")
            for j, nm in enumerate(("lru_ba", "lru_bi", "lru_lambda")):
                ph.dma(col[:, j:j + 1], dr[nm][l, d].rearrange("(c o) -> c o", o=1)[cs, :], [], [col])
            ph.act(col[:, 4:5], col[:, 2:3], AF.Exp, [col], [col], scale=-1.0)
            ph.act(col[:, 5:6], col[:, 4:5], AF.Ln, [col], [col], bias=1.0)
            ph.ts("dve", col[:, 2:3], col[:, 5:6], -8.0, None, ALU.mult, None, [col], [col])
            ph.ts("dve", col[:, 3:4], col[:, 5:6], -16.0, None, ALU.mult, None, [col], [col])
            for bi, (t0, n) in enumerate(blocks):
                pr = ph.pb[(2 * bi) % 8]
                pi = ph.pb[(2 * bi + 1) % 8]
                ph.mm(pr[:, 0:n], wbd[:, 0, :], xcb[:, t0:t0 + n], True, True, [wbd, xcb], [pr])
                ph.mm(pi[:, 0:n], wbd[:, 1, :], xcb[:, t0:t0 + n], True, True, [wbd, xcb], [pi])
                ph.act(abuf[:, t0:t0 + n], pr[:, 0:n], AF.Sigmoid, [pr, col], [abuf], bias=col[:, 0:1])
                ph.act(ibuf[:, t0:t0 + n], pi[:, 0:n], AF.Sigmoid, [pi, col], [ibuf], bias=col[:, 1:2])
            ph.act(mbuf[:], abuf[:], AF.Exp, [abuf, col], [mbuf], scale=col[:, 3:4])
            ph.act(abuf[:], abuf[:], AF.Exp, [abuf, col], [abuf], scale=col[:, 2:3])
            ph.act(mbuf[:], mbuf[:], AF.Sqrt, [mbuf], [mbuf], bias=1.0, scale=-1.0)
            ph.tt("pool", ibuf[:], ibuf[:], mbuf[:], ALU.mult, [ibuf, mbuf], [ibuf])
            ph.tt("dve", ibuf[:], ibuf[:], xc[:], ALU.mult, [ibuf, xc], [ibuf])
            dst = ysum if d == 0 else hbuf
            if d == 0:
                ph.add("dve", lambda e, dst=dst, abuf=abuf, ibuf=ibuf: e.tensor_tensor_scan(out=dst[:, :], data0=abuf[:, :], data1=ibuf[:, :], initial=0.0,
                                                                      op0=ALU.mult, op1=ALU.add), [abuf, ibuf], [dst])
            else:
                ph.add("dve", lambda e, dst=dst, abuf=abuf, ibuf=ibuf: e.tensor_tensor_scan(out=dst[:, 0:NCTX][:, ::-1], data0=abuf[:, 0:NCTX][:, ::-1],
                                                                      data1=ibuf[:, 0:NCTX][:, ::-1], initial=0.0,
                                                                      op0=ALU.mult, op1=ALU.add), [abuf, ibuf], [dst])
                ph.add("dve", lambda e, dst=dst, abuf=abuf, ibuf=ibuf: e.tensor_tensor_scan(out=dst[:, NCTX:T][:, ::-1], data0=abuf[:, NCTX:T][:, ::-1],
                                                                      data1=ibuf[:, NCTX:T][:, ::-1], initial=dst[:, 0:1],
                                                                      op0=ALU.mult, op1=ALU.add), [abuf, ibuf, dst], [dst])
                ph.tt("pool", ysum[:], ysum[:], hbuf[:], ALU.add, [ysum, hbuf], [ysum])
        ph.tt("pool", yo[:], ysum[:], rg[:], ALU.mult, [ysum, rg], [yo])
        ph.dma(dr["yT"][0, cs, :], yo[:], [yo], [])


def phase_attn(ph, dr, l, with_ctx):
    NB = 2
    kT = [ph.sb([96, T], BF16, "kT") for _ in range(NB)]
    qT = [ph.sb([96, T], BF16, "qT") for _ in range(NB)]
    kTg = [ph.sb([128, T], BF16, "kTg") for _ in range(NB)]
    qTg = [ph.sb([128, T], BF16, "qTg") for _ in range(NB)]
    for t_ in kTg + qTg:
        ph.memset("pool", t_[64:128, :], 0.0, [t_])
    va = [ph.sb([128, NT, 128], BF16, "va") for _ in range(NB)]
    for v_ in va:
        ph.memset("pool", v_[:, :, 64:128], 1.0, [v_])
    NPT = 3
    GS = 3
    pT = [ph.sb([128, GS * 512], BF16, "pT") for _ in range(NPT)]
    osb = [ph.sb([64, 512], F32, "osb") for _ in range(2)]
    yo = [ph.sb([64, 512], BF16, "yo") for _ in range(2)]
    sp_ = [Tl(ph.ps[:, 0:1536], "S0"), Tl(ph.ps[:, 1536:3072], "S1")]
    acc = [ph.pb[6], ph.pb[7]]
    heads = [(br, h) for br in (1, 2) for h in range(8)]

    def load_head(hi):
        br, h = heads[hi]
        k_, q_, v_ = (kT if br == 1 else kTg)[hi % NB], (qT if br == 1 else qTg)[hi % NB], va[hi % NB]
        if br == 1:
            ph.dma(k_[0:96, :], dr["kmT"][h], [], [k_])
            ph.dma(q_[0:96, :], dr["qmT"][h], [], [q_])
            vsrc = dr["vm"][:, h * 64:(h + 1) * 64].rearrange("(c p) d -> p c d", p=128)
        else:
            ph.dma(k_[0:64, :], dr["kgT"][h // 4], [], [k_])
            ph.dma(q_[0:64, :], dr["qgT"][h], [], [q_])
            vsrc = dr["vg"][:, (h // 4) * 64:(h // 4 + 1) * 64].rearrange("(c p) d -> p c d", p=128)
        ph.dma(v_[:, 0:17, 0:64], vsrc[:, 0:17, :], [], [v_])
        ph.dma(v_[:, 17:NT, 0:64], vsrc[:, 17:NT, :], [], [v_])

    items = []
    blk = 0
    for hi, (br, h) in enumerate(heads):
        d = 96 if br == 1 else 64
        scale = MLA_SCALE if br == 1 else GQA_SCALE
        qblocks = [(t0, n, NT) for (t0, n) in token_blocks(NCTX, T)]
        if with_ctx:
            qblocks = [(0, NCTX, NCTX // 128)] + qblocks
        first = True
        for (t0, n, nkc) in qblocks:
            for g0 in range(0, nkc, GS):
                items.append(dict(hi=hi, br=br, h=h, d=d, scale=scale, t0=t0, n=n, nkc=nkc, g0=g0, gn=min(GS, nkc - g0), blk=blk,
                                  pre=first, last=(g0 + GS >= nkc), idx=len(items)))
                first = False
            blk += 1

    def S(it):
        i = it["idx"]
        k_, q_ = (kT if it["br"] == 1 else kTg)[it["hi"] % NB], (qT if it["br"] == 1 else qTg)[it["hi"] % NB]
        s_, p_ = sp_[i % 2], pT[i % NPT]
        n, t0, gn = it["n"], it["t0"], it["gn"]
        d = 96 if it["br"] == 1 else 128
        for u in range(gn):
            kc = it["g0"] + u
            ph.mm(s_[:, u * 512:u * 512 + n], k_[0:d, kc * 128:(kc + 1) * 128], q_[0:d, t0:t0 + n], True, True, [k_, q_], [s_])
        if n == 512:
            ph.act(p_[:, 0:gn * 512], s_[:, 0:gn * 512], AF.Exp, [s_], [p_], scale=it["scale"])
        else:
            sv = s_[:, :].rearrange("p (u t) -> p u t", u=GS)[:, 0:gn, 0:n]
            pv = p_[:, :].rearrange("p (u t) -> p u t", u=GS)[:, 0:gn, 0:n]
            ph.act(pv, sv, AF.Exp, [s_], [p_], scale=it["scale"])

    def PV(it):
        i = it["idx"]
        v_, p_ = va[it["hi"] % NB], pT[i % NPT]
        a_ = acc[it["blk"] % 2]
        n = it["n"]
        for u in range(it["gn"]):
            kc = it["g0"] + u
            ph.mm(a_[:, 0:n], v_[:, kc, :], p_[:, u * 512:u * 512 + n], kc == 0, kc == it["nkc"] - 1, [v_, p_], [a_])
        if it["last"]:
            o_, y_ = osb[it["blk"] % 2], yo[it["blk"] % 2]
            t0, h, br = it["t0"], it["h"], it["br"]
            ph.recip(o_[0:64, 0:n], a_[64:128, 0:n], [a_], [o_])
            ph.tt("dve", y_[:, 0:n], a_[0:64, 0:n], o_[0:64, 0:n], ALU.mult, [o_, a_], [y_])
            ph.dma(dr["yT"][br, h * 64:(h + 1) * 64, t0:t0 + n], y_[:, 0:n], [y_], [], eng="pool")

    load_head(0)
    N = len(items)
    for i in range(N + 1):
        if i < N:
            S(items[i])
        if 0 <= i - 1 < N:
            PV(items[i - 1])
        if i < N and items[i]["pre"] and items[i]["hi"] + 1 < len(heads):
            load_head(items[i]["hi"] + 1)


def phase_merge(ph, dr, l, xsrc, tstart):
    eps_const(ph)
    wbr = ph.sb([128, 3, 4, D], BF16, "wbr")
    wo = ph.sb([128, 8, D], BF16, "wo")
    stg = [ph.sb([128, 2, D], F32, "stg") for _ in range(2)]
    si = 0
    for k in range(3):
        for hf in range(2):
            s = stg[si % 2]
            ph.dma(s[:], dr["w_branch"][l, k].rearrange("(c p) n -> p c n", p=128)[:, hf * 2:(hf + 1) * 2, :], [], [s])
            ph.cp("dve" if si % 2 else "pool", wbr[:, k, hf * 2:(hf + 1) * 2, :], s[:], [s], [wbr])
            si += 1
    for hf in range(4):
        s = stg[si % 2]
        ph.dma(s[:], dr["w_out"][l].rearrange("(c p) n -> p c n", p=128)[:, hf * 2:(hf + 1) * 2, :], [], [s])
        ph.cp("dve" if si % 2 else "pool", wo[:, hf * 2:(hf + 1) * 2, :], s[:], [s], [wo])
        si += 1
    wr = ph.sb([128, 8, 36], F32, "wr")
    ph.dma(wr[:, :, 0:4], dr["moe_wg"][l].rearrange("(c p) n -> p c n", p=128), [], [wr])
    ph.dma(wr[:, :, 4:36], dr["moe_we"][l].rearrange("(c p) n -> p c n", p=128), [], [wr])
    br_ = ph.sb([128, 36], F32, "br")
    ph.dma(br_[:, 0:4], dr["moe_bg"][l:l + 1, :].partition_broadcast(128), [], [br_])
    ph.dma(br_[:, 4:36], dr["moe_be"][l:l + 1, :].partition_broadcast(128), [], [br_])
    wrb = ph.sb([128, 8, 36], BF16, "wrb")
    ph.cp("pool", wrb[:], wr[:], [wr], [wrb])
    identb = make_identity(ph, BF16, "identb")
    hb16 = [ph.sb([128, D], BF16, "hb16") for _ in range(2)]
    G = {}
    for row in ((0, 1) if tstart == 0 else (0,)):
        G[row] = (load_bc(ph, dr, row, 2, "Ga"), load_bc(ph, dr, row, 4, "Af"), load_bc(ph, dr, row, 3, "Bf"))
    yb = [ph.sb([128, 3, 4, 512], BF16, "yb") for _ in range(2)]
    gb = [ph.sb([128, 24, 512], BF16, "gb") for _ in range(1)]
    mg = [ph.sb([128, 8, 512], BF16, "mgd") for _ in range(2)]
    tmpf = [ph.sb([128, 512], F32, "tmpf") for _ in range(3)]
    accf = ph.sb([128, 512], F32, "accf")
    xt = [ph.sb([128, D], F32, "xt") for _ in range(2)]
    x2 = [ph.sb([128, D], F32, "x2") for _ in range(2)]
    junk = ph.sb([128, D], F32, "junk")
    hTb = [ph.sb([128, 8, 128], BF16, "hTb") for _ in range(2)]
    ss = ph.sb([128, 1], F32, "ss")
    rstd = ph.sb([128, 1], F32, "rstd")
    sm = ph.sb([128, 64], F32, "sm")
    lg = ph.sb([128, 36], F32, "lg")
    mk = ph.sb([128, 4], F32, "mk")
    es = ph.sb([128, 4, 8], F32, "es")
    e8 = ph.sb([128, 8], F32, "e8")
    m8 = ph.sb([128, 8], F32, "m8")
    cb_ = [ph.sb([128, 32], F32, "comb") for _ in range(2)]
    c1 = ph.sb([128, 32], F32, "c1")
    c2 = ph.sb([128, 32], F32, "c2")
    ctr = {"pb": 0, "t": 0}

    def npb():
        ctr["pb"] += 1
        return ph.pb[ctr["pb"] % 8]

    blocks = token_blocks(tstart, T)
    bufs_of_block = {}

    def PRO(bi, t0, n):
        sl = slice(t0, t0 + n)
        y_, g_, m_ = yb[bi % 2], gb[0], mg[bi % 2]
        for k in range(3):
            ph.dma(y_[:, k, :, 0:n], dr["yT"][k, :, sl].rearrange("(c p) t -> p c t", p=128), [], [y_])
        for k in range(3):
            ph.dma(g_[:, k * 8:(k + 1) * 8, 0:n], dr["gatesT"][k * D:(k + 1) * D, sl].rearrange("(c p) t -> p c t", p=128), [], [g_])
        for oc in range(8):
            for k in range(3):
                pb = npb()
                for c in range(4):
                    ph.mm(pb[:, 0:n], wbr[:, k, c, oc * 128:(oc + 1) * 128], y_[:, k, c, 0:n], c == 0, c == 3, [wbr, y_], [pb])
                if k == 0:
                    ph.tt("dve", accf[:, 0:n], pb[:, 0:n], g_[:, oc, 0:n], ALU.mult, [pb, g_], [accf])
                else:
                    tf = tmpf[ctr["t"] % 3]
                    ctr["t"] += 1
                    ph.tt("dve", tf[:, 0:n], pb[:, 0:n], g_[:, k * 8 + oc, 0:n], ALU.mult, [pb, g_], [tf])
                    if k == 1:
                        ph.tt("pool", accf[:, 0:n], accf[:, 0:n], tf[:, 0:n], ALU.add, [accf, tf], [accf])
                    else:
                        ph.tt("pool", m_[:, oc, 0:n], accf[:, 0:n], tf[:, 0:n], ALU.add, [accf, tf], [m_])

    items = []
    for bi, (t0, n) in enumerate(blocks):
        for j in range(n // 128):
            items.append(dict(bi=bi, t0=t0, n=n, j=j, gi=len(items)))

    def A(it):
        bi, t0, n, j, gi = it["bi"], it["t0"], it["n"], it["j"], it["gi"]
        if j == 0:
            PRO(bi, t0, n)
        m_ = mg[bi % 2]
        tok = t0 + j * 128
        row = 1 if tok < NCTX else 0
        Ga, Af, Bf = G[row]
        x_, xo = xt[gi % 2], x2[gi % 2]
        ph.dma(x_[:], xsrc[tok:tok + 128, :], [], [x_])
        for hf in range(2):
            pb = npb()
            for c in range(8):
                ph.mm(pb[:, :], m_[:, c, j * 128:(j + 1) * 128], wo[:, c, hf * 512:(hf + 1) * 512], c == 0, c == 7, [m_, wo], [pb])
            ph.tt("dve", junk[:, hf * 512:(hf + 1) * 512], pb[:, :], Ga[:, hf * 512:(hf + 1) * 512], ALU.mult, [pb, Ga], [junk])
        ph.tt("pool", xo[:], junk[:], x_[:], ALU.add, [junk, x_], [xo])
        ph.dma(dr["x2"][tok:tok + 128, :], xo[:], [xo], [], eng="pool")
        hbt = hb16[gi % 2]
        rms_modulate(ph, xo, Af, Bf, hbt, junk, ss, rstd)
        ph.dma(dr["h2tok"][tok:tok + 128, :], hbt[:], [hbt], [], eng="pool")

    def B(it):
        gi = it["gi"]
        tok = it["t0"] + it["j"] * 128
        hbt = hb16[gi % 2]
        hb_ = hTb[gi % 2]
        pb = npb()
        pbv = pb.t.bitcast(BF16)
        for c in range(8):
            ph.tr(pbv[:, c * 128:(c + 1) * 128], hbt[:, c * 128:(c + 1) * 128], identb[:], [hbt, identb], [pb])
        ph.cp("act", hb_[:, :, :], pbv[:, :].rearrange("p (c t) -> p c t", t=128), [pb], [hb_])
        pb = npb()
        for c in range(8):
            ph.mm(pb[:, 0:36], hb_[:, c, :], wrb[:, c, :], c == 0, c == 7, [hb_, wrb], [pb])
        ph.tt("dve", lg[:], pb[:, 0:36], br_[:], ALU.add, [pb, br_], [lg])
        ph.add("dve", lambda e: e.tensor_reduce(out=sm[:, 0:1], in_=lg[:, 0:4], axis=AX.X, op=ALU.max), _bufs([lg]), _bufs([sm]))
        ph.ts("dve", mk[:], lg[:, 0:4], sm[:, 0:1], None, ALU.is_equal, None, [lg, sm], [mk])
        ph.ts("dve", sm[:, 1:2], sm[:, 0:1], -1.0, None, ALU.mult, None, [sm], [sm])
        ph.act(sm[:, 8:12], lg[:, 0:4], AF.Exp, [lg, sm], [sm], bias=sm[:, 1:2], accum=sm[:, 2:3])
        ph.recip(sm[:, 3:4], sm[:, 2:3], [sm], [sm])
        ev = lg[:, 4:36].rearrange("p (g e) -> p g e", e=8)
        ph.tt("dve", es[:], ev, mk[:, :].unsqueeze(2).to_broadcast([128, 4, 8]), ALU.mult, [lg, mk], [es])
        ph.add("dve", lambda e: e.tensor_reduce(out=e8[:], in_=es[:].rearrange("p g e -> p e g"), axis=AX.X, op=ALU.add),
               _bufs([es]), _bufs([e8]))
        ph.add("dve", lambda e: e.max(out=m8[:], in_=e8[:]), _bufs([e8]), _bufs([m8]))
        ph.tt("dve", sm[:, 4:5], m8[:, 0:1], m8[:, 1:2], ALU.subtract, [m8], [sm])
        ph.act(sm[:, 5:6], sm[:, 4:5], AF.Sigmoid, [sm], [sm])
        ph.tt("dve", sm[:, 5:6], sm[:, 5:6], sm[:, 3:4], ALU.mult, [sm], [sm])
        ph.tt("dve", sm[:, 6:7], sm[:, 3:4], sm[:, 5:6], ALU.subtract, [sm], [sm])
        cbt = cb_[gi % 2]
        ph.ts("dve", c1[:], lg[:, 4:36], m8[:, 0:1], sm[:, 5:6], ALU.is_equal, ALU.mult, [lg, m8, sm], [c1])
        ph.ts("dve", c2[:], lg[:, 4:36], m8[:, 1:2], sm[:, 6:7], ALU.is_equal, ALU.mult, [lg, m8, sm], [c2])
        ph.tt("dve", c1[:], c1[:], c2[:], ALU.add, [c1, c2], [c1])
        ph.tt("dve", cbt[:].rearrange("p (g e) -> p g e", e=8), c1[:].rearrange("p (g e) -> p g e", e=8),
              mk[:, :].unsqueeze(2).to_broadcast([128, 4, 8]), ALU.mult, [c1, mk], [cbt])
        ph.dma(dr["comb"][tok:tok + 128, :], cbt[:], [cbt], [], eng="pool")

    N = len(items)
    for i in range(N + 1):
        if i < N:
            A(items[i])
        if i >= 1:
            B(items[i - 1])


def phase_moe(ph, dr, l, tstart, final):
    eps_const(ph)
    ntok = T - tstart
    half = ntok // 2
    assert half % 128 == 0
    nth = half // 128
    Gf = {}
    for row in ((0, 1) if tstart == 0 else (0,)):
        Gf[row] = load_bc(ph, dr, row, 5, "Gf")
    if final:
        gfin = ph.sb([128, D], F32, "gfin")
        ph.dma(gfin[:], dr["g_final"].rearrange("(o n) -> o n", o=1).partition_broadcast(128), [], [gfin])
    hT = ph.sb([128, 8, half], BF16, "hT")
    acc = ph.sb([128, nth, D], F32, "acc")
    comb = ph.sb([128, nth, NE], F32, "comb")
    s13 = [ph.sb([128, 8, 256], F32, "s13") for _ in range(2)]
    s2 = [ph.sb([128, 2, D], F32, "s2") for _ in range(1)]
    w13 = [ph.sb([128, 8, 512], BF16, "w13") for _ in range(2)]
    w2 = [ph.sb([128, 2, D], BF16, "w2") for _ in range(2)]
    sg = [ph.sb([128, 2, 512], F32, "sg") for _ in range(2)]
    he = [ph.sb([128, 2, 512], BF16, "he") for _ in range(2)]
    xt = [ph.sb([128, D], F32, "xt") for _ in range(2)]
    junk = ph.sb([128, D], F32, "junk")
    ss = ph.sb([128, 1], F32, "ss")
    rstd = ph.sb([128, 1], F32, "rstd")
    ctr = {"pd": 0}
    for hf in range(2):
        h0 = tstart + hf * half
        ph.dma(hT[:, :, :], dr["h2T"][:, h0:h0 + half].rearrange("(c p) t -> p c t", p=128), [], [hT])
        ph.dma(comb[:, :, :], dr["comb"][h0:h0 + half, :].rearrange("(j p) e -> p j e", p=128), [], [comb])

        def load_w(e_):
            a2, b13, b2 = s2[0], w13[e_ % 2], w2[e_ % 2]
            ph.dma(s13[0][:], dr["moe_w1"][l, e_].rearrange("(c p) n -> p c n", p=128), [], [s13[0]])
            ph.dma(s13[1][:], dr["moe_w3"][l, e_].rearrange("(c p) n -> p c n", p=128), [], [s13[1]])
            ph.dma(a2[:], dr["moe_w2"][l, e_].rearrange("(c p) n -> p c n", p=128), [], [a2])
            ph.cp("pool", b13[:, :, 0:256], s13[0][:], [s13[0]], [b13])
            ph.cp("pool", b13[:, :, 256:512], s13[1][:], [s13[1]], [b13])
            ph.cp("pool", b2[:], a2[:], [a2], [b2])

        items = []
        for e_ in range(NE):
            for bi_, (b0, n) in enumerate(token_blocks(0, half)):
                items.append(dict(e=e_, b0=b0, n=n, first=(bi_ == 0), idx=len(items)))

        def UP(it):
            i, e_, b0, n = it["idx"], it["e"], it["b0"], it["n"]
            b13 = w13[e_ % 2]
            s_, h_ = sg[i % 2], he[i % 2]
            for m in range(2):
                pg, pu = ph.pb[2 * m], ph.pb[2 * m + 1]
                for c in range(8):
                    ph.mm(pg[:, 0:n], b13[:, c, m * 128:(m + 1) * 128], hT[:, c, b0:b0 + n], c == 0, c == 7, [b13, hT], [pg])
                for c in range(8):
                    ph.mm(pu[:, 0:n], b13[:, c, 256 + m * 128:256 + (m + 1) * 128], hT[:, c, b0:b0 + n], c == 0, c == 7, [b13, hT], [pu])
                ph.act(s_[:, m, 0:n], pg[:, 0:n], AF.Silu, [pg], [s_])
                ph.tt("dve", h_[:, m, 0:n], pu[:, 0:n], s_[:, m, 0:n], ALU.mult, [pu, s_], [h_])

        def DN(it):
            i, e_, b0, n = it["idx"], it["e"], it["b0"], it["n"]
            b2 = w2[e_ % 2]
            h_ = he[i % 2]
            for j in range(n // 128):
                tj = (b0 // 128) + j
                for nh in range(2):
                    pd = ph.pb[4 + ctr["pd"] % 4]
                    ctr["pd"] += 1
                    for c in range(2):
                        ph.mm(pd[:, :], h_[:, c, j * 128:(j + 1) * 128], b2[:, c, nh * 512:(nh + 1) * 512], c == 0, c == 1, [h_, b2], [pd])
                    asl = acc[:, tj, nh * 512:(nh + 1) * 512]
                    if e_ == 0:
                        ph.ts("dve", asl, pd[:, :], comb[:, tj, e_:e_ + 1], None, ALU.mult, None, [pd, comb], [acc])
                    else:
                        ph.stt("dve", asl, pd[:, :], comb[:, tj, e_:e_ + 1], asl, ALU.mult, ALU.add, [pd, comb, acc], [acc])

        load_w(0)
        N = len(items)
        for i in range(N + 1):
            if i < N:
                UP(items[i])
            if i >= 1:
                DN(items[i - 1])
            if i < N and items[i]["first"] and items[i]["e"] + 1 < NE:
                load_w(items[i]["e"] + 1)
        for tj in range(nth):
            tok = h0 + tj * 128
            row = 1 if tok < NCTX else 0
            x_ = xt[tj % 2]
            ph.dma(x_[:], dr["x2"][tok:tok + 128, :], [], [x_])
            ph.tt("pool", acc[:, tj, :], acc[:, tj, :], Gf[row][:], ALU.mult, [acc, Gf[row]], [acc])
            ph.tt("pool", x_[:], x_[:], acc[:, tj, :], ALU.add, [x_, acc], [x_])
            if not final:
                ph.dma(dr["xres"][tok:tok + 128, :], x_[:], [x_], [], eng="pool")
            else:
                ph.act(junk[:], x_[:], AF.Square, [x_], [junk, ss], accum=ss[:, 0:1])
                ph.act(rstd[:, 0:1], ss[:, 0:1], AF.Sqrt, [ss], [rstd], bias=ph.epsc[:, 0:1], scale=1.0 / D)
                ph.recip(rstd[:, 0:1], rstd[:, 0:1], [rstd], [rstd])
                ph.stt("dve", x_[:], x_[:], rstd[:, 0:1], gfin[:], ALU.mult, ALU.mult, [x_, rstd, gfin], [x_])
                ph.dma(dr["out"][tok - NCTX:tok - NCTX + 128, :], x_[:], [x_], [], eng="pool")


SL = 512
I32 = mybir.dt.int32
BIG = 1.0e9


def n_chunks(tstart):
    return (2 * (T - tstart)) // SL + NE


def phase_route(ph, dr, l, tstart):
    ntt = (T - tstart) // 128
    nch_tot = n_chunks(tstart)
    W = ntt * NE
    comb = ph.sb([128, ntt, NE], F32, "comb")
    ph.dma(comb[:], dr["comb"][tstart:T, :].rearrange("(j p) e -> p j e", p=128), [], [comb])
    ltri = ph.sb([128, 128], BF16, "ltri")
    ph.dma(ltri[:], dr["cst_ltri"][:, :], [], [ltri])
    iota = ph.sb([128, 128], F32, "iota")
    ph.dma(iota[:], dr["cst_iota"][:, :], [], [iota])
    pidx = ph.sb([128, 1], F32, "pidx")
    ph.dma(pidx[:], dr["cst_pidx"][:, :], [], [pidx])
    ones = ph.sb([128, 128], BF16, "ones")
    ph.memset("pool", ones[:], 1.0, [ones])
    M = ph.sb([128, ntt, NE], BF16, "M")
    Mf = ph.sb([128, ntt, NE], F32, "Mf")
    ph.ts("dve", Mf[:], comb[:], 0.0, None, ALU.is_gt, None, [comb], [Mf])
    ph.cp("dve", M[:], Mf[:], [Mf], [M])
    rank = ph.sb([128, ntt, NE], F32, "rank")
    tot = ph.sb([128, ntt, NE], F32, "tot")
    Mfl = M[:, :, :].rearrange("p j e -> p (j e)")
    for (dst, lhs, pbi) in ((rank, ltri, 0), (tot, ones, 4)):
        dfl = dst[:, :, :].rearrange("p j e -> p (j e)")
        for i, c0 in enumerate(range(0, W, 512)):
            n = min(512, W - c0)
            pb = ph.pb[pbi + i]
            ph.mm(pb[:, 0:n], lhs[:], Mfl[:, c0:c0 + n], True, True, [lhs, M], [pb])
            ph.cp("act", dfl[:, c0:c0 + n], pb[:, 0:n], [pb], [dst])
    carry = ph.sb([128, ntt + 1, NE], F32, "carry")
    ph.memset("dve", carry[:, 0, :], 0.0, [carry])
    for j in range(ntt):
        ph.tt("dve", carry[:, j + 1, :], carry[:, j, :], tot[:, j, :], ALU.add, [carry, tot], [carry])
    cnt = carry[:, ntt, :]
    NK = (T - tstart) // SL + 1
    thr = ph.sb([128, NK], F32, "thr")
    ph.ts("dve", thr[:], iota[:, 0:NK], float(SL), None, ALU.mult, None, [iota], [thr])
    cmpk = ph.sb([128, NE, NK], F32, "cmpk")
    ph.tt("dve", cmpk[:], cnt.unsqueeze(2).to_broadcast([128, NE, NK]), thr[:, :].unsqueeze(1).to_broadcast([128, NE, NK]), ALU.is_gt,
          [carry, thr], [cmpk])
    sc = ph.sb([128, 8, NE], F32, "sc")
    ph.add("dve", lambda e: e.tensor_reduce(out=sc[:, 0, :], in_=cmpk[:], axis=AX.X, op=ALU.add), _bufs([cmpk]), _bufs([sc]))
    ph.memset("dve", sc[:, 4, :], 1.0, [sc])
    ph.add("dve", lambda e: e.tensor_tensor_scan(out=sc[:, 1, :], data0=sc[:, 4, :], data1=sc[:, 0, :], initial=0.0,
                                                 op0=ALU.mult, op1=ALU.add), _bufs([sc]), _bufs([sc]))
    ph.tt("dve", sc[:, 2, :], sc[:, 1, :], sc[:, 0, :], ALU.subtract, [sc], [sc])
    ph.ts("dve", sc[:, 3, :], sc[:, 2, :], float(SL), None, ALU.mult, None, [sc], [sc])
    posv = ph.sb([128, ntt, NE], F32, "posv")
    ph.tt("dve", posv[:], rank[:], carry[:, 0:ntt, :], ALU.add, [rank, carry], [posv])
    ph.stt("dve", posv[:], posv[:], 1.0, sc[:, 3, :].unsqueeze(1).to_broadcast([128, ntt, NE]), ALU.add, ALU.add, [posv, sc], [posv])
    ph.tt("dve", posv[:], posv[:], Mf[:], ALU.mult, [posv, Mf], [posv])
    pw = ph.sb([128, ntt, 4], F32, "pw")
    eq1 = ph.sb([128, ntt, NE], F32, "eq1")
    tmp = ph.sb([128, ntt, NE], F32, "tmp")
    ph.add("dve", lambda e: e.tensor_reduce(out=pw[:, :, 0], in_=posv[:], axis=AX.X, op=ALU.max), _bufs([posv]), _bufs([pw]))
    ph.tt("dve", eq1[:], posv[:], pw[:, :, 0:1].to_broadcast([128, ntt, NE]), ALU.is_equal, [posv, pw], [eq1])
    ph.tt("dve", tmp[:], eq1[:], posv[:], ALU.mult, [eq1, posv], [tmp])
    ph.tt("dve", tmp[:], posv[:], tmp[:], ALU.subtract, [posv, tmp], [tmp])
    ph.add("dve", lambda e: e.tensor_reduce(out=pw[:, :, 1], in_=tmp[:], axis=AX.X, op=ALU.max), _bufs([tmp]), _bufs([pw]))
    ph.tt("dve", tmp[:], eq1[:], comb[:], ALU.mult, [eq1, comb], [tmp])
    ph.add("dve", lambda e: e.tensor_reduce(out=pw[:, :, 2], in_=tmp[:], axis=AX.X, op=ALU.add), _bufs([tmp]), _bufs([pw]))
    ph.add("dve", lambda e: e.tensor_reduce(out=pw[:, :, 3], in_=comb[:], axis=AX.X, op=ALU.add), _bufs([comb]), _bufs([pw]))
    ph.tt("dve", pw[:, :, 3], pw[:, :, 3], pw[:, :, 2], ALU.subtract, [pw], [pw])
    posf = ph.sb([128, ntt, 2], F32, "posf")
    ph.ts("dve", posf[:], pw[:, :, 0:2], -1.0, None, ALU.add, None, [pw], [posf])
    posi = ph.sb([128, ntt, 2], I32, "posi")
    ph.cp("dve", posi[:], posf[:], [posf], [posi])
    ph.dma(dr["posi"][tstart:T, :].rearrange("(j p) k -> p j k", p=128), posi[:], [posi], [])
    ph.dma(dr["posw"][tstart:T, :].rearrange("(j p) k -> p j k", p=128), pw[:, :, 2:4], [pw], [])
    cmpc = ph.sb([128, nch_tot, NE], F32, "cmpc")
    ph.tt("dve", cmpc[:], sc[:, 2, :].unsqueeze(1).to_broadcast([128, nch_tot, NE]),
          iota[:, 0:nch_tot].unsqueeze(2).to_broadcast([128, nch_tot, NE]), ALU.is_le, [sc, iota], [cmpc])
    wf = ph.sb([128, 4, nch_tot], F32, "wf")
    ph.add("dve", lambda e: e.tensor_reduce(out=wf[:, 0, :], in_=cmpc[:], axis=AX.X, op=ALU.add), _bufs([cmpc]), _bufs([wf]))
    ph.ts("dve", wf[:, 1, :], wf[:, 0, :], -1.0, 128.0, ALU.add, ALU.mult, [wf], [wf])
    ph.ts("dve", wf[:, 1, :], wf[:, 1, :], pidx[:, 0:1], None, ALU.add, None, [wf, pidx], [wf])
    ph.ts("dve", wf[:, 2, :], iota[:, 0:nch_tot], sc[:, 1, NE - 1:NE], BIG, ALU.is_ge, ALU.mult, [iota, sc], [wf])
    ph.tt("dve", wf[:, 1, :], wf[:, 1, :], wf[:, 2, :], ALU.add, [wf], [wf])
    widx = ph.sb([128, nch_tot], I32, "widx")
    ph.cp("dve", widx[:], wf[:, 1, :], [wf], [widx])
    ph.dma(dr["widx"][:, 0:nch_tot], widx[:], [widx], [])
    xs_t = Tl(dr["xsort"], "xsort")
    xt = [ph.sb([128, D], BF16, "xt") for _ in range(3)]
    bound = nch_tot * SL - 1
    for j in range(ntt):
        x_ = xt[j % 3]
        tok = tstart + j * 128
        ph.dma(x_[:], dr["h2tok"][tok:tok + 128, :], [], [x_])
        for k in range(2):
            ph.add("pool", lambda e, x_=x_, j=j, k=k: e.indirect_dma_start(
                out=dr["xsort"][:, :], out_offset=bass.IndirectOffsetOnAxis(ap=posi[:, j, k:k + 1], axis=0),
                in_=x_[:, :], in_offset=None, bounds_check=ph.breg(e, bound), oob_is_err=False), _bufs([x_, posi]), _bufs([]), dma=True)


def phase_moe_sparse(ph, dr, l, tstart, final):
    eps_const(ph)
    nch_tot = n_chunks(tstart)
    ntt = (T - tstart) // 128
    widx = ph.sb([128, nch_tot], I32, "widx")
    ph.dma(widx[:], dr["widx"][:, 0:nch_tot], [], [widx])
    identb = make_identity(ph, BF16, "identb")
    st = [[ph.sb([128, 2048], F32, "st") for _ in range(3)] for _ in range(2)]
    w13 = [ph.sb([128, 8, 512], BF16, "w13") for _ in range(2)]
    w2 = [ph.sb([128, 2, D], BF16, "w2") for _ in range(2)]
    for t_ in st[0] + st[1]:
        ph.memset("pool", t_[:], 0.0, [t_])
    xs = [ph.sb([128, D], BF16, "xs") for _ in range(3)]
    xT = [ph.sb([128, 8, SL], BF16, "xT") for _ in range(2)]
    sg = [ph.sb([128, 2, SL], F32, "sg") for _ in range(2)]
    he = [ph.sb([128, 2, SL], BF16, "he") for _ in range(2)]
    yb = [ph.sb([128, D], BF16, "yb") for _ in range(3)]
    srcs = (dr["w1r%d" % l], dr["w3r%d" % l], dr["w2r%d" % l])
    ctr = {"x": 0, "y": 0, "pd": 0, "pt": 0}

    def LOADW(c):
        for i in range(3):
            s_ = st[c % 2][i]
            ph.add("pool", lambda e, s_=s_, i=i: e.indirect_dma_start(
                out=s_[:, :], out_offset=None, in_=srcs[i][:, :],
                in_offset=bass.IndirectOffsetOnAxis(ap=widx[:, c:c + 1], axis=0), bounds_check=ph.breg(e, NE * 128 - 1), oob_is_err=False),
                _bufs([widx]), _bufs([s_]), dma=True)
        b13, b2 = w13[c % 2], w2[c % 2]
        ph.cp("dve", b13[:, :, 0:256], st[c % 2][0][:, :].rearrange("p (k n) -> p k n", n=256), [st[c % 2][0]], [b13])
        ph.cp("act", b13[:, :, 256:512], st[c % 2][1][:, :].rearrange("p (k n) -> p k n", n=256), [st[c % 2][1]], [b13])
        ph.cp("dve", b2[:, :, :], st[c % 2][2][:, :].rearrange("p (k n) -> p k n", n=D), [st[c % 2][2]], [b2])

    def LOADX(c):
        xT_ = xT[c % 2]
        for s4 in range(SL // 128):
            x_ = xs[ctr["x"] % 3]
            ctr["x"] += 1
            r0 = c * SL + s4 * 128
            ph.dma(x_[:], dr["xsort"][r0:r0 + 128, :], [], [x_])
            pb = ph.pb[6 + ctr["pt"] % 2]
            ctr["pt"] += 1
            pbv = pb.t.bitcast(BF16)
            for k in range(8):
                ph.tr(pbv[:, k * 128:(k + 1) * 128], x_[:, k * 128:(k + 1) * 128], identb[:], [x_, identb], [pb])
            ph.cp("act" if s4 % 2 else "dve", xT_[:, :, s4 * 128:(s4 + 1) * 128], pbv[:, :].rearrange("p (k t) -> p k t", t=128), [pb], [xT_])

    def UP(c):
        b13, xT_ = w13[c % 2], xT[c % 2]
        s_, h_ = sg[c % 2], he[c % 2]
        for m in range(2):
            pg, pu = ph.pb[2 * m], ph.pb[2 * m + 1]
            for k in range(8):
                ph.mm(pg[:, :], b13[:, k, m * 128:(m + 1) * 128], xT_[:, k, :], k == 0, k == 7, [b13, xT_], [pg])
            for k in range(8):
                ph.mm(pu[:, :], b13[:, k, 256 + m * 128:256 + (m + 1) * 128], xT_[:, k, :], k == 0, k == 7, [b13, xT_], [pu])
            ph.act(s_[:, m, :], pg[:, :], AF.Silu, [pg], [s_])
            ph.tt("dve", h_[:, m, :], pu[:, :], s_[:, m, :], ALU.mult, [pu, s_], [h_])

    def DN(c):
        b2, h_ = w2[c % 2], he[c % 2]
        for s4 in range(SL // 128):
            y_ = yb[ctr["y"] % 3]
            ctr["y"] += 1
            for nh in range(2):
                pd = ph.pb[4 + ctr["pd"] % 2]
                ctr["pd"] += 1
                for k in range(2):
                    ph.mm(pd[:, :], h_[:, k, s4 * 128:(s4 + 1) * 128], b2[:, k, nh * 512:(nh + 1) * 512], k == 0, k == 1, [h_, b2], [pd])
                ph.cp("act" if nh else "dve", y_[:, nh * 512:(nh + 1) * 512], pd[:, :], [pd], [y_])
            r0 = c * SL + s4 * 128
            ph.dma(dr["ysort"][r0:r0 + 128, :], y_[:], [y_], [])

    LOADW(0)
    LOADX(0)
    for c in range(nch_tot + 1):
        if c < nch_tot:
            UP(c)
        if c >= 1:
            DN(c - 1)
        if c + 1 < nch_tot:
            LOADW(c + 1)
            LOADX(c + 1)

    Gf = {}
    for row in ((0, 1) if tstart == 0 else (0,)):
        Gf[row] = load_bc(ph, dr, row, 5, "Gf")
    if final:
        gfin = ph.sb([128, D], F32, "gfin")
        ph.dma(gfin[:], dr["g_final"].rearrange("(o n) -> o n", o=1).partition_broadcast(128), [], [gfin])
    posi = ph.sb([128, ntt, 2], I32, "posi")
    posw = ph.sb([128, ntt, 2], F32, "posw")
    ph.dma(posi[:], dr["posi"][tstart:T, :].rearrange("(j p) k -> p j k", p=128), [], [posi])
    ph.dma(posw[:], dr["posw"][tstart:T, :].rearrange("(j p) k -> p j k", p=128), [], [posw])
    ya = [[ph.sb([128, D], BF16, "ya") for _ in range(2)] for _ in range(2)]
    for a_ in ya[0] + ya[1]:
        ph.memset("pool", a_[:], 0.0, [a_])
    acc = [ph.sb([128, D], F32, "acc") for _ in range(2)]
    xt = [ph.sb([128, D], F32, "xt") for _ in range(2)]
    junk = ph.sb([128, D], F32, "junk")
    ss = ph.sb([128, 1], F32, "ss")
    rstd = ph.sb([128, 1], F32, "rstd")
    ys_t = Tl(dr["ysort"], "ysort")
    bound = nch_tot * SL - 1
    for j in range(ntt):
        tok = tstart + j * 128
        row = 1 if tok < NCTX else 0
        for k in range(2):
            a_ = ya[j % 2][k]
            ph.add("pool", lambda e, a_=a_, j=j, k=k: e.indirect_dma_start(
                out=a_[:, :], out_offset=None, in_=dr["ysort"][:, :],
                in_offset=bass.IndirectOffsetOnAxis(ap=posi[:, j, k:k + 1], axis=0), bounds_check=ph.breg(e, bound), oob_is_err=False),
                _bufs([posi]), _bufs([a_]), dma=True)
        x_, ac = xt[j % 2], acc[j % 2]
        ph.dma(x_[:], dr["x2"][tok:tok + 128, :], [], [x_])
        ph.ts("dve", ac[:], ya[j % 2][0][:], posw[:, j, 0:1], None, ALU.mult, None, [ya[j % 2][0], posw], [ac])
        ph.stt("dve", ac[:], ya[j % 2][1][:], posw[:, j, 1:2], ac[:], ALU.mult, ALU.add, [ya[j % 2][1], posw, ac], [ac])
        ph.tt("dve", ac[:], ac[:], Gf[row][:], ALU.mult, [ac, Gf[row]], [ac])
        ph.tt("dve", x_[:], x_[:], ac[:], ALU.add, [x_, ac], [x_])
        if not final:
            ph.dma(dr["xres"][tok:tok + 128, :], x_[:], [x_], [])
        else:
            ph.act(junk[:], x_[:], AF.Square, [x_], [junk, ss], accum=ss[:, 0:1])
            ph.act(rstd[:, 0:1], ss[:, 0:1], AF.Sqrt, [ss], [rstd], bias=ph.epsc[:, 0:1], scale=1.0 / D)
            ph.recip(rstd[:, 0:1], rstd[:, 0:1], [rstd], [rstd])
            ph.stt("dve", x_[:], x_[:], rstd[:, 0:1], gfin[:], ALU.mult, ALU.mult, [x_, rstd, gfin], [x_])
            ph.dma(dr["out"][tok - NCTX:tok - NCTX + 128, :], x_[:], [x_], [])


WEIGHTS = [("w_mod", [2, D, 6 * D]), ("b_mod", [2, 6 * D]), ("g_mix", [2, D]), ("g_ffn", [2, D]), ("w_in", [2, D, DIN]),
           ("conv_w", [2, 4, 512]), ("conv_b", [2, 512]), ("lru_wa", [2, 2, 8, 64, 64]), ("lru_ba", [2, 2, 512]),
           ("lru_wi", [2, 2, 8, 64, 64]), ("lru_bi", [2, 2, 512]), ("lru_lambda", [2, 2, 512]), ("mla_gq", [2, 256]),
           ("mla_wuq", [2, 256, 768]), ("mla_gkv", [2, 128]), ("mla_wukv", [2, 128, 1024]), ("gqa_gq", [2, 64]),
           ("gqa_gk", [2, 64]), ("w_branch", [2, 3, 512, D]), ("w_out", [2, D, D]), ("moe_wg", [2, D, 4]), ("moe_bg", [2, 4]),
           ("moe_we", [2, D, 32]), ("moe_be", [2, 32]), ("g_final", [D])]
RELAID = [("w1r0", [NE * 128, 2048]), ("w3r0", [NE * 128, 2048]), ("w2r0", [NE * 128, 2048]),
          ("w1r1", [NE * 128, 2048]), ("w3r1", [NE * 128, 2048]), ("w2r1", [NE * 128, 2048])]

SCRATCH = [("modv", [2, 6 * D], F32), ("xres", [T, D], F32), ("x2", [T, D], F32), ("xrT", [512, T], BF16),
           ("rgT", [512, T], BF16), ("gatesT", [3 * D, T], BF16), ("kmT", [8, 96, T], BF16), ("qmT", [8, 96, T], BF16),
           ("vm", [T, 512], BF16), ("kgT", [2, 64, T], BF16), ("qgT", [8, 64, T], BF16), ("vg", [T, 128], BF16),
           ("yT", [3, 512, T], BF16), ("h2tok", [T, D], BF16), ("comb", [T, NE], F32),
           ("xsort", [(2 * T // SL + NE) * SL, D], BF16), ("ysort", [(2 * T // SL + NE) * SL, D], BF16),
           ("posi", [T, 2], I32), ("posw", [T, 2], F32), ("widx", [128, 2 * T // SL + NE], I32)]


def build_nc(phases=None, debug=()):
    nc = bass.Bass("TRN2", target_bir_lowering=False)
    dr = {}
    dr["xin"] = nc.dram_tensor("xin", [T, D], F32, kind="ExternalInput").ap()
    dr["cc"] = nc.dram_tensor("cc", [2, D], F32, kind="ExternalInput").ap()
    dr["ropem"] = nc.dram_tensor("ropem", [2, 32, T], BF16, kind="ExternalInput").ap()
    dr["ropeg"] = nc.dram_tensor("ropeg", [2, 64, T], BF16, kind="ExternalInput").ap()
    for nm, shp in WEIGHTS + RELAID:
        dr[nm] = nc.dram_tensor(nm, shp, F32, kind="ExternalInput").ap()
    dr["cst_ltri"] = nc.dram_tensor("cst_ltri", [128, 128], BF16, kind="ExternalInput").ap()
    dr["cst_iota"] = nc.dram_tensor("cst_iota", [128, 128], F32, kind="ExternalInput").ap()
    dr["cst_pidx"] = nc.dram_tensor("cst_pidx", [128, 1], F32, kind="ExternalInput").ap()
    dr["out"] = nc.dram_tensor("out", [SEQ, D], F32, kind="ExternalOutput").ap()
    for nm, shp, dt in SCRATCH:
        if nm in debug:
            dr[nm] = nc.dram_tensor(nm, shp, dt, kind="ExternalOutput").ap()
        else:
            dr[nm] = nc.dram_tensor(nm, shp, dt).ap()
    ps = nc.alloc_psum_tensor("ps", [128, 4096], F32)
    for l in range(2):
        xsrc = dr["xin"] if l == 0 else dr["xres"]
        last = l == 1
        tstart = NCTX if last else 0
        plan = [("mod", phase_mod, (dr, l)), ("inproj", phase_inproj, (dr, l, xsrc)), ("lru", phase_lru, (dr, l)),
                ("attn", phase_attn, (dr, l, not last)), ("merge", phase_merge, (dr, l, xsrc, tstart)),
                ("route", phase_route, (dr, l, tstart)), ("moe", phase_moe_sparse, (dr, l, tstart, last))]
        for nm, fn, args in plan:
            if phases is not None and (l, nm) not in phases:
                continue
            run_phase(nc, ps, fn, *args)
    return nc


def rope_consts():
    def tab(rot):
        q = rot // 4
        pos = np.arange(SEQ)
        row = (pos // 64).astype(np.float32)
        col = (pos % 64).astype(np.float32)
        freqs = (np.float32(10000.0) ** (-np.arange(q, dtype=np.float32) / np.float32(q))).astype(np.float32)
        ang = np.concatenate([row[:, None] * freqs, col[:, None] * freqs], axis=-1).astype(np.float32)
        cos, sin = np.cos(ang).T, np.sin(ang).T
        C = np.ones((rot, T), np.float32)
        S = np.zeros((rot, T), np.float32)
        C[:, NCTX:] = np.concatenate([cos, cos], axis=0)
        S[:, NCTX:] = np.concatenate([-sin, sin], axis=0)
        return np.stack([C, S]).astype(ml_dtypes.bfloat16)
    return tab(32), tab(64)


def host_shared(inputs):
    shared = {nm: np.ascontiguousarray(np.asarray(inputs[nm], np.float32)) for nm, _ in WEIGHTS}
    ropem, ropeg = rope_consts()
    shared["ropem"] = ropem
    shared["ropeg"] = ropeg
    for l in range(2):
        w1 = np.asarray(inputs["moe_w1"][l], np.float32).reshape(NE, 8, 128, DE).transpose(0, 2, 1, 3)
        w3 = np.asarray(inputs["moe_w3"][l], np.float32).reshape(NE, 8, 128, DE).transpose(0, 2, 1, 3)
        w2 = np.asarray(inputs["moe_w2"][l], np.float32).reshape(NE, 2, 128, D).transpose(0, 2, 1, 3)
        shared["w1r%d" % l] = np.ascontiguousarray(w1).reshape(NE * 128, 2048)
        shared["w3r%d" % l] = np.ascontiguousarray(w3).reshape(NE * 128, 2048)
        shared["w2r%d" % l] = np.ascontiguousarray(w2).reshape(NE * 128, 2048)
    shared["cst_ltri"] = np.triu(np.ones((128, 128), np.float32), 1).astype(ml_dtypes.bfloat16)
    shared["cst_iota"] = np.ascontiguousarray(np.broadcast_to(np.arange(128, dtype=np.float32)[None, :], (128, 128)))
    shared["cst_pidx"] = np.arange(128, dtype=np.float32).reshape(128, 1)
    return shared


_CACHE = {}


def kernel(**inputs):
    x = np.asarray(inputs["x"], np.float32)
    ctx = np.asarray(inputs["ctx"], np.float32)
    c = np.asarray(inputs["c"], np.float32)
    c_ctx = np.asarray(inputs["c_ctx"], np.float32)
    B = x.shape[0]
    if "nc" not in _CACHE:
        _CACHE["nc"] = build_nc()
    nc = _CACHE["nc"]
    shared = host_shared(inputs)
    in_maps = []
    for b in range(B):
        m = dict(shared)
        m["xin"] = np.ascontiguousarray(np.concatenate([ctx[b], x[b]], axis=0))
        m["cc"] = np.ascontiguousarray(np.stack([c[b], c_ctx], axis=0))
        in_maps.append(m)
    res = run_bass_kernel_spmd(nc, in_maps, core_ids=list(range(B)))
    return np.stack([np.asarray(r["out"], np.float32) for r in res.results], axis=0)
```

```python
import numpy as np
import ml_dtypes
import concourse.bass as bass
import concourse.mybir as mybir
from concourse.bass_utils import run_bass_kernel_spmd

F32 = mybir.dt.float32
BF16 = mybir.dt.bfloat16
AF = mybir.ActivationFunctionType
ALU = mybir.AluOpType
AX = mybir.AxisListType

D = 1024
NCTX = 256
SEQ = 4096
T = NCTX + SEQ
NT = T // 128
DIN = 5280
EPS = 1e-6
C_XR, C_CKV, C_KR, C_GK, C_GV, C_RG, C_CQ, C_GQ, C_MG = 0, 512, 640, 672, 800, 928, 1440, 1696, 2208
MLA_SCALE = 96 ** -0.5
GQA_SCALE = 64 ** -0.5
NE = 32
DE = 256
SL = 512
I32 = mybir.dt.int32
BIG = 1.0e9


def n_chunks(tstart):
    return (2 * (T - tstart)) // SL + NE

import os as _os
DBG_SKIP_ROUTER = bool(_os.environ.get('DBG_SKIP_ROUTER'))
DBG_STOP = int(_os.environ.get('DBG_STOP', '99'))


class Buf:
    __slots__ = ("name", "lastw", "readers")

    def __init__(self, name=""):
        self.name = name
        self.lastw = None
        self.readers = []


class Op:
    __slots__ = ("eng", "fn", "deps", "dma", "tok", "needed", "idx")


class Prog:
    COMPUTE = ("pe", "act", "dve", "pool")
    NPOOL = 12
    UID = 0

    def __init__(self, nc):
        self.nc = nc
        self.ops = []
        self.q = {k: [] for k in ("pe", "act", "dve", "pool", "sp")}

    def add(self, eng, fn, reads=(), writes=(), dma=False):
        op = Op()
        op.eng, op.fn, op.dma, op.tok, op.needed = eng, fn, dma, None, False
        op.idx = len(self.ops)
        deps = set()
        for b in reads:
            if b.lastw is not None:
                deps.add(b.lastw)
        for b in writes:
            if b.lastw is not None:
                deps.add(b.lastw)
            deps.update(b.readers)
        op.deps = deps
        for b in reads:
            if b in writes:
                continue
            if not dma:
                b.readers = [r for r in b.readers if self.ops[r].dma or self.ops[r].eng != eng]
            b.readers.append(op.idx)
        for b in writes:
            b.lastw = op.idx
            b.readers = []
        self.ops.append(op)
        self.q[eng].append(op)
        return op

    def emit(self):
        nc, ops = self.nc, self.ops
        for op in ops:
            for d in op.deps:
                dop = ops[d]
                if dop.eng == "pe" and op.eng == "pe" and not dop.dma and not op.dma:
                    continue
                dop.needed = True
        Prog.UID += 1
        u = Prog.UID
        sems = {k: nc.alloc_semaphore("s%d_%s" % (u, k)) for k in self.COMPUTE}
        dsem = {k: [nc.alloc_semaphore("d%d_%s_%d" % (u, k, i)) for i in range(self.NPOOL)] for k in self.q}
        cnt = {k: 0 for k in self.COMPUTE}
        dcnt = {k: 0 for k in self.q}
        prewait = {}
        for op in ops:
            if op.dma:
                k = dcnt[op.eng]
                dcnt[op.eng] += 1
                s = dsem[op.eng][k % self.NPOOL]
                op.tok = (s, 16 * (k // self.NPOOL + 1))
                if k >= self.NPOOL:
                    prewait[op.idx] = (s, 16 * (k // self.NPOOL))
            elif op.needed:
                cnt[op.eng] += 1
                op.tok = (sems[op.eng], cnt[op.eng])
        engines = {"pe": "tensor", "act": "scalar", "dve": "vector", "pool": "gpsimd", "sp": "sync"}
        with nc.Block() as block:
            def make(k):
                def body(e):
                    known = {}
                    for op in self.q[k]:
                        waits = []
                        if op.idx in prewait:
                            waits.append(prewait[op.idx])
                        for d in sorted(op.deps):
                            dop = ops[d]
                            if dop.tok is None:
                                continue
                            if dop.eng == "pe" and k == "pe" and not dop.dma and not op.dma:
                                continue
                            waits.append(dop.tok)
                        for (s, v) in waits:
                            if known.get(id(s), 0) >= v:
                                continue
                            known[id(s)] = v
                            e.wait_ge(s, v)
                        ins = op.fn(e)
                        if op.tok is not None:
                            ins.then_inc(op.tok[0], 16 if op.dma else 1)
                    if k == "sp":
                        for kk in self.q:
                            n = dcnt[kk]
                            for j in range(min(n, self.NPOOL)):
                                uses = (n - j + self.NPOOL - 1) // self.NPOOL
                                e.wait_ge(dsem[kk][j], 16 * uses)
                return body
            for k, attr in engines.items():
                getattr(block, attr)(make(k))


class Tl:
    def __init__(self, t, name=""):
        self.t = t
        self.b = Buf(name)

    def __getitem__(self, k):
        return self.t[k]


def _bufs(lst):
    return [x.b if isinstance(x, Tl) else x for x in lst]


class Ph:
    def __init__(self, nc, ps):
        self.nc = nc
        self.P = Prog(nc)
        self.ps = ps
        self.pb = [Tl(ps[:, i * 512:(i + 1) * 512], "pb%d" % i) for i in range(8)]
        self.n = 0
        Ph.UID += 1
        self.uid = Ph.UID

    UID = 0

    def sb(self, shape, dt=F32, name=None):
        self.n += 1
        t = self.nc.alloc_sbuf_tensor("%s_%d_%d" % (name or "t", self.uid, self.n), list(shape), dt)
        return Tl(t, name or "t")

    def add(self, eng, fn, r, w, dma=False):
        return self.P.add(eng, fn, _bufs(r), _bufs(w), dma=dma)

    def dma(self, out, in_, r=(), w=(), eng="sp", **kw):
        return self.add(eng, lambda e: e.dma_start(out=out, in_=in_, **kw), r, w, dma=True)

    def mm(self, out, lhsT, rhs, start, stop, r, w):
        return self.add("pe", lambda e: e.matmul(out, lhsT=lhsT, rhs=rhs, start=start, stop=stop), r, w)

    def tr(self, out, in_, ident, r, w):
        return self.add("pe", lambda e: e.transpose(out=out, in_=in_, identity=ident), r, w)

    def act(self, out, in_, func, r, w, bias=None, scale=None, accum=None):
        kw = {}
        if bias is not None:
            kw["bias"] = bias
        if scale is not None:
            kw["scale"] = scale
        if accum is not None:
            kw["accum_out"] = accum
        return self.add("act", lambda e: e.activation(out=out, in_=in_, func=func, **kw), r, w)

    def tt(self, eng, out, in0, in1, op, r, w):
        return self.add(eng, lambda e: e.tensor_tensor(out=out, in0=in0, in1=in1, op=op), r, w)

    def ts(self, eng, out, in0, s1, s2, op0, op1, r, w):
        if op1 is None:
            return self.add(eng, lambda e: e.tensor_scalar(out=out, in0=in0, scalar1=s1, scalar2=None, op0=op0), r, w)
        return self.add(eng, lambda e: e.tensor_scalar(out=out, in0=in0, scalar1=s1, scalar2=s2, op0=op0, op1=op1), r, w)

    def stt(self, eng, out, in0, sc, in1, op0, op1, r, w):
        return self.add(eng, lambda e: e.scalar_tensor_tensor(out=out, in0=in0, scalar=sc, in1=in1, op0=op0, op1=op1), r, w)

    def cp(self, eng, out, in_, r, w):
        if eng == "act":
            return self.add("act", lambda e: e.activation(out=out, in_=in_, func=AF.Copy), r, w)
        return self.add(eng, lambda e: e.tensor_copy(out=out, in_=in_), r, w)

    def memset(self, eng, ap, val, w):
        return self.add(eng, lambda e: e.memset(ap, val), [], w)

    def recip(self, out, in_, r, w):
        return self.add("dve", lambda e: e.reciprocal(out=out, in_=in_), r, w)

    def breg(self, e, val):
        if not hasattr(self, "_regs"):
            self._regs = {}
        if val not in self._regs:
            r = e.alloc_register("bnd_%d_%d" % (self.uid, val))
            e.reg_mov(r, val)
            self._regs[val] = r
        return self._regs[val]

    def finish(self):
        self.P.emit()


def run_phase(nc, ps, fn, *args):
    with nc.cleanup_on_exit():
        ph = Ph(nc, ps)
        fn(ph, *args)
        ph.finish()
        nc.all_engine_barrier()


def token_blocks(t0, t1, bs=512):
    out = []
    t = t0
    while t < t1:
        n = min(bs, t1 - t)
        out.append((t, n))
        t += n
    return out


def make_identity(ph, dt, name):
    idf = ph.sb([128, 128], F32, name + "f")
    ph.memset("pool", idf[:], 0.0, [idf])
    ph.add("pool", lambda e: e.affine_select(out=idf[:], in_=idf[:], pattern=[[-1, 128]], compare_op=ALU.not_equal,
                                            fill=1.0, base=0, channel_multiplier=1), [idf], [idf])
    if dt == F32:
        return idf
    idb = ph.sb([128, 128], dt, name)
    ph.cp("pool", idb[:], idf[:], [idf], [idb])
    return idb


def phase_mod(ph, dr, l):
    cc = ph.sb([128, 2, 8], F32, "cc")
    sc = ph.sb([128, 2, 8], F32, "sc")
    ph.dma(cc[:], dr["cc"].rearrange("r (p k) -> p r k", k=8), [], [cc])
    ph.act(sc[:], cc[:], AF.Silu, [cc], [sc])
    mods = ph.sb([2, 6 * D], F32, "mods")
    bm = ph.sb([2, 6 * D], F32, "bm")
    ph.dma(bm[:], dr["b_mod"][l:l + 1, :].partition_broadcast(2), [], [bm])
    gm = ph.sb([2, D], F32, "gm")
    gf = ph.sb([2, D], F32, "gf")
    ph.dma(gm[:], dr["g_mix"][l:l + 1, :].partition_broadcast(2), [], [gm])
    ph.dma(gf[:], dr["g_ffn"][l:l + 1, :].partition_broadcast(2), [], [gf])
    wv = dr["w_mod"][l].rearrange("(p k) n -> p k n", k=8)
    wb = [ph.sb([128, 8, 512], F32, "wb") for _ in range(2)]
    for nb in range(12):
        w = wb[nb % 2]
        ph.dma(w[:], wv[:, :, nb * 512:(nb + 1) * 512], [], [w])
        pb = ph.pb[nb % 2]
        for k in range(8):
            ph.mm(pb[0:2, :], sc[:, :, k], w[:, k, :], k == 0, k == 7, [sc, w], [pb])
        ph.tt("dve", mods[:, nb * 512:(nb + 1) * 512], pb[0:2, :], bm[:, nb * 512:(nb + 1) * 512], ALU.add, [pb, bm], [mods])
    ph.stt("dve", mods[:, D:2 * D], mods[:, D:2 * D], 1.0, gm[:], ALU.add, ALU.mult, [mods, gm], [mods])
    ph.stt("dve", mods[:, 4 * D:5 * D], mods[:, 4 * D:5 * D], 1.0, gf[:], ALU.add, ALU.mult, [mods, gf], [mods])
    ph.dma(dr["modv"][:, :], mods[:], [mods], [])


def load_bc(ph, dr, row, idx, name):
    t = ph.sb([128, D], F32, name)
    ph.dma(t[:], dr["modv"][row:row + 1, idx * D:(idx + 1) * D].partition_broadcast(128), [], [t])
    return t


def rms_modulate(ph, xt, A, B, hout, junk, ss, rstd, h32=None):
    ph.act(junk[:], xt[:], AF.Square, [xt], [junk, ss], accum=ss[:, 0:1])
    ph.act(rstd[:, 0:1], ss[:, 0:1], AF.Sqrt, [ss], [rstd], bias=ph.epsc[:, 0:1], scale=1.0 / D)
    ph.recip(rstd[:, 0:1], rstd[:, 0:1], [rstd], [rstd])
    tmp = h32 if h32 is not None else junk
    ph.stt("dve", tmp[:], xt[:], rstd[:, 0:1], A[:], ALU.mult, ALU.mult, [xt, rstd, A], [tmp])
    ph.tt("dve", hout[:], tmp[:], B[:], ALU.add, [tmp, B], [hout])


def eps_const(ph):
    ph.epsc = ph.sb([128, 1], F32, "eps")
    ph.memset("pool", ph.epsc[:], EPS, [ph.epsc])


def phase_inproj(ph, dr, l, xsrc):
    nc = ph.nc
    eps_const(ph)
    win = ph.sb([128, 8, DIN], BF16, "win")
    stg = [ph.sb([128, 1320], F32, "stg") for _ in range(2)]
    wv = dr["w_in"][l].rearrange("(k p) n -> p k n", p=128)
    i = 0
    for k in range(8):
        for c4 in range(4):
            s = stg[i % 2]
            ph.dma(s[:], wv[:, k, c4 * 1320:(c4 + 1) * 1320], [], [s])
            ph.cp("dve" if i % 2 == 0 else "pool", win[:, k, c4 * 1320:(c4 + 1) * 1320], s[:], [s], [win])
            i += 1
    wkr = ph.sb([128, 8, 96], BF16, "wkr")
    wkrs = ph.sb([128, 8, 96], BF16, "wkrs")
    ph.memset("pool", wkr[:], 0.0, [wkr])
    ph.memset("pool", wkrs[:], 0.0, [wkrs])
    ph.cp("pool", wkr[:, :, 64:96], win[:, :, C_KR:C_KR + 32], [win], [wkr])
    ph.cp("pool", wkrs[:, :, 64:80], win[:, :, C_KR + 16:C_KR + 32], [win], [wkrs])
    ph.cp("pool", wkrs[:, :, 80:96], win[:, :, C_KR:C_KR + 16], [win], [wkrs])
    wgks = ph.sb([128, 8, 128], BF16, "wgks")
    wgqs = ph.sb([128, 8, 512], BF16, "wgqs")
    for (dst, c0, n) in ((wgks, C_GK, 128), (wgqs, C_GQ, 512)):
        sv = win[:, :, c0:c0 + n].rearrange("p k (h two d) -> p k h two d", two=2, d=32)
        dv = dst[:, :, :].rearrange("p k (h two d) -> p k h two d", two=2, d=32)
        ph.cp("pool", dv[:, :, :, 0, :], sv[:, :, :, 1, :], [win], [dst])
        ph.cp("pool", dv[:, :, :, 1, :], sv[:, :, :, 0, :], [win], [dst])
    gq = ph.sb([128, 2], F32, "gq")
    ph.dma(gq[:], dr["mla_gq"][l].rearrange("(c p) -> p c", p=128), [], [gq], allow_slow_non_contiguous=True)
    gkv = ph.sb([128, 1], F32, "gkv")
    ph.dma(gkv[:], dr["mla_gkv"][l].rearrange("(p o) -> p o", o=1), [], [gkv])
    wuqf = ph.sb([128, 2, 768], F32, "wuqf")
    ph.dma(wuqf[:], dr["mla_wuq"][l].rearrange("(c p) n -> p c n", p=128), [], [wuqf])
    wuq = ph.sb([128, 2, 768], BF16, "wuq")
    wuqs = ph.sb([128, 2, 768], BF16, "wuqs")
    for c in range(2):
        ph.ts("dve", wuq[:, c, :], wuqf[:, c, :], gq[:, c:c + 1], None, ALU.mult, None, [wuqf, gq], [wuq])
    ph.cp("pool", wuqs[:], wuq[:], [wuq], [wuqs])
    v1 = wuq[:, :, :].rearrange("p c (h d) -> p c h d", d=96)
    v2 = wuqs[:, :, :].rearrange("p c (h d) -> p c h d", d=96)
    ph.cp("pool", v2[:, :, :, 64:80], v1[:, :, :, 80:96], [wuq], [wuqs])
    ph.cp("pool", v2[:, :, :, 80:96], v1[:, :, :, 64:80], [wuq], [wuqs])
    wkvf = ph.sb([128, 1024], F32, "wkvf")
    ph.dma(wkvf[:], dr["mla_wukv"][l], [], [wkvf])
    wkv = ph.sb([128, 2, 512], BF16, "wkv")
    sv = wkvf[:, :].rearrange("p (h two d) -> p two h d", two=2, d=64)
    for two in range(2):
        ph.ts("dve", wkv[:, two, :].rearrange("p (h d) -> p h d", d=64), sv[:, two, :, :], gkv[:, 0:1], None, ALU.mult, None,
              [wkvf, gkv], [wkv])
    ones = ph.sb([128, 128], BF16, "ones")
    ph.memset("pool", ones[:], 1.0, [ones])
    bones = ph.sb([128, 128], BF16, "bones")
    ph.memset("pool", bones[:], 0.0, [bones])
    ph.memset("pool", bones[0:64, 0:64], 1.0, [bones])
    ph.memset("pool", bones[64:128, 64:128], 1.0, [bones])
    ident = make_identity(ph, BF16, "ident")
    gcol = ph.sb([128, 4], F32, "gcol")
    for j, nm in ((0, "gqa_gq"), (2, "gqa_gk")):
        src = dr[nm][l].rearrange("(d o) -> d o", o=1)
        for hh in range(2):
            ph.dma(gcol[hh * 64:hh * 64 + 64, j:j + 1], src[0:64, :], [], [gcol])
            ph.dma(gcol[hh * 64:hh * 64 + 32, j + 1:j + 2], src[32:64, :], [], [gcol])
            ph.dma(gcol[hh * 64 + 32:hh * 64 + 64, j + 1:j + 2], src[0:32, :], [], [gcol])
    Al = load_bc(ph, dr, 0, 1, "Al")
    Bl = load_bc(ph, dr, 0, 0, "Bl")
    Ac = load_bc(ph, dr, 1, 1, "Ac")
    Bc = load_bc(ph, dr, 1, 0, "Bc")

    xt = [ph.sb([128, D], F32, "xt") for _ in range(2)]
    junk = ph.sb([128, D], F32, "junk")
    hb = [ph.sb([128, D], BF16, "hb") for _ in range(2)]
    ss = ph.sb([128, 1], F32, "ss")
    rstd = ph.sb([128, 1], F32, "rstd")
    hT = [ph.sb([128, 8, 512], BF16, "hT") for _ in range(2)]
    tabm = [ph.sb([96, 2, 512], BF16, "tabm") for _ in range(2)]
    tabg = [ph.sb([128, 2, 512], BF16, "tabg") for _ in range(2)]
    NOB = 6
    ob = [ph.sb([128, 512], BF16, "ob") for _ in range(NOB)]
    NF = 6
    fb = [ph.sb([128, 512], F32, "fb") for _ in range(NF)]
    nck = ph.sb([128, 512], BF16, "nck")
    ncq = [ph.sb([128, 512], BF16, "ncq") for _ in range(2)]
    ctr = {"ob": 0, "fb": 0, "pb": 0, "ev": 0}

    def nob():
        ctr["ob"] += 1
        return ob[ctr["ob"] % NOB]

    def nfb():
        ctr["fb"] += 1
        return fb[ctr["fb"] % NF]

    def npb():
        ctr["pb"] += 1
        return ph.pb[ctr["pb"] % 8]

    def evac_eng():
        ctr["ev"] += 1
        return "act" if ctr["ev"] % 2 == 0 else "dve"

    def proj(lhs_tile, c0, m, hTb, n, extra_r=()):
        pb = npb()
        for k in range(8):
            ph.mm(pb[0:m, 0:n], lhs_tile[:, k, c0:c0 + m], hTb[:, k, 0:n], k == 0, k == 7, [lhs_tile, hTb], [pb])
        return pb

    def store(dst_ap, src_tile, src_ap, eng="pool"):
        ph.dma(dst_ap, src_ap, [src_tile], [], eng=eng)

    blocks = token_blocks(0, T)

    def PREP(bi, t0, n):
        hTb = hT[bi % 2]
        tm = tabm[bi % 2]
        tg = tabg[bi % 2]
        ph.dma(tm[64:96, :, 0:n], dr["ropem"][:, :, t0:t0 + n].rearrange("a r t -> r a t"), [], [tm])
        for hh in range(2):
            ph.dma(tg[hh * 64:hh * 64 + 64, :, 0:n], dr["ropeg"][:, :, t0:t0 + n].rearrange("a r t -> r a t"), [], [tg])
        for j in range(n // 128):
            tok = t0 + j * 128
            x_ = xt[j % 2]
            h_ = hb[j % 2]
            ph.dma(x_[:], xsrc[tok:tok + 128, :], [], [x_])
            isctx = tok < NCTX
            rms_modulate(ph, x_, Ac if isctx else Al, Bc if isctx else Bl, h_, junk, ss, rstd)
            pb = npb()
            pbv = pb.t.bitcast(BF16)
            for k in range(8):
                ph.tr(pbv[:, k * 128:(k + 1) * 128], h_[:, k * 128:(k + 1) * 128], ident[:], [h_, ident], [pb])
            ph.cp(evac_eng(), hTb[:, :, j * 128:(j + 1) * 128], pbv[:, :].rearrange("p (k t) -> p k t", t=128), [pb], [hTb])

    def MAIN(bi, t0, n, part):
        hTb = hT[bi % 2]
        tm = tabm[bi % 2]
        tg = tabg[bi % 2]
        sl = slice(t0, t0 + n)
        if part == 1:
            for c in range(4):
                pb = proj(win, C_XR + c * 128, 128, hTb, n)
                o = nob()
                ph.cp(evac_eng(), o[:, 0:n], pb[:, 0:n], [pb], [o])
                store(dr["xrT"][c * 128:(c + 1) * 128, sl], o, o[:, 0:n])
            for c in range(4):
                pb = proj(win, C_RG + c * 128, 128, hTb, n)
                o = nob()
                ph.act(o[:, 0:n], pb[:, 0:n], AF.Gelu_apprx_tanh, [pb], [o])
                store(dr["rgT"][c * 128:(c + 1) * 128, sl], o, o[:, 0:n])
            for c in range(24):
                pb = proj(win, C_MG + c * 128, 128, hTb, n)
                o = nob()
                ph.act(o[:, 0:n], pb[:, 0:n], AF.Sigmoid, [pb], [o])
                store(dr["gatesT"][c * 128:(c + 1) * 128, sl], o, o[:, 0:n])
            return
        for j in range(n // 128):
            pb = npb()
            for k in range(8):
                ph.mm(pb[:, 0:128], hTb[:, k, j * 128:(j + 1) * 128], win[:, k, C_GV:C_GV + 128], k == 0, k == 7, [hTb, win], [pb])
            o = nob()
            ph.cp(evac_eng(), o[:, 0:128], pb[:, 0:128], [pb], [o])
            store(dr["vg"][t0 + j * 128:t0 + (j + 1) * 128, :], o, o[:, 0:128])

        def rstd_bc(sq_list, ones_t, count):
            pb = npb()
            for i_, sq in enumerate(sq_list):
                ph.mm(pb[:, 0:n], ones_t[:], sq[:, 0:n], i_ == 0, i_ == len(sq_list) - 1, [ones_t, sq], [pb])
            r_ = nfb()
            ph.act(r_[:, 0:n], pb[:, 0:n], AF.Sqrt, [pb], [r_], bias=ph.epsc[:, 0:1], scale=1.0 / count)
            ph.recip(r_[:, 0:n], r_[:, 0:n], [r_], [r_])
            return r_

        pa = proj(win, C_CKV, 128, hTb, n)
        sq = nob()
        ph.act(sq[:, 0:n], pa[:, 0:n], AF.Square, [pa], [sq])
        r_ = rstd_bc([sq], ones, 128)
        ph.tt("dve", nck[:, 0:n], pa[:, 0:n], r_[:, 0:n], ALU.mult, [pa, r_], [nck])
        for hp in range(4):
            pb = npb()
            ph.mm(pb[:, 0:n], wkv[:, 0, hp * 128:(hp + 1) * 128], nck[:, 0:n], True, True, [wkv, nck], [pb])
            o = nob()
            ph.cp(evac_eng(), o[:, 0:n], pb[:, 0:n], [pb], [o])
            for hh in range(2):
                store(dr["kmT"][2 * hp + hh, 0:64, sl], o, o[hh * 64:hh * 64 + 64, 0:n])
        for j in range(n // 128):
            pb = npb()
            ph.mm(pb[:, :], nck[:, j * 128:(j + 1) * 128], wkv[:, 1, :], True, True, [nck, wkv], [pb])
            o = nob()
            ph.cp(evac_eng(), o[:, :], pb[:, :], [pb], [o])
            store(dr["vm"][t0 + j * 128:t0 + (j + 1) * 128, :], o, o[:, :])

        def rope96(pa, pb_, dst_tile):
            t1 = nfb()
            t2 = nfb()
            ph.tt("dve", t1[64:96, 0:n], pa[64:96, 0:n], tm[64:96, 0, 0:n], ALU.mult, [pa, tm], [t1])
            ph.tt("dve", t2[64:96, 0:n], pb_[64:96, 0:n], tm[64:96, 1, 0:n], ALU.mult, [pb_, tm], [t2])
            ph.tt("pool", dst_tile[64:96, 0:n], t1[64:96, 0:n], t2[64:96, 0:n], ALU.add, [t1, t2], [dst_tile])

        pa = proj(wkr, 0, 96, hTb, n)
        pb_ = proj(wkrs, 0, 96, hTb, n)
        o = nob()
        rope96(pa, pb_, o)
        for h in range(8):
            store(dr["kmT"][h, 64:96, sl], o, o[64:96, 0:n], eng="sp" if h % 2 else "pool")
        pc = [proj(win, C_CQ + c * 128, 128, hTb, n) for c in range(2)]
        sqs = []
        for c in range(2):
            s_ = nob()
            ph.act(s_[:, 0:n], pc[c][:, 0:n], AF.Square, [pc[c]], [s_])
            sqs.append(s_)
        r_ = rstd_bc(sqs, ones, 256)
        for c in range(2):
            ph.tt("dve", ncq[c][:, 0:n], pc[c][:, 0:n], r_[:, 0:n], ALU.mult, [pc[c], r_], [ncq[c]])
        for h in range(8):
            pa = npb()
            pb_ = npb()
            for c in range(2):
                ph.mm(pa[0:96, 0:n], wuq[:, c, h * 96:(h + 1) * 96], ncq[c][:, 0:n], c == 0, c == 1, [wuq, ncq[c]], [pa])
            for c in range(2):
                ph.mm(pb_[0:96, 0:n], wuqs[:, c, h * 96:(h + 1) * 96], ncq[c][:, 0:n], c == 0, c == 1, [wuqs, ncq[c]], [pb_])
            o = nob()
            ph.cp("act", o[0:64, 0:n], pa[0:64, 0:n], [pa], [o])
            rope96(pa, pb_, o)
            store(dr["qmT"][h, :, sl], o, o[0:96, 0:n], eng="sp" if h % 2 else "pool")

        def gqa_chunk(c0, wsw, csw, gj, dst_fn):
            pa = proj(win, c0, 128, hTb, n)
            pb_ = proj(wsw, csw, 128, hTb, n)
            sq = nob()
            ph.act(sq[:, 0:n], pa[:, 0:n], AF.Square, [pa], [sq])
            r_ = rstd_bc([sq], bones, 64)
            t1 = nfb()
            t2 = nfb()
            ph.stt("dve", t1[:, 0:n], pa[:, 0:n], gcol[:, gj:gj + 1], tg[:, 0, 0:n], ALU.mult, ALU.mult, [pa, gcol, tg], [t1])
            ph.stt("dve", t2[:, 0:n], pb_[:, 0:n], gcol[:, gj + 1:gj + 2], tg[:, 1, 0:n], ALU.mult, ALU.mult, [pb_, gcol, tg], [t2])
            ph.tt("pool", t1[:, 0:n], t1[:, 0:n], t2[:, 0:n], ALU.add, [t1, t2], [t1])
            o = nob()
            ph.tt("pool", o[:, 0:n], t1[:, 0:n], r_[:, 0:n], ALU.mult, [t1, r_], [o])
            dst_fn(o)

        def st_gk(o):
            for hh in range(2):
                store(dr["kgT"][hh, :, sl], o, o[hh * 64:hh * 64 + 64, 0:n])
        gqa_chunk(C_GK, wgks, 0, 2, st_gk)
        for c in range(4):
            def st_gq(o, c=c):
                for hh in range(2):
                    store(dr["qgT"][2 * c + hh, :, sl], o, o[hh * 64:hh * 64 + 64, 0:n])
            gqa_chunk(C_GQ + c * 128, wgqs, c * 128, 0, st_gq)


    PREP(0, *blocks[0])
    for bi, (t0, n) in enumerate(blocks):
        MAIN(bi, t0, n, 1)
        if bi + 1 < len(blocks):
            PREP(bi + 1, *blocks[bi + 1])
        MAIN(bi, t0, n, 2)


def phase_lru(ph, dr, l):
    blocks = token_blocks(0, T)
    xp = ph.sb([128, T + 6], BF16, "xp")
    xc = ph.sb([128, T], F32, "xc")
    xcb = ph.sb([128, T], BF16, "xcb")
    rg = ph.sb([128, T], BF16, "rg")
    ysum = ph.sb([128, T], F32, "ysum")
    abufs = [ph.sb([128, T], F32, "abuf") for _ in range(2)]
    ibufs = [ph.sb([128, T], F32, "ibuf") for _ in range(2)]
    mbufs = [ph.sb([128, T], F32, "mbuf") for _ in range(2)]
    hbuf = ph.sb([128, T], F32, "hbuf")
    yo = ph.sb([128, T], BF16, "yo")
    for c in range(4):
        cs = slice(c * 128, (c + 1) * 128)
        ph.memset("pool", xp[:, 0:2], 0.0, [xp])
        ph.memset("pool", xp[:, 258:261], 0.0, [xp])
        ph.memset("pool", xp[:, T + 5:T + 6], 0.0, [xp])
        ph.dma(xp[:, 2:258], dr["xrT"][cs, 0:NCTX], [], [xp])
        ph.dma(xp[:, 261:261 + SEQ], dr["xrT"][cs, NCTX:T], [], [xp])
        cw = ph.sb([128, 4], F32, "cw")
        ph.dma(cw[:], dr["conv_w"][l].rearrange("j c -> c j")[cs, :], [], [cw], allow_slow_non_contiguous=True)
        cb = ph.sb([128, 1], F32, "cb")
        ph.dma(cb[:], dr["conv_b"][l].rearrange("(c o) -> c o", o=1)[cs, :], [], [cb])
        for (o0, i0, n) in ((0, 0, NCTX), (NCTX, 259, SEQ)):
            ph.ts("dve", xc[:, o0:o0 + n], xp[:, i0:i0 + n], cw[:, 0:1], cb[:, 0:1], ALU.mult, ALU.add, [xp, cw, cb], [xc])
            for j in range(1, 4):
                ph.stt("dve", xc[:, o0:o0 + n], xp[:, i0 + j:i0 + j + n], cw[:, j:j + 1], xc[:, o0:o0 + n],
                       ALU.mult, ALU.add, [xp, cw, xc], [xc])
        ph.cp("act", xcb[:], xc[:], [xc], [xcb])
        ph.dma(rg[:], dr["rgT"][cs, :], [], [rg])
        for d in range(2):
            abuf, ibuf, mbuf = abufs[d], ibufs[d], mbufs[d]
            wst = ph.sb([128, 2, 128], F32, "wst")
            ph.memset("pool", wst[:], 0.0, [wst])
            for g_, nm in enumerate(("lru_wa", "lru_wi")):
                for hb_ in range(2):
                    ph.dma(wst[hb_ * 64:hb_ * 64 + 64, g_, hb_ * 64:hb_ * 64 + 64], dr[nm][l, d, 2 * c + hb_], [wst], [wst])
            wbd = ph.sb([128, 2, 128], BF16, "wbd")
            ph.cp("pool", wbd[:], wst[:], [wst], [wbd])
            col = ph.sb([128, 8], F32, "col")
            for j, nm in enumerate(("lru_ba", "lru_bi", "lru_lambda")):
                ph.dma(col[:, j:j + 1], dr[nm][l, d].rearrange("(c o) -> c o", o=1)[cs, :], [], [col])
            ph.act(col[:, 4:5], col[:, 2:3], AF.Exp, [col], [col], scale=-1.0)
            ph.act(col[:, 5:6], col[:, 4:5], AF.Ln, [col], [col], bias=1.0)
            ph.ts("dve", col[:, 2:3], col[:, 5:6], -8.0, None, ALU.mult, None, [col], [col])
            ph.ts("dve", col[:, 3:4], col[:, 5:6], -16.0, None, ALU.mult, None, [col], [col])
            for bi, (t0, n) in enumerate(blocks):
                pr = ph.pb[(2 * bi) % 8]
                pi = ph.pb[(2 * bi + 1) % 8]
                ph.mm(pr[:, 0:n], wbd[:, 0, :], xcb[:, t0:t0 + n], True, True, [wbd, xcb], [pr])
                ph.mm(pi[:, 0:n], wbd[:, 1, :], xcb[:, t0:t0 + n], True, True, [wbd, xcb], [pi])
                ph.act(abuf[:, t0:t0 + n], pr[:, 0:n], AF.Sigmoid, [pr, col], [abuf], bias=col[:, 0:1])
                ph.act(ibuf[:, t0:t0 + n], pi[:, 0:n], AF.Sigmoid, [pi, col], [ibuf], bias=col[:, 1:2])
            ph.act(mbuf[:], abuf[:], AF.Exp, [abuf, col], [mbuf], scale=col[:, 3:4])
            ph.act(abuf[:], abuf[:], AF.Exp, [abuf, col], [abuf], scale=col[:, 2:3])
            ph.act(mbuf[:], mbuf[:], AF.Sqrt, [mbuf], [mbuf], bias=1.0, scale=-1.0)
            ph.tt("pool", ibuf[:], ibuf[:], mbuf[:], ALU.mult, [ibuf, mbuf], [ibuf])
            ph.tt("dve", ibuf[:], ibuf[:], xc[:], ALU.mult, [ibuf, xc], [ibuf])
            dst = ysum if d == 0 else hbuf
            if d == 0:
                ph.add("dve", lambda e, dst=dst, abuf=abuf, ibuf=ibuf: e.tensor_tensor_scan(out=dst[:, :], data0=abuf[:, :], data1=ibuf[:, :], initial=0.0,
                                                                      op0=ALU.mult, op1=ALU.add), [abuf, ibuf], [dst])
            else:
                ph.add("dve", lambda e, dst=dst, abuf=abuf, ibuf=ibuf: e.tensor_tensor_scan(out=dst[:, 0:NCTX][:, ::-1], data0=abuf[:, 0:NCTX][:, ::-1],
                                                                      data1=ibuf[:, 0:NCTX][:, ::-1], initial=0.0,
                                                                      op0=ALU.mult, op1=ALU.add), [abuf, ibuf], [dst])
                ph.add("dve", lambda e, dst=dst, abuf=abuf, ibuf=ibuf: e.tensor_tensor_scan(out=dst[:, NCTX:T][:, ::-1], data0=abuf[:, NCTX:T][:, ::-1],
                                                                      data1=ibuf[:, NCTX:T][:, ::-1], initial=dst[:, 0:1],
                                                                      op0=ALU.mult, op1=ALU.add), [abuf, ibuf, dst], [dst])
                ph.tt("pool", ysum[:], ysum[:], hbuf[:], ALU.add, [ysum, hbuf], [ysum])
        ph.tt("pool", yo[:], ysum[:], rg[:], ALU.mult, [ysum, rg], [yo])
        ph.dma(dr["yT"][0, cs, :], yo[:], [yo], [])


def phase_attn(ph, dr, l, with_ctx):
    NB = 2
    kT = [ph.sb([96, T], BF16, "kT") for _ in range(NB)]
    qT = [ph.sb([96, T], BF16, "qT") for _ in range(NB)]
    kTg = [ph.sb([128, T], BF16, "kTg") for _ in range(NB)]
    qTg = [ph.sb([128, T], BF16, "qTg") for _ in range(NB)]
    for t_ in kTg + qTg:
        ph.memset("pool", t_[64:128, :], 0.0, [t_])
    va = [ph.sb([128, NT, 128], BF16, "va") for _ in range(NB)]
    for v_ in va:
        ph.memset("pool", v_[:, :, 64:128], 1.0, [v_])
    NPT = 3
    GS = 3
    pT = [ph.sb([128, GS * 512], BF16, "pT") for _ in range(NPT)]
    osb = [ph.sb([64, 512], F32, "osb") for _ in range(2)]
    yo = [ph.sb([64, 512], BF16, "yo") for _ in range(2)]
    sp_ = [Tl(ph.ps[:, 0:1536], "S0"), Tl(ph.ps[:, 1536:3072], "S1")]
    acc = [ph.pb[6], ph.pb[7]]
    heads = [(br, h) for br in (1, 2) for h in range(8)]

    def load_head(hi):
        br, h = heads[hi]
        k_, q_, v_ = (kT if br == 1 else kTg)[hi % NB], (qT if br == 1 else qTg)[hi % NB], va[hi % NB]
        if br == 1:
            ph.dma(k_[0:96, :], dr["kmT"][h], [], [k_])
            ph.dma(q_[0:96, :], dr["qmT"][h], [], [q_])
            vsrc = dr["vm"][:, h * 64:(h + 1) * 64].rearrange("(c p) d -> p c d", p=128)
        else:
            ph.dma(k_[0:64, :], dr["kgT"][h // 4], [], [k_])
            ph.dma(q_[0:64, :], dr["qgT"][h], [], [q_])
            vsrc = dr["vg"][:, (h // 4) * 64:(h // 4 + 1) * 64].rearrange("(c p) d -> p c d", p=128)
        ph.dma(v_[:, 0:17, 0:64], vsrc[:, 0:17, :], [], [v_])
        ph.dma(v_[:, 17:NT, 0:64], vsrc[:, 17:NT, :], [], [v_])

    items = []
    blk = 0
    for hi, (br, h) in enumerate(heads):
        d = 96 if br == 1 else 64
        scale = MLA_SCALE if br == 1 else GQA_SCALE
        qblocks = [(t0, n, NT) for (t0, n) in token_blocks(NCTX, T)]
        if with_ctx:
            qblocks = [(0, NCTX, NCTX // 128)] + qblocks
        first = True
        for (t0, n, nkc) in qblocks:
            for g0 in range(0, nkc, GS):
                items.append(dict(hi=hi, br=br, h=h, d=d, scale=scale, t0=t0, n=n, nkc=nkc, g0=g0, gn=min(GS, nkc - g0), blk=blk,
                                  pre=first, last=(g0 + GS >= nkc), idx=len(items)))
                first = False
            blk += 1

    def S(it):
        i = it["idx"]
        k_, q_ = (kT if it["br"] == 1 else kTg)[it["hi"] % NB], (qT if it["br"] == 1 else qTg)[it["hi"] % NB]
        s_, p_ = sp_[i % 2], pT[i % NPT]
        n, t0, gn = it["n"], it["t0"], it["gn"]
        d = 96 if it["br"] == 1 else 128
        for u in range(gn):
            kc = it["g0"] + u
            ph.mm(s_[:, u * 512:u * 512 + n], k_[0:d, kc * 128:(kc + 1) * 128], q_[0:d, t0:t0 + n], True, True, [k_, q_], [s_])
        if n == 512:
            ph.act(p_[:, 0:gn * 512], s_[:, 0:gn * 512], AF.Exp, [s_], [p_], scale=it["scale"])
        else:
            sv = s_[:, :].rearrange("p (u t) -> p u t", u=GS)[:, 0:gn, 0:n]
            pv = p_[:, :].rearrange("p (u t) -> p u t", u=GS)[:, 0:gn, 0:n]
            ph.act(pv, sv, AF.Exp, [s_], [p_], scale=it["scale"])

    def PV(it):
        i = it["idx"]
        v_, p_ = va[it["hi"] % NB], pT[i % NPT]
        a_ = acc[it["blk"] % 2]
        n = it["n"]
        for u in range(it["gn"]):
            kc = it["g0"] + u
            ph.mm(a_[:, 0:n], v_[:, kc, :], p_[:, u * 512:u * 512 + n], kc == 0, kc == it["nkc"] - 1, [v_, p_], [a_])
        if it["last"]:
            o_, y_ = osb[it["blk"] % 2], yo[it["blk"] % 2]
            t0, h, br = it["t0"], it["h"], it["br"]
            ph.recip(o_[0:64, 0:n], a_[64:128, 0:n], [a_], [o_])
            ph.tt("dve", y_[:, 0:n], a_[0:64, 0:n], o_[0:64, 0:n], ALU.mult, [o_, a_], [y_])
            ph.dma(dr["yT"][br, h * 64:(h + 1) * 64, t0:t0 + n], y_[:, 0:n], [y_], [], eng="pool")

    load_head(0)
    N = len(items)
    for i in range(N + 1):
        if i < N:
            S(items[i])
        if 0 <= i - 1 < N:
            PV(items[i - 1])
        if i < N and items[i]["pre"] and items[i]["hi"] + 1 < len(heads):
            load_head(items[i]["hi"] + 1)


def phase_merge(ph, dr, l, xsrc, tstart):
    eps_const(ph)
    wbr = ph.sb([128, 3, 4, D], BF16, "wbr")
    wo = ph.sb([128, 8, D], BF16, "wo")
    stg = [ph.sb([128, 2, D], F32, "stg") for _ in range(2)]
    si = 0
    for k in range(3):
        for hf in range(2):
            s = stg[si % 2]
            ph.dma(s[:], dr["w_branch"][l, k].rearrange("(c p) n -> p c n", p=128)[:, hf * 2:(hf + 1) * 2, :], [], [s])
            ph.cp("dve" if si % 2 else "pool", wbr[:, k, hf * 2:(hf + 1) * 2, :], s[:], [s], [wbr])
            si += 1
    for hf in range(4):
        s = stg[si % 2]
        ph.dma(s[:], dr["w_out"][l].rearrange("(c p) n -> p c n", p=128)[:, hf * 2:(hf + 1) * 2, :], [], [s])
        ph.cp("dve" if si % 2 else "pool", wo[:, hf * 2:(hf + 1) * 2, :], s[:], [s], [wo])
        si += 1
    wr = ph.sb([128, 8, 36], F32, "wr")
    ph.dma(wr[:, :, 0:4], dr["moe_wg"][l].rearrange("(c p) n -> p c n", p=128), [], [wr])
    ph.dma(wr[:, :, 4:36], dr["moe_we"][l].rearrange("(c p) n -> p c n", p=128), [], [wr])
    br_ = ph.sb([128, 36], F32, "br")
    ph.dma(br_[:, 0:4], dr["moe_bg"][l:l + 1, :].partition_broadcast(128), [], [br_])
    ph.dma(br_[:, 4:36], dr["moe_be"][l:l + 1, :].partition_broadcast(128), [], [br_])
    wrb = ph.sb([128, 8, 36], BF16, "wrb")
    ph.cp("pool", wrb[:], wr[:], [wr], [wrb])
    identb = make_identity(ph, BF16, "identb")
    hb16 = [ph.sb([128, D], BF16, "hb16") for _ in range(2)]
    G = {}
    for row in ((0, 1) if tstart == 0 else (0,)):
        G[row] = (load_bc(ph, dr, row, 2, "Ga"), load_bc(ph, dr, row, 4, "Af"), load_bc(ph, dr, row, 3, "Bf"))
    yb = [ph.sb([128, 3, 4, 512], BF16, "yb") for _ in range(2)]
    gb = [ph.sb([128, 24, 512], BF16, "gb") for _ in range(1)]
    mg = [ph.sb([128, 8, 512], BF16, "mgd") for _ in range(2)]
    tmpf = [ph.sb([128, 512], F32, "tmpf") for _ in range(4)]
    accfs = [ph.sb([128, 512], F32, "accf") for _ in range(3)]
    xt = [ph.sb([128, D], F32, "xt") for _ in range(2)]
    x2 = [ph.sb([128, D], F32, "x2") for _ in range(2)]
    junk = ph.sb([128, D], F32, "junk")
    hTb = [ph.sb([128, 8, 128], BF16, "hTb") for _ in range(2)]
    ss = ph.sb([128, 1], F32, "ss")
    rstd = ph.sb([128, 1], F32, "rstd")
    sm = ph.sb([128, 64], F32, "sm")
    lg = ph.sb([128, 36], F32, "lg")
    mk = ph.sb([128, 4], F32, "mk")
    es = ph.sb([128, 4, 8], F32, "es")
    e8 = ph.sb([128, 8], F32, "e8")
    m8 = ph.sb([128, 8], F32, "m8")
    cb_ = [ph.sb([128, 32], F32, "comb") for _ in range(2)]
    c1 = ph.sb([128, 32], F32, "c1")
    c2 = ph.sb([128, 32], F32, "c2")
    ctr = {"pb": 0, "t": 0}

    def npb():
        ctr["pb"] += 1
        return ph.pb[ctr["pb"] % 8]

    blocks = token_blocks(tstart, T)
    zt = ph.sb([128, SL * D // 128], BF16, "zt")
    ph.memset("pool", zt[:], 0.0, [zt])
    zstate = {"c": 0}
    nzc = n_chunks(tstart)
    ntile_tot = (T - tstart) // 128
    zper = -(-nzc // ntile_tot)

    def ZFILL():
        for _ in range(zper):
            c = zstate["c"]
            if c >= nzc:
                return
            zstate["c"] += 1
            ph.dma(dr["xsort"][c * SL:(c + 1) * SL, :].rearrange("(p r) d -> p (r d)", p=128), zt[:], [zt], [])

    def PRO(bi, t0, n):
        sl = slice(t0, t0 + n)
        y_, g_, m_ = yb[bi % 2], gb[0], mg[bi % 2]
        for k in range(3):
            ph.dma(y_[:, k, :, 0:n], dr["yT"][k, :, sl].rearrange("(c p) t -> p c t", p=128), [], [y_])
        for k in range(3):
            ph.dma(g_[:, k * 8:(k + 1) * 8, 0:n], dr["gatesT"][k * D:(k + 1) * D, sl].rearrange("(c p) t -> p c t", p=128), [], [g_])
        for oc in range(8):
            accf = accfs[oc % 3]
            for k in range(3):
                pb = npb()
                for c in range(4):
                    ph.mm(pb[:, 0:n], wbr[:, k, c, oc * 128:(oc + 1) * 128], y_[:, k, c, 0:n], c == 0, c == 3, [wbr, y_], [pb])
                if k == 0:
                    ph.tt("dve", accf[:, 0:n], pb[:, 0:n], g_[:, oc, 0:n], ALU.mult, [pb, g_], [accf])
                else:
                    tf = tmpf[ctr["t"] % 4]
                    ctr["t"] += 1
                    ph.tt("dve", tf[:, 0:n], pb[:, 0:n], g_[:, k * 8 + oc, 0:n], ALU.mult, [pb, g_], [tf])
                    if k == 1:
                        ph.tt("pool", accf[:, 0:n], accf[:, 0:n], tf[:, 0:n], ALU.add, [accf, tf], [accf])
                    else:
                        ph.tt("pool", m_[:, oc, 0:n], accf[:, 0:n], tf[:, 0:n], ALU.add, [accf, tf], [m_])

    items = []
    for bi, (t0, n) in enumerate(blocks):
        for j in range(n // 128):
            items.append(dict(bi=bi, t0=t0, n=n, j=j, gi=len(items)))

    def A(it):
        bi, t0, n, j, gi = it["bi"], it["t0"], it["n"], it["j"], it["gi"]
        if j == 0:
            PRO(bi, t0, n)
        m_ = mg[bi % 2]
        tok = t0 + j * 128
        row = 1 if tok < NCTX else 0
        Ga, Af, Bf = G[row]
        x_, xo = xt[gi % 2], x2[gi % 2]
        ph.dma(x_[:], xsrc[tok:tok + 128, :], [], [x_])
        for hf in range(2):
            pb = npb()
            for c in range(8):
                ph.mm(pb[:, :], m_[:, c, j * 128:(j + 1) * 128], wo[:, c, hf * 512:(hf + 1) * 512], c == 0, c == 7, [m_, wo], [pb])
            ph.tt("dve", junk[:, hf * 512:(hf + 1) * 512], pb[:, :], Ga[:, hf * 512:(hf + 1) * 512], ALU.mult, [pb, Ga], [junk])
        ph.tt("pool", xo[:], junk[:], x_[:], ALU.add, [junk, x_], [xo])
        ph.dma(dr["x2"][tok:tok + 128, :], xo[:], [xo], [], eng="pool")
        hbt = hb16[gi % 2]
        rms_modulate(ph, xo, Af, Bf, hbt, junk, ss, rstd)
        ph.dma(dr["h2tok"][tok:tok + 128, :], hbt[:], [hbt], [], eng="pool")
        ZFILL()

    def B(it):
        gi = it["gi"]
        tok = it["t0"] + it["j"] * 128
        hbt = hb16[gi % 2]
        hb_ = hTb[gi % 2]
        pb = npb()
        pbv = pb.t.bitcast(BF16)
        for c in range(8):
            ph.tr(pbv[:, c * 128:(c + 1) * 128], hbt[:, c * 128:(c + 1) * 128], identb[:], [hbt, identb], [pb])
        ph.cp("act", hb_[:, :, :], pbv[:, :].rearrange("p (c t) -> p c t", t=128), [pb], [hb_])
        pb = npb()
        for c in range(8):
            ph.mm(pb[:, 0:36], hb_[:, c, :], wrb[:, c, :], c == 0, c == 7, [hb_, wrb], [pb])
        ph.tt("dve", lg[:], pb[:, 0:36], br_[:], ALU.add, [pb, br_], [lg])
        ph.add("dve", lambda e: e.tensor_reduce(out=sm[:, 0:1], in_=lg[:, 0:4], axis=AX.X, op=ALU.max), _bufs([lg]), _bufs([sm]))
        ph.ts("dve", mk[:], lg[:, 0:4], sm[:, 0:1], None, ALU.is_equal, None, [lg, sm], [mk])
        ph.ts("dve", sm[:, 1:2], sm[:, 0:1], -1.0, None, ALU.mult, None, [sm], [sm])
        ph.act(sm[:, 8:12], lg[:, 0:4], AF.Exp, [lg, sm], [sm], bias=sm[:, 1:2], accum=sm[:, 2:3])
        ph.recip(sm[:, 3:4], sm[:, 2:3], [sm], [sm])
        ev = lg[:, 4:36].rearrange("p (g e) -> p g e", e=8)
        ph.tt("dve", es[:], ev, mk[:, :].unsqueeze(2).to_broadcast([128, 4, 8]), ALU.mult, [lg, mk], [es])
        ph.add("dve", lambda e: e.tensor_reduce(out=e8[:], in_=es[:].rearrange("p g e -> p e g"), axis=AX.X, op=ALU.add),
               _bufs([es]), _bufs([e8]))
        ph.add("dve", lambda e: e.max(out=m8[:], in_=e8[:]), _bufs([e8]), _bufs([m8]))
        ph.tt("dve", sm[:, 4:5], m8[:, 0:1], m8[:, 1:2], ALU.subtract, [m8], [sm])
        ph.act(sm[:, 5:6], sm[:, 4:5], AF.Sigmoid, [sm], [sm])
        ph.tt("dve", sm[:, 5:6], sm[:, 5:6], sm[:, 3:4], ALU.mult, [sm], [sm])
        ph.tt("dve", sm[:, 6:7], sm[:, 3:4], sm[:, 5:6], ALU.subtract, [sm], [sm])
        cbt = cb_[gi % 2]
        ph.ts("dve", c1[:], lg[:, 4:36], m8[:, 0:1], sm[:, 5:6], ALU.is_equal, ALU.mult, [lg, m8, sm], [c1])
        ph.ts("dve", c2[:], lg[:, 4:36], m8[:, 1:2], sm[:, 6:7], ALU.is_equal, ALU.mult, [lg, m8, sm], [c2])
        ph.tt("dve", c1[:], c1[:], c2[:], ALU.add, [c1, c2], [c1])
        ph.tt("dve", cbt[:].rearrange("p (g e) -> p g e", e=8), c1[:].rearrange("p (g e) -> p g e", e=8),
              mk[:, :].unsqueeze(2).to_broadcast([128, 4, 8]), ALU.mult, [c1, mk], [cbt])
        ph.dma(dr["comb"][tok:tok + 128, :], cbt[:], [cbt], [], eng="pool")

    N = len(items)
    for i in range(N + 1):
        if i < N:
            A(items[i])
        if i >= 1:
            B(items[i - 1])


def phase_moe(ph, dr, l, tstart, final):
    eps_const(ph)
    ntok = T - tstart
    half = ntok // 2
    assert half % 128 == 0
    nth = half // 128
    Gf = {}
    for row in ((0, 1) if tstart == 0 else (0,)):
        Gf[row] = load_bc(ph, dr, row, 5, "Gf")
    if final:
        gfin = ph.sb([128, D], F32, "gfin")
        ph.dma(gfin[:], dr["g_final"].rearrange("(o n) -> o n", o=1).partition_broadcast(128), [], [gfin])
    hT = ph.sb([128, 8, half], BF16, "hT")
    acc = ph.sb([128, nth, D], F32, "acc")
    comb = ph.sb([128, nth, NE], F32, "comb")
    s13 = [ph.sb([128, 8, 256], F32, "s13") for _ in range(2)]
    s2 = [ph.sb([128, 2, D], F32, "s2") for _ in range(1)]
    w13 = [ph.sb([128, 8, 512], BF16, "w13") for _ in range(2)]
    w2 = [ph.sb([128, 2, D], BF16, "w2") for _ in range(2)]
    sg = [ph.sb([128, 2, 512], F32, "sg") for _ in range(2)]
    he = [ph.sb([128, 2, 512], BF16, "he") for _ in range(2)]
    xt = [ph.sb([128, D], F32, "xt") for _ in range(2)]
    junk = ph.sb([128, D], F32, "junk")
    ss = ph.sb([128, 1], F32, "ss")
    rstd = ph.sb([128, 1], F32, "rstd")
    ctr = {"pd": 0}
    for hf in range(2):
        h0 = tstart + hf * half
        ph.dma(hT[:, :, :], dr["h2T"][:, h0:h0 + half].rearrange("(c p) t -> p c t", p=128), [], [hT])
        ph.dma(comb[:, :, :], dr["comb"][h0:h0 + half, :].rearrange("(j p) e -> p j e", p=128), [], [comb])

        def load_w(e_):
            a2, b13, b2 = s2[0], w13[e_ % 2], w2[e_ % 2]
            ph.dma(s13[0][:], dr["moe_w1"][l, e_].rearrange("(c p) n -> p c n", p=128), [], [s13[0]])
            ph.dma(s13[1][:], dr["moe_w3"][l, e_].rearrange("(c p) n -> p c n", p=128), [], [s13[1]])
            ph.dma(a2[:], dr["moe_w2"][l, e_].rearrange("(c p) n -> p c n", p=128), [], [a2])
            ph.cp("pool", b13[:, :, 0:256], s13[0][:], [s13[0]], [b13])
            ph.cp("pool", b13[:, :, 256:512], s13[1][:], [s13[1]], [b13])
            ph.cp("pool", b2[:], a2[:], [a2], [b2])

        items = []
        for e_ in range(NE):
            for bi_, (b0, n) in enumerate(token_blocks(0, half)):
                items.append(dict(e=e_, b0=b0, n=n, first=(bi_ == 0), idx=len(items)))

        def UP(it):
            i, e_, b0, n = it["idx"], it["e"], it["b0"], it["n"]
            b13 = w13[e_ % 2]
            s_, h_ = sg[i % 2], he[i % 2]
            for m in range(2):
                pg, pu = ph.pb[2 * m], ph.pb[2 * m + 1]
                for c in range(8):
                    ph.mm(pg[:, 0:n], b13[:, c, m * 128:(m + 1) * 128], hT[:, c, b0:b0 + n], c == 0, c == 7, [b13, hT], [pg])
                for c in range(8):
                    ph.mm(pu[:, 0:n], b13[:, c, 256 + m * 128:256 + (m + 1) * 128], hT[:, c, b0:b0 + n], c == 0, c == 7, [b13, hT], [pu])
                ph.act(s_[:, m, 0:n], pg[:, 0:n], AF.Silu, [pg], [s_])
                ph.tt("dve", h_[:, m, 0:n], pu[:, 0:n], s_[:, m, 0:n], ALU.mult, [pu, s_], [h_])

        def DN(it):
            i, e_, b0, n = it["idx"], it["e"], it["b0"], it["n"]
            b2 = w2[e_ % 2]
            h_ = he[i % 2]
            for j in range(n // 128):
                tj = (b0 // 128) + j
                for nh in range(2):
                    pd = ph.pb[4 + ctr["pd"] % 4]
                    ctr["pd"] += 1
                    for c in range(2):
                        ph.mm(pd[:, :], h_[:, c, j * 128:(j + 1) * 128], b2[:, c, nh * 512:(nh + 1) * 512], c == 0, c == 1, [h_, b2], [pd])
                    asl = acc[:, tj, nh * 512:(nh + 1) * 512]
                    if e_ == 0:
                        ph.ts("dve", asl, pd[:, :], comb[:, tj, e_:e_ + 1], None, ALU.mult, None, [pd, comb], [acc])
                    else:
                        ph.stt("dve", asl, pd[:, :], comb[:, tj, e_:e_ + 1], asl, ALU.mult, ALU.add, [pd, comb, acc], [acc])

        load_w(0)
        N = len(items)
        for i in range(N + 1):
            if i < N:
                UP(items[i])
            if i >= 1:
                DN(items[i - 1])
            if i < N and items[i]["first"] and items[i]["e"] + 1 < NE:
                load_w(items[i]["e"] + 1)
        for tj in range(nth):
            tok = h0 + tj * 128
            row = 1 if tok < NCTX else 0
            x_ = xt[tj % 2]
            ph.dma(x_[:], dr["x2"][tok:tok + 128, :], [], [x_])
            ph.tt("pool", acc[:, tj, :], acc[:, tj, :], Gf[row][:], ALU.mult, [acc, Gf[row]], [acc])
            ph.tt("pool", x_[:], x_[:], acc[:, tj, :], ALU.add, [x_, acc], [x_])
            if not final:
                ph.dma(dr["xres"][tok:tok + 128, :], x_[:], [x_], [], eng="pool")
            else:
                ph.act(junk[:], x_[:], AF.Square, [x_], [junk, ss], accum=ss[:, 0:1])
                ph.act(rstd[:, 0:1], ss[:, 0:1], AF.Sqrt, [ss], [rstd], bias=ph.epsc[:, 0:1], scale=1.0 / D)
                ph.recip(rstd[:, 0:1], rstd[:, 0:1], [rstd], [rstd])
                ph.stt("dve", x_[:], x_[:], rstd[:, 0:1], gfin[:], ALU.mult, ALU.mult, [x_, rstd, gfin], [x_])
                ph.dma(dr["out"][tok - NCTX:tok - NCTX + 128, :], x_[:], [x_], [], eng="pool")


def phase_route(ph, dr, l, tstart):
    ntt = (T - tstart) // 128
    nch_tot = n_chunks(tstart)
    W = ntt * NE
    comb = ph.sb([128, ntt, NE], F32, "comb")
    ph.dma(comb[:], dr["comb"][tstart:T, :].rearrange("(j p) e -> p j e", p=128), [], [comb])
    ltri = ph.sb([128, 128], BF16, "ltri")
    ph.dma(ltri[:], dr["cst_ltri"][:, :], [], [ltri])
    iota = ph.sb([128, 128], F32, "iota")
    ph.dma(iota[:], dr["cst_iota"][:, :], [], [iota])
    pidx = ph.sb([128, 1], F32, "pidx")
    ph.dma(pidx[:], dr["cst_pidx"][:, :], [], [pidx])
    ones = ph.sb([128, 128], BF16, "ones")
    ph.memset("pool", ones[:], 1.0, [ones])
    M = ph.sb([128, ntt, NE], BF16, "M")
    Mf = ph.sb([128, ntt, NE], F32, "Mf")
    ph.ts("dve", Mf[:], comb[:], 0.0, None, ALU.is_gt, None, [comb], [Mf])
    ph.cp("dve", M[:], Mf[:], [Mf], [M])
    rank = ph.sb([128, ntt, NE], F32, "rank")
    tot = ph.sb([128, ntt, NE], F32, "tot")
    Mfl = M[:, :, :].rearrange("p j e -> p (j e)")
    for (dst, lhs, pbi) in ((rank, ltri, 0), (tot, ones, 4)):
        dfl = dst[:, :, :].rearrange("p j e -> p (j e)")
        for i, c0 in enumerate(range(0, W, 512)):
            n = min(512, W - c0)
            pb = ph.pb[pbi + i]
            ph.mm(pb[:, 0:n], lhs[:], Mfl[:, c0:c0 + n], True, True, [lhs, M], [pb])
            ph.cp("act", dfl[:, c0:c0 + n], pb[:, 0:n], [pb], [dst])
    carry = ph.sb([128, ntt + 1, NE], F32, "carry")
    ph.memset("dve", carry[:, 0, :], 0.0, [carry])
    for j in range(ntt):
        ph.tt("dve", carry[:, j + 1, :], carry[:, j, :], tot[:, j, :], ALU.add, [carry, tot], [carry])
    cnt = carry[:, ntt, :]
    NK = (T - tstart) // SL + 1
    thr = ph.sb([128, NK], F32, "thr")
    ph.ts("dve", thr[:], iota[:, 0:NK], float(SL), None, ALU.mult, None, [iota], [thr])
    cmpk = ph.sb([128, NE, NK], F32, "cmpk")
    ph.tt("dve", cmpk[:], cnt.unsqueeze(2).to_broadcast([128, NE, NK]), thr[:, :].unsqueeze(1).to_broadcast([128, NE, NK]), ALU.is_gt,
          [carry, thr], [cmpk])
    sc = ph.sb([128, 8, NE], F32, "sc")
    ph.add("dve", lambda e: e.tensor_reduce(out=sc[:, 0, :], in_=cmpk[:], axis=AX.X, op=ALU.add), _bufs([cmpk]), _bufs([sc]))
    ph.memset("dve", sc[:, 4, :], 1.0, [sc])
    ph.add("dve", lambda e: e.tensor_tensor_scan(out=sc[:, 1, :], data0=sc[:, 4, :], data1=sc[:, 0, :], initial=0.0,
                                                 op0=ALU.mult, op1=ALU.add), _bufs([sc]), _bufs([sc]))
    ph.tt("dve", sc[:, 2, :], sc[:, 1, :], sc[:, 0, :], ALU.subtract, [sc], [sc])
    ph.ts("dve", sc[:, 3, :], sc[:, 2, :], float(SL), None, ALU.mult, None, [sc], [sc])
    posv = ph.sb([128, ntt, NE], F32, "posv")
    ph.tt("dve", posv[:], rank[:], carry[:, 0:ntt, :], ALU.add, [rank, carry], [posv])
    ph.stt("dve", posv[:], posv[:], 1.0, sc[:, 3, :].unsqueeze(1).to_broadcast([128, ntt, NE]), ALU.add, ALU.add, [posv, sc], [posv])
    ph.tt("dve", posv[:], posv[:], Mf[:], ALU.mult, [posv, Mf], [posv])
    pw = ph.sb([128, ntt, 4], F32, "pw")
    eq1 = ph.sb([128, ntt, NE], F32, "eq1")
    tmp = ph.sb([128, ntt, NE], F32, "tmp")
    ph.add("dve", lambda e: e.tensor_reduce(out=pw[:, :, 0], in_=posv[:], axis=AX.X, op=ALU.max), _bufs([posv]), _bufs([pw]))
    ph.tt("dve", eq1[:], posv[:], pw[:, :, 0:1].to_broadcast([128, ntt, NE]), ALU.is_equal, [posv, pw], [eq1])
    ph.tt("dve", tmp[:], eq1[:], posv[:], ALU.mult, [eq1, posv], [tmp])
    ph.tt("dve", tmp[:], posv[:], tmp[:], ALU.subtract, [posv, tmp], [tmp])
    ph.add("dve", lambda e: e.tensor_reduce(out=pw[:, :, 1], in_=tmp[:], axis=AX.X, op=ALU.max), _bufs([tmp]), _bufs([pw]))
    ph.tt("dve", tmp[:], eq1[:], comb[:], ALU.mult, [eq1, comb], [tmp])
    ph.add("dve", lambda e: e.tensor_reduce(out=pw[:, :, 2], in_=tmp[:], axis=AX.X, op=ALU.add), _bufs([tmp]), _bufs([pw]))
    ph.add("dve", lambda e: e.tensor_reduce(out=pw[:, :, 3], in_=comb[:], axis=AX.X, op=ALU.add), _bufs([comb]), _bufs([pw]))
    ph.tt("dve", pw[:, :, 3], pw[:, :, 3], pw[:, :, 2], ALU.subtract, [pw], [pw])
    posf = ph.sb([128, ntt, 2], F32, "posf")
    ph.ts("dve", posf[:], pw[:, :, 0:2], -1.0, None, ALU.add, None, [pw], [posf])
    posi = ph.sb([128, ntt, 2], I32, "posi")
    ph.cp("dve", posi[:], posf[:], [posf], [posi])
    ph.dma(dr["posi"][tstart:T, :].rearrange("(j p) k -> p j k", p=128), posi[:], [posi], [])
    ph.dma(dr["posw"][tstart:T, :].rearrange("(j p) k -> p j k", p=128), pw[:, :, 2:4], [pw], [])
    cmpc = ph.sb([128, nch_tot, NE], F32, "cmpc")
    ph.tt("dve", cmpc[:], sc[:, 2, :].unsqueeze(1).to_broadcast([128, nch_tot, NE]),
          iota[:, 0:nch_tot].unsqueeze(2).to_broadcast([128, nch_tot, NE]), ALU.is_le, [sc, iota], [cmpc])
    wf = ph.sb([128, 4, nch_tot], F32, "wf")
    ph.add("dve", lambda e: e.tensor_reduce(out=wf[:, 0, :], in_=cmpc[:], axis=AX.X, op=ALU.add), _bufs([cmpc]), _bufs([wf]))
    ph.ts("dve", wf[:, 1, :], wf[:, 0, :], -1.0, 128.0, ALU.add, ALU.mult, [wf], [wf])
    ph.ts("dve", wf[:, 1, :], wf[:, 1, :], pidx[:, 0:1], None, ALU.add, None, [wf, pidx], [wf])
    ph.ts("dve", wf[:, 2, :], iota[:, 0:nch_tot], sc[:, 1, NE - 1:NE], BIG, ALU.is_ge, ALU.mult, [iota, sc], [wf])
    ph.tt("dve", wf[:, 1, :], wf[:, 1, :], wf[:, 2, :], ALU.add, [wf], [wf])
    widx = ph.sb([128, nch_tot], I32, "widx")
    ph.cp("dve", widx[:], wf[:, 1, :], [wf], [widx])
    ph.dma(dr["widx"][:, 0:nch_tot], widx[:], [widx], [])
    xs_t = Tl(dr["xsort"], "xsort")
    xt = [ph.sb([128, D], BF16, "xt") for _ in range(3)]
    bound = nch_tot * SL - 1
    for j in range(ntt):
        x_ = xt[j % 3]
        tok = tstart + j * 128
        ph.dma(x_[:], dr["h2tok"][tok:tok + 128, :], [], [x_])
        for k in range(2):
            ph.add("pool", lambda e, x_=x_, j=j, k=k: e.indirect_dma_start(
                out=dr["xsort"][:, :], out_offset=bass.IndirectOffsetOnAxis(ap=posi[:, j, k:k + 1], axis=0),
                in_=x_[:, :], in_offset=None, bounds_check=ph.breg(e, bound), oob_is_err=False), _bufs([x_, posi]), _bufs([]), dma=True)


def phase_moe_sparse(ph, dr, l, tstart, final):
    eps_const(ph)
    nch_tot = n_chunks(tstart)
    ntt = (T - tstart) // 128
    widx = ph.sb([128, nch_tot], I32, "widx")
    ph.dma(widx[:], dr["widx"][:, 0:nch_tot], [], [widx])
    identb = make_identity(ph, BF16, "identb")
    st = [[ph.sb([128, 2048], F32, "st") for _ in range(3)] for _ in range(2)]
    w13 = [ph.sb([128, 8, 512], BF16, "w13") for _ in range(2)]
    w2 = [ph.sb([128, 2, D], BF16, "w2") for _ in range(2)]
    for t_ in st[0] + st[1]:
        ph.memset("pool", t_[:], 0.0, [t_])
    xs = [ph.sb([128, D], BF16, "xs") for _ in range(3)]
    xT = [ph.sb([128, 8, SL], BF16, "xT") for _ in range(2)]
    sg = [ph.sb([128, 2, SL], F32, "sg") for _ in range(2)]
    he = [ph.sb([128, 2, SL], BF16, "he") for _ in range(2)]
    yb = [ph.sb([128, D], BF16, "yb") for _ in range(3)]
    srcs = (dr["w1r%d" % l], dr["w3r%d" % l], dr["w2r%d" % l])
    ctr = {"x": 0, "y": 0, "pd": 0, "pt": 0}

    def LOADW(c):
        for i in range(3):
            s_ = st[c % 2][i]
            ph.add("pool", lambda e, s_=s_, i=i: e.indirect_dma_start(
                out=s_[:, :], out_offset=None, in_=srcs[i][:, :],
                in_offset=bass.IndirectOffsetOnAxis(ap=widx[:, c:c + 1], axis=0), bounds_check=ph.breg(e, NE * 128 - 1), oob_is_err=False),
                _bufs([widx]), _bufs([s_]), dma=True)
        b13, b2 = w13[c % 2], w2[c % 2]
        ph.cp("dve", b13[:, :, 0:256], st[c % 2][0][:, :].rearrange("p (k n) -> p k n", n=256), [st[c % 2][0]], [b13])
        ph.cp("act", b13[:, :, 256:512], st[c % 2][1][:, :].rearrange("p (k n) -> p k n", n=256), [st[c % 2][1]], [b13])
        ph.cp("dve", b2[:, :, :], st[c % 2][2][:, :].rearrange("p (k n) -> p k n", n=D), [st[c % 2][2]], [b2])

    def LOADX(c):
        xT_ = xT[c % 2]
        for s4 in range(SL // 128):
            x_ = xs[ctr["x"] % 3]
            ctr["x"] += 1
            r0 = c * SL + s4 * 128
            ph.dma(x_[:], dr["xsort"][r0:r0 + 128, :], [], [x_])
            pb = ph.pb[6 + ctr["pt"] % 2]
            ctr["pt"] += 1
            pbv = pb.t.bitcast(BF16)
            for k in range(8):
                ph.tr(pbv[:, k * 128:(k + 1) * 128], x_[:, k * 128:(k + 1) * 128], identb[:], [x_, identb], [pb])
            ph.cp("act" if s4 % 2 else "dve", xT_[:, :, s4 * 128:(s4 + 1) * 128], pbv[:, :].rearrange("p (k t) -> p k t", t=128), [pb], [xT_])

    def UP(c):
        b13, xT_ = w13[c % 2], xT[c % 2]
        s_, h_ = sg[c % 2], he[c % 2]
        for m in range(2):
            pg, pu = ph.pb[2 * m], ph.pb[2 * m + 1]
            for k in range(8):
                ph.mm(pg[:, :], b13[:, k, m * 128:(m + 1) * 128], xT_[:, k, :], k == 0, k == 7, [b13, xT_], [pg])
            for k in range(8):
                ph.mm(pu[:, :], b13[:, k, 256 + m * 128:256 + (m + 1) * 128], xT_[:, k, :], k == 0, k == 7, [b13, xT_], [pu])
            ph.act(s_[:, m, :], pg[:, :], AF.Silu, [pg], [s_])
            ph.tt("dve", h_[:, m, :], pu[:, :], s_[:, m, :], ALU.mult, [pu, s_], [h_])

    def DN(c):
        b2, h_ = w2[c % 2], he[c % 2]
        for s4 in range(SL // 128):
            y_ = yb[ctr["y"] % 3]
            ctr["y"] += 1
            for nh in range(2):
                pd = ph.pb[4 + ctr["pd"] % 2]
                ctr["pd"] += 1
                for k in range(2):
                    ph.mm(pd[:, :], h_[:, k, s4 * 128:(s4 + 1) * 128], b2[:, k, nh * 512:(nh + 1) * 512], k == 0, k == 1, [h_, b2], [pd])
                ph.cp("act" if nh else "dve", y_[:, nh * 512:(nh + 1) * 512], pd[:, :], [pd], [y_])
            r0 = c * SL + s4 * 128
            ph.dma(dr["ysort"][r0:r0 + 128, :], y_[:], [y_], [])

    LOADW(0)
    LOADX(0)
    for c in range(nch_tot + 1):
        if c < nch_tot:
            UP(c)
        if c >= 1:
            DN(c - 1)
        if c + 1 < nch_tot:
            LOADW(c + 1)
            LOADX(c + 1)

    Gf = {}
    for row in ((0, 1) if tstart == 0 else (0,)):
        Gf[row] = load_bc(ph, dr, row, 5, "Gf")
    if final:
        gfin = ph.sb([128, D], F32, "gfin")
        ph.dma(gfin[:], dr["g_final"].rearrange("(o n) -> o n", o=1).partition_broadcast(128), [], [gfin])
    posi = ph.sb([128, ntt, 2], I32, "posi")
    posw = ph.sb([128, ntt, 2], F32, "posw")
    ph.dma(posi[:], dr["posi"][tstart:T, :].rearrange("(j p) k -> p j k", p=128), [], [posi])
    ph.dma(posw[:], dr["posw"][tstart:T, :].rearrange("(j p) k -> p j k", p=128), [], [posw])
    ya = [[ph.sb([128, D], BF16, "ya") for _ in range(2)] for _ in range(2)]
    for a_ in ya[0] + ya[1]:
        ph.memset("pool", a_[:], 0.0, [a_])
    acc = [ph.sb([128, D], F32, "acc") for _ in range(2)]
    xt = [ph.sb([128, D], F32, "xt") for _ in range(2)]
    junk = ph.sb([128, D], F32, "junk")
    ss = ph.sb([128, 1], F32, "ss")
    rstd = ph.sb([128, 1], F32, "rstd")
    ys_t = Tl(dr["ysort"], "ysort")
    bound = nch_tot * SL - 1
    for j in range(ntt):
        tok = tstart + j * 128
        row = 1 if tok < NCTX else 0
        for k in range(2):
            a_ = ya[j % 2][k]
            ph.add("pool", lambda e, a_=a_, j=j, k=k: e.indirect_dma_start(
                out=a_[:, :], out_offset=None, in_=dr["ysort"][:, :],
                in_offset=bass.IndirectOffsetOnAxis(ap=posi[:, j, k:k + 1], axis=0), bounds_check=ph.breg(e, bound), oob_is_err=False),
                _bufs([posi]), _bufs([a_]), dma=True)
        x_, ac = xt[j % 2], acc[j % 2]
        ph.dma(x_[:], dr["x2"][tok:tok + 128, :], [], [x_])
        ph.ts("dve", ac[:], ya[j % 2][0][:], posw[:, j, 0:1], None, ALU.mult, None, [ya[j % 2][0], posw], [ac])
        ph.stt("dve", ac[:], ya[j % 2][1][:], posw[:, j, 1:2], ac[:], ALU.mult, ALU.add, [ya[j % 2][1], posw, ac], [ac])
        ph.tt("dve", ac[:], ac[:], Gf[row][:], ALU.mult, [ac, Gf[row]], [ac])
        ph.tt("dve", x_[:], x_[:], ac[:], ALU.add, [x_, ac], [x_])
        if not final:
            ph.dma(dr["xres"][tok:tok + 128, :], x_[:], [x_], [])
        else:
            ph.act(junk[:], x_[:], AF.Square, [x_], [junk, ss], accum=ss[:, 0:1])
            ph.act(rstd[:, 0:1], ss[:, 0:1], AF.Sqrt, [ss], [rstd], bias=ph.epsc[:, 0:1], scale=1.0 / D)
            ph.recip(rstd[:, 0:1], rstd[:, 0:1], [rstd], [rstd])
            ph.stt("dve", x_[:], x_[:], rstd[:, 0:1], gfin[:], ALU.mult, ALU.mult, [x_, rstd, gfin], [x_])
            ph.dma(dr["out"][tok - NCTX:tok - NCTX + 128, :], x_[:], [x_], [])


WEIGHTS = [("w_mod", [2, D, 6 * D]), ("b_mod", [2, 6 * D]), ("g_mix", [2, D]), ("g_ffn", [2, D]), ("w_in", [2, D, DIN]),
           ("conv_w", [2, 4, 512]), ("conv_b", [2, 512]), ("lru_wa", [2, 2, 8, 64, 64]), ("lru_ba", [2, 2, 512]),
           ("lru_wi", [2, 2, 8, 64, 64]), ("lru_bi", [2, 2, 512]), ("lru_lambda", [2, 2, 512]), ("mla_gq", [2, 256]),
           ("mla_wuq", [2, 256, 768]), ("mla_gkv", [2, 128]), ("mla_wukv", [2, 128, 1024]), ("gqa_gq", [2, 64]),
           ("gqa_gk", [2, 64]), ("w_branch", [2, 3, 512, D]), ("w_out", [2, D, D]), ("moe_wg", [2, D, 4]), ("moe_bg", [2, 4]),
           ("moe_we", [2, D, 32]), ("moe_be", [2, 32]), ("g_final", [D])]
RELAID = [("w1r0", [NE * 128, 2048]), ("w3r0", [NE * 128, 2048]), ("w2r0", [NE * 128, 2048]),
          ("w1r1", [NE * 128, 2048]), ("w3r1", [NE * 128, 2048]), ("w2r1", [NE * 128, 2048])]

SCRATCH = [("modv", [2, 6 * D], F32), ("xres", [T, D], F32), ("x2", [T, D], F32), ("xrT", [512, T], BF16),
           ("rgT", [512, T], BF16), ("gatesT", [3 * D, T], BF16), ("kmT", [8, 96, T], BF16), ("qmT", [8, 96, T], BF16),
           ("vm", [T, 512], BF16), ("kgT", [2, 64, T], BF16), ("qgT", [8, 64, T], BF16), ("vg", [T, 128], BF16),
           ("yT", [3, 512, T], BF16), ("h2tok", [T, D], BF16), ("comb", [T, NE], F32),
           ("xsort", [(2 * T // SL + NE) * SL, D], BF16), ("ysort", [(2 * T // SL + NE) * SL, D], BF16),
           ("posi", [T, 2], I32), ("posw", [T, 2], F32), ("widx", [128, 2 * T // SL + NE], I32)]


def build_nc(phases=None, debug=()):
    nc = bass.Bass("TRN2", target_bir_lowering=False)
    dr = {}
    dr["xin"] = nc.dram_tensor("xin", [T, D], F32, kind="ExternalInput").ap()
    dr["cc"] = nc.dram_tensor("cc", [2, D], F32, kind="ExternalInput").ap()
    dr["ropem"] = nc.dram_tensor("ropem", [2, 32, T], BF16, kind="ExternalInput").ap()
    dr["ropeg"] = nc.dram_tensor("ropeg", [2, 64, T], BF16, kind="ExternalInput").ap()
    for nm, shp in WEIGHTS + RELAID:
        dr[nm] = nc.dram_tensor(nm, shp, F32, kind="ExternalInput").ap()
    dr["cst_ltri"] = nc.dram_tensor("cst_ltri", [128, 128], BF16, kind="ExternalInput").ap()
    dr["cst_iota"] = nc.dram_tensor("cst_iota", [128, 128], F32, kind="ExternalInput").ap()
    dr["cst_pidx"] = nc.dram_tensor("cst_pidx", [128, 1], F32, kind="ExternalInput").ap()
    dr["out"] = nc.dram_tensor("out", [SEQ, D], F32, kind="ExternalOutput").ap()
    for nm, shp, dt in SCRATCH:
        if nm in debug:
            dr[nm] = nc.dram_tensor(nm, shp, dt, kind="ExternalOutput").ap()
        else:
            dr[nm] = nc.dram_tensor(nm, shp, dt).ap()
    ps = nc.alloc_psum_tensor("ps", [128, 4096], F32)
    for l in range(2):
        xsrc = dr["xin"] if l == 0 else dr["xres"]
        last = l == 1
        tstart = NCTX if last else 0
        plan = [("mod", phase_mod, (dr, l)), ("inproj", phase_inproj, (dr, l, xsrc)), ("lru", phase_lru, (dr, l)),
                ("attn", phase_attn, (dr, l, not last)), ("merge", phase_merge, (dr, l, xsrc, tstart)),
                ("route", phase_route, (dr, l, tstart)), ("moe", phase_moe_sparse, (dr, l, tstart, last))]
        for nm, fn, args in plan:
            if phases is not None and (l, nm) not in phases:
                continue
            run_phase(nc, ps, fn, *args)
    return nc


def rope_consts():
    def tab(rot):
        q = rot // 4
        pos = np.arange(SEQ)
        row = (pos // 64).astype(np.float32)
        col = (pos % 64).astype(np.float32)
        freqs = (np.float32(10000.0) ** (-np.arange(q, dtype=np.float32) / np.float32(q))).astype(np.float32)
        ang = np.concatenate([row[:, None] * freqs, col[:, None] * freqs], axis=-1).astype(np.float32)
        cos, sin = np.cos(ang).T, np.sin(ang).T
        C = np.ones((rot, T), np.float32)
        S = np.zeros((rot, T), np.float32)
        C[:, NCTX:] = np.concatenate([cos, cos], axis=0)
        S[:, NCTX:] = np.concatenate([-sin, sin], axis=0)
        return np.stack([C, S]).astype(ml_dtypes.bfloat16)
    return tab(32), tab(64)


def host_shared(inputs):
    shared = {nm: np.ascontiguousarray(np.asarray(inputs[nm], np.float32)) for nm, _ in WEIGHTS}
    ropem, ropeg = rope_consts()
    shared["ropem"] = ropem
    shared["ropeg"] = ropeg
    for l in range(2):
        w1 = np.asarray(inputs["moe_w1"][l], np.float32).reshape(NE, 8, 128, DE).transpose(0, 2, 1, 3)
        w3 = np.asarray(inputs["moe_w3"][l], np.float32).reshape(NE, 8, 128, DE).transpose(0, 2, 1, 3)
        w2 = np.asarray(inputs["moe_w2"][l], np.float32).reshape(NE, 2, 128, D).transpose(0, 2, 1, 3)
        shared["w1r%d" % l] = np.ascontiguousarray(w1).reshape(NE * 128, 2048)
        shared["w3r%d" % l] = np.ascontiguousarray(w3).reshape(NE * 128, 2048)
        shared["w2r%d" % l] = np.ascontiguousarray(w2).reshape(NE * 128, 2048)
    shared["cst_ltri"] = np.triu(np.ones((128, 128), np.float32), 1).astype(ml_dtypes.bfloat16)
    shared["cst_iota"] = np.ascontiguousarray(np.broadcast_to(np.arange(128, dtype=np.float32)[None, :], (128, 128)))
    shared["cst_pidx"] = np.arange(128, dtype=np.float32).reshape(128, 1)
    return shared


_CACHE = {}


def kernel(**inputs):
    x = np.asarray(inputs["x"], np.float32)
    ctx = np.asarray(inputs["ctx"], np.float32)
    c = np.asarray(inputs["c"], np.float32)
    c_ctx = np.asarray(inputs["c_ctx"], np.float32)
    B = x.shape[0]
    if "nc" not in _CACHE:
        _CACHE["nc"] = build_nc()
    nc = _CACHE["nc"]
    shared = host_shared(inputs)
    in_maps = []
    for b in range(B):
        m = dict(shared)
        m["xin"] = np.ascontiguousarray(np.concatenate([ctx[b], x[b]], axis=0))
        m["cc"] = np.ascontiguousarray(np.stack([c[b], c_ctx], axis=0))
        in_maps.append(m)
    res = run_bass_kernel_spmd(nc, in_maps, core_ids=list(range(B)))
    return np.stack([np.asarray(r["out"], np.float32) for r in res.results], axis=0)
```
